# Optimizing a Trainium2 kernel written in Bass

```python
import math
import jax
import jax.numpy as jnp
from jax import lax
import numpy as np

D_MODEL = 1024
BATCH = 8
SEQ = 8192
DEPTH = 1

HEAD_DIM = 64
MEM_LEN = 256
GRID_W = 64
NA_HEADS = D_MODEL // (2 * HEAD_DIM)
NA_WIN_ROWS = 8
NA_WIN_COLS = 16
DIL_PAIRS = ((128, 1), (512, 4), (2048, 16))
DIL_GROUPS = len(DIL_PAIRS)
DIL_HEADS_PER_GROUP = D_MODEL // (4 * HEAD_DIM)
DIL_HEADS = DIL_GROUPS * DIL_HEADS_PER_GROUP
DIL_BLOCK = 128
MEM_HEADS = 4
T5_BUCKETS = 32
T5_MAX_DIST = 1024
N_GROUPS = 4
EXPERTS_PER_GROUP = 8
N_EXPERTS = N_GROUPS * EXPERTS_PER_GROUP
TOP_K = 2
D_EXPERT = D_MODEL // 2
MOE_BLOCK = 256
EPS = 1e-6
NEG_INF = -1e30

NA_W = NA_HEADS * HEAD_DIM
DIL_W = DIL_HEADS * HEAD_DIM
DIL_OUT_W = DIL_HEADS_PER_GROUP * HEAD_DIM
MEM_W = MEM_HEADS * HEAD_DIM
IN_COLS = 3 * NA_W + 3 * DIL_W + MEM_W
MIX_W = NA_W + DIL_OUT_W + MEM_W
IN_SPLITS = (NA_W, 2 * NA_W, 3 * NA_W, 3 * NA_W + DIL_W, 3 * NA_W + 2 * DIL_W, 3 * NA_W + 3 * DIL_W)

kernel_name = 'hybrid_natten_dilated_memory_hmoe_encoder'


def rms_norm(x, g):
    xf = x.astype(jnp.float32)
    y = xf * lax.rsqrt(jnp.mean(xf * xf, axis=-1, keepdims=True) + EPS)
    return (y * g.astype(jnp.float32)).astype(x.dtype)


def t5_bucket(rel):
    nb = T5_BUCKETS // 2
    max_exact = nb // 2
    n = jnp.abs(rel)
    upper = (rel > 0).astype(jnp.int32) * nb
    nf = jnp.maximum(n, 1).astype(jnp.float32)
    large = max_exact + (jnp.log(nf / max_exact) / math.log(T5_MAX_DIST / max_exact) * (nb - max_exact)).astype(jnp.int32)
    large = jnp.minimum(large, nb - 1)
    return upper + jnp.where(n < max_exact, n, large)


def neighborhood_attention(q, k, v, rpb):
    b, s, h, dh = q.shape
    rows = s // GRID_W
    kh = min(NA_WIN_ROWS, rows)
    kw = NA_WIN_COLS
    kg = k.reshape(b, rows, GRID_W, h, dh).transpose(0, 3, 1, 2, 4)
    vg = v.reshape(b, rows, GRID_W, h, dh).transpose(0, 3, 1, 2, 4)
    qg = q.reshape(b, rows, GRID_W, h, dh).transpose(1, 0, 3, 2, 4)
    cols = jnp.arange(GRID_W)
    c0 = jnp.clip(cols - kw // 2, 0, GRID_W - kw)
    col_idx = c0[:, None] + jnp.arange(kw)[None, :]
    dc = col_idx - cols[:, None] + (NA_WIN_COLS - 1)
    scale = HEAD_DIM ** -0.5

    def row_fn(args):
        q_r, r = args
        r0 = jnp.clip(r - kh // 2, 0, rows - kh)
        k_rows = lax.dynamic_slice_in_dim(kg, r0, kh, axis=2)
        v_rows = lax.dynamic_slice_in_dim(vg, r0, kh, axis=2)
        k_win = k_rows[:, :, :, col_idx]
        v_win = v_rows[:, :, :, col_idx]
        dr = r0 + jnp.arange(kh) - r + (NA_WIN_ROWS - 1)
        bias = rpb[:, dr[:, None, None], dc[None]].transpose(0, 2, 1, 3)
        logits = jnp.einsum('bhcd,bhacjd->bhcaj', q_r, k_win).astype(jnp.float32) * scale + bias.astype(jnp.float32)[None]
        p = jax.nn.softmax(logits, axis=(-2, -1)).astype(v.dtype)
        return jnp.einsum('bhcaj,bhacjd->bhcd', p, v_win)

    out = lax.map(row_fn, (qg, jnp.arange(rows)))
    return out.transpose(1, 0, 3, 2, 4).reshape(b, s, h * dh)


def dilated_window_attention(q, k, v, offs, bias, half):
    b, s, h, dh = q.shape
    scale = HEAD_DIM ** -0.5
    kp = jnp.pad(k, ((0, 0), (half, half), (0, 0), (0, 0)))
    vp = jnp.pad(v, ((0, 0), (half, half), (0, 0), (0, 0)))
    local = jnp.arange(DIL_BLOCK)[:, None] + offs[None, :] + half
    bias32 = bias.astype(jnp.float32)
    n_blk = s // DIL_BLOCK

    def block_fn(i):
        s0 = i * DIL_BLOCK
        qb = lax.dynamic_slice_in_dim(q, s0, DIL_BLOCK, axis=1)
        kb = lax.dynamic_slice_in_dim(kp, s0, DIL_BLOCK + 2 * half, axis=1)
        vb = lax.dynamic_slice_in_dim(vp, s0, DIL_BLOCK + 2 * half, axis=1)
        kgat = kb[:, local]
        vgat = vb[:, local]
        key_pos = s0 + local - half
        valid = (key_pos >= 0) & (key_pos < s)
        logits = jnp.einsum('bqhd,bqjhd->bqhj', qb, kgat).astype(jnp.float32) * scale + bias32[None, None]
        logits = jnp.where(valid[None, :, None, :], logits, NEG_INF)
        m = jnp.max(logits, axis=-1)
        e = jnp.exp(logits - m[..., None])
        den = jnp.sum(e, axis=-1)
        o = jnp.einsum('bqhj,bqjhd->bqhd', e, vgat.astype(jnp.float32)) / den[..., None]
        return o, m, den

    o, m, den = lax.map(block_fn, jnp.arange(n_blk))
    o = o.transpose(1, 0, 2, 3, 4).reshape(b, s, h, dh)
    m = m.transpose(1, 0, 2, 3).reshape(b, s, h)
    den = den.transpose(1, 0, 2, 3).reshape(b, s, h)
    return o, m, den


def hierarchical_moe(h, w_r1, b_r1, w_r2, b_r2, w1, w3, w2):
    b, s, d = h.shape
    n = b * s
    t = h.reshape(n, d)
    grp_logits = (t @ w_r1).astype(jnp.float32) + b_r1.astype(jnp.float32)
    grp_prob = jax.nn.softmax(grp_logits, axis=-1)
    grp_idx = jnp.argmax(grp_logits, axis=-1)
    grp_gate = jnp.take_along_axis(grp_prob, grp_idx[:, None], axis=-1)
    fine_logits = ((t @ w_r2).astype(jnp.float32) + b_r2.astype(jnp.float32)).reshape(n, N_GROUPS, EXPERTS_PER_GROUP)
    fine_sel = jnp.take_along_axis(fine_logits, grp_idx[:, None, None], axis=1)[:, 0]
    top_val, top_idx = lax.top_k(fine_sel, TOP_K)
    gate = grp_gate * jax.nn.softmax(top_val, axis=-1)
    exp_idx = grp_idx[:, None] * EXPERTS_PER_GROUP + top_idx
    n_rows = n * TOP_K
    e_flat = exp_idx.reshape(-1)
    g_flat = gate.reshape(-1)
    tok_flat = jnp.repeat(jnp.arange(n), TOP_K)
    order = jnp.argsort(e_flat)
    e_sorted = e_flat[order]
    counts = jnp.bincount(e_flat, length=N_EXPERTS)
    cnt_start = jnp.cumsum(counts) - counts
    padded = (counts + MOE_BLOCK - 1) // MOE_BLOCK * MOE_BLOCK
    pad_end = jnp.cumsum(padded)
    pad_start = pad_end - padded
    dest = pad_start[e_sorted] + jnp.arange(n_rows) - cnt_start[e_sorted]
    cap = n_rows + N_EXPERTS * MOE_BLOCK
    row_tok = jnp.full((cap,), n, jnp.int32).at[dest].set(tok_flat[order])
    row_gate = jnp.zeros((cap,), jnp.float32).at[dest].set(g_flat[order])
    n_blk = cap // MOE_BLOCK
    blk_exp = jnp.minimum(jnp.searchsorted(pad_end, jnp.arange(n_blk) * MOE_BLOCK, side='right'), N_EXPERTS - 1)
    t_pad = jnp.concatenate([t, jnp.zeros((1, d), t.dtype)], axis=0)
    xs = t_pad[row_tok].reshape(n_blk, MOE_BLOCK, d)

    def expert_block(args):
        xb, e = args
        a = jax.nn.silu(xb @ w1[e]) * (xb @ w3[e])
        return a @ w2[e]

    yb = lax.map(expert_block, (xs, blk_exp)).reshape(cap, d)
    y = jnp.zeros((n + 1, d), h.dtype).at[row_tok].add(yb * row_gate[:, None].astype(h.dtype))
    return y[:n].reshape(b, s, d)


def setup_inputs(seed: int = 0) -> dict:
    key = jax.random.key(seed)
    ks = jax.random.split(key, 18)
    f32 = jnp.float32

    def nrm(k, shape, sc):
        return jax.random.normal(k, shape, f32) * sc

    L = DEPTH
    return {
        'x': nrm(ks[0], (BATCH, SEQ, D_MODEL), 1.0),
        'mem': nrm(ks[1], (BATCH, MEM_LEN, D_MODEL), 1.0),
        'g_mix': 1.0 + nrm(ks[2], (L, D_MODEL), 0.02),
        'w_in': nrm(ks[3], (L, D_MODEL, IN_COLS), D_MODEL ** -0.5),
        'qk_gain': 1.0 + nrm(ks[4], (L, 3, 2, HEAD_DIM), 0.02),
        'na_rpb': nrm(ks[5], (L, NA_HEADS, 2 * NA_WIN_ROWS - 1, 2 * NA_WIN_COLS - 1), 0.1),
        't5_table': nrm(ks[6], (T5_BUCKETS, DIL_HEADS), 0.1),
        'g_mem': 1.0 + nrm(ks[7], (L, D_MODEL), 0.02),
        'w_mem_kv': nrm(ks[8], (L, D_MODEL, 2 * MEM_W), D_MODEL ** -0.5),
        'w_out': nrm(ks[9], (L, MIX_W, D_MODEL), MIX_W ** -0.5),
        'g_ffn': 1.0 + nrm(ks[10], (L, D_MODEL), 0.02),
        'w_r1': nrm(ks[11], (L, D_MODEL, N_GROUPS), D_MODEL ** -0.5),
        'b_r1': nrm(ks[12], (L, N_GROUPS), 0.01),
        'w_r2': nrm(ks[13], (L, D_MODEL, N_EXPERTS), D_MODEL ** -0.5),
        'b_r2': nrm(ks[14], (L, N_EXPERTS), 0.01),
        'w1': nrm(ks[15], (L, N_EXPERTS, D_MODEL, D_EXPERT), D_MODEL ** -0.5),
        'w3': nrm(ks[16], (L, N_EXPERTS, D_MODEL, D_EXPERT), D_MODEL ** -0.5),
        'w2': nrm(ks[17], (L, N_EXPERTS, D_EXPERT, D_MODEL), D_EXPERT ** -0.5),
    }


def reference(x, mem, g_mix, w_in, qk_gain, na_rpb, t5_table, g_mem, w_mem_kv, w_out, g_ffn, w_r1, b_r1, w_r2, b_r2, w1, w3, w2):
    b, s, _ = x.shape
    hd = HEAD_DIM
    scale = HEAD_DIM ** -0.5
    t5 = t5_table.reshape(T5_BUCKETS, DIL_GROUPS, DIL_HEADS_PER_GROUP)
    for l in range(DEPTH):
        h = rms_norm(x, g_mix[l])
        qa, ka, va, qd, kd, vd, qm = jnp.split(h @ w_in[l], IN_SPLITS, axis=-1)

        qa = rms_norm(qa.reshape(b, s, NA_HEADS, hd), qk_gain[l, 0, 0])
        ka = rms_norm(ka.reshape(b, s, NA_HEADS, hd), qk_gain[l, 0, 1])
        va = va.reshape(b, s, NA_HEADS, hd)
        out_na = neighborhood_attention(qa, ka, va, na_rpb[l])

        dshape = (b, s, DIL_GROUPS, DIL_HEADS_PER_GROUP, hd)
        qd = rms_norm(qd.reshape(dshape), qk_gain[l, 1, 0])
        kd = rms_norm(kd.reshape(dshape), qk_gain[l, 1, 1])
        vd = vd.reshape(dshape)
        o_list, m_list, d_list = [], [], []
        for g, (win, dil) in enumerate(DIL_PAIRS):
            half = win // 2
            n_side = half // dil
            offs = jnp.arange(-n_side, n_side + 1) * dil
            bias = t5[t5_bucket(offs), g].T
            o, m, den = dilated_window_attention(qd[:, :, g], kd[:, :, g], vd[:, :, g], offs, bias, half)
            o_list.append(o)
            m_list.append(m)
            d_list.append(den)
        m_all = jnp.stack(m_list)
        wts = jnp.stack(d_list) * jnp.exp(m_all - jnp.max(m_all, axis=0, keepdims=True))
        out_dil = jnp.sum(wts[..., None] * jnp.stack(o_list), axis=0) / jnp.sum(wts, axis=0)[..., None]
        out_dil = out_dil.astype(x.dtype).reshape(b, s, DIL_OUT_W)

        kvm = rms_norm(mem, g_mem[l]) @ w_mem_kv[l]
        km, vm = jnp.split(kvm, 2, axis=-1)
        km = rms_norm(km.reshape(b, -1, MEM_HEADS, hd), qk_gain[l, 2, 1])
        vm = vm.reshape(b, -1, MEM_HEADS, hd)
        qm = rms_norm(qm.reshape(b, s, MEM_HEADS, hd), qk_gain[l, 2, 0])
        logits_m = jnp.einsum('bshd,bmhd->bhsm', qm, km).astype(jnp.float32) * scale
        p_m = jax.nn.softmax(logits_m, axis=-1).astype(vm.dtype)
        out_mem = jnp.einsum('bhsm,bmhd->bshd', p_m, vm).reshape(b, s, MEM_W)

        x = x + jnp.concatenate([out_na, out_dil, out_mem], axis=-1) @ w_out[l]
        x = x + hierarchical_moe(rms_norm(x, g_ffn[l]), w_r1[l], b_r1[l], w_r2[l], b_r2[l], w1[l], w3[l], w2[l])
    return x
```

```python
import math
import os as _os
from contextlib import ExitStack
import numpy as np
import concourse.bass as bass
import concourse.mybir as mybir
from concourse.bass_utils import run_bass_kernel_spmd

F32 = mybir.dt.float32
BF16 = mybir.dt.bfloat16
I32 = mybir.dt.int32
AF = mybir.ActivationFunctionType
ALU = mybir.AluOpType
AX = mybir.AxisListType

S = 8192
D = 1024
NT = S // 128
EPS = 1e-6
NEG = -30000.0
N_EXP = 32
MOE_B = 256
NSET = 3
CAP = 2 * S + N_EXP * MOE_B
NBLK = CAP // MOE_B
SAME_ENGINE_SYNC = True
WSKIP = True


class Eng:
    def __init__(self, name, eng, sem):
        self.name, self.eng, self.sem = name, eng, sem
        self.count = 0
        self.waited = {}


class Buf:
    def __init__(self, name):
        self.name = name
        self.w = {}
        self.r = {}
        self.dw = None
        self.dr = None


class Sched:
    def __init__(self, nc, es):
        self.nc, self.es = nc, es
        self.engs = {}
        for nm, e in (("pe", nc.tensor), ("act", nc.scalar), ("dve", nc.vector), ("pool", nc.gpsimd), ("sp", nc.sync)):
            self.engs[nm] = Eng(nm, e, es.enter_context(nc.semaphore("sem_" + nm)))
        self.nsem = 5
        self.all_bufs = []

    def buf(self, name):
        b = Buf(name)
        self.all_bufs.append(b)
        return b

    def newsem(self, name):
        self.nsem += 1
        return self.es.enter_context(self.nc.semaphore(name))

    def _wait(self, E, key, sem, val):
        if val <= 0 or E.waited.get(key, 0) >= val:
            return
        E.eng.wait_ge(sem, val)
        E.waited[key] = val

    def _deps(self, E, reads, writes):
        for b in reads:
            for f, n in b.w.items():
                self._dep_eng(E, f, n)
            if b.dw is not None:
                self._wait(E, id(b.dw[0]), b.dw[0], b.dw[1])
        for b in writes:
            for f, n in b.w.items():
                self._dep_eng(E, f, n)
            for f, n in b.r.items():
                self._dep_eng(E, f, n)
            if b.dw is not None:
                self._wait(E, id(b.dw[0]), b.dw[0], b.dw[1])
            if b.dr is not None:
                self._wait(E, id(b.dr[0]), b.dr[0], b.dr[1])

    def _dep_eng(self, E, f, n):
        if f == E.name and (f == "pe" or not SAME_ENGINE_SYNC):
            return
        F = self.engs[f]
        self._wait(E, f, F.sem, n)

    def op(self, ename, ins_fn, reads=(), writes=()):
        E = self.engs[ename]
        self._deps(E, reads, writes)
        ins = ins_fn(E.eng)
        E.count += 1
        ins.then_inc(E.sem, 1)
        for b in reads:
            b.r[ename] = E.count
        for b in writes:
            b.w[ename] = E.count
        return ins

    def dma(self, ename, out, in_, reads=(), writes=(), extra_wait=(), indirect=None, **kw):
        E = self.engs[ename]
        self._deps(E, reads, writes)
        for b in extra_wait:
            self._deps(E, [b], [])
        if indirect is None:
            ins = E.eng.dma_start(out=out, in_=in_, **kw)
        else:
            ins = E.eng.indirect_dma_start(out=out, in_=in_, **indirect, **kw)
        if writes:
            b = writes[0]
            if b.dw is None:
                b.dw = [self.newsem("ld_" + b.name), 0]
            b.dw[1] += 16
            ins.then_inc(b.dw[0], 16)
            for b2 in writes[1:]:
                b2.dw = b.dw
        elif reads:
            b = reads[0]
            if b.dr is None:
                b.dr = [self.newsem("st_" + b.name), 0]
            b.dr[1] += 16
            ins.then_inc(b.dr[0], 16)
        return ins

    def fence_stores(self, ename):
        E = self.engs[ename]
        for b in self.all_bufs:
            if b.dr is not None:
                self._wait(E, id(b.dr[0]), b.dr[0], b.dr[1])

    def fence_all(self, ename):
        E = self.engs[ename]
        for f, F in self.engs.items():
            if f != ename and F.count > 0:
                self._wait(E, f, F.sem, F.count)
        self.fence_stores(ename)


class Ring:
    def __init__(self, sch, es, nc, name, shape, dtype, n, psum=False):
        self.tiles, self.bufs, self.i = [], [], 0
        for k in range(n):
            nm = f"{name}{k}"
            t = es.enter_context(nc.psum_tensor(nm, shape, dtype) if psum else nc.sbuf_tensor(nm, shape, dtype))
            self.tiles.append(t)
            self.bufs.append(sch.buf(nm))

    def next(self):
        k = self.i % len(self.tiles)
        self.i += 1
        return self.tiles[k], self.bufs[k]


def t5_bucket_np(rel):
    nb = 16
    max_exact = 8
    n = np.abs(rel)
    upper = (rel > 0).astype(np.int32) * nb
    nf = np.maximum(n, 1).astype(np.float32)
    large = max_exact + (np.log(nf / max_exact) / math.log(1024 / max_exact) * (nb - max_exact)).astype(np.int32)
    large = np.minimum(large, nb - 1)
    return upper + np.where(n < max_exact, n, large)


def na_plan():
    def r0(r):
        return min(max(r - 4, 0), 120)
    tiles = {}
    steps = []
    for n in range(64):
        rows = (2 * n, 2 * n + 1)
        lo = min(r0(r) for r in rows)
        hi = max(r0(r) + 7 for r in rows)
        st = []
        for m in range(lo // 2, hi // 2 + 1):
            key = []
            for kl in range(2):
                kr = 2 * m + kl
                for ql in range(2):
                    r = rows[ql]
                    ok = r0(r) <= kr <= r0(r) + 7
                    key.append(kr - r + 7 if ok else -1)
            key = tuple(key)
            if all(k < 0 for k in key):
                continue
            if key not in tiles:
                tiles[key] = len(tiles)
            st.append((m, tiles[key]))
        steps.append(st)
    return steps, list(tiles.keys())


def na_bias_tiles(rpb, tile_keys):
    cols = np.arange(64)
    c0 = np.clip(cols - 8, 0, 48)
    kc = cols[:, None]
    qc = cols[None, :]
    colok = (kc >= c0[None, :]) & (kc < c0[None, :] + 16)
    dc = np.clip(kc - qc + 15, 0, 30)
    out = np.full((len(tile_keys), 128, 8, 128), NEG, np.float32)
    for t, key in enumerate(tile_keys):
        i = 0
        for kl in range(2):
            for ql in range(2):
                dr = key[i]
                i += 1
                if dr < 0:
                    continue
                blk = np.where(colok[None], rpb[:, dr][:, dc], NEG)
                out[t, kl * 64:(kl + 1) * 64, :, ql * 64:(ql + 1) * 64] = blk.transpose(1, 0, 2)
    return out


DIL = ((128, 1), (512, 4), (2048, 16))


def dil_plan():
    tl = []
    for g, (win, d) in enumerate(DIL):
        half = win // 2
        lo = -((half + 127) // 128)
        hi = (half + 127) // 128
        for o in range(lo, hi + 1):
            tl.append((g, o))
    return tl


def dil_bias_tiles(t5_table, tl):
    t5 = t5_table.reshape(32, 3, 4)
    out = np.full((len(tl), 128, 4, 128), NEG, np.float32)
    kk = np.arange(128)[:, None]
    qq = np.arange(128)[None, :]
    for t, (g, o) in enumerate(tl):
        win, d = DIL[g]
        rel = o * 128 + kk - qq
        ok = (np.abs(rel) <= win // 2) & (rel % d == 0)
        bk = t5_bucket_np(rel)
        for h in range(4):
            out[t, :, h, :] = np.where(ok, t5[bk, g, h], NEG)
    return out


QK_TILES = []
for i in range(4):
    QK_TILES.append((0 + 128 * i, 0))
for i in range(4):
    QK_TILES.append((512 + 128 * i, 1))
for i in range(6):
    QK_TILES.append((1536 + 128 * i, 2))
for i in range(6):
    QK_TILES.append((2304 + 128 * i, 3))
for i in range(2):
    QK_TILES.append((3840 + 128 * i, 4))
T_NAQ, T_NAK, T_DQ, T_DK, T_MQ = 0, 4, 8, 14, 20
NQK = len(QK_TILES)
V_SEGS = ((1024, 512, 0), (3072, 512, 512), (3584, 256, 1024))


def build_program(n_na_tiles, n_dil_tiles, na_steps, dil_tl, debug=False, phases=(1, 2, 3, 4)):
    nc = bass.Bass("TRN2", target_bir_lowering=False)
    dk = "ExternalOutput" if debug else "Internal"

    def din(name, shape, dt=F32):
        return nc.dram_tensor(name, list(shape), dt, kind="ExternalInput").ap()

    x = din("x", [S, D])
    mem = din("mem", [256, D])
    w_in = din("w_in", [D, 4096])
    w_mem = din("w_mem", [D, 512])
    w_out = din("w_out", [D, D])
    gvec = din("gvec", [128, 24])
    gains = din("gains", [128, 6])
    gffn_b = din("gffn_b", [128, D])
    ident_in = din("ident", [128, 128])
    na_bias = din("na_bias", [n_na_tiles, 128, 8, 128])
    dil_bias = din("dil_bias", [n_dil_tiles, 128, 4, 128])
    w_r = din("w_r", [D, 36])
    b_r = din("b_r", [128, 36])
    w1 = din("w1", [N_EXP * 256, 2048])
    w3 = din("w3", [N_EXP * 256, 2048])
    w2 = din("w2", [N_EXP * 256, 2048])
    iota_in = din("iota", [128, 256])
    pidx_in = din("pidx", [128, 1])
    tri_in = din("tri", [128, 128])
    out = nc.dram_tensor("out", [S, D], F32, kind="ExternalOutput").ap()

    qk_scr = nc.dram_tensor("qk_scr", [NQK, 128, S], BF16, kind=dk).ap()
    v_scr = nc.dram_tensor("v_scr", [S, 1280], BF16, kind=dk).ap()
    mix_scr = nc.dram_tensor("mix_scr", [S, D], BF16, kind=dk).ap()
    km_scr = nc.dram_tensor("km_scr", [2, 128, 256], BF16, kind=dk).ap()
    vm_scr = nc.dram_tensor("vm_scr", [256, 256], BF16, kind=dk).ap()
    h2_scr = nc.dram_tensor("h2_scr", [S, D], BF16, kind=dk).ap()
    xs_scr = nc.dram_tensor("xs_scr", [CAP, D], BF16, kind=dk).ap()
    yb_scr = nc.dram_tensor("yb_scr", [CAP, D], F32, kind=dk).ap()
    dbg_out = nc.dram_tensor("dbg_out", [128, 4 * NT + 2 * NBLK], F32, kind=dk).ap()

    with ExitStack() as es:
        sch = Sched(nc, es)

        def sb(name, shape, dt):
            return es.enter_context(nc.sbuf_tensor(name, list(shape), dt))

        def ps(name, shape, dt=F32):
            return es.enter_context(nc.psum_tensor(name, list(shape), dt))

        identf = sb("identf", [128, 128], F32)
        identb = sb("identb", [128, 128], BF16)
        blk1 = sb("blk1", [128, 128], BF16)
        gv = sb("gv", [128, 24], F32)
        gn = sb("gn", [128, 6], F32)
        b_const = sch.buf("const")
        sch.dma("sp", identf[:], ident_in[:, :], writes=[b_const])
        sch.dma("sp", gv[:], gvec[:, :], writes=[b_const])
        sch.dma("sp", gn[:], gains[:, :], writes=[b_const])
        sch.op("dve", lambda e: e.tensor_copy(out=identb[:], in_=identf[:]), reads=[b_const], writes=[b_const])
        sch.op("dve", lambda e: e.memset(blk1[:], 0.0), writes=[b_const])
        sch.op("dve", lambda e: e.memset(blk1[0:64, 0:64], 1.0), writes=[b_const])
        sch.op("dve", lambda e: e.memset(blk1[64:128, 64:128], 1.0), writes=[b_const])
        gq = gn[:].rearrange("p (a b) -> p a b", b=2)[:, :, 0:1]
        sch.op("dve", lambda e: e.tensor_scalar(out=gq, in0=gq, scalar1=0.125, scalar2=None, op0=ALU.mult),
               reads=[b_const], writes=[b_const])

        def projection(es1, src, n_tok, wsrc, ncols, gcol0, qk_tiles, qk_dst, v_segs, v_dst, tag):
            def sb1(name, shape, dt):
                return es1.enter_context(nc.sbuf_tensor(tag + name, list(shape), dt))
            W = sb1("W", [128, 8, ncols], BF16)
            bW = sch.buf(tag + "W")
            wst = Ring(sch, es1, nc, tag + "wst", [128, 2048], F32, 2)
            nhalf = (ncols + 2047) // 2048
            for kc in range(8):
                for hf in range(nhalf):
                    c0 = hf * 2048
                    cw = min(2048, ncols - c0)
                    t, b = wst.next()
                    sch.dma("sp", t[:, 0:cw], wsrc[kc * 128:(kc + 1) * 128, c0:c0 + cw], writes=[b])
                    if (kc * nhalf + hf) % 2 == 0:
                        sch.op("dve", lambda e, t=t, kc=kc, c0=c0, cw=cw: e.tensor_scalar(
                            out=W[:, kc, c0:c0 + cw], in0=t[:, 0:cw], scalar1=gv[:, gcol0 + kc:gcol0 + kc + 1],
                            scalar2=None, op0=ALU.mult), reads=[b, b_const], writes=[bW])
                    else:
                        sch.op("act", lambda e, t=t, kc=kc, c0=c0, cw=cw: e.activation(
                            out=W[:, kc, c0:c0 + cw], in_=t[:, 0:cw], func=AF.Copy, scale=gv[:, gcol0 + kc:gcol0 + kc + 1]),
                            reads=[b, b_const], writes=[bW])
            CH = min(512, n_tok)
            TT = CH // 128
            xt_r = Ring(sch, es1, nc, tag + "xt", [128, TT, D], F32, 2)
            hn_r = Ring(sch, es1, nc, tag + "hn", [128, TT, D], BF16, 2)
            hT_r = Ring(sch, es1, nc, tag + "hT", [128, 8, CH], BF16, 2)
            st_r = Ring(sch, es1, nc, tag + "st", [128, 8], F32, 2)
            junk = sb1("junk", [128, D], BF16)
            bjunk = sch.buf(tag + "junk")
            sq_r = Ring(sch, es1, nc, tag + "sq", [128, CH], BF16, 2)
            sd_r = Ring(sch, es1, nc, tag + "sd", [128, CH], F32, 2)
            rs_r = Ring(sch, es1, nc, tag + "rs", [128, CH], F32, 2)
            qn_r = Ring(sch, es1, nc, tag + "qn", [128, CH], BF16, 3)
            vo_r = Ring(sch, es1, nc, tag + "vo", [128, 1280], BF16, 2)
            tpb = [es1.enter_context(nc.psum_tensor(f"{tag}tp{i}", [128, 2, 512], BF16)) for i in range(1)]
            tpbuf = [sch.buf(tag + "tp0")]
            pbank = [None] + [es1.enter_context(nc.psum_tensor(f"{tag}pb{i}", [128, 512], F32)) for i in range(1, 8)]
            pbuf = [None] + [sch.buf(f"{tag}pb{i}") for i in range(1, 8)]
            for ck in range(n_tok // CH):
                t0 = ck * CH
                xt, bxt = xt_r.next()
                sch.dma("sp", xt[:], src[t0:t0 + CH, :].rearrange("(t p) d -> p t d", p=128), writes=[bxt])
                stt, bst = st_r.next()
                for t in range(TT):
                    sch.op("act", lambda e, t=t: e.activation(out=junk[:], in_=xt[:, t, :], func=AF.Square,
                                                               accum_out=stt[:, t:t + 1]),
                           reads=[bxt], writes=[bjunk, bst])
                sch.op("act", lambda e: e.activation(out=stt[:, 4:4 + TT], in_=stt[:, 0:TT], func=AF.Sqrt,
                                                      bias=EPS, scale=1.0 / D), reads=[bst], writes=[bst])
                sch.op("dve", lambda e: e.reciprocal(out=stt[:, 4:4 + TT], in_=stt[:, 4:4 + TT]),
                       reads=[bst], writes=[bst])
                hn, bhn = hn_r.next()
                for t in range(TT):
                    sch.op("act", lambda e, t=t: e.activation(out=hn[:, t, :], in_=xt[:, t, :], func=AF.Copy,
                                                               scale=stt[:, 4 + t:5 + t]),
                           reads=[bxt, bst], writes=[bhn])
                hT, bhT = hT_r.next()
                for kc in range(8):
                    bank = 0
                    sl = kc % 2
                    for t in range(TT):
                        sch.op("pe", lambda e, t=t, kc=kc, bank=bank, sl=sl: e.transpose(
                            out=tpb[bank][:, sl, t * 128:(t + 1) * 128], in_=hn[:, t, kc * 128:(kc + 1) * 128],
                            identity=identb[:]), reads=[bhn, b_const], writes=[tpbuf[bank]])
                    if sl == 1:
                        sch.op("dve", lambda e, kc=kc, bank=bank: e.tensor_copy(
                            out=hT[:, kc - 1:kc + 1, :], in_=tpb[bank][:, :, 0:CH]),
                            reads=[tpbuf[bank]], writes=[bhT])
                def qk_mm(j):
                    c0, gc = qk_tiles[j]
                    qb = 1 + (j % 3)
                    for kc in range(8):
                        sch.op("pe", lambda e, kc=kc, c0=c0, qb=qb: e.matmul(
                            pbank[qb][:, 0:CH], W[:, kc, c0:c0 + 128], hT[:, kc, :], start=(kc == 0), stop=(kc == 7)),
                            reads=[bW, bhT], writes=[pbuf[qb]])
                    sq, bsq = sq_r.next()
                    sch.op("act", lambda e, qb=qb, sq=sq: e.activation(out=sq[:], in_=pbank[qb][:, 0:CH], func=AF.Square),
                           reads=[pbuf[qb]], writes=[bsq])
                    return sq, bsq

                def qk_epi(j, sq, bsq):
                    c0, gc = qk_tiles[j]
                    qb = 1 + (j % 3)
                    sbk = 4 + (j % 2)
                    sch.op("pe", lambda e, sbk=sbk, sq=sq: e.matmul(pbank[sbk][:, 0:CH], blk1[:], sq[:], start=True, stop=True),
                           reads=[bsq, b_const], writes=[pbuf[sbk]])
                    sd, bsd = sd_r.next()
                    sch.op("act", lambda e, sbk=sbk, sd=sd: e.activation(out=sd[:], in_=pbank[sbk][:, 0:CH], func=AF.Sqrt,
                                                                         bias=EPS, scale=1.0 / 64),
                           reads=[pbuf[sbk]], writes=[bsd])
                    rs, brs = rs_r.next()
                    sch.op("dve", lambda e, sd=sd, rs=rs: e.reciprocal(out=rs[:], in_=sd[:]), reads=[bsd], writes=[brs])
                    qn, bqn = qn_r.next()
                    sch.op("dve", lambda e, qb=qb, rs=rs, qn=qn, gc=gc: e.scalar_tensor_tensor(
                        out=qn[:], in0=pbank[qb][:, 0:CH], scalar=gn[:, gc:gc + 1], in1=rs[:], op0=ALU.mult, op1=ALU.mult),
                        reads=[pbuf[qb], brs, b_const], writes=[bqn])
                    sch.dma("pool", qk_dst(j, t0, CH), qn[:], reads=[bqn])

                prev = None
                for j in range(len(qk_tiles) + 1):
                    cur_ = qk_mm(j) if j < len(qk_tiles) else None
                    if prev is not None:
                        qk_epi(j - 1, *prev)
                    prev = cur_
                for t in range(TT):
                    vo, bvo = vo_r.next()
                    for si, (c0, cw, d0) in enumerate(v_segs):
                        vb = 6 + ((t * len(v_segs) + si) % 2)
                        for kc in range(8):
                            sch.op("pe", lambda e, kc=kc, c0=c0, cw=cw, vb=vb, t=t: e.matmul(
                                pbank[vb][:, 0:cw], hT[:, kc, t * 128:(t + 1) * 128], W[:, kc, c0:c0 + cw],
                                start=(kc == 0), stop=(kc == 7)), reads=[bW, bhT], writes=[pbuf[vb]])
                        sch.op("act", lambda e, vb=vb, cw=cw, d0=d0, vo=vo: e.activation(
                            out=vo[:, d0:d0 + cw], in_=pbank[vb][:, 0:cw], func=AF.Copy),
                            reads=[pbuf[vb]], writes=[bvo])
                    vw = sum(s_[1] for s_ in v_segs)
                    sch.dma("pool", v_dst(t0 + t * 128, vw), vo[:, 0:vw], reads=[bvo])

        if 1 in phases:
            with ExitStack() as es1:
                projection(es1, x, S, w_in, 4096, 0, QK_TILES,
                           lambda j, t0, n: qk_scr[j, :, t0:t0 + n], V_SEGS,
                           lambda t0, vw: v_scr[t0:t0 + 128, 0:vw], "p1")
                sch.fence_all("sp")
                sch.fence_all("pool")
            with ExitStack() as es1:
                projection(es1, mem, 256, w_mem, 512, 8, [(0, 5), (128, 5)],
                           lambda j, t0, n: km_scr[j, :, t0:t0 + n], ((256, 256, 0),),
                           lambda t0, vw: vm_scr[t0:t0 + 128, 0:vw], "pm")
                sch.fence_all("sp")
                sch.fence_all("pool")


        def attn_pass(tag, qsrc, ksrc, sk, vsrc, vruns, slots, steps_fn, OH, bias, mix_col0):
            with ExitStack() as e2:
                def sb2(name, shape, dt):
                    return e2.enter_context(nc.sbuf_tensor(tag + name, list(shape), dt))
                nkb = sk // 128
                QT = [sb2(f"QT{i}", [128, S], BF16) for i in range(len(qsrc))]
                KT = [sb2(f"KT{i}", [128, sk], BF16) for i in range(len(ksrc))]
                nv = sum(c for _, c in vruns)
                V1 = sb2("V1", [128, nkb, nv, 65], BF16)
                bin_ = sch.buf(tag + "in")
                SKIP = _os.environ.get("P2SKIP", "")
                for i, a in enumerate(qsrc if "q" not in SKIP else []):
                    for hf in range(2):
                        sch.dma("sp", QT[i][:, hf * S // 2:(hf + 1) * S // 2], a[:, hf * S // 2:(hf + 1) * S // 2], writes=[bin_])
                for i, a in enumerate(ksrc if "k" not in SKIP else []):
                    sch.dma("sp", KT[i][:], a, writes=[bin_])
                s0 = 0
                for (vc0, cnt) in (vruns if "v" not in SKIP else []):
                    for c in range(cnt):
                        vv = vsrc[:, vc0 + c * 64:vc0 + (c + 1) * 64].rearrange("(b p) d -> p b d", p=128)
                        for b0 in range(0, nkb, 16):
                            b1 = min(nkb, b0 + 16)
                            sch.dma("sp", V1[:, b0:b1, s0 + c, 0:64], vv[:, b0:b1, :], writes=[bin_])
                    s0 += cnt
                if "m" not in SKIP:
                    sch.op("pool", lambda e: e.memset(V1[:, :, :, 64:65], 1.0), writes=[bin_])
                EB = None
                if bias is not None:
                    bd, h0, Hs = bias
                    ntile = bd.shape[0]
                    Hh = Hs // 2
                    EB = sb2("EB", [128, 2, ntile, Hh, 128], BF16)
                    ebs = Ring(sch, e2, nc, tag + "ebs", [128, Hs, 128], F32, 2)
                    for t in range(ntile):
                        st, bst = ebs.next()
                        sch.dma("sp", st[:], bd[t, :, h0:h0 + Hs, :], writes=[bst])
                        sch.op("act", lambda e, st=st, t=t: e.activation(
                            out=EB[:, :, t, :, :], in_=st[:].rearrange("p (i f) q -> p f i q", f=2), func=AF.Exp),
                               reads=[bst], writes=[bin_])
                sps = Ring(sch, e2, nc, tag + "sps", [128, 512], F32, 4, psum=True)
                ops_ = Ring(sch, e2, nc, tag + "ops", [128, 512], F32, 2, psum=True)
                pr = Ring(sch, e2, nc, tag + "P", [128, 512], BF16, 5)
                rd_r = Ring(sch, e2, nc, tag + "rd", [128, 8], F32, 2)
                mx_r = Ring(sch, e2, nc, tag + "mx", [128, 4, OH * 64], BF16, 2)
                mx_state = [None, None]
                recs = []
                for n in range(NT):
                    pend = ([], [])
                    groups = []
                    for (m, tid, sl) in steps_fn(n):
                        for si in sl:
                            hf = slots[si][2]
                            pend[hf].append((m, tid, slots[si]))
                            if len(pend[hf]) == 4:
                                groups.append(list(pend[hf]))
                                pend[hf].clear()
                    for hf in range(2):
                        if pend[hf]:
                            groups.append(list(pend[hf]))
                    for gi, grp in enumerate(groups):
                        recs.append(dict(n=n, grp=grp, first=(gi == 0), last=(gi == len(groups) - 1)))

                def emitA(rc):
                    n, grp = rc["n"], rc["grp"]
                    sp_, bsp = sps.next()
                    for i, (m, tid, (qt, kt, half, vs, oh, bh)) in enumerate(grp):
                        pl = slice(half * 64, half * 64 + 64)
                        sch.op("pe", lambda e, i=i, m=m, qt=qt, kt=kt, pl=pl, sp_=sp_: e.matmul(
                            sp_[:, i * 128:(i + 1) * 128], KT[kt][pl, m * 128:(m + 1) * 128],
                            QT[qt][pl, n * 128:(n + 1) * 128], start=True, stop=True),
                            reads=[bin_], writes=[bsp])
                    w = len(grp) * 128
                    P, bP = pr.next()
                    rc["P"], rc["bP"] = P, bP
                    sch.op("act", lambda e, sp_=sp_, P=P, w=w: e.activation(out=P[:, 0:w], in_=sp_[:, 0:w], func=AF.Exp),
                           reads=[bsp], writes=[bP])
                    if EB is not None:
                        def eoff(job):
                            return (job[2][2] * ntile + job[1]) * Hh + job[2][5] // 2
                        i = 0
                        while i < len(grp):
                            off = eoff(grp[i])
                            j = i + 1
                            while j < len(grp) and eoff(grp[j]) == off + (j - i):
                                j += 1
                            ebv = EB[:].rearrange("p f t h q -> p (f t h) q")[:, off:off + (j - i), :]
                            pv = P[:, i * 128:j * 128].rearrange("p (a q) -> p a q", q=128)
                            sch.op("dve", lambda e, pv=pv, ebv=ebv: e.tensor_tensor(out=pv, in0=pv, in1=ebv, op=ALU.mult),
                                   reads=[bP, bin_], writes=[bP])
                            i = j

                cur_o = [None, None]

                def emitB(rc):
                    n, grp, P, bP = rc["n"], rc["grp"], rc["P"], rc["bP"]
                    if rc["first"]:
                        cur_o[0], cur_o[1] = ops_.next()
                    oacc, boacc = cur_o
                    for i, (m, tid, (qt, kt, half, vs, oh, bh)) in enumerate(grp):
                        fst = rc["first"] and i == 0
                        lst = rc["last"] and i == len(grp) - 1
                        sch.op("pe", lambda e, i=i, m=m, vs=vs, oh=oh, P=P, oacc=oacc, fst=fst, lst=lst: e.matmul(
                            oacc[:, oh * 65:(oh + 1) * 65], P[:, i * 128:(i + 1) * 128], V1[:, m, vs, :],
                            start=fst, stop=lst, skip_group_check=True),
                            reads=[bP, bin_], writes=[boacc])
                    if not rc["last"]:
                        return
                    rd, brd = rd_r.next()
                    ov = oacc[:, 0:OH * 65].rearrange("p (h c) -> p h c", c=65)
                    sch.op("dve", lambda e, rd=rd, ov=ov: e.reciprocal(out=rd[:, 0:OH], in_=ov[:, :, 64]),
                           reads=[boacc], writes=[brd])
                    if n % 4 == 0:
                        mx_state[0], mx_state[1] = mx_r.next()
                    mx, bmx = mx_state
                    for h in range(OH):
                        sch.op("dve", lambda e, h=h, rd=rd, oacc=oacc, mx=mx: e.tensor_scalar(
                            out=mx[:, n % 4, h * 64:(h + 1) * 64], in0=oacc[:, h * 65:h * 65 + 64],
                            scalar1=rd[:, h:h + 1], scalar2=None, op0=ALU.mult),
                            reads=[boacc, brd], writes=[bmx])
                    if n % 4 == 3:
                        r0_ = (n - 3) * 128
                        sch.dma("pool", mix_scr[r0_:r0_ + 512, mix_col0:mix_col0 + OH * 64].rearrange("(t p) c -> p t c", p=128),
                                mx[:], reads=[bmx])

                LA = 2
                for i in range(len(recs) + LA):
                    if i < len(recs):
                        emitA(recs[i])
                    if i - LA >= 0:
                        emitB(recs[i - LA])
                sch.fence_all("sp")
                sch.fence_all("pool")

        SEL = _os.environ.get("P2SEL", "na0,na1,dl0,dl1,mm").split(",")
        if 2 in phases:
            for ps_ in range(2):
                if f"na{ps_}" not in SEL:
                    continue
                slots = [(h // 2, h // 2, h % 2, h, h, h) for h in range(4)]
                attn_pass(f"na{ps_}", [qk_scr[T_NAQ + 2 * ps_ + i] for i in range(2)],
                          [qk_scr[T_NAK + 2 * ps_ + i] for i in range(2)], S, v_scr, [(ps_ * 256, 4)], slots,
                          lambda n: [(m, tid, [0, 1, 2, 3]) for (m, tid) in na_steps[n]], 4,
                          (na_bias, ps_ * 4, 4), ps_ * 256)
            for ps_ in range(2):
                if f"dl{ps_}" not in SEL:
                    continue
                slots = []
                for g in range(3):
                    for hh in range(2):
                        slots.append((g, g, hh, g * 2 + hh, hh, hh))

                def dsteps(n):
                    st = []
                    for t, (g, o) in enumerate(dil_tl):
                        m = n + o
                        if 0 <= m < NT:
                            st.append((m, t, [g * 2, g * 2 + 1]))
                    return st
                attn_pass(f"dl{ps_}", [qk_scr[T_DQ + 2 * g + ps_] for g in range(3)],
                          [qk_scr[T_DK + 2 * g + ps_] for g in range(3)], S, v_scr,
                          [(512 + g * 256 + ps_ * 128, 2) for g in range(3)], slots, dsteps, 2,
                          (dil_bias, ps_ * 2, 2), 512 + ps_ * 128)
            slots = [(h // 2, h // 2, h % 2, h, h, 0) for h in range(4)]
            if "mm" in SEL:
              attn_pass("mm", [qk_scr[T_MQ + i] for i in range(2)], [km_scr[i] for i in range(2)], 256, vm_scr,
                      [(0, 4)], slots, lambda n: [(0, None, [0, 1, 2, 3]), (1, None, [0, 1, 2, 3])], 4, None, 768)


        g1 = sb("g1", [128, NT], F32)
        g2 = sb("g2", [128, NT], F32)
        dst1i = sb("dst1i", [128, NT], I32)
        dst2i = sb("dst2i", [128, NT], I32)
        widx = sb("widx", [128, NBLK * 2], I32)
        bRt = sch.buf("routeout")
        if 3 in phases:
            with ExitStack() as e3:
                cur = [e3]
                def sb3(name, shape, dt):
                    return cur[0].enter_context(nc.sbuf_tensor("p3" + name, list(shape), dt))
                L = sb3("L", [128, NT, 36], F32)
                bL = sch.buf("L")
                e3m = ExitStack()
                e3m.__enter__()
                cur[0] = e3m
                Wo = sb3("Wo", [128, 8, D], BF16)
                bWo = sch.buf("Wo")
                wst = Ring(sch, cur[0], nc, "p3wst", [128, D], F32, 2)
                for kc in range(8):
                    t, b = wst.next()
                    sch.dma("sp", t[:], w_out[kc * 128:(kc + 1) * 128, :], writes=[b])
                    sch.op("dve", lambda e, t=t, kc=kc: e.tensor_copy(out=Wo[:, kc, :], in_=t[:]), reads=[b], writes=[bWo])
                wr = sb3("wr", [128, 8, 36], F32)
                br = sb3("br", [128, 36], F32)
                gB = sb3("gB", [128, D], F32)
                bwr = sch.buf("wr")
                sch.dma("sp", wr[:], w_r.rearrange("(kc p) n -> p kc n", p=128), writes=[bwr])
                sch.dma("sp", br[:], b_r[:, :], writes=[bwr])
                sch.dma("sp", gB[:], gffn_b[:, :], writes=[bwr])
                for kc in range(8):
                    sch.op("dve", lambda e, kc=kc: e.tensor_scalar(out=wr[:, kc, :], in0=wr[:, kc, :],
                                                                    scalar1=gv[:, 16 + kc:17 + kc], scalar2=None, op0=ALU.mult),
                           reads=[bwr, b_const], writes=[bwr])
                wrh = sb3("wrh", [128, 8, 36], BF16)
                wrl = sb3("wrl", [128, 8, 36], BF16)
                sch.op("dve", lambda e: e.tensor_copy(out=wrh[:], in_=wr[:]), reads=[bwr], writes=[bwr])
                sch.op("dve", lambda e: e.tensor_tensor(out=wrl[:], in0=wr[:], in1=wrh[:], op=ALU.subtract), reads=[bwr], writes=[bwr])
                mx_r = Ring(sch, cur[0], nc, "p3mx", [128, D], BF16, 2)
                xt_r = Ring(sch, cur[0], nc, "p3xt", [128, D], F32, 2)
                mT_r = Ring(sch, cur[0], nc, "p3mT", [128, 8, 128], BF16, 2)
                x1_r = Ring(sch, cur[0], nc, "p3x1", [128, D], F32, 2)
                hn_r = Ring(sch, cur[0], nc, "p3hn", [128, D], F32, 2)
                hi_r = Ring(sch, cur[0], nc, "p3hi", [128, D], BF16, 2)
                lo_r = Ring(sch, cur[0], nc, "p3lo", [128, D], BF16, 2)
                hg_r = Ring(sch, cur[0], nc, "p3hg", [128, D], BF16, 2)
                hiT_r = Ring(sch, cur[0], nc, "p3hiT", [128, 8, 128], BF16, 2)
                loT_r = Ring(sch, cur[0], nc, "p3loT", [128, 8, 128], BF16, 2)
                st_r = Ring(sch, cur[0], nc, "p3st", [128, 4], F32, 2)
                junk = sb3("junk", [128, D], BF16)
                bjunk = sch.buf("p3junk")
                with ExitStack() as e3p:
                    tpm = e3p.enter_context(nc.psum_tensor("p3tpm", [128, 8, 128], BF16))
                    btpm = sch.buf("p3tpm")
                    yps = Ring(sch, e3p, nc, "p3yps", [128, 512], F32, 4, psum=True)
                    tph = e3p.enter_context(nc.psum_tensor("p3tph", [128, 8, 128], BF16))
                    tpl = e3p.enter_context(nc.psum_tensor("p3tpl", [128, 8, 128], BF16))
                    btph, btpl = sch.buf("p3tph"), sch.buf("p3tpl")
                    lps = e3p.enter_context(nc.psum_tensor("p3lps", [128, 512], F32))
                    blps = sch.buf("p3lps")
                    for n in range(NT):
                        r0_ = n * 128
                        mxt, bmx = mx_r.next()
                        xt, bxt = xt_r.next()
                        sch.dma("sp", mxt[:], mix_scr[r0_:r0_ + 128, :], writes=[bmx])
                        sch.dma("sp", xt[:], x[r0_:r0_ + 128, :], writes=[bxt])
                        for kc in range(8):
                            sch.op("pe", lambda e, kc=kc, mxt=mxt: e.transpose(out=tpm[:, kc, :], in_=mxt[:, kc * 128:(kc + 1) * 128],
                                                                      identity=identb[:]), reads=[bmx, b_const], writes=[btpm])
                        mT, bmT = mT_r.next()
                        sch.op("dve", lambda e, mT=mT: e.tensor_copy(out=mT[:], in_=tpm[:]), reads=[btpm], writes=[bmT])
                        x1, bx1 = x1_r.next()
                        for hf in range(2):
                            yp, byp = yps.next()
                            for kc in range(8):
                                sch.op("pe", lambda e, kc=kc, hf=hf, yp=yp, mT=mT: e.matmul(
                                    yp[:], mT[:, kc, :], Wo[:, kc, hf * 512:(hf + 1) * 512], start=(kc == 0), stop=(kc == 7)),
                                    reads=[bmT, bWo], writes=[byp])
                            sch.op("dve", lambda e, hf=hf, yp=yp, x1=x1, xt=xt: e.tensor_tensor(
                                out=x1[:, hf * 512:(hf + 1) * 512], in0=yp[:], in1=xt[:, hf * 512:(hf + 1) * 512], op=ALU.add),
                                reads=[byp, bxt], writes=[bx1])
                        sch.dma("pool", out[r0_:r0_ + 128, :], x1[:], reads=[bx1])
                        stt, bst = st_r.next()
                        sch.op("act", lambda e, x1=x1, stt=stt: e.activation(out=junk[:], in_=x1[:], func=AF.Square,
                                                                           accum_out=stt[:, 0:1]), reads=[bx1], writes=[bjunk, bst])
                        sch.op("act", lambda e, stt=stt: e.activation(out=stt[:, 1:2], in_=stt[:, 0:1], func=AF.Sqrt,
                                                                       bias=EPS, scale=1.0 / D), reads=[bst], writes=[bst])
                        sch.op("dve", lambda e, stt=stt: e.reciprocal(out=stt[:, 1:2], in_=stt[:, 1:2]), reads=[bst], writes=[bst])
                        hn, bhn = hn_r.next()
                        sch.op("act", lambda e, hn=hn, x1=x1, stt=stt: e.activation(out=hn[:], in_=x1[:], func=AF.Copy,
                                                                                   scale=stt[:, 1:2]), reads=[bx1, bst], writes=[bhn])
                        hi, bhi = hi_r.next()
                        lo, blo = lo_r.next()
                        hg, bhg = hg_r.next()
                        sch.op("act", lambda e, hi=hi, hn=hn: e.activation(out=hi[:], in_=hn[:], func=AF.Copy), reads=[bhn], writes=[bhi])
                        sch.op("dve", lambda e, hi=hi, lo=lo, hn=hn: e.tensor_tensor(out=lo[:], in0=hn[:], in1=hi[:], op=ALU.subtract),
                               reads=[bhn, bhi], writes=[blo])
                        sch.op("pool", lambda e, hg=hg, hn=hn: e.tensor_tensor(out=hg[:], in0=hn[:], in1=gB[:], op=ALU.mult),
                               reads=[bhn, bwr], writes=[bhg])
                        sch.dma("pool", h2_scr[r0_:r0_ + 128, :], hg[:], reads=[bhg])
                        for kc in range(8):
                            sch.op("pe", lambda e, kc=kc, hi=hi: e.transpose(out=tph[:, kc, :], in_=hi[:, kc * 128:(kc + 1) * 128],
                                                                            identity=identb[:]), reads=[bhi, b_const], writes=[btph])
                        for kc in range(8):
                            sch.op("pe", lambda e, kc=kc, lo=lo: e.transpose(out=tpl[:, kc, :], in_=lo[:, kc * 128:(kc + 1) * 128],
                                                                            identity=identb[:]), reads=[blo, b_const], writes=[btpl])
                        hiT, bhiT = hiT_r.next()
                        loT, bloT = loT_r.next()
                        sch.op("act", lambda e, hiT=hiT: e.activation(out=hiT[:], in_=tph[:], func=AF.Copy), reads=[btph], writes=[bhiT])
                        sch.op("act", lambda e, loT=loT: e.activation(out=loT[:], in_=tpl[:], func=AF.Copy), reads=[btpl], writes=[bloT])
                        k = 0
                        for (aa, ba, ww) in ((hiT, bhiT, wrh), (hiT, bhiT, wrl), (loT, bloT, wrh)):
                            for kc in range(8):
                                sch.op("pe", lambda e, kc=kc, aa=aa, ww=ww, k=k: e.matmul(lps[:, 0:36], aa[:, kc, :], ww[:, kc, :],
                                                                                     start=(k == 0), stop=(k == 23)),
                                       reads=[ba, bwr], writes=[blps])
                                k += 1
                        sch.op("dve", lambda e, n=n: e.tensor_tensor(out=L[:, n, :], in0=lps[:, 0:36], in1=br[:], op=ALU.add),
                               reads=[blps, bwr], writes=[bL])

                e3m.close()
                cur[0] = e3
                def sbr(name, shape, dt=F32):
                    return e3.enter_context(nc.sbuf_tensor("rt" + name, list(shape), dt))
                bR = sch.buf("route")
                gl = L[:, :, 0:4]
                fl = L[:, :, 4:36]
                gmax = sbr("gmax", [128, NT]); G1 = sbr("G1", [128, NT, 4]); ge = sbr("ge", [128, NT, 4])
                gsum = sbr("gsum", [128, NT]); pen = sbr("pen", [128, NT, 4]); flm = sbr("flm", [128, NT, 32])
                m1 = sbr("m1", [128, NT]); oh1 = sbr("oh1", [128, NT, 32]); m2 = sbr("m2", [128, NT])
                oh2 = sbr("oh2", [128, NT, 32]); dd = sbr("dd", [128, NT])

                def R(eng, fn, extra_w=()):
                    sch.op(eng, fn, reads=[bL, bR, b_const], writes=[bR] + list(extra_w))

                def bc(a, n):
                    return a[:, :].unsqueeze(2).broadcast_to([128, NT, n])
                R("dve", lambda e: e.tensor_reduce(out=gmax[:], in_=gl, axis=AX.X, op=ALU.max))
                R("dve", lambda e: e.tensor_tensor(out=G1[:], in0=gl, in1=bc(gmax, 4), op=ALU.is_ge))
                R("dve", lambda e: e.tensor_tensor(out=ge[:], in0=gl, in1=bc(gmax, 4), op=ALU.subtract))
                R("act", lambda e: e.activation(out=ge[:], in_=ge[:], func=AF.Exp))
                R("dve", lambda e: e.tensor_reduce(out=gsum[:], in_=ge[:], axis=AX.X, op=ALU.add))
                R("dve", lambda e: e.reciprocal(out=gsum[:], in_=gsum[:]))
                R("dve", lambda e: e.tensor_scalar(out=pen[:], in0=G1[:], scalar1=1e9, scalar2=-1e9, op0=ALU.mult, op1=ALU.add))
                R("dve", lambda e: e.tensor_tensor(
                    out=flm[:].rearrange("p t (g e) -> p t g e", e=8), in0=fl.rearrange("p t (g e) -> p t g e", e=8),
                    in1=pen[:].unsqueeze(3).broadcast_to([128, NT, 4, 8]), op=ALU.add))
                R("dve", lambda e: e.tensor_reduce(out=m1[:], in_=flm[:], axis=AX.X, op=ALU.max))
                R("dve", lambda e: e.tensor_tensor(out=oh1[:], in0=flm[:], in1=bc(m1, 32), op=ALU.is_ge))
                R("dve", lambda e: e.scalar_tensor_tensor(out=flm[:], in0=oh1[:], scalar=-1e9, in1=flm[:], op0=ALU.mult, op1=ALU.add))
                R("dve", lambda e: e.tensor_reduce(out=m2[:], in_=flm[:], axis=AX.X, op=ALU.max))
                R("dve", lambda e: e.tensor_tensor(out=oh2[:], in0=flm[:], in1=bc(m2, 32), op=ALU.is_ge))
                R("dve", lambda e: e.tensor_tensor(out=dd[:], in0=m2[:], in1=m1[:], op=ALU.subtract))
                R("act", lambda e: e.activation(out=dd[:], in_=dd[:], func=AF.Exp))
                R("dve", lambda e: e.tensor_scalar(out=g1[:], in0=dd[:], scalar1=1.0, scalar2=None, op0=ALU.add), [bRt])
                R("dve", lambda e: e.reciprocal(out=g1[:], in_=g1[:]), [bRt])
                R("dve", lambda e: e.tensor_tensor(out=g1[:], in0=g1[:], in1=gsum[:], op=ALU.mult), [bRt])
                R("dve", lambda e: e.tensor_tensor(out=g2[:], in0=g1[:], in1=dd[:], op=ALU.mult), [bRt])
                selb = sbr("selb", [128, NT * 32], BF16)
                trif = sbr("trif", [128, 128]); trib = sbr("trib", [128, 128], BF16); oneb = sbr("oneb", [128, 128], BF16)
                iot = sbr("iot", [128, 256]); pid2 = sbr("pid2", [128, 1])
                sch.dma("sp", trif[:], tri_in[:, :], writes=[bR])
                sch.dma("sp", iot[:], iota_in[:, :], writes=[bR])
                sch.dma("sp", pid2[:], pidx_in[:, :], writes=[bR])
                R("dve", lambda e: e.tensor_copy(out=trib[:], in_=trif[:]))
                R("dve", lambda e: e.memset(oneb[:], 1.0))
                R("dve", lambda e: e.tensor_scalar(out=pid2[:], in0=pid2[:], scalar1=2.0, scalar2=None, op0=ALU.mult))
                R("dve", lambda e: e.tensor_tensor(out=selb[:], in0=oh1[:].rearrange("p t e -> p (t e)"),
                                                   in1=oh2[:].rearrange("p t e -> p (t e)"), op=ALU.add))
                Cs = sbr("Cs", [128, NT, 32]); Ta = sbr("Ta", [128, NT, 32]); Tb = sbr("Tb", [128, NT, 32]); T0 = sbr("T0", [128, NT, 32])
                with ExitStack() as e3r:
                    cps = [e3r.enter_context(nc.psum_tensor(f"rtc{j}", [128, 512], F32)) for j in range(4)]
                    tps = [e3r.enter_context(nc.psum_tensor(f"rtt{j}", [128, 512], F32)) for j in range(4)]
                    bcp, btp = sch.buf("rtc"), sch.buf("rtt")
                    Cf = Cs[:].rearrange("p t e -> p (t e)")
                    T0f = T0[:].rearrange("p t e -> p (t e)")
                    for j in range(4):
                        sch.op("pe", lambda e, j=j: e.matmul(cps[j][:], trib[:], selb[:, j * 512:(j + 1) * 512], start=True, stop=True),
                               reads=[bR], writes=[bcp])
                        sch.op("pe", lambda e, j=j: e.matmul(tps[j][:], oneb[:], selb[:, j * 512:(j + 1) * 512], start=True, stop=True),
                               reads=[bR], writes=[btp])
                    for j in range(4):
                        sch.op("act", lambda e, j=j: e.activation(out=Cf[:, j * 512:(j + 1) * 512], in_=cps[j][:], func=AF.Copy),
                               reads=[bcp], writes=[bR])
                        sch.op("dve", lambda e, j=j: e.tensor_copy(out=T0f[:, j * 512:(j + 1) * 512], in_=tps[j][:]),
                               reads=[btp], writes=[bR])
                src, dstb = T0, Ta
                for sft in (1, 2, 4, 8, 16, 32):
                    R("dve", lambda e, src=src, dstb=dstb, sft=sft: e.tensor_copy(out=dstb[:, 0:sft, :], in_=src[:, 0:sft, :]))
                    R("dve", lambda e, src=src, dstb=dstb, sft=sft: e.tensor_tensor(
                        out=dstb[:, sft:NT, :], in0=src[:, sft:NT, :], in1=src[:, 0:NT - sft, :], op=ALU.add))
                    src, dstb = dstb, (Tb if dstb is Ta else Ta)
                Inc = src
                cnt = sbr("cnt", [128, 32]); nbk = sbr("nbk", [128, 32]); cmp1 = sbr("cmp1", [128, 32, 128])
                i128 = sbr("i128", [128, 128]); sa = sbr("sa", [128, 32]); sb_ = sbr("sb_", [128, 32])
                psr = sbr("psr", [128, 32]); pend = sbr("pend", [128, 32])
                R("dve", lambda e: e.tensor_copy(out=cnt[:], in_=Inc[:, NT - 1, :]))
                R("dve", lambda e: e.tensor_scalar(out=i128[:], in0=iot[:, 0:128], scalar1=float(MOE_B), scalar2=None, op0=ALU.mult))
                R("dve", lambda e: e.tensor_tensor(out=cmp1[:], in0=cnt[:, :].unsqueeze(2).broadcast_to([128, 32, 128]),
                                                   in1=i128[:, :].unsqueeze(1).broadcast_to([128, 32, 128]), op=ALU.is_gt))
                R("dve", lambda e: e.tensor_reduce(out=nbk[:], in_=cmp1[:], axis=AX.X, op=ALU.add))
                src, dstb = nbk, sa
                for sft in (1, 2, 4, 8, 16):
                    R("dve", lambda e, src=src, dstb=dstb, sft=sft: e.tensor_copy(out=dstb[:, 0:sft], in_=src[:, 0:sft]))
                    R("dve", lambda e, src=src, dstb=dstb, sft=sft: e.tensor_tensor(
                        out=dstb[:, sft:32], in0=src[:, sft:32], in1=src[:, 0:32 - sft], op=ALU.add))
                    src, dstb = dstb, (sb_ if dstb is sa else sa)
                R("dve", lambda e, src=src: e.tensor_copy(out=pend[:], in_=src[:]))
                R("dve", lambda e: e.tensor_tensor(out=psr[:], in0=pend[:], in1=nbk[:], op=ALU.subtract))
                R("dve", lambda e: e.tensor_scalar(out=psr[:], in0=psr[:], scalar1=float(MOE_B), scalar2=None, op0=ALU.mult))
                R("dve", lambda e, Inc=Inc: e.tensor_tensor(out=Cs[:], in0=Cs[:], in1=Inc[:], op=ALU.add))
                R("dve", lambda e: e.tensor_tensor(out=Cs[:], in0=Cs[:], in1=T0[:], op=ALU.subtract))
                R("dve", lambda e: e.tensor_tensor(out=Cs[:], in0=Cs[:], in1=psr[:, :].unsqueeze(1).broadcast_to([128, NT, 32]), op=ALU.add))
                d1 = sbr("d1", [128, NT]); d2 = sbr("d2", [128, NT])
                R("dve", lambda e: e.tensor_tensor(out=Ta[:], in0=Cs[:], in1=oh1[:], op=ALU.mult))
                R("dve", lambda e: e.tensor_reduce(out=d1[:], in_=Ta[:], axis=AX.X, op=ALU.add))
                R("dve", lambda e: e.tensor_tensor(out=Tb[:], in0=Cs[:], in1=oh2[:], op=ALU.mult))
                R("dve", lambda e: e.tensor_reduce(out=d2[:], in_=Tb[:], axis=AX.X, op=ALU.add))
                R("dve", lambda e: e.tensor_copy(out=dst1i[:], in_=d1[:]), [bRt])
                R("dve", lambda e: e.tensor_copy(out=dst2i[:], in_=d2[:]), [bRt])
                cmp2 = sbr("cmp2", [128, NBLK, 32]); bex = sbr("bex", [128, NBLK]); need = sbr("need", [128, NBLK])
                w0 = sbr("w0", [128, NBLK]); w1f = sbr("w1f", [128, NBLK, 2])
                R("dve", lambda e: e.tensor_tensor(out=cmp2[:], in0=iot[:, 0:NBLK].unsqueeze(2).broadcast_to([128, NBLK, 32]),
                                                   in1=pend[:, :].unsqueeze(1).broadcast_to([128, NBLK, 32]), op=ALU.is_ge))
                R("dve", lambda e: e.tensor_reduce(out=bex[:], in_=cmp2[:], axis=AX.X, op=ALU.add))
                R("dve", lambda e: e.tensor_scalar(out=bex[:], in0=bex[:], scalar1=float(N_EXP - 1), scalar2=None, op0=ALU.min))
                R("dve", lambda e: e.memset(need[:], 1.0))
                if WSKIP:
                    R("dve", lambda e: e.tensor_tensor(out=need[:, NSET:NBLK], in0=bex[:, NSET:NBLK], in1=bex[:, 0:NBLK - NSET], op=ALU.not_equal))
                R("dve", lambda e: e.tensor_scalar(out=w0[:], in0=bex[:], scalar1=256.0, scalar2=pid2[:, 0:1], op0=ALU.mult, op1=ALU.add))
                R("dve", lambda e: e.tensor_tensor(out=w0[:], in0=w0[:], in1=need[:], op=ALU.mult))
                R("dve", lambda e: e.tensor_scalar(out=need[:], in0=need[:], scalar1=-float(1 << 30), scalar2=float(1 << 30),
                                                   op0=ALU.mult, op1=ALU.add))
                R("dve", lambda e: e.tensor_tensor(out=w0[:], in0=w0[:], in1=need[:], op=ALU.add))
                R("dve", lambda e: e.tensor_copy(out=w1f[:, :, 0], in_=w0[:]))
                R("dve", lambda e: e.tensor_scalar(out=w1f[:, :, 1], in0=w0[:], scalar1=1.0, scalar2=None, op0=ALU.add))
                R("dve", lambda e: e.tensor_copy(out=widx[:], in_=w1f[:].rearrange("p b h -> p (b h)")), [bRt])
                if debug:
                    dbg = sbr("dbg", [128, 4 * NT + 2 * NBLK])
                    R("dve", lambda e: e.tensor_copy(out=dbg[:, 0:NT], in_=d1[:]))
                    R("dve", lambda e: e.tensor_copy(out=dbg[:, NT:2 * NT], in_=d2[:]))
                    R("dve", lambda e: e.tensor_copy(out=dbg[:, 2 * NT:3 * NT], in_=g1[:]))
                    R("dve", lambda e: e.tensor_copy(out=dbg[:, 3 * NT:4 * NT], in_=g2[:]))
                    R("dve", lambda e: e.tensor_copy(out=dbg[:, 4 * NT:4 * NT + NBLK], in_=bex[:]))
                    R("dve", lambda e: e.tensor_copy(out=dbg[:, 4 * NT + NBLK:4 * NT + 2 * NBLK], in_=w0[:]))
                    sch.dma("sp", dbg_out[:, :], dbg[:], reads=[bR])
                sch.fence_all("sp")
                sch.fence_all("pool")
                hs_r = Ring(sch, e3, nc, "p3hs", [128, D], BF16, 3)
                for n in range(NT):
                    hs, bhs = hs_r.next()
                    sch.dma("sp", hs[:], h2_scr[n * 128:(n + 1) * 128, :], writes=[bhs])
                    for dsti in (dst1i, dst2i):
                        sch.dma("pool", xs_scr[:, :], hs[:], reads=[bhs, bRt],
                                indirect=dict(out_offset=bass.IndirectOffsetOnAxis(dsti[:, n:n + 1], 0), in_offset=None))
                sch.fence_all("sp")
                sch.fence_all("pool")

        if 4 in phases:
            with ExitStack() as e4:
                def sb4(name, shape, dt):
                    return e4.enter_context(nc.sbuf_tensor("p4" + name, list(shape), dt))
                SUB = MOE_B // 128
                Wb = [[sb4(f"W{i}_{s_}", [128, 4096], BF16) for s_ in range(NSET)] for i in range(3)]
                bWb = [[[sch.buf(f"p4W{i}_{s_}_{h}") for h in range(2)] for s_ in range(NSET)] for i in range(3)]
                wsrc = (w1, w3, w2)
                bnd_reg = nc.gpsimd.alloc_register("wbnd")
                nc.gpsimd.reg_mov(bnd_reg, N_EXP * 256 - 1)
                xs_r = Ring(sch, e4, nc, "p4xs", [128, D], BF16, 3)
                xT_r = Ring(sch, e4, nc, "p4xT", [128, 8, 128], BF16, 3)
                sg_r = Ring(sch, e4, nc, "p4sg", [128, 512], BF16, 2)
                am_r = Ring(sch, e4, nc, "p4am", [128, 512], BF16, 2)
                aT_r = Ring(sch, e4, nc, "p4aT", [128, 4, 128], BF16, 2 * SUB + 1)
                ys_r = Ring(sch, e4, nc, "p4ys", [128, D], F32, 3)
                with ExitStack() as e4p:
                    tpx = e4p.enter_context(nc.psum_tensor("p4tpx", [128, 8, 128], BF16))
                    btpx = sch.buf("p4tpx")
                    a1p = Ring(sch, e4p, nc, "p4a1", [128, 512], F32, 2, psum=True)
                    a3p = Ring(sch, e4p, nc, "p4a3", [128, 512], F32, 2, psum=True)
                    ypp = Ring(sch, e4p, nc, "p4yp", [128, 512], F32, 2, psum=True)
                    tpa = e4p.enter_context(nc.psum_tensor("p4tpa", [128, 4, 128], BF16))
                    btpa = sch.buf("p4tpa")

                    def stageX(b):
                        st_ = b % NSET
                        for i in range(3):
                            for hf in range(2):
                                sch.dma("pool", Wb[i][st_][:, hf * 2048:(hf + 1) * 2048], wsrc[i][:, :], reads=[bRt], writes=[bWb[i][st_][hf]],
                                        indirect=dict(out_offset=None, in_offset=bass.IndirectOffsetOnAxis(widx[:, 2 * b + hf:2 * b + hf + 1], 0),
                                                      bounds_check=bnd_reg, oob_is_err=False))
                        res = []
                        for sb_ in range(SUB):
                            r0_ = b * MOE_B + sb_ * 128
                            xs, bxs = xs_r.next()
                            sch.dma("sp", xs[:], xs_scr[r0_:r0_ + 128, :], writes=[bxs])
                            for kc in range(8):
                                sch.op("pe", lambda e, kc=kc, xs=xs: e.transpose(out=tpx[:, kc, :], in_=xs[:, kc * 128:(kc + 1) * 128],
                                                                                identity=identb[:]), reads=[bxs, b_const], writes=[btpx])
                            xT, bxT = xT_r.next()
                            sch.op("dve", lambda e, xT=xT: e.tensor_copy(out=xT[:], in_=tpx[:]), reads=[btpx], writes=[bxT])
                            a1, ba1 = a1p.next()
                            a3, ba3 = a3p.next()
                            for (ap_, bap, Wx, bWx) in ((a1, ba1, Wb[0][st_], bWb[0][st_]), (a3, ba3, Wb[1][st_], bWb[1][st_])):
                                for kc in range(8):
                                    sch.op("pe", lambda e, kc=kc, ap_=ap_, Wx=Wx, xT=xT: e.matmul(
                                        ap_[:], xT[:, kc, :], Wx[:, kc * 512:(kc + 1) * 512],
                                        start=(kc == 0), stop=(kc == 7)), reads=[bWx[kc // 4], bxT], writes=[bap])
                            sg, bsg = sg_r.next()
                            sch.op("act", lambda e, a1=a1, sg=sg: e.activation(out=sg[:], in_=a1[:], func=AF.Silu), reads=[ba1], writes=[bsg])
                            am, bam = am_r.next()
                            sch.op("dve", lambda e, am=am, a3=a3, sg=sg: e.tensor_tensor(out=am[:], in0=a3[:], in1=sg[:], op=ALU.mult),
                                   reads=[ba3, bsg], writes=[bam])
                            for nch in range(4):
                                sch.op("pe", lambda e, nch=nch, am=am: e.transpose(out=tpa[:, nch, :], in_=am[:, nch * 128:(nch + 1) * 128],
                                                                                  identity=identb[:]), reads=[bam, b_const], writes=[btpa])
                            aT, baT = aT_r.next()
                            sch.op("act", lambda e, aT=aT: e.activation(out=aT[:], in_=tpa[:], func=AF.Copy), reads=[btpa], writes=[baT])
                            res.append((aT, baT))
                        return res

                    def stageY(b, res):
                        st_ = b % NSET
                        W2b = Wb[2][st_]
                        for sb_, (aT, baT) in enumerate(res):
                            r0_ = b * MOE_B + sb_ * 128
                            ys, bys = ys_r.next()
                            for hf in range(2):
                                yp, byp = ypp.next()
                                for nch in range(4):
                                    sch.op("pe", lambda e, nch=nch, hf=hf, yp=yp, aT=aT, W2b=W2b: e.matmul(
                                        yp[:], aT[:, nch, :], W2b[:, nch * 1024 + hf * 512:nch * 1024 + (hf + 1) * 512],
                                        start=(nch == 0), stop=(nch == 3)), reads=[baT, bWb[2][st_][nch // 2]], writes=[byp])
                                sch.op("act", lambda e, hf=hf, yp=yp, ys=ys: e.activation(out=ys[:, hf * 512:(hf + 1) * 512], in_=yp[:], func=AF.Copy),
                                       reads=[byp], writes=[bys])
                            sch.dma("act", yb_scr[r0_:r0_ + 128, :], ys[:], reads=[bys])

                    prevx = None
                    for b in range(NBLK + 1):
                        curx = stageX(b) if b < NBLK else None
                        if prevx is not None:
                            stageY(b - 1, prevx)
                        prevx = curx
                sch.fence_all("sp")
                sch.fence_all("pool")
                sch.fence_all("act")
                y1_r = Ring(sch, e4, nc, "p4y1", [128, D], F32, 2)
                y2_r = Ring(sch, e4, nc, "p4y2", [128, D], F32, 2)
                xo_r = Ring(sch, e4, nc, "p4xo", [128, D], F32, 2)
                for n in range(NT):
                    y1, by1 = y1_r.next()
                    y2, by2 = y2_r.next()
                    xo, bxo = xo_r.next()
                    sch.dma("pool", y1[:], yb_scr[:, :], reads=[bRt], writes=[by1],
                            indirect=dict(out_offset=None, in_offset=bass.IndirectOffsetOnAxis(dst1i[:, n:n + 1], 0)))
                    sch.dma("pool", y2[:], yb_scr[:, :], reads=[bRt], writes=[by2],
                            indirect=dict(out_offset=None, in_offset=bass.IndirectOffsetOnAxis(dst2i[:, n:n + 1], 0)))
                    sch.dma("sp", xo[:], out[n * 128:(n + 1) * 128, :], writes=[bxo])
                    sch.op("dve", lambda e, n=n, y1=y1, xo=xo: e.scalar_tensor_tensor(
                        out=xo[:], in0=y1[:], scalar=g1[:, n:n + 1], in1=xo[:], op0=ALU.mult, op1=ALU.add),
                        reads=[by1, bxo, bRt], writes=[bxo])
                    sch.op("dve", lambda e, n=n, y2=y2, xo=xo: e.scalar_tensor_tensor(
                        out=xo[:], in0=y2[:], scalar=g2[:, n:n + 1], in1=xo[:], op0=ALU.mult, op1=ALU.add),
                        reads=[by2, bxo, bRt], writes=[bxo])
                    sch.dma("act", out[n * 128:(n + 1) * 128, :], xo[:], reads=[bxo])
                sch.fence_all("sp")
                sch.fence_all("pool")
                sch.fence_all("act")

        for en in ("sp", "pool", "act", "dve", "pe"):
            sch.fence_all(en)
        if _os.environ.get("DRYPRINT"):
            print("counts", {k: v.count for k, v in sch.engs.items()}, "nsem", sch.nsem)
    return nc


_CACHE = {}


def kernel(x, mem, g_mix, w_in, qk_gain, na_rpb, t5_table, g_mem, w_mem_kv, w_out, g_ffn, w_r1, b_r1, w_r2, b_r2,
           w1, w3, w2, _debug=False, _phases=(1, 2, 3, 4), _cores=8):
    f32 = np.float32
    x = np.asarray(x, f32); mem = np.asarray(mem, f32)
    na_steps, na_keys = na_plan()
    dil_tl = dil_plan()
    nab = na_bias_tiles(np.asarray(na_rpb, f32)[0], na_keys)
    dlb = dil_bias_tiles(np.asarray(t5_table, f32), dil_tl)
    key = (len(na_keys), len(dil_tl), _debug, tuple(_phases))
    if key not in _CACHE:
        _CACHE[key] = build_program(len(na_keys), len(dil_tl), na_steps, dil_tl, debug=_debug, phases=_phases)
    nc = _CACHE[key]

    def pk(v):
        return np.asarray(v, f32).reshape(8, 128).T
    gvec = np.ascontiguousarray(np.concatenate([pk(g_mix[0]), pk(g_mem[0]), pk(g_ffn[0])], axis=1))
    qg = np.asarray(qk_gain, f32)[0]
    gains = np.ascontiguousarray(np.tile(qg.reshape(6, 64), (1, 2)).T)
    shared = {
        "w_in": np.ascontiguousarray(np.asarray(w_in, f32)[0]),
        "w_mem": np.ascontiguousarray(np.asarray(w_mem_kv, f32)[0]),
        "w_out": np.ascontiguousarray(np.asarray(w_out, f32)[0]),
        "gvec": gvec, "gains": gains,
        "gffn_b": np.ascontiguousarray(np.broadcast_to(np.asarray(g_ffn, f32)[0][None, :], (128, D))),
        "ident": np.eye(128, dtype=f32),
        "na_bias": nab, "dil_bias": dlb,
        "w_r": np.ascontiguousarray(np.concatenate([np.asarray(w_r1, f32)[0], np.asarray(w_r2, f32)[0]], axis=1)),
        "b_r": np.ascontiguousarray(np.broadcast_to(
            np.concatenate([np.asarray(b_r1, f32)[0], np.asarray(b_r2, f32)[0]])[None, :], (128, 36))),
        "w1": np.ascontiguousarray(np.asarray(w1, f32)[0].reshape(N_EXP, 8, 128, 512).transpose(0, 2, 1, 3)).reshape(N_EXP * 256, 2048),
        "w3": np.ascontiguousarray(np.asarray(w3, f32)[0].reshape(N_EXP, 8, 128, 512).transpose(0, 2, 1, 3)).reshape(N_EXP * 256, 2048),
        "w2": np.ascontiguousarray(np.asarray(w2, f32)[0].reshape(N_EXP, 4, 128, 1024).transpose(0, 2, 1, 3)).reshape(N_EXP * 256, 2048),
        "iota": np.ascontiguousarray(np.broadcast_to(np.arange(256, dtype=f32)[None, :], (128, 256))),
        "pidx": np.arange(128, dtype=f32).reshape(128, 1),
        "tri": np.triu(np.ones((128, 128), f32), 1),
    }
    in_maps = []
    for c in range(_cores):
        m = dict(shared)
        m["x"] = np.ascontiguousarray(x[c])
        m["mem"] = np.ascontiguousarray(mem[c])
        in_maps.append(m)
    res = run_bass_kernel_spmd(nc, in_maps, core_ids=list(range(_cores)))
    if _debug:
        return res.results
    return np.stack([r["out"] for r in res.results], axis=0)
```

```python
import math
import os as _os
from contextlib import ExitStack
import numpy as np
import concourse.bass as bass
import concourse.mybir as mybir
from concourse.bass_utils import run_bass_kernel_spmd

F32 = mybir.dt.float32
BF16 = mybir.dt.bfloat16
I32 = mybir.dt.int32
AF = mybir.ActivationFunctionType
ALU = mybir.AluOpType
AX = mybir.AxisListType

S = 8192
D = 1024
NT = S // 128
EPS = 1e-6
NEG = -30000.0
N_EXP = 32
MOE_B = 256
NSET = 3
CAP = 2 * S + N_EXP * MOE_B
NBLK = CAP // MOE_B
SAME_ENGINE_SYNC = True
WSKIP = True
LNEXP = bool(int(_os.environ.get("LNEXP", "1")))


class Eng:
    def __init__(self, name, eng, sem):
        self.name, self.eng, self.sem = name, eng, sem
        self.count = 0
        self.waited = {}


class Buf:
    def __init__(self, name):
        self.name = name
        self.w = {}
        self.r = {}
        self.dw = None
        self.dr = None


class Sched:
    def __init__(self, nc, es):
        self.nc, self.es = nc, es
        self.engs = {}
        for nm, e in (("pe", nc.tensor), ("act", nc.scalar), ("dve", nc.vector), ("pool", nc.gpsimd), ("sp", nc.sync)):
            self.engs[nm] = Eng(nm, e, es.enter_context(nc.semaphore("sem_" + nm)))
        self.nsem = 5
        self.all_bufs = []

    def buf(self, name):
        b = Buf(name)
        self.all_bufs.append(b)
        return b

    def newsem(self, name):
        self.nsem += 1
        return self.es.enter_context(self.nc.semaphore(name))

    def _wait(self, E, key, sem, val):
        if val <= 0 or E.waited.get(key, 0) >= val:
            return
        E.eng.wait_ge(sem, val)
        E.waited[key] = val

    def _deps(self, E, reads, writes, skip_dw=False):
        for b in reads:
            for f, n in b.w.items():
                self._dep_eng(E, f, n)
            if b.dw is not None:
                self._wait(E, id(b.dw[0]), b.dw[0], b.dw[1])
        for b in writes:
            for f, n in b.w.items():
                self._dep_eng(E, f, n)
            for f, n in b.r.items():
                self._dep_eng(E, f, n)
            if b.dw is not None and not skip_dw:
                self._wait(E, id(b.dw[0]), b.dw[0], b.dw[1])
            if b.dr is not None:
                self._wait(E, id(b.dr[0]), b.dr[0], b.dr[1])

    def _dep_eng(self, E, f, n):
        if f == E.name and (f == "pe" or not SAME_ENGINE_SYNC):
            return
        F = self.engs[f]
        self._wait(E, f, F.sem, n)

    def op(self, ename, ins_fn, reads=(), writes=()):
        E = self.engs[ename]
        self._deps(E, reads, writes)
        ins = ins_fn(E.eng)
        E.count += 1
        ins.then_inc(E.sem, 1)
        for b in reads:
            b.r[ename] = E.count
        for b in writes:
            b.w[ename] = E.count
        return ins

    def dma(self, ename, out, in_, reads=(), writes=(), extra_wait=(), indirect=None, disjoint=False, **kw):
        E = self.engs[ename]
        self._deps(E, reads, writes, skip_dw=disjoint)
        for b in extra_wait:
            self._deps(E, [b], [])
        if indirect is None:
            ins = E.eng.dma_start(out=out, in_=in_, **kw)
        else:
            ins = E.eng.indirect_dma_start(out=out, in_=in_, **indirect, **kw)
        if writes:
            b = writes[0]
            if b.dw is None:
                b.dw = [self.newsem("ld_" + b.name), 0]
            b.dw[1] += 16
            ins.then_inc(b.dw[0], 16)
            for b2 in writes[1:]:
                b2.dw = b.dw
        elif reads:
            b = reads[0]
            if b.dr is None:
                b.dr = [self.newsem("st_" + b.name), 0]
            b.dr[1] += 16
            ins.then_inc(b.dr[0], 16)
        return ins

    def fence_stores(self, ename):
        E = self.engs[ename]
        for b in self.all_bufs:
            if b.dr is not None:
                self._wait(E, id(b.dr[0]), b.dr[0], b.dr[1])

    def fence_all(self, ename):
        E = self.engs[ename]
        for f, F in self.engs.items():
            if f != ename and F.count > 0:
                self._wait(E, f, F.sem, F.count)
        self.fence_stores(ename)


class Ring:
    def __init__(self, sch, es, nc, name, shape, dtype, n, psum=False):
        self.tiles, self.bufs, self.i = [], [], 0
        for k in range(n):
            nm = f"{name}{k}"
            t = es.enter_context(nc.psum_tensor(nm, shape, dtype) if psum else nc.sbuf_tensor(nm, shape, dtype))
            self.tiles.append(t)
            self.bufs.append(sch.buf(nm))

    def next(self):
        k = self.i % len(self.tiles)
        self.i += 1
        return self.tiles[k], self.bufs[k]


def t5_bucket_np(rel):
    nb = 16
    max_exact = 8
    n = np.abs(rel)
    upper = (rel > 0).astype(np.int32) * nb
    nf = np.maximum(n, 1).astype(np.float32)
    large = max_exact + (np.log(nf / max_exact) / math.log(1024 / max_exact) * (nb - max_exact)).astype(np.int32)
    large = np.minimum(large, nb - 1)
    return upper + np.where(n < max_exact, n, large)


def na_plan():
    def r0(r):
        return min(max(r - 4, 0), 120)
    tiles = {}
    steps = []
    for n in range(64):
        rows = (2 * n, 2 * n + 1)
        lo = min(r0(r) for r in rows)
        hi = max(r0(r) + 7 for r in rows)
        st = []
        for m in range(lo // 2, hi // 2 + 1):
            key = []
            for kl in range(2):
                kr = 2 * m + kl
                for ql in range(2):
                    r = rows[ql]
                    ok = r0(r) <= kr <= r0(r) + 7
                    key.append(kr - r + 7 if ok else -1)
            key = tuple(key)
            if all(k < 0 for k in key):
                continue
            if key not in tiles:
                tiles[key] = len(tiles)
            st.append((m, tiles[key]))
        steps.append(st)
    return steps, list(tiles.keys())


def na_bias_tiles(rpb, tile_keys):
    cols = np.arange(64)
    c0 = np.clip(cols - 8, 0, 48)
    kc = cols[:, None]
    qc = cols[None, :]
    colok = (kc >= c0[None, :]) & (kc < c0[None, :] + 16)
    dc = np.clip(kc - qc + 15, 0, 30)
    out = np.full((len(tile_keys), 128, 8, 128), NEG, np.float32)
    for t, key in enumerate(tile_keys):
        i = 0
        for kl in range(2):
            for ql in range(2):
                dr = key[i]
                i += 1
                if dr < 0:
                    continue
                blk = np.where(colok[None], rpb[:, dr][:, dc], NEG)
                out[t, kl * 64:(kl + 1) * 64, :, ql * 64:(ql + 1) * 64] = blk.transpose(1, 0, 2)
    return out


DIL = ((128, 1), (512, 4), (2048, 16))


def dil_plan():
    tl = []
    for g, (win, d) in enumerate(DIL):
        half = win // 2
        lo = -((half + 127) // 128)
        hi = (half + 127) // 128
        for o in range(lo, hi + 1):
            tl.append((g, o))
    return tl


def dil_bias_tiles(t5_table, tl):
    t5 = t5_table.reshape(32, 3, 4)
    out = np.full((len(tl), 128, 4, 128), NEG, np.float32)
    kk = np.arange(128)[:, None]
    qq = np.arange(128)[None, :]
    for t, (g, o) in enumerate(tl):
        win, d = DIL[g]
        rel = o * 128 + kk - qq
        ok = (np.abs(rel) <= win // 2) & (rel % d == 0)
        bk = t5_bucket_np(rel)
        for h in range(4):
            out[t, :, h, :] = np.where(ok, t5[bk, g, h], NEG)
    return out


QK_TILES = []
for i in range(4):
    QK_TILES.append((0 + 128 * i, 0))
for i in range(4):
    QK_TILES.append((512 + 128 * i, 1))
for i in range(6):
    QK_TILES.append((1536 + 128 * i, 2))
for i in range(6):
    QK_TILES.append((2304 + 128 * i, 3))
for i in range(2):
    QK_TILES.append((3840 + 128 * i, 4))
T_NAQ, T_NAK, T_DQ, T_DK, T_MQ = 0, 4, 8, 14, 20
NQK = len(QK_TILES)
V_SEGS = ((1024, 512, 0), (3072, 512, 512), (3584, 256, 1024))


def build_program(n_na_tiles, n_dil_tiles, na_steps, dil_tl, debug=False, phases=(1, 2, 3, 4)):
    nc = bass.Bass("TRN2", target_bir_lowering=False)
    dk = "ExternalOutput" if debug else "Internal"

    def din(name, shape, dt=F32):
        return nc.dram_tensor(name, list(shape), dt, kind="ExternalInput").ap()

    x = din("x", [S, D])
    mem = din("mem", [256, D])
    w_in = din("w_in", [D, 4096])
    w_mem = din("w_mem", [D, 512])
    w_out = din("w_out", [D, D])
    gvec = din("gvec", [128, 24])
    gains = din("gains", [128, 6])
    gffn_b = din("gffn_b", [128, D])
    ident_in = din("ident", [128, 128])
    na_bias = din("na_bias", [n_na_tiles, 128, 8, 128])
    dil_bias = din("dil_bias", [n_dil_tiles, 128, 4, 128])
    w_r = din("w_r", [D, 36])
    b_r = din("b_r", [128, 36])
    w1 = din("w1", [N_EXP * 256, 2048])
    w3 = din("w3", [N_EXP * 256, 2048])
    w2 = din("w2", [N_EXP * 256, 2048])
    iota_in = din("iota", [128, 256])
    pidx_in = din("pidx", [128, 1])
    tri_in = din("tri", [128, 128])
    out = nc.dram_tensor("out", [S, D], F32, kind="ExternalOutput").ap()

    qk_scr = nc.dram_tensor("qk_scr", [NQK, 128, S], BF16, kind=dk).ap()
    v_scr = nc.dram_tensor("v_scr", [S, 1280], BF16, kind=dk).ap()
    mix_scr = nc.dram_tensor("mix_scr", [S, D], BF16, kind=dk).ap()
    km_scr = nc.dram_tensor("km_scr", [2, 128, 256], BF16, kind=dk).ap()
    vm_scr = nc.dram_tensor("vm_scr", [256, 256], BF16, kind=dk).ap()
    h2_scr = nc.dram_tensor("h2_scr", [S, D], BF16, kind=dk).ap()
    xs_scr = nc.dram_tensor("xs_scr", [CAP, D], BF16, kind=dk).ap()
    yb_scr = nc.dram_tensor("yb_scr", [CAP, D], F32, kind=dk).ap()
    dbg_out = nc.dram_tensor("dbg_out", [128, 4 * NT + 2 * NBLK], F32, kind=dk).ap()

    with ExitStack() as es:
        sch = Sched(nc, es)

        def sb(name, shape, dt):
            return es.enter_context(nc.sbuf_tensor(name, list(shape), dt))

        def ps(name, shape, dt=F32):
            return es.enter_context(nc.psum_tensor(name, list(shape), dt))

        identf = sb("identf", [128, 128], F32)
        identb = sb("identb", [128, 128], BF16)
        blk1 = sb("blk1", [128, 128], BF16)
        gv = sb("gv", [128, 24], F32)
        gn = sb("gn", [128, 6], F32)
        epsb = sb("epsb", [128, 1], F32)
        b_const = sch.buf("const")
        sch.dma("sp", identf[:], ident_in[:, :], writes=[b_const])
        sch.dma("sp", gv[:], gvec[:, :], writes=[b_const])
        sch.dma("sp", gn[:], gains[:, :], writes=[b_const])
        sch.op("dve", lambda e: e.tensor_copy(out=identb[:], in_=identf[:]), reads=[b_const], writes=[b_const])
        sch.op("dve", lambda e: e.memset(blk1[:], 0.0), writes=[b_const])
        sch.op("dve", lambda e: e.memset(epsb[:], EPS), writes=[b_const])
        sch.op("dve", lambda e: e.memset(blk1[0:64, 0:64], 1.0), writes=[b_const])
        sch.op("dve", lambda e: e.memset(blk1[64:128, 64:128], 1.0), writes=[b_const])
        gq = gn[:].rearrange("p (a b) -> p a b", b=2)[:, :, 0:1]
        sch.op("dve", lambda e: e.tensor_scalar(out=gq, in0=gq, scalar1=0.125, scalar2=None, op0=ALU.mult),
               reads=[b_const], writes=[b_const])

        def projection(es1, src, n_tok, wsrc, ncols, gcol0, qk_tiles, qk_dst, v_segs, v_dst, tag):
            def sb1(name, shape, dt):
                return es1.enter_context(nc.sbuf_tensor(tag + name, list(shape), dt))
            W = sb1("W", [128, 8, ncols], BF16)
            bW = sch.buf(tag + "W")
            wst = Ring(sch, es1, nc, tag + "wst", [128, 2048], F32, 2)
            nhalf = (ncols + 2047) // 2048
            for kc in range(8):
                for hf in range(nhalf):
                    c0 = hf * 2048
                    cw = min(2048, ncols - c0)
                    t, b = wst.next()
                    sch.dma("sp", t[:, 0:cw], wsrc[kc * 128:(kc + 1) * 128, c0:c0 + cw], writes=[b])
                    if (kc * nhalf + hf) % 2 == 0:
                        sch.op("dve", lambda e, t=t, kc=kc, c0=c0, cw=cw: e.tensor_scalar(
                            out=W[:, kc, c0:c0 + cw], in0=t[:, 0:cw], scalar1=gv[:, gcol0 + kc:gcol0 + kc + 1],
                            scalar2=None, op0=ALU.mult), reads=[b, b_const], writes=[bW])
                    else:
                        sch.op("act", lambda e, t=t, kc=kc, c0=c0, cw=cw: e.activation(
                            out=W[:, kc, c0:c0 + cw], in_=t[:, 0:cw], func=AF.Copy, scale=gv[:, gcol0 + kc:gcol0 + kc + 1]),
                            reads=[b, b_const], writes=[bW])
            CH = min(512, n_tok)
            TT = CH // 128
            xt_r = Ring(sch, es1, nc, tag + "xt", [128, TT, D], F32, 2)
            hn_r = Ring(sch, es1, nc, tag + "hn", [128, TT, D], BF16, 2)
            hT_r = Ring(sch, es1, nc, tag + "hT", [128, 8, CH], BF16, 2)
            st_r = Ring(sch, es1, nc, tag + "st", [128, 8], F32, 2)
            junk = sb1("junk", [128, D], BF16)
            bjunk = sch.buf(tag + "junk")
            sq_r = Ring(sch, es1, nc, tag + "sq", [128, CH], BF16, 2)
            sd_r = Ring(sch, es1, nc, tag + "sd", [128, CH], F32, 2)
            rs_r = Ring(sch, es1, nc, tag + "rs", [128, CH], F32, 2)
            qn_r = Ring(sch, es1, nc, tag + "qn", [128, CH], BF16, 3)
            vo_r = Ring(sch, es1, nc, tag + "vo", [128, 1280], BF16, 2)
            tpb = [es1.enter_context(nc.psum_tensor(f"{tag}tp{i}", [128, 2, 512], BF16)) for i in range(1)]
            tpbuf = [sch.buf(tag + "tp0")]
            pbank = [None] + [es1.enter_context(nc.psum_tensor(f"{tag}pb{i}", [128, 512], F32)) for i in range(1, 8)]
            pbuf = [None] + [sch.buf(f"{tag}pb{i}") for i in range(1, 8)]
            for ck in range(n_tok // CH):
                t0 = ck * CH
                xt, bxt = xt_r.next()
                sch.dma("sp", xt[:], src[t0:t0 + CH, :].rearrange("(t p) d -> p t d", p=128), writes=[bxt])
                stt, bst = st_r.next()
                for t in range(TT):
                    sch.op("act", lambda e, t=t: e.activation(out=junk[:], in_=xt[:, t, :], func=AF.Square,
                                                               accum_out=stt[:, t:t + 1]),
                           reads=[bxt], writes=[bjunk, bst])
                sch.op("act", lambda e: e.activation(out=stt[:, 4:4 + TT], in_=stt[:, 0:TT], func=AF.Sqrt,
                                                      bias=EPS, scale=1.0 / D), reads=[bst], writes=[bst])
                sch.op("dve", lambda e: e.reciprocal(out=stt[:, 4:4 + TT], in_=stt[:, 4:4 + TT]),
                       reads=[bst], writes=[bst])
                hn, bhn = hn_r.next()
                for t in range(TT):
                    sch.op("act", lambda e, t=t: e.activation(out=hn[:, t, :], in_=xt[:, t, :], func=AF.Copy,
                                                               scale=stt[:, 4 + t:5 + t]),
                           reads=[bxt, bst], writes=[bhn])
                hT, bhT = hT_r.next()
                for kc in range(8):
                    bank = 0
                    sl = kc % 2
                    for t in range(TT):
                        sch.op("pe", lambda e, t=t, kc=kc, bank=bank, sl=sl: e.transpose(
                            out=tpb[bank][:, sl, t * 128:(t + 1) * 128], in_=hn[:, t, kc * 128:(kc + 1) * 128],
                            identity=identb[:]), reads=[bhn, b_const], writes=[tpbuf[bank]])
                    if sl == 1:
                        sch.op("dve", lambda e, kc=kc, bank=bank: e.tensor_copy(
                            out=hT[:, kc - 1:kc + 1, :], in_=tpb[bank][:, :, 0:CH]),
                            reads=[tpbuf[bank]], writes=[bhT])
                def qk_mm(j):
                    c0, gc = qk_tiles[j]
                    qb = 1 + (j % 3)
                    for kc in range(8):
                        sch.op("pe", lambda e, kc=kc, c0=c0, qb=qb: e.matmul(
                            pbank[qb][:, 0:CH], W[:, kc, c0:c0 + 128], hT[:, kc, :], start=(kc == 0), stop=(kc == 7)),
                            reads=[bW, bhT], writes=[pbuf[qb]])
                    sq, bsq = sq_r.next()
                    sch.op("act", lambda e, qb=qb, sq=sq: e.activation(out=sq[:], in_=pbank[qb][:, 0:CH], func=AF.Square),
                           reads=[pbuf[qb]], writes=[bsq])
                    return sq, bsq

                def qk_epi(j, sq, bsq):
                    c0, gc = qk_tiles[j]
                    qb = 1 + (j % 3)
                    sbk = 4 + (j % 2)
                    sch.op("pe", lambda e, sbk=sbk, sq=sq: e.matmul(pbank[sbk][:, 0:CH], blk1[:], sq[:], start=True, stop=True),
                           reads=[bsq, b_const], writes=[pbuf[sbk]])
                    sd, bsd = sd_r.next()
                    rs, brs = rs_r.next()
                    if LNEXP:
                        sch.op("act", lambda e, sbk=sbk, sd=sd: e.activation(out=sd[:], in_=pbank[sbk][:, 0:CH], func=AF.Ln,
                                                                             bias=epsb[:, 0:1], scale=1.0 / 64),
                               reads=[pbuf[sbk], b_const], writes=[bsd])
                        sch.op("act", lambda e, sd=sd, rs=rs: e.activation(out=rs[:], in_=sd[:], func=AF.Exp, scale=-0.5),
                               reads=[bsd], writes=[brs])
                    else:
                        sch.op("act", lambda e, sbk=sbk, sd=sd: e.activation(out=sd[:], in_=pbank[sbk][:, 0:CH], func=AF.Sqrt,
                                                                             bias=EPS, scale=1.0 / 64),
                               reads=[pbuf[sbk]], writes=[bsd])
                        sch.op("dve", lambda e, sd=sd, rs=rs: e.reciprocal(out=rs[:], in_=sd[:]), reads=[bsd], writes=[brs])
                    qn, bqn = qn_r.next()
                    sch.op("dve", lambda e, qb=qb, rs=rs, qn=qn, gc=gc: e.scalar_tensor_tensor(
                        out=qn[:], in0=pbank[qb][:, 0:CH], scalar=gn[:, gc:gc + 1], in1=rs[:], op0=ALU.mult, op1=ALU.mult),
                        reads=[pbuf[qb], brs, b_const], writes=[bqn])
                    sch.dma("pool", qk_dst(j, t0, CH), qn[:], reads=[bqn])

                prev = None
                for j in range(len(qk_tiles) + 1):
                    cur_ = qk_mm(j) if j < len(qk_tiles) else None
                    if prev is not None:
                        qk_epi(j - 1, *prev)
                    prev = cur_
                for t in range(TT):
                    vo, bvo = vo_r.next()
                    for si, (c0, cw, d0) in enumerate(v_segs):
                        vb = 6 + ((t * len(v_segs) + si) % 2)
                        for kc in range(8):
                            sch.op("pe", lambda e, kc=kc, c0=c0, cw=cw, vb=vb, t=t: e.matmul(
                                pbank[vb][:, 0:cw], hT[:, kc, t * 128:(t + 1) * 128], W[:, kc, c0:c0 + cw],
                                start=(kc == 0), stop=(kc == 7)), reads=[bW, bhT], writes=[pbuf[vb]])
                        sch.op("act", lambda e, vb=vb, cw=cw, d0=d0, vo=vo: e.activation(
                            out=vo[:, d0:d0 + cw], in_=pbank[vb][:, 0:cw], func=AF.Copy),
                            reads=[pbuf[vb]], writes=[bvo])
                    vw = sum(s_[1] for s_ in v_segs)
                    sch.dma("pool", v_dst(t0 + t * 128, vw), vo[:, 0:vw], reads=[bvo])

        if 1 in phases:
            with ExitStack() as es1:
                projection(es1, x, S, w_in, 4096, 0, QK_TILES,
                           lambda j, t0, n: qk_scr[j, :, t0:t0 + n], V_SEGS,
                           lambda t0, vw: v_scr[t0:t0 + 128, 0:vw], "p1")
                sch.fence_all("sp")
                sch.fence_all("pool")
            with ExitStack() as es1:
                projection(es1, mem, 256, w_mem, 512, 8, [(0, 5), (128, 5)],
                           lambda j, t0, n: km_scr[j, :, t0:t0 + n], ((256, 256, 0),),
                           lambda t0, vw: vm_scr[t0:t0 + 128, 0:vw], "pm")
                sch.fence_all("sp")
                sch.fence_all("pool")


        def attn_pass(tag, qsrc, ksrc, sk, vsrc, vruns, slots, steps_fn, OH, bias, mix_col0):
            with ExitStack() as e2:
                def sb2(name, shape, dt):
                    return e2.enter_context(nc.sbuf_tensor(tag + name, list(shape), dt))
                nkb = sk // 128
                QT = [sb2(f"QT{i}", [128, S], BF16) for i in range(len(qsrc))]
                KT = [sb2(f"KT{i}", [128, sk], BF16) for i in range(len(ksrc))]
                nv = sum(c for _, c in vruns)
                V1 = sb2("V1", [128, nkb, nv, 65], BF16)
                bin_ = sch.buf(tag + "in")
                SKIP = _os.environ.get("P2SKIP", "")
                for i, a in enumerate(qsrc if "q" not in SKIP else []):
                    for hf in range(2):
                        sch.dma("sp", QT[i][:, hf * S // 2:(hf + 1) * S // 2], a[:, hf * S // 2:(hf + 1) * S // 2], writes=[bin_], disjoint=True)
                for i, a in enumerate(ksrc if "k" not in SKIP else []):
                    sch.dma("sp", KT[i][:], a, writes=[bin_], disjoint=True)
                s0 = 0
                for (vc0, cnt) in (vruns if "v" not in SKIP else []):
                    for c in range(cnt):
                        vv = vsrc[:, vc0 + c * 64:vc0 + (c + 1) * 64].rearrange("(b p) d -> p b d", p=128)
                        for b0 in range(0, nkb, 16):
                            b1 = min(nkb, b0 + 16)
                            sch.dma("sp", V1[:, b0:b1, s0 + c, 0:64], vv[:, b0:b1, :], writes=[bin_], disjoint=True)
                    s0 += cnt
                if "m" not in SKIP:
                    sch.op("pool", lambda e: e.memset(V1[:, :, :, 64:65], 1.0), writes=[bin_])
                EB = None
                if bias is not None:
                    bd, h0, Hs = bias
                    ntile = bd.shape[0]
                    Hh = Hs // 2
                    EB = sb2("EB", [128, 2, ntile, Hh, 128], BF16)
                    ebs = Ring(sch, e2, nc, tag + "ebs", [128, Hs, 128], F32, 2)
                    for t in range(ntile):
                        st, bst = ebs.next()
                        sch.dma("sp", st[:], bd[t, :, h0:h0 + Hs, :], writes=[bst])
                        sch.op("act", lambda e, st=st, t=t: e.activation(
                            out=EB[:, :, t, :, :], in_=st[:].rearrange("p (i f) q -> p f i q", f=2), func=AF.Exp),
                               reads=[bst], writes=[bin_])
                sps = Ring(sch, e2, nc, tag + "sps", [128, 512], F32, 4, psum=True)
                ops_ = Ring(sch, e2, nc, tag + "ops", [128, 512], F32, 2, psum=True)
                pr = Ring(sch, e2, nc, tag + "P", [128, 512], BF16, 5)
                rd_r = Ring(sch, e2, nc, tag + "rd", [128, 8], F32, 2)
                mx_r = Ring(sch, e2, nc, tag + "mx", [128, 4, OH * 64], BF16, 2)
                mx_state = [None, None]
                recs = []
                for n in range(NT):
                    pend = ([], [])
                    groups = []
                    for (m, tid, sl) in steps_fn(n):
                        for si in sl:
                            hf = slots[si][2]
                            pend[hf].append((m, tid, slots[si]))
                            if len(pend[hf]) == 4:
                                groups.append(list(pend[hf]))
                                pend[hf].clear()
                    for hf in range(2):
                        if pend[hf]:
                            groups.append(list(pend[hf]))
                    for gi, grp in enumerate(groups):
                        recs.append(dict(n=n, grp=grp, first=(gi == 0), last=(gi == len(groups) - 1)))

                def emitA(rc):
                    n, grp = rc["n"], rc["grp"]
                    sp_, bsp = sps.next()
                    for i, (m, tid, (qt, kt, half, vs, oh, bh)) in enumerate(grp):
                        pl = slice(half * 64, half * 64 + 64)
                        sch.op("pe", lambda e, i=i, m=m, qt=qt, kt=kt, pl=pl, sp_=sp_: e.matmul(
                            sp_[:, i * 128:(i + 1) * 128], KT[kt][pl, m * 128:(m + 1) * 128],
                            QT[qt][pl, n * 128:(n + 1) * 128], start=True, stop=True),
                            reads=[bin_], writes=[bsp])
                    w = len(grp) * 128
                    P, bP = pr.next()
                    rc["P"], rc["bP"] = P, bP
                    sch.op("act", lambda e, sp_=sp_, P=P, w=w: e.activation(out=P[:, 0:w], in_=sp_[:, 0:w], func=AF.Exp),
                           reads=[bsp], writes=[bP])
                    if EB is not None:
                        def eoff(job):
                            return (job[2][2] * ntile + job[1]) * Hh + job[2][5] // 2
                        i = 0
                        while i < len(grp):
                            off = eoff(grp[i])
                            j = i + 1
                            while j < len(grp) and eoff(grp[j]) == off + (j - i):
                                j += 1
                            ebv = EB[:].rearrange("p f t h q -> p (f t h) q")[:, off:off + (j - i), :]
                            pv = P[:, i * 128:j * 128].rearrange("p (a q) -> p a q", q=128)
                            sch.op("dve", lambda e, pv=pv, ebv=ebv: e.tensor_tensor(out=pv, in0=pv, in1=ebv, op=ALU.mult),
                                   reads=[bP, bin_], writes=[bP])
                            i = j

                cur_o = [None, None]

                def emitB(rc):
                    n, grp, P, bP = rc["n"], rc["grp"], rc["P"], rc["bP"]
                    if rc["first"]:
                        cur_o[0], cur_o[1] = ops_.next()
                    oacc, boacc = cur_o
                    for i, (m, tid, (qt, kt, half, vs, oh, bh)) in enumerate(grp):
                        fst = rc["first"] and i == 0
                        lst = rc["last"] and i == len(grp) - 1
                        sch.op("pe", lambda e, i=i, m=m, vs=vs, oh=oh, P=P, oacc=oacc, fst=fst, lst=lst: e.matmul(
                            oacc[:, oh * 65:(oh + 1) * 65], P[:, i * 128:(i + 1) * 128], V1[:, m, vs, :],
                            start=fst, stop=lst, skip_group_check=True),
                            reads=[bP, bin_], writes=[boacc])
                    if not rc["last"]:
                        return
                    rd, brd = rd_r.next()
                    ov = oacc[:, 0:OH * 65].rearrange("p (h c) -> p h c", c=65)
                    sch.op("dve", lambda e, rd=rd, ov=ov: e.reciprocal(out=rd[:, 0:OH], in_=ov[:, :, 64]),
                           reads=[boacc], writes=[brd])
                    if n % 4 == 0:
                        mx_state[0], mx_state[1] = mx_r.next()
                    mx, bmx = mx_state
                    for h in range(OH):
                        sch.op("dve", lambda e, h=h, rd=rd, oacc=oacc, mx=mx: e.tensor_scalar(
                            out=mx[:, n % 4, h * 64:(h + 1) * 64], in0=oacc[:, h * 65:h * 65 + 64],
                            scalar1=rd[:, h:h + 1], scalar2=None, op0=ALU.mult),
                            reads=[boacc, brd], writes=[bmx])
                    if n % 4 == 3:
                        r0_ = (n - 3) * 128
                        sch.dma("pool", mix_scr[r0_:r0_ + 512, mix_col0:mix_col0 + OH * 64].rearrange("(t p) c -> p t c", p=128),
                                mx[:], reads=[bmx])

                LA = 2
                for i in range(len(recs) + LA):
                    if i < len(recs):
                        emitA(recs[i])
                    if i - LA >= 0:
                        emitB(recs[i - LA])
                sch.fence_all("sp")
                sch.fence_all("pool")

        SEL = _os.environ.get("P2SEL", "na0,na1,dl0,dl1,mm").split(",")
        if 2 in phases:
            for ps_ in range(2):
                if f"na{ps_}" not in SEL:
                    continue
                slots = [(h // 2, h // 2, h % 2, h, h, h) for h in range(4)]
                attn_pass(f"na{ps_}", [qk_scr[T_NAQ + 2 * ps_ + i] for i in range(2)],
                          [qk_scr[T_NAK + 2 * ps_ + i] for i in range(2)], S, v_scr, [(ps_ * 256, 4)], slots,
                          lambda n: [(m, tid, [0, 1, 2, 3]) for (m, tid) in na_steps[n]], 4,
                          (na_bias, ps_ * 4, 4), ps_ * 256)
            for ps_ in range(2):
                if f"dl{ps_}" not in SEL:
                    continue
                slots = []
                for g in range(3):
                    for hh in range(2):
                        slots.append((g, g, hh, g * 2 + hh, hh, hh))

                def dsteps(n):
                    st = []
                    for t, (g, o) in enumerate(dil_tl):
                        m = n + o
                        if 0 <= m < NT:
                            st.append((m, t, [g * 2, g * 2 + 1]))
                    return st
                attn_pass(f"dl{ps_}", [qk_scr[T_DQ + 2 * g + ps_] for g in range(3)],
                          [qk_scr[T_DK + 2 * g + ps_] for g in range(3)], S, v_scr,
                          [(512 + g * 256 + ps_ * 128, 2) for g in range(3)], slots, dsteps, 2,
                          (dil_bias, ps_ * 2, 2), 512 + ps_ * 128)
            slots = [(h // 2, h // 2, h % 2, h, h, 0) for h in range(4)]
            if "mm" in SEL:
              attn_pass("mm", [qk_scr[T_MQ + i] for i in range(2)], [km_scr[i] for i in range(2)], 256, vm_scr,
                      [(0, 4)], slots, lambda n: [(0, None, [0, 1, 2, 3]), (1, None, [0, 1, 2, 3])], 4, None, 768)


        g1 = sb("g1", [128, NT], F32)
        g2 = sb("g2", [128, NT], F32)
        dst1i = sb("dst1i", [128, NT], I32)
        dst2i = sb("dst2i", [128, NT], I32)
        widx = sb("widx", [128, NBLK * 2], I32)
        bRt = sch.buf("routeout")
        if 3 in phases:
            with ExitStack() as e3:
                cur = [e3]
                def sb3(name, shape, dt):
                    return cur[0].enter_context(nc.sbuf_tensor("p3" + name, list(shape), dt))
                L = sb3("L", [128, NT, 36], F32)
                bL = sch.buf("L")
                e3m = ExitStack()
                e3m.__enter__()
                cur[0] = e3m
                Wo = sb3("Wo", [128, 8, D], BF16)
                bWo = sch.buf("Wo")
                wst = Ring(sch, cur[0], nc, "p3wst", [128, D], F32, 2)
                for kc in range(8):
                    t, b = wst.next()
                    sch.dma("sp", t[:], w_out[kc * 128:(kc + 1) * 128, :], writes=[b])
                    sch.op("dve", lambda e, t=t, kc=kc: e.tensor_copy(out=Wo[:, kc, :], in_=t[:]), reads=[b], writes=[bWo])
                wr = sb3("wr", [128, 8, 36], F32)
                br = sb3("br", [128, 36], F32)
                gB = sb3("gB", [128, D], F32)
                bwr = sch.buf("wr")
                sch.dma("sp", wr[:], w_r.rearrange("(kc p) n -> p kc n", p=128), writes=[bwr])
                sch.dma("sp", br[:], b_r[:, :], writes=[bwr])
                sch.dma("sp", gB[:], gffn_b[:, :], writes=[bwr])
                for kc in range(8):
                    sch.op("dve", lambda e, kc=kc: e.tensor_scalar(out=wr[:, kc, :], in0=wr[:, kc, :],
                                                                    scalar1=gv[:, 16 + kc:17 + kc], scalar2=None, op0=ALU.mult),
                           reads=[bwr, b_const], writes=[bwr])
                wrh = sb3("wrh", [128, 8, 36], BF16)
                wrl = sb3("wrl", [128, 8, 36], BF16)
                sch.op("dve", lambda e: e.tensor_copy(out=wrh[:], in_=wr[:]), reads=[bwr], writes=[bwr])
                sch.op("dve", lambda e: e.tensor_tensor(out=wrl[:], in0=wr[:], in1=wrh[:], op=ALU.subtract), reads=[bwr], writes=[bwr])
                mx_r = Ring(sch, cur[0], nc, "p3mx", [128, D], BF16, 2)
                xt_r = Ring(sch, cur[0], nc, "p3xt", [128, D], F32, 2)
                mT_r = Ring(sch, cur[0], nc, "p3mT", [128, 8, 128], BF16, 2)
                x1_r = Ring(sch, cur[0], nc, "p3x1", [128, D], F32, 2)
                hn_r = Ring(sch, cur[0], nc, "p3hn", [128, D], F32, 2)
                hi_r = Ring(sch, cur[0], nc, "p3hi", [128, D], BF16, 3)
                lo_r = Ring(sch, cur[0], nc, "p3lo", [128, D], BF16, 3)
                hg_r = Ring(sch, cur[0], nc, "p3hg", [128, D], BF16, 2)
                hiT_r = Ring(sch, cur[0], nc, "p3hiT", [128, 8, 128], BF16, 2)
                loT_r = Ring(sch, cur[0], nc, "p3loT", [128, 8, 128], BF16, 2)
                st_r = Ring(sch, cur[0], nc, "p3st", [128, 4], F32, 2)
                junk = sb3("junk", [128, D], BF16)
                bjunk = sch.buf("p3junk")
                with ExitStack() as e3p:
                    tpm = e3p.enter_context(nc.psum_tensor("p3tpm", [128, 8, 128], BF16))
                    btpm = sch.buf("p3tpm")
                    yps = Ring(sch, e3p, nc, "p3yps", [128, 512], F32, 4, psum=True)
                    tph = e3p.enter_context(nc.psum_tensor("p3tph", [128, 8, 128], BF16))
                    tpl = e3p.enter_context(nc.psum_tensor("p3tpl", [128, 8, 128], BF16))
                    btph, btpl = sch.buf("p3tph"), sch.buf("p3tpl")
                    lps = e3p.enter_context(nc.psum_tensor("p3lps", [128, 512], F32))
                    blps = sch.buf("p3lps")
                    def p3A(n):
                        r0_ = n * 128
                        mxt, bmx = mx_r.next()
                        xt, bxt = xt_r.next()
                        sch.dma("sp", mxt[:], mix_scr[r0_:r0_ + 128, :], writes=[bmx])
                        sch.dma("sp", xt[:], x[r0_:r0_ + 128, :], writes=[bxt])
                        for kc in range(8):
                            sch.op("pe", lambda e, kc=kc, mxt=mxt: e.transpose(out=tpm[:, kc, :], in_=mxt[:, kc * 128:(kc + 1) * 128],
                                                                      identity=identb[:]), reads=[bmx, b_const], writes=[btpm])
                        mT, bmT = mT_r.next()
                        sch.op("dve", lambda e, mT=mT: e.tensor_copy(out=mT[:], in_=tpm[:]), reads=[btpm], writes=[bmT])
                        x1, bx1 = x1_r.next()
                        for hf in range(2):
                            yp, byp = yps.next()
                            for kc in range(8):
                                sch.op("pe", lambda e, kc=kc, hf=hf, yp=yp, mT=mT: e.matmul(
                                    yp[:], mT[:, kc, :], Wo[:, kc, hf * 512:(hf + 1) * 512], start=(kc == 0), stop=(kc == 7)),
                                    reads=[bmT, bWo], writes=[byp])
                            sch.op("dve", lambda e, hf=hf, yp=yp, x1=x1, xt=xt: e.tensor_tensor(
                                out=x1[:, hf * 512:(hf + 1) * 512], in0=yp[:], in1=xt[:, hf * 512:(hf + 1) * 512], op=ALU.add),
                                reads=[byp, bxt], writes=[bx1])
                        sch.dma("pool", out[r0_:r0_ + 128, :], x1[:], reads=[bx1])
                        stt, bst = st_r.next()
                        sch.op("act", lambda e, x1=x1, stt=stt: e.activation(out=junk[:], in_=x1[:], func=AF.Square,
                                                                           accum_out=stt[:, 0:1]), reads=[bx1], writes=[bjunk, bst])
                        sch.op("act", lambda e, stt=stt: e.activation(out=stt[:, 1:2], in_=stt[:, 0:1], func=AF.Sqrt,
                                                                       bias=EPS, scale=1.0 / D), reads=[bst], writes=[bst])
                        sch.op("dve", lambda e, stt=stt: e.reciprocal(out=stt[:, 1:2], in_=stt[:, 1:2]), reads=[bst], writes=[bst])
                        hn, bhn = hn_r.next()
                        sch.op("act", lambda e, hn=hn, x1=x1, stt=stt: e.activation(out=hn[:], in_=x1[:], func=AF.Copy,
                                                                                   scale=stt[:, 1:2]), reads=[bx1, bst], writes=[bhn])
                        hi, bhi = hi_r.next()
                        lo, blo = lo_r.next()
                        hg, bhg = hg_r.next()
                        sch.op("act", lambda e, hi=hi, hn=hn: e.activation(out=hi[:], in_=hn[:], func=AF.Copy), reads=[bhn], writes=[bhi])
                        sch.op("dve", lambda e, hi=hi, lo=lo, hn=hn: e.tensor_tensor(out=lo[:], in0=hn[:], in1=hi[:], op=ALU.subtract),
                               reads=[bhn, bhi], writes=[blo])
                        sch.op("pool", lambda e, hg=hg, hn=hn: e.tensor_tensor(out=hg[:], in0=hn[:], in1=gB[:], op=ALU.mult),
                               reads=[bhn, bwr], writes=[bhg])
                        sch.dma("pool", h2_scr[r0_:r0_ + 128, :], hg[:], reads=[bhg])
                        return hi, bhi, lo, blo

                    def p3B(n, hi, bhi, lo, blo):
                        for kc in range(8):
                            sch.op("pe", lambda e, kc=kc, hi=hi: e.transpose(out=tph[:, kc, :], in_=hi[:, kc * 128:(kc + 1) * 128],
                                                                            identity=identb[:]), reads=[bhi, b_const], writes=[btph])
                        for kc in range(8):
                            sch.op("pe", lambda e, kc=kc, lo=lo: e.transpose(out=tpl[:, kc, :], in_=lo[:, kc * 128:(kc + 1) * 128],
                                                                            identity=identb[:]), reads=[blo, b_const], writes=[btpl])
                        hiT, bhiT = hiT_r.next()
                        loT, bloT = loT_r.next()
                        sch.op("act", lambda e, hiT=hiT: e.activation(out=hiT[:], in_=tph[:], func=AF.Copy), reads=[btph], writes=[bhiT])
                        sch.op("act", lambda e, loT=loT: e.activation(out=loT[:], in_=tpl[:], func=AF.Copy), reads=[btpl], writes=[bloT])
                        k = 0
                        for (aa, ba, ww) in ((hiT, bhiT, wrh), (hiT, bhiT, wrl), (loT, bloT, wrh)):
                            for kc in range(8):
                                sch.op("pe", lambda e, kc=kc, aa=aa, ww=ww, k=k: e.matmul(lps[:, 0:36], aa[:, kc, :], ww[:, kc, :],
                                                                                     start=(k == 0), stop=(k == 23)),
                                       reads=[ba, bwr], writes=[blps])
                                k += 1
                        sch.op("dve", lambda e, n=n: e.tensor_tensor(out=L[:, n, :], in0=lps[:, 0:36], in1=br[:], op=ALU.add),
                               reads=[blps, bwr], writes=[bL])


                    prev3 = None
                    for n in range(NT + 1):
                        cur3 = p3A(n) if n < NT else None
                        if prev3 is not None:
                            p3B(n - 1, *prev3)
                        prev3 = cur3
                e3m.close()
                cur[0] = e3
                def sbr(name, shape, dt=F32):
                    return e3.enter_context(nc.sbuf_tensor("rt" + name, list(shape), dt))
                bR = sch.buf("route")
                gl = L[:, :, 0:4]
                fl = L[:, :, 4:36]
                gmax = sbr("gmax", [128, NT]); G1 = sbr("G1", [128, NT, 4]); ge = sbr("ge", [128, NT, 4])
                gsum = sbr("gsum", [128, NT]); pen = sbr("pen", [128, NT, 4]); flm = sbr("flm", [128, NT, 32])
                m1 = sbr("m1", [128, NT]); oh1 = sbr("oh1", [128, NT, 32]); m2 = sbr("m2", [128, NT])
                oh2 = sbr("oh2", [128, NT, 32]); dd = sbr("dd", [128, NT])

                def R(eng, fn, extra_w=()):
                    sch.op(eng, fn, reads=[bL, bR, b_const], writes=[bR] + list(extra_w))

                def bc(a, n):
                    return a[:, :].unsqueeze(2).broadcast_to([128, NT, n])
                R("dve", lambda e: e.tensor_reduce(out=gmax[:], in_=gl, axis=AX.X, op=ALU.max))
                R("dve", lambda e: e.tensor_tensor(out=G1[:], in0=gl, in1=bc(gmax, 4), op=ALU.is_ge))
                R("dve", lambda e: e.tensor_tensor(out=ge[:], in0=gl, in1=bc(gmax, 4), op=ALU.subtract))
                R("act", lambda e: e.activation(out=ge[:], in_=ge[:], func=AF.Exp))
                R("dve", lambda e: e.tensor_reduce(out=gsum[:], in_=ge[:], axis=AX.X, op=ALU.add))
                R("dve", lambda e: e.reciprocal(out=gsum[:], in_=gsum[:]))
                R("dve", lambda e: e.tensor_scalar(out=pen[:], in0=G1[:], scalar1=1e9, scalar2=-1e9, op0=ALU.mult, op1=ALU.add))
                R("dve", lambda e: e.tensor_tensor(
                    out=flm[:].rearrange("p t (g e) -> p t g e", e=8), in0=fl.rearrange("p t (g e) -> p t g e", e=8),
                    in1=pen[:].unsqueeze(3).broadcast_to([128, NT, 4, 8]), op=ALU.add))
                R("dve", lambda e: e.tensor_reduce(out=m1[:], in_=flm[:], axis=AX.X, op=ALU.max))
                R("dve", lambda e: e.tensor_tensor(out=oh1[:], in0=flm[:], in1=bc(m1, 32), op=ALU.is_ge))
                R("dve", lambda e: e.scalar_tensor_tensor(out=flm[:], in0=oh1[:], scalar=-1e9, in1=flm[:], op0=ALU.mult, op1=ALU.add))
                R("dve", lambda e: e.tensor_reduce(out=m2[:], in_=flm[:], axis=AX.X, op=ALU.max))
                R("dve", lambda e: e.tensor_tensor(out=oh2[:], in0=flm[:], in1=bc(m2, 32), op=ALU.is_ge))
                R("dve", lambda e: e.tensor_tensor(out=dd[:], in0=m2[:], in1=m1[:], op=ALU.subtract))
                R("act", lambda e: e.activation(out=dd[:], in_=dd[:], func=AF.Exp))
                R("dve", lambda e: e.tensor_scalar(out=g1[:], in0=dd[:], scalar1=1.0, scalar2=None, op0=ALU.add), [bRt])
                R("dve", lambda e: e.reciprocal(out=g1[:], in_=g1[:]), [bRt])
                R("dve", lambda e: e.tensor_tensor(out=g1[:], in0=g1[:], in1=gsum[:], op=ALU.mult), [bRt])
                R("dve", lambda e: e.tensor_tensor(out=g2[:], in0=g1[:], in1=dd[:], op=ALU.mult), [bRt])
                selb = sbr("selb", [128, NT * 32], BF16)
                trif = sbr("trif", [128, 128]); trib = sbr("trib", [128, 128], BF16); oneb = sbr("oneb", [128, 128], BF16)
                iot = sbr("iot", [128, 256]); pid2 = sbr("pid2", [128, 1])
                sch.dma("sp", trif[:], tri_in[:, :], writes=[bR])
                sch.dma("sp", iot[:], iota_in[:, :], writes=[bR])
                sch.dma("sp", pid2[:], pidx_in[:, :], writes=[bR])
                R("dve", lambda e: e.tensor_copy(out=trib[:], in_=trif[:]))
                R("dve", lambda e: e.memset(oneb[:], 1.0))
                R("dve", lambda e: e.tensor_scalar(out=pid2[:], in0=pid2[:], scalar1=2.0, scalar2=None, op0=ALU.mult))
                R("dve", lambda e: e.tensor_tensor(out=selb[:], in0=oh1[:].rearrange("p t e -> p (t e)"),
                                                   in1=oh2[:].rearrange("p t e -> p (t e)"), op=ALU.add))
                Cs = sbr("Cs", [128, NT, 32]); Ta = sbr("Ta", [128, NT, 32]); Tb = sbr("Tb", [128, NT, 32]); T0 = sbr("T0", [128, NT, 32])
                with ExitStack() as e3r:
                    cps = [e3r.enter_context(nc.psum_tensor(f"rtc{j}", [128, 512], F32)) for j in range(4)]
                    tps = [e3r.enter_context(nc.psum_tensor(f"rtt{j}", [128, 512], F32)) for j in range(4)]
                    bcp, btp = sch.buf("rtc"), sch.buf("rtt")
                    Cf = Cs[:].rearrange("p t e -> p (t e)")
                    T0f = T0[:].rearrange("p t e -> p (t e)")
                    for j in range(4):
                        sch.op("pe", lambda e, j=j: e.matmul(cps[j][:], trib[:], selb[:, j * 512:(j + 1) * 512], start=True, stop=True),
                               reads=[bR], writes=[bcp])
                        sch.op("pe", lambda e, j=j: e.matmul(tps[j][:], oneb[:], selb[:, j * 512:(j + 1) * 512], start=True, stop=True),
                               reads=[bR], writes=[btp])
                    for j in range(4):
                        sch.op("act", lambda e, j=j: e.activation(out=Cf[:, j * 512:(j + 1) * 512], in_=cps[j][:], func=AF.Copy),
                               reads=[bcp], writes=[bR])
                        sch.op("dve", lambda e, j=j: e.tensor_copy(out=T0f[:, j * 512:(j + 1) * 512], in_=tps[j][:]),
                               reads=[btp], writes=[bR])
                src, dstb = T0, Ta
                for sft in (1, 2, 4, 8, 16, 32):
                    R("dve", lambda e, src=src, dstb=dstb, sft=sft: e.tensor_copy(out=dstb[:, 0:sft, :], in_=src[:, 0:sft, :]))
                    R("dve", lambda e, src=src, dstb=dstb, sft=sft: e.tensor_tensor(
                        out=dstb[:, sft:NT, :], in0=src[:, sft:NT, :], in1=src[:, 0:NT - sft, :], op=ALU.add))
                    src, dstb = dstb, (Tb if dstb is Ta else Ta)
                Inc = src
                cnt = sbr("cnt", [128, 32]); nbk = sbr("nbk", [128, 32]); cmp1 = sbr("cmp1", [128, 32, 128])
                i128 = sbr("i128", [128, 128]); sa = sbr("sa", [128, 32]); sb_ = sbr("sb_", [128, 32])
                psr = sbr("psr", [128, 32]); pend = sbr("pend", [128, 32])
                R("dve", lambda e: e.tensor_copy(out=cnt[:], in_=Inc[:, NT - 1, :]))
                R("dve", lambda e: e.tensor_scalar(out=i128[:], in0=iot[:, 0:128], scalar1=float(MOE_B), scalar2=None, op0=ALU.mult))
                R("dve", lambda e: e.tensor_tensor(out=cmp1[:], in0=cnt[:, :].unsqueeze(2).broadcast_to([128, 32, 128]),
                                                   in1=i128[:, :].unsqueeze(1).broadcast_to([128, 32, 128]), op=ALU.is_gt))
                R("dve", lambda e: e.tensor_reduce(out=nbk[:], in_=cmp1[:], axis=AX.X, op=ALU.add))
                src, dstb = nbk, sa
                for sft in (1, 2, 4, 8, 16):
                    R("dve", lambda e, src=src, dstb=dstb, sft=sft: e.tensor_copy(out=dstb[:, 0:sft], in_=src[:, 0:sft]))
                    R("dve", lambda e, src=src, dstb=dstb, sft=sft: e.tensor_tensor(
                        out=dstb[:, sft:32], in0=src[:, sft:32], in1=src[:, 0:32 - sft], op=ALU.add))
                    src, dstb = dstb, (sb_ if dstb is sa else sa)
                R("dve", lambda e, src=src: e.tensor_copy(out=pend[:], in_=src[:]))
                R("dve", lambda e: e.tensor_tensor(out=psr[:], in0=pend[:], in1=nbk[:], op=ALU.subtract))
                R("dve", lambda e: e.tensor_scalar(out=psr[:], in0=psr[:], scalar1=float(MOE_B), scalar2=None, op0=ALU.mult))
                R("dve", lambda e, Inc=Inc: e.tensor_tensor(out=Cs[:], in0=Cs[:], in1=Inc[:], op=ALU.add))
                R("dve", lambda e: e.tensor_tensor(out=Cs[:], in0=Cs[:], in1=T0[:], op=ALU.subtract))
                R("dve", lambda e: e.tensor_tensor(out=Cs[:], in0=Cs[:], in1=psr[:, :].unsqueeze(1).broadcast_to([128, NT, 32]), op=ALU.add))
                d1 = sbr("d1", [128, NT]); d2 = sbr("d2", [128, NT])
                R("dve", lambda e: e.tensor_tensor(out=Ta[:], in0=Cs[:], in1=oh1[:], op=ALU.mult))
                R("dve", lambda e: e.tensor_reduce(out=d1[:], in_=Ta[:], axis=AX.X, op=ALU.add))
                R("dve", lambda e: e.tensor_tensor(out=Tb[:], in0=Cs[:], in1=oh2[:], op=ALU.mult))
                R("dve", lambda e: e.tensor_reduce(out=d2[:], in_=Tb[:], axis=AX.X, op=ALU.add))
                R("dve", lambda e: e.tensor_copy(out=dst1i[:], in_=d1[:]), [bRt])
                R("dve", lambda e: e.tensor_copy(out=dst2i[:], in_=d2[:]), [bRt])
                cmp2 = sbr("cmp2", [128, NBLK, 32]); bex = sbr("bex", [128, NBLK]); need = sbr("need", [128, NBLK])
                w0 = sbr("w0", [128, NBLK]); w1f = sbr("w1f", [128, NBLK, 2])
                R("dve", lambda e: e.tensor_tensor(out=cmp2[:], in0=iot[:, 0:NBLK].unsqueeze(2).broadcast_to([128, NBLK, 32]),
                                                   in1=pend[:, :].unsqueeze(1).broadcast_to([128, NBLK, 32]), op=ALU.is_ge))
                R("dve", lambda e: e.tensor_reduce(out=bex[:], in_=cmp2[:], axis=AX.X, op=ALU.add))
                R("dve", lambda e: e.tensor_scalar(out=bex[:], in0=bex[:], scalar1=float(N_EXP - 1), scalar2=None, op0=ALU.min))
                R("dve", lambda e: e.memset(need[:], 1.0))
                if WSKIP:
                    R("dve", lambda e: e.tensor_tensor(out=need[:, NSET:NBLK], in0=bex[:, NSET:NBLK], in1=bex[:, 0:NBLK - NSET], op=ALU.not_equal))
                R("dve", lambda e: e.tensor_scalar(out=w0[:], in0=bex[:], scalar1=256.0, scalar2=pid2[:, 0:1], op0=ALU.mult, op1=ALU.add))
                R("dve", lambda e: e.tensor_tensor(out=w0[:], in0=w0[:], in1=need[:], op=ALU.mult))
                R("dve", lambda e: e.tensor_scalar(out=need[:], in0=need[:], scalar1=-float(1 << 30), scalar2=float(1 << 30),
                                                   op0=ALU.mult, op1=ALU.add))
                R("dve", lambda e: e.tensor_tensor(out=w0[:], in0=w0[:], in1=need[:], op=ALU.add))
                R("dve", lambda e: e.tensor_copy(out=w1f[:, :, 0], in_=w0[:]))
                R("dve", lambda e: e.tensor_scalar(out=w1f[:, :, 1], in0=w0[:], scalar1=1.0, scalar2=None, op0=ALU.add))
                R("dve", lambda e: e.tensor_copy(out=widx[:], in_=w1f[:].rearrange("p b h -> p (b h)")), [bRt])
                if debug:
                    dbg = sbr("dbg", [128, 4 * NT + 2 * NBLK])
                    R("dve", lambda e: e.tensor_copy(out=dbg[:, 0:NT], in_=d1[:]))
                    R("dve", lambda e: e.tensor_copy(out=dbg[:, NT:2 * NT], in_=d2[:]))
                    R("dve", lambda e: e.tensor_copy(out=dbg[:, 2 * NT:3 * NT], in_=g1[:]))
                    R("dve", lambda e: e.tensor_copy(out=dbg[:, 3 * NT:4 * NT], in_=g2[:]))
                    R("dve", lambda e: e.tensor_copy(out=dbg[:, 4 * NT:4 * NT + NBLK], in_=bex[:]))
                    R("dve", lambda e: e.tensor_copy(out=dbg[:, 4 * NT + NBLK:4 * NT + 2 * NBLK], in_=w0[:]))
                    sch.dma("sp", dbg_out[:, :], dbg[:], reads=[bR])
                sch.fence_all("sp")
                sch.fence_all("pool")
                hs_r = Ring(sch, e3, nc, "p3hs", [128, D], BF16, 4)
                for n in range(NT):
                    hs, bhs = hs_r.next()
                    sch.dma("sp", hs[:], h2_scr[n * 128:(n + 1) * 128, :], writes=[bhs])
                    for dsti in (dst1i, dst2i):
                        sch.dma("pool", xs_scr[:, :], hs[:], reads=[bhs, bRt],
                                indirect=dict(out_offset=bass.IndirectOffsetOnAxis(dsti[:, n:n + 1], 0), in_offset=None))
                sch.fence_all("sp")
                sch.fence_all("pool")

        if 4 in phases:
            with ExitStack() as e4:
                def sb4(name, shape, dt):
                    return e4.enter_context(nc.sbuf_tensor("p4" + name, list(shape), dt))
                SUB = MOE_B // 128
                Wb = [[sb4(f"W{i}_{s_}", [128, 4096], BF16) for s_ in range(NSET)] for i in range(3)]
                bWb = [[sch.buf(f"p4W{i}_{s_}") for s_ in range(NSET)] for i in range(3)]
                wsrc = (w1, w3, w2)
                bnd_reg = nc.gpsimd.alloc_register("wbnd")
                nc.gpsimd.reg_mov(bnd_reg, N_EXP * 256 - 1)
                xs_r = Ring(sch, e4, nc, "p4xs", [128, D], BF16, 3)
                xT_r = Ring(sch, e4, nc, "p4xT", [128, 8, 128], BF16, 3)
                sg_r = Ring(sch, e4, nc, "p4sg", [128, 512], BF16, 2)
                am_r = Ring(sch, e4, nc, "p4am", [128, 512], BF16, 2)
                aT_r = Ring(sch, e4, nc, "p4aT", [128, 4, 128], BF16, 2 * SUB + 1)
                ys_r = Ring(sch, e4, nc, "p4ys", [128, D], F32, 3)
                with ExitStack() as e4p:
                    tpx = e4p.enter_context(nc.psum_tensor("p4tpx", [128, 8, 128], BF16))
                    btpx = sch.buf("p4tpx")
                    a1p = Ring(sch, e4p, nc, "p4a1", [128, 512], F32, 2, psum=True)
                    a3p = Ring(sch, e4p, nc, "p4a3", [128, 512], F32, 2, psum=True)
                    ypp = Ring(sch, e4p, nc, "p4yp", [128, 512], F32, 2, psum=True)
                    tpa = e4p.enter_context(nc.psum_tensor("p4tpa", [128, 4, 128], BF16))
                    btpa = sch.buf("p4tpa")

                    def stageX(b):
                        st_ = b % NSET
                        for i in range(3):
                            for hf in range(2):
                                sch.dma("pool", Wb[i][st_][:, hf * 2048:(hf + 1) * 2048], wsrc[i][:, :], reads=[bRt], writes=[bWb[i][st_]], disjoint=(hf == 1),
                                        indirect=dict(out_offset=None, in_offset=bass.IndirectOffsetOnAxis(widx[:, 2 * b + hf:2 * b + hf + 1], 0),
                                                      bounds_check=bnd_reg, oob_is_err=False))
                        res = []
                        for sb_ in range(SUB):
                            r0_ = b * MOE_B + sb_ * 128
                            xs, bxs = xs_r.next()
                            sch.dma("sp", xs[:], xs_scr[r0_:r0_ + 128, :], writes=[bxs])
                            for kc in range(8):
                                sch.op("pe", lambda e, kc=kc, xs=xs: e.transpose(out=tpx[:, kc, :], in_=xs[:, kc * 128:(kc + 1) * 128],
                                                                                identity=identb[:]), reads=[bxs, b_const], writes=[btpx])
                            xT, bxT = xT_r.next()
                            sch.op("dve", lambda e, xT=xT: e.tensor_copy(out=xT[:], in_=tpx[:]), reads=[btpx], writes=[bxT])
                            a1, ba1 = a1p.next()
                            a3, ba3 = a3p.next()
                            for (ap_, bap, Wx, bWx) in ((a1, ba1, Wb[0][st_], bWb[0][st_]), (a3, ba3, Wb[1][st_], bWb[1][st_])):
                                for kc in range(8):
                                    sch.op("pe", lambda e, kc=kc, ap_=ap_, Wx=Wx, xT=xT: e.matmul(
                                        ap_[:], xT[:, kc, :], Wx[:, kc * 512:(kc + 1) * 512],
                                        start=(kc == 0), stop=(kc == 7)), reads=[bWx, bxT], writes=[bap])
                            sg, bsg = sg_r.next()
                            sch.op("act", lambda e, a1=a1, sg=sg: e.activation(out=sg[:], in_=a1[:], func=AF.Silu), reads=[ba1], writes=[bsg])
                            am, bam = am_r.next()
                            sch.op("dve", lambda e, am=am, a3=a3, sg=sg: e.tensor_tensor(out=am[:], in0=a3[:], in1=sg[:], op=ALU.mult),
                                   reads=[ba3, bsg], writes=[bam])
                            for nch in range(4):
                                sch.op("pe", lambda e, nch=nch, am=am: e.transpose(out=tpa[:, nch, :], in_=am[:, nch * 128:(nch + 1) * 128],
                                                                                  identity=identb[:]), reads=[bam, b_const], writes=[btpa])
                            aT, baT = aT_r.next()
                            sch.op("act", lambda e, aT=aT: e.activation(out=aT[:], in_=tpa[:], func=AF.Copy), reads=[btpa], writes=[baT])
                            res.append((aT, baT))
                        return res

                    def stageY(b, res):
                        st_ = b % NSET
                        W2b = Wb[2][st_]
                        for sb_, (aT, baT) in enumerate(res):
                            r0_ = b * MOE_B + sb_ * 128
                            ys, bys = ys_r.next()
                            for hf in range(2):
                                yp, byp = ypp.next()
                                for nch in range(4):
                                    sch.op("pe", lambda e, nch=nch, hf=hf, yp=yp, aT=aT, W2b=W2b: e.matmul(
                                        yp[:], aT[:, nch, :], W2b[:, nch * 1024 + hf * 512:nch * 1024 + (hf + 1) * 512],
                                        start=(nch == 0), stop=(nch == 3)), reads=[baT, bWb[2][st_]], writes=[byp])
                                sch.op("act", lambda e, hf=hf, yp=yp, ys=ys: e.activation(out=ys[:, hf * 512:(hf + 1) * 512], in_=yp[:], func=AF.Copy),
                                       reads=[byp], writes=[bys])
                            sch.dma("act", yb_scr[r0_:r0_ + 128, :], ys[:], reads=[bys])

                    prevx = None
                    for b in range(NBLK + 1):
                        curx = stageX(b) if b < NBLK else None
                        if prevx is not None:
                            stageY(b - 1, prevx)
                        prevx = curx
                sch.fence_all("sp")
                sch.fence_all("pool")
                sch.fence_all("act")
                y1_r = Ring(sch, e4, nc, "p4y1", [128, D], F32, 3)
                y2_r = Ring(sch, e4, nc, "p4y2", [128, D], F32, 3)
                xo_r = Ring(sch, e4, nc, "p4xo", [128, D], F32, 3)
                for n in range(NT):
                    y1, by1 = y1_r.next()
                    y2, by2 = y2_r.next()
                    xo, bxo = xo_r.next()
                    sch.dma("pool", y1[:], yb_scr[:, :], reads=[bRt], writes=[by1],
                            indirect=dict(out_offset=None, in_offset=bass.IndirectOffsetOnAxis(dst1i[:, n:n + 1], 0)))
                    sch.dma("pool", y2[:], yb_scr[:, :], reads=[bRt], writes=[by2],
                            indirect=dict(out_offset=None, in_offset=bass.IndirectOffsetOnAxis(dst2i[:, n:n + 1], 0)))
                    sch.dma("sp", xo[:], out[n * 128:(n + 1) * 128, :], writes=[bxo])
                    sch.op("dve", lambda e, n=n, y1=y1, xo=xo: e.scalar_tensor_tensor(
                        out=xo[:], in0=y1[:], scalar=g1[:, n:n + 1], in1=xo[:], op0=ALU.mult, op1=ALU.add),
                        reads=[by1, bxo, bRt], writes=[bxo])
                    sch.op("dve", lambda e, n=n, y2=y2, xo=xo: e.scalar_tensor_tensor(
                        out=xo[:], in0=y2[:], scalar=g2[:, n:n + 1], in1=xo[:], op0=ALU.mult, op1=ALU.add),
                        reads=[by2, bxo, bRt], writes=[bxo])
                    sch.dma("act", out[n * 128:(n + 1) * 128, :], xo[:], reads=[bxo])
                sch.fence_all("sp")
                sch.fence_all("pool")
                sch.fence_all("act")

        for en in ("sp", "pool", "act", "dve", "pe"):
            sch.fence_all(en)
        if _os.environ.get("DRYPRINT"):
            print("counts", {k: v.count for k, v in sch.engs.items()}, "nsem", sch.nsem)
    return nc


_CACHE = {}


def kernel(x, mem, g_mix, w_in, qk_gain, na_rpb, t5_table, g_mem, w_mem_kv, w_out, g_ffn, w_r1, b_r1, w_r2, b_r2,
           w1, w3, w2, _debug=False, _phases=(1, 2, 3, 4), _cores=8):
    f32 = np.float32
    x = np.asarray(x, f32); mem = np.asarray(mem, f32)
    na_steps, na_keys = na_plan()
    dil_tl = dil_plan()
    nab = na_bias_tiles(np.asarray(na_rpb, f32)[0], na_keys)
    dlb = dil_bias_tiles(np.asarray(t5_table, f32), dil_tl)
    key = (len(na_keys), len(dil_tl), _debug, tuple(_phases))
    if key not in _CACHE:
        _CACHE[key] = build_program(len(na_keys), len(dil_tl), na_steps, dil_tl, debug=_debug, phases=_phases)
    nc = _CACHE[key]

    def pk(v):
        return np.asarray(v, f32).reshape(8, 128).T
    gvec = np.ascontiguousarray(np.concatenate([pk(g_mix[0]), pk(g_mem[0]), pk(g_ffn[0])], axis=1))
    qg = np.asarray(qk_gain, f32)[0]
    gains = np.ascontiguousarray(np.tile(qg.reshape(6, 64), (1, 2)).T)
    shared = {
        "w_in": np.ascontiguousarray(np.asarray(w_in, f32)[0]),
        "w_mem": np.ascontiguousarray(np.asarray(w_mem_kv, f32)[0]),
        "w_out": np.ascontiguousarray(np.asarray(w_out, f32)[0]),
        "gvec": gvec, "gains": gains,
        "gffn_b": np.ascontiguousarray(np.broadcast_to(np.asarray(g_ffn, f32)[0][None, :], (128, D))),
        "ident": np.eye(128, dtype=f32),
        "na_bias": nab, "dil_bias": dlb,
        "w_r": np.ascontiguousarray(np.concatenate([np.asarray(w_r1, f32)[0], np.asarray(w_r2, f32)[0]], axis=1)),
        "b_r": np.ascontiguousarray(np.broadcast_to(
            np.concatenate([np.asarray(b_r1, f32)[0], np.asarray(b_r2, f32)[0]])[None, :], (128, 36))),
        "w1": np.ascontiguousarray(np.asarray(w1, f32)[0].reshape(N_EXP, 8, 128, 512).transpose(0, 2, 1, 3)).reshape(N_EXP * 256, 2048),
        "w3": np.ascontiguousarray(np.asarray(w3, f32)[0].reshape(N_EXP, 8, 128, 512).transpose(0, 2, 1, 3)).reshape(N_EXP * 256, 2048),
        "w2": np.ascontiguousarray(np.asarray(w2, f32)[0].reshape(N_EXP, 4, 128, 1024).transpose(0, 2, 1, 3)).reshape(N_EXP * 256, 2048),
        "iota": np.ascontiguousarray(np.broadcast_to(np.arange(256, dtype=f32)[None, :], (128, 256))),
        "pidx": np.arange(128, dtype=f32).reshape(128, 1),
        "tri": np.triu(np.ones((128, 128), f32), 1),
    }
    in_maps = []
    for c in range(_cores):
        m = dict(shared)
        m["x"] = np.ascontiguousarray(x[c])
        m["mem"] = np.ascontiguousarray(mem[c])
        in_maps.append(m)
    res = run_bass_kernel_spmd(nc, in_maps, core_ids=list(range(_cores)))
    if _debug:
        return res.results
    return np.stack([r["out"] for r in res.results], axis=0)
```

```python
import math
import os as _os
from contextlib import ExitStack
import numpy as np
import concourse.bass as bass
import concourse.mybir as mybir
from concourse.bass_utils import run_bass_kernel_spmd

F32 = mybir.dt.float32
BF16 = mybir.dt.bfloat16
I32 = mybir.dt.int32
AF = mybir.ActivationFunctionType
ALU = mybir.AluOpType
AX = mybir.AxisListType

S = 8192
D = 1024
NT = S // 128
EPS = 1e-6
NEG = -30000.0
N_EXP = 32
MOE_B = 256
NSET = 3
CAP = 2 * S + N_EXP * MOE_B
NBLK = CAP // MOE_B
SAME_ENGINE_SYNC = True
WSKIP = True
LNEXP = bool(int(_os.environ.get("LNEXP", "1")))


class Eng:
    def __init__(self, name, eng, sem):
        self.name, self.eng, self.sem = name, eng, sem
        self.count = 0
        self.waited = {}


class Buf:
    def __init__(self, name):
        self.name = name
        self.w = {}
        self.r = {}
        self.dw = None
        self.dr = None


class Sched:
    def __init__(self, nc, es):
        self.nc, self.es = nc, es
        self.engs = {}
        for nm, e in (("pe", nc.tensor), ("act", nc.scalar), ("dve", nc.vector), ("pool", nc.gpsimd), ("sp", nc.sync)):
            self.engs[nm] = Eng(nm, e, es.enter_context(nc.semaphore("sem_" + nm)))
        self.nsem = 5
        self.all_bufs = []

    def buf(self, name):
        b = Buf(name)
        self.all_bufs.append(b)
        return b

    def newsem(self, name):
        self.nsem += 1
        return self.es.enter_context(self.nc.semaphore(name))

    def _wait(self, E, key, sem, val):
        if val <= 0 or E.waited.get(key, 0) >= val:
            return
        E.eng.wait_ge(sem, val)
        E.waited[key] = val

    def _deps(self, E, reads, writes, skip_dw=False):
        for b in reads:
            for f, n in b.w.items():
                self._dep_eng(E, f, n)
            if b.dw is not None:
                self._wait(E, id(b.dw[0]), b.dw[0], b.dw[1])
        for b in writes:
            for f, n in b.w.items():
                self._dep_eng(E, f, n)
            for f, n in b.r.items():
                self._dep_eng(E, f, n)
            if b.dw is not None and not skip_dw:
                self._wait(E, id(b.dw[0]), b.dw[0], b.dw[1])
            if b.dr is not None:
                self._wait(E, id(b.dr[0]), b.dr[0], b.dr[1])

    def _dep_eng(self, E, f, n):
        if f == E.name and (f == "pe" or not SAME_ENGINE_SYNC):
            return
        F = self.engs[f]
        self._wait(E, f, F.sem, n)

    def op(self, ename, ins_fn, reads=(), writes=()):
        E = self.engs[ename]
        self._deps(E, reads, writes)
        ins = ins_fn(E.eng)
        E.count += 1
        ins.then_inc(E.sem, 1)
        for b in reads:
            b.r[ename] = E.count
        for b in writes:
            b.w[ename] = E.count
        return ins

    def dma(self, ename, out, in_, reads=(), writes=(), extra_wait=(), indirect=None, disjoint=False, **kw):
        E = self.engs[ename]
        self._deps(E, reads, writes, skip_dw=disjoint)
        for b in extra_wait:
            self._deps(E, [b], [])
        if indirect is None:
            ins = E.eng.dma_start(out=out, in_=in_, **kw)
        else:
            ins = E.eng.indirect_dma_start(out=out, in_=in_, **indirect, **kw)
        if writes:
            b = writes[0]
            if b.dw is None:
                b.dw = [self.newsem("ld_" + b.name), 0]
            b.dw[1] += 16
            ins.then_inc(b.dw[0], 16)
            for b2 in writes[1:]:
                b2.dw = b.dw
        elif reads:
            b = reads[0]
            if b.dr is None:
                b.dr = [self.newsem("st_" + b.name), 0]
            b.dr[1] += 16
            ins.then_inc(b.dr[0], 16)
        return ins

    def fence_stores(self, ename):
        E = self.engs[ename]
        for b in self.all_bufs:
            if b.dr is not None:
                self._wait(E, id(b.dr[0]), b.dr[0], b.dr[1])

    def fence_all(self, ename):
        E = self.engs[ename]
        for f, F in self.engs.items():
            if f != ename and F.count > 0:
                self._wait(E, f, F.sem, F.count)
        self.fence_stores(ename)


class Ring:
    def __init__(self, sch, es, nc, name, shape, dtype, n, psum=False):
        self.tiles, self.bufs, self.i = [], [], 0
        for k in range(n):
            nm = f"{name}{k}"
            t = es.enter_context(nc.psum_tensor(nm, shape, dtype) if psum else nc.sbuf_tensor(nm, shape, dtype))
            self.tiles.append(t)
            self.bufs.append(sch.buf(nm))

    def next(self):
        k = self.i % len(self.tiles)
        self.i += 1
        return self.tiles[k], self.bufs[k]


def t5_bucket_np(rel):
    nb = 16
    max_exact = 8
    n = np.abs(rel)
    upper = (rel > 0).astype(np.int32) * nb
    nf = np.maximum(n, 1).astype(np.float32)
    large = max_exact + (np.log(nf / max_exact) / math.log(1024 / max_exact) * (nb - max_exact)).astype(np.int32)
    large = np.minimum(large, nb - 1)
    return upper + np.where(n < max_exact, n, large)


def na_plan():
    def r0(r):
        return min(max(r - 4, 0), 120)
    tiles = {}
    steps = []
    for n in range(64):
        rows = (2 * n, 2 * n + 1)
        lo = min(r0(r) for r in rows)
        hi = max(r0(r) + 7 for r in rows)
        st = []
        for m in range(lo // 2, hi // 2 + 1):
            key = []
            for kl in range(2):
                kr = 2 * m + kl
                for ql in range(2):
                    r = rows[ql]
                    ok = r0(r) <= kr <= r0(r) + 7
                    key.append(kr - r + 7 if ok else -1)
            key = tuple(key)
            if all(k < 0 for k in key):
                continue
            if key not in tiles:
                tiles[key] = len(tiles)
            st.append((m, tiles[key]))
        steps.append(st)
    return steps, list(tiles.keys())


def na_bias_tiles(rpb, tile_keys):
    cols = np.arange(64)
    c0 = np.clip(cols - 8, 0, 48)
    kc = cols[:, None]
    qc = cols[None, :]
    colok = (kc >= c0[None, :]) & (kc < c0[None, :] + 16)
    dc = np.clip(kc - qc + 15, 0, 30)
    out = np.full((len(tile_keys), 128, 8, 128), NEG, np.float32)
    for t, key in enumerate(tile_keys):
        i = 0
        for kl in range(2):
            for ql in range(2):
                dr = key[i]
                i += 1
                if dr < 0:
                    continue
                blk = np.where(colok[None], rpb[:, dr][:, dc], NEG)
                out[t, kl * 64:(kl + 1) * 64, :, ql * 64:(ql + 1) * 64] = blk.transpose(1, 0, 2)
    return out


DIL = ((128, 1), (512, 4), (2048, 16))


def dil_plan():
    tl = []
    for g, (win, d) in enumerate(DIL):
        half = win // 2
        lo = -((half + 127) // 128)
        hi = (half + 127) // 128
        for o in range(lo, hi + 1):
            tl.append((g, o))
    return tl


def dil_bias_tiles(t5_table, tl):
    t5 = t5_table.reshape(32, 3, 4)
    out = np.full((len(tl), 128, 4, 128), NEG, np.float32)
    kk = np.arange(128)[:, None]
    qq = np.arange(128)[None, :]
    for t, (g, o) in enumerate(tl):
        win, d = DIL[g]
        rel = o * 128 + kk - qq
        ok = (np.abs(rel) <= win // 2) & (rel % d == 0)
        bk = t5_bucket_np(rel)
        for h in range(4):
            out[t, :, h, :] = np.where(ok, t5[bk, g, h], NEG)
    return out


QK_TILES = []
for i in range(4):
    QK_TILES.append((0 + 128 * i, 0))
for i in range(4):
    QK_TILES.append((512 + 128 * i, 1))
for i in range(6):
    QK_TILES.append((1536 + 128 * i, 2))
for i in range(6):
    QK_TILES.append((2304 + 128 * i, 3))
for i in range(2):
    QK_TILES.append((3840 + 128 * i, 4))
T_NAQ, T_NAK, T_DQ, T_DK, T_MQ = 0, 4, 8, 14, 20
NQK = len(QK_TILES)
V_SEGS = ((1024, 512, 0), (3072, 512, 512), (3584, 256, 1024))


def build_program(n_na_tiles, n_dil_tiles, na_steps, dil_tl, debug=False, phases=(1, 2, 3, 4)):
    nc = bass.Bass("TRN2", target_bir_lowering=False)
    dk = "ExternalOutput" if debug else "Internal"

    def din(name, shape, dt=F32):
        return nc.dram_tensor(name, list(shape), dt, kind="ExternalInput").ap()

    x = din("x", [S, D])
    mem = din("mem", [256, D])
    w_in = din("w_in", [D, 4096])
    w_mem = din("w_mem", [D, 512])
    w_out = din("w_out", [D, D])
    gvec = din("gvec", [128, 24])
    gains = din("gains", [128, 6])
    gffn_b = din("gffn_b", [128, D])
    ident_in = din("ident", [128, 128])
    na_bias = din("na_bias", [n_na_tiles, 128, 8, 128])
    dil_bias = din("dil_bias", [n_dil_tiles, 128, 4, 128])
    w_r = din("w_r", [D, 36])
    b_r = din("b_r", [128, 36])
    w1 = din("w1", [N_EXP * 256, 2048])
    w3 = din("w3", [N_EXP * 256, 2048])
    w2 = din("w2", [N_EXP * 256, 2048])
    iota_in = din("iota", [128, 256])
    pidx_in = din("pidx", [128, 1])
    tri_in = din("tri", [128, 128])
    out = nc.dram_tensor("out", [S, D], F32, kind="ExternalOutput").ap()

    qk_scr = nc.dram_tensor("qk_scr", [NQK, 128, S], BF16, kind=dk).ap()
    v_scr = nc.dram_tensor("v_scr", [S, 1280], BF16, kind=dk).ap()
    mix_scr = nc.dram_tensor("mix_scr", [S, D], BF16, kind=dk).ap()
    km_scr = nc.dram_tensor("km_scr", [2, 128, 256], BF16, kind=dk).ap()
    vm_scr = nc.dram_tensor("vm_scr", [256, 256], BF16, kind=dk).ap()
    h2_scr = nc.dram_tensor("h2_scr", [S, D], BF16, kind=dk).ap()
    xs_scr = nc.dram_tensor("xs_scr", [CAP, D], BF16, kind=dk).ap()
    yb_scr = nc.dram_tensor("yb_scr", [CAP, D], F32, kind=dk).ap()
    dbg_out = nc.dram_tensor("dbg_out", [128, 4 * NT + 2 * NBLK], F32, kind=dk).ap()

    with ExitStack() as es:
        sch = Sched(nc, es)

        def sb(name, shape, dt):
            return es.enter_context(nc.sbuf_tensor(name, list(shape), dt))

        def ps(name, shape, dt=F32):
            return es.enter_context(nc.psum_tensor(name, list(shape), dt))

        identf = sb("identf", [128, 128], F32)
        identb = sb("identb", [128, 128], BF16)
        blk1 = sb("blk1", [128, 128], BF16)
        gv = sb("gv", [128, 24], F32)
        gn = sb("gn", [128, 6], F32)
        epsb = sb("epsb", [128, 1], F32)
        b_const = sch.buf("const")
        sch.dma("sp", identf[:], ident_in[:, :], writes=[b_const])
        sch.dma("sp", gv[:], gvec[:, :], writes=[b_const])
        sch.dma("sp", gn[:], gains[:, :], writes=[b_const])
        sch.op("dve", lambda e: e.tensor_copy(out=identb[:], in_=identf[:]), reads=[b_const], writes=[b_const])
        sch.op("dve", lambda e: e.memset(blk1[:], 0.0), writes=[b_const])
        sch.op("dve", lambda e: e.memset(epsb[:], EPS), writes=[b_const])
        sch.op("dve", lambda e: e.memset(blk1[0:64, 0:64], 1.0), writes=[b_const])
        sch.op("dve", lambda e: e.memset(blk1[64:128, 64:128], 1.0), writes=[b_const])
        gq = gn[:].rearrange("p (a b) -> p a b", b=2)[:, :, 0:1]
        sch.op("dve", lambda e: e.tensor_scalar(out=gq, in0=gq, scalar1=0.125, scalar2=None, op0=ALU.mult),
               reads=[b_const], writes=[b_const])

        def projection(es1, src, n_tok, wsrc, ncols, gcol0, qk_tiles, qk_dst, v_segs, v_dst, tag):
            def sb1(name, shape, dt):
                return es1.enter_context(nc.sbuf_tensor(tag + name, list(shape), dt))
            W = sb1("W", [128, 8, ncols], BF16)
            bW = sch.buf(tag + "W")
            wst = Ring(sch, es1, nc, tag + "wst", [128, 2048], F32, 2)
            nhalf = (ncols + 2047) // 2048
            for kc in range(8):
                for hf in range(nhalf):
                    c0 = hf * 2048
                    cw = min(2048, ncols - c0)
                    t, b = wst.next()
                    sch.dma("sp", t[:, 0:cw], wsrc[kc * 128:(kc + 1) * 128, c0:c0 + cw], writes=[b])
                    if (kc * nhalf + hf) % 2 == 0:
                        sch.op("dve", lambda e, t=t, kc=kc, c0=c0, cw=cw: e.tensor_scalar(
                            out=W[:, kc, c0:c0 + cw], in0=t[:, 0:cw], scalar1=gv[:, gcol0 + kc:gcol0 + kc + 1],
                            scalar2=None, op0=ALU.mult), reads=[b, b_const], writes=[bW])
                    else:
                        sch.op("act", lambda e, t=t, kc=kc, c0=c0, cw=cw: e.activation(
                            out=W[:, kc, c0:c0 + cw], in_=t[:, 0:cw], func=AF.Copy, scale=gv[:, gcol0 + kc:gcol0 + kc + 1]),
                            reads=[b, b_const], writes=[bW])
            CH = min(512, n_tok)
            TT = CH // 128
            xt_r = Ring(sch, es1, nc, tag + "xt", [128, TT, D], F32, 2)
            hn_r = Ring(sch, es1, nc, tag + "hn", [128, TT, D], BF16, 2)
            hT_r = Ring(sch, es1, nc, tag + "hT", [128, 8, CH], BF16, 2)
            st_r = Ring(sch, es1, nc, tag + "st", [128, 8], F32, 2)
            junk = sb1("junk", [128, D], BF16)
            bjunk = sch.buf(tag + "junk")
            sq_r = Ring(sch, es1, nc, tag + "sq", [128, CH], BF16, 2)
            sd_r = Ring(sch, es1, nc, tag + "sd", [128, CH], F32, 2)
            rs_r = Ring(sch, es1, nc, tag + "rs", [128, CH], F32, 2)
            qn_r = Ring(sch, es1, nc, tag + "qn", [128, CH], BF16, 3)
            vo_r = Ring(sch, es1, nc, tag + "vo", [128, 1280], BF16, 2)
            tpb = [es1.enter_context(nc.psum_tensor(f"{tag}tp{i}", [128, 2, 512], BF16)) for i in range(1)]
            tpbuf = [sch.buf(tag + "tp0")]
            pbank = [None] + [es1.enter_context(nc.psum_tensor(f"{tag}pb{i}", [128, 512], F32)) for i in range(1, 8)]
            pbuf = [None] + [sch.buf(f"{tag}pb{i}") for i in range(1, 8)]
            for ck in range(n_tok // CH):
                t0 = ck * CH
                xt, bxt = xt_r.next()
                sch.dma("sp", xt[:], src[t0:t0 + CH, :].rearrange("(t p) d -> p t d", p=128), writes=[bxt])
                stt, bst = st_r.next()
                for t in range(TT):
                    sch.op("act", lambda e, t=t: e.activation(out=junk[:], in_=xt[:, t, :], func=AF.Square,
                                                               accum_out=stt[:, t:t + 1]),
                           reads=[bxt], writes=[bjunk, bst])
                sch.op("act", lambda e: e.activation(out=stt[:, 4:4 + TT], in_=stt[:, 0:TT], func=AF.Sqrt,
                                                      bias=EPS, scale=1.0 / D), reads=[bst], writes=[bst])
                sch.op("dve", lambda e: e.reciprocal(out=stt[:, 4:4 + TT], in_=stt[:, 4:4 + TT]),
                       reads=[bst], writes=[bst])
                hn, bhn = hn_r.next()
                for t in range(TT):
                    sch.op("act", lambda e, t=t: e.activation(out=hn[:, t, :], in_=xt[:, t, :], func=AF.Copy,
                                                               scale=stt[:, 4 + t:5 + t]),
                           reads=[bxt, bst], writes=[bhn])
                hT, bhT = hT_r.next()
                for kc in range(8):
                    bank = 0
                    sl = kc % 2
                    for t in range(TT):
                        sch.op("pe", lambda e, t=t, kc=kc, bank=bank, sl=sl: e.transpose(
                            out=tpb[bank][:, sl, t * 128:(t + 1) * 128], in_=hn[:, t, kc * 128:(kc + 1) * 128],
                            identity=identb[:]), reads=[bhn, b_const], writes=[tpbuf[bank]])
                    if sl == 1:
                        sch.op("dve", lambda e, kc=kc, bank=bank: e.tensor_copy(
                            out=hT[:, kc - 1:kc + 1, :], in_=tpb[bank][:, :, 0:CH]),
                            reads=[tpbuf[bank]], writes=[bhT])
                def qk_mm(j):
                    c0, gc = qk_tiles[j]
                    qb = 1 + (j % 3)
                    for kc in range(8):
                        sch.op("pe", lambda e, kc=kc, c0=c0, qb=qb: e.matmul(
                            pbank[qb][:, 0:CH], W[:, kc, c0:c0 + 128], hT[:, kc, :], start=(kc == 0), stop=(kc == 7)),
                            reads=[bW, bhT], writes=[pbuf[qb]])
                    sq, bsq = sq_r.next()
                    sch.op("act", lambda e, qb=qb, sq=sq: e.activation(out=sq[:], in_=pbank[qb][:, 0:CH], func=AF.Square),
                           reads=[pbuf[qb]], writes=[bsq])
                    return sq, bsq

                def qk_epi(j, sq, bsq):
                    c0, gc = qk_tiles[j]
                    qb = 1 + (j % 3)
                    sbk = 4 + (j % 2)
                    sch.op("pe", lambda e, sbk=sbk, sq=sq: e.matmul(pbank[sbk][:, 0:CH], blk1[:], sq[:], start=True, stop=True),
                           reads=[bsq, b_const], writes=[pbuf[sbk]])
                    sd, bsd = sd_r.next()
                    rs, brs = rs_r.next()
                    if LNEXP:
                        sch.op("act", lambda e, sbk=sbk, sd=sd: e.activation(out=sd[:], in_=pbank[sbk][:, 0:CH], func=AF.Ln,
                                                                             bias=epsb[:, 0:1], scale=1.0 / 64),
                               reads=[pbuf[sbk], b_const], writes=[bsd])
                        sch.op("act", lambda e, sd=sd, rs=rs: e.activation(out=rs[:], in_=sd[:], func=AF.Exp, scale=-0.5),
                               reads=[bsd], writes=[brs])
                    else:
                        sch.op("act", lambda e, sbk=sbk, sd=sd: e.activation(out=sd[:], in_=pbank[sbk][:, 0:CH], func=AF.Sqrt,
                                                                             bias=EPS, scale=1.0 / 64),
                               reads=[pbuf[sbk]], writes=[bsd])
                        sch.op("dve", lambda e, sd=sd, rs=rs: e.reciprocal(out=rs[:], in_=sd[:]), reads=[bsd], writes=[brs])
                    qn, bqn = qn_r.next()
                    sch.op("dve", lambda e, qb=qb, rs=rs, qn=qn, gc=gc: e.scalar_tensor_tensor(
                        out=qn[:], in0=pbank[qb][:, 0:CH], scalar=gn[:, gc:gc + 1], in1=rs[:], op0=ALU.mult, op1=ALU.mult),
                        reads=[pbuf[qb], brs, b_const], writes=[bqn])
                    sch.dma("pool", qk_dst(j, t0, CH), qn[:], reads=[bqn])

                prev = None
                for j in range(len(qk_tiles) + 1):
                    cur_ = qk_mm(j) if j < len(qk_tiles) else None
                    if prev is not None:
                        qk_epi(j - 1, *prev)
                    prev = cur_
                for t in range(TT):
                    vo, bvo = vo_r.next()
                    for si, (c0, cw, d0) in enumerate(v_segs):
                        vb = 6 + ((t * len(v_segs) + si) % 2)
                        for kc in range(8):
                            sch.op("pe", lambda e, kc=kc, c0=c0, cw=cw, vb=vb, t=t: e.matmul(
                                pbank[vb][:, 0:cw], hT[:, kc, t * 128:(t + 1) * 128], W[:, kc, c0:c0 + cw],
                                start=(kc == 0), stop=(kc == 7)), reads=[bW, bhT], writes=[pbuf[vb]])
                        sch.op("act", lambda e, vb=vb, cw=cw, d0=d0, vo=vo: e.activation(
                            out=vo[:, d0:d0 + cw], in_=pbank[vb][:, 0:cw], func=AF.Copy),
                            reads=[pbuf[vb]], writes=[bvo])
                    vw = sum(s_[1] for s_ in v_segs)
                    sch.dma("pool", v_dst(t0 + t * 128, vw), vo[:, 0:vw], reads=[bvo])

        if 1 in phases:
            with ExitStack() as es1:
                projection(es1, x, S, w_in, 4096, 0, QK_TILES,
                           lambda j, t0, n: qk_scr[j, :, t0:t0 + n], V_SEGS,
                           lambda t0, vw: v_scr[t0:t0 + 128, 0:vw], "p1")
                sch.fence_all("sp")
                sch.fence_all("pool")
            with ExitStack() as es1:
                projection(es1, mem, 256, w_mem, 512, 8, [(0, 5), (128, 5)],
                           lambda j, t0, n: km_scr[j, :, t0:t0 + n], ((256, 256, 0),),
                           lambda t0, vw: vm_scr[t0:t0 + 128, 0:vw], "pm")
                sch.fence_all("sp")
                sch.fence_all("pool")


        def attn_pass(tag, qsrc, ksrc, sk, vsrc, vruns, slots, steps_fn, OH, bias, mix_col0):
            with ExitStack() as e2:
                def sb2(name, shape, dt):
                    return e2.enter_context(nc.sbuf_tensor(tag + name, list(shape), dt))
                nkb = sk // 128
                QT = [sb2(f"QT{i}", [128, S], BF16) for i in range(len(qsrc))]
                KT = [sb2(f"KT{i}", [128, sk], BF16) for i in range(len(ksrc))]
                nv = sum(c for _, c in vruns)
                V1 = sb2("V1", [128, nkb, nv, 65], BF16)
                bin_ = sch.buf(tag + "in")
                SKIP = _os.environ.get("P2SKIP", "")
                for i, a in enumerate(qsrc if "q" not in SKIP else []):
                    for hf in range(2):
                        sch.dma("sp", QT[i][:, hf * S // 2:(hf + 1) * S // 2], a[:, hf * S // 2:(hf + 1) * S // 2], writes=[bin_], disjoint=True)
                for i, a in enumerate(ksrc if "k" not in SKIP else []):
                    sch.dma("sp", KT[i][:], a, writes=[bin_], disjoint=True)
                s0 = 0
                for (vc0, cnt) in (vruns if "v" not in SKIP else []):
                    for c in range(cnt):
                        vv = vsrc[:, vc0 + c * 64:vc0 + (c + 1) * 64].rearrange("(b p) d -> p b d", p=128)
                        for b0 in range(0, nkb, 16):
                            b1 = min(nkb, b0 + 16)
                            sch.dma("sp", V1[:, b0:b1, s0 + c, 0:64], vv[:, b0:b1, :], writes=[bin_], disjoint=True)
                    s0 += cnt
                if "m" not in SKIP:
                    sch.op("pool", lambda e: e.memset(V1[:, :, :, 64:65], 1.0), writes=[bin_])
                EB = None
                if bias is not None:
                    bd, h0, Hs = bias
                    ntile = bd.shape[0]
                    Hh = Hs // 2
                    EB = sb2("EB", [128, 2, ntile, Hh, 128], BF16)
                    ebs = Ring(sch, e2, nc, tag + "ebs", [128, Hs, 128], F32, 1)
                    for t in range(ntile):
                        st, bst = ebs.next()
                        sch.dma("sp", st[:], bd[t, :, h0:h0 + Hs, :], writes=[bst])
                        sch.op("act", lambda e, st=st, t=t: e.activation(
                            out=EB[:, :, t, :, :], in_=st[:].rearrange("p (i f) q -> p f i q", f=2), func=AF.Exp),
                               reads=[bst], writes=[bin_])
                sps = Ring(sch, e2, nc, tag + "sps", [128, 512], F32, 4, psum=True)
                ops_ = Ring(sch, e2, nc, tag + "ops", [128, 512], F32, 2, psum=True)
                pr = Ring(sch, e2, nc, tag + "P", [128, 512], BF16, 5)
                rd_r = Ring(sch, e2, nc, tag + "rd", [128, 8], F32, 2)
                mx_r = Ring(sch, e2, nc, tag + "mx", [128, 4, OH * 64], BF16, 2)
                mx_state = [None, None]
                recs = []
                for n in range(NT):
                    pend = ([], [])
                    groups = []
                    for (m, tid, sl) in steps_fn(n):
                        for si in sl:
                            hf = slots[si][2]
                            pend[hf].append((m, tid, slots[si]))
                            if len(pend[hf]) == 4:
                                groups.append(list(pend[hf]))
                                pend[hf].clear()
                    for hf in range(2):
                        if pend[hf]:
                            groups.append(list(pend[hf]))
                    for gi, grp in enumerate(groups):
                        recs.append(dict(n=n, grp=grp, first=(gi == 0), last=(gi == len(groups) - 1)))

                def emitA(rcs):
                    for rc in rcs:
                        rc["sp"], rc["bsp"] = sps.next()
                    for i in range(4):
                        for rc in rcs:
                            grp = rc["grp"]
                            if i >= len(grp):
                                continue
                            n = rc["n"]
                            (m, tid, (qt, kt, half, vs, oh, bh)) = grp[i]
                            pl = slice(half * 64, half * 64 + 64)
                            sp_ = rc["sp"]
                            sch.op("pe", lambda e, i=i, m=m, qt=qt, kt=kt, pl=pl, sp_=sp_, n=n: e.matmul(
                                sp_[:, i * 128:(i + 1) * 128], KT[kt][pl, m * 128:(m + 1) * 128],
                                QT[qt][pl, n * 128:(n + 1) * 128], start=True, stop=True),
                                reads=[bin_], writes=[rc["bsp"]])
                    for rc in rcs:
                        grp, sp_, bsp = rc["grp"], rc["sp"], rc["bsp"]
                        w = len(grp) * 128
                        P, bP = pr.next()
                        rc["P"], rc["bP"] = P, bP
                        sch.op("act", lambda e, sp_=sp_, P=P, w=w: e.activation(out=P[:, 0:w], in_=sp_[:, 0:w], func=AF.Exp),
                               reads=[bsp], writes=[bP])
                        if EB is not None:
                            def eoff(job):
                                return (job[2][2] * ntile + job[1]) * Hh + job[2][5] // 2
                            i = 0
                            while i < len(grp):
                                off = eoff(grp[i])
                                j = i + 1
                                while j < len(grp) and eoff(grp[j]) == off + (j - i):
                                    j += 1
                                ebv = EB[:].rearrange("p f t h q -> p (f t h) q")[:, off:off + (j - i), :]
                                pv = P[:, i * 128:j * 128].rearrange("p (a q) -> p a q", q=128)
                                sch.op("dve", lambda e, pv=pv, ebv=ebv: e.tensor_tensor(out=pv, in0=pv, in1=ebv, op=ALU.mult),
                                       reads=[bP, bin_], writes=[bP])
                                i = j

                cur_o = [None, None]

                def emitB(rc):
                    n, grp, P, bP = rc["n"], rc["grp"], rc["P"], rc["bP"]
                    if rc["first"]:
                        cur_o[0], cur_o[1] = ops_.next()
                    oacc, boacc = cur_o
                    for i, (m, tid, (qt, kt, half, vs, oh, bh)) in enumerate(grp):
                        fst = rc["first"] and i == 0
                        lst = rc["last"] and i == len(grp) - 1
                        sch.op("pe", lambda e, i=i, m=m, vs=vs, oh=oh, P=P, oacc=oacc, fst=fst, lst=lst: e.matmul(
                            oacc[:, oh * 65:(oh + 1) * 65], P[:, i * 128:(i + 1) * 128], V1[:, m, vs, :],
                            start=fst, stop=lst, skip_group_check=True),
                            reads=[bP, bin_], writes=[boacc])
                    if not rc["last"]:
                        return
                    rd, brd = rd_r.next()
                    ov = oacc[:, 0:OH * 65].rearrange("p (h c) -> p h c", c=65)
                    sch.op("dve", lambda e, rd=rd, ov=ov: e.reciprocal(out=rd[:, 0:OH], in_=ov[:, :, 64]),
                           reads=[boacc], writes=[brd])
                    if n % 4 == 0:
                        mx_state[0], mx_state[1] = mx_r.next()
                    mx, bmx = mx_state
                    for h in range(OH):
                        sch.op("dve", lambda e, h=h, rd=rd, oacc=oacc, mx=mx: e.tensor_scalar(
                            out=mx[:, n % 4, h * 64:(h + 1) * 64], in0=oacc[:, h * 65:h * 65 + 64],
                            scalar1=rd[:, h:h + 1], scalar2=None, op0=ALU.mult),
                            reads=[boacc, brd], writes=[bmx])
                    if n % 4 == 3:
                        r0_ = (n - 3) * 128
                        sch.dma("pool", mix_scr[r0_:r0_ + 512, mix_col0:mix_col0 + OH * 64].rearrange("(t p) c -> p t c", p=128),
                                mx[:], reads=[bmx])

                units = []
                i = 0
                while i < len(recs):
                    if i + 1 < len(recs) and recs[i]["grp"][0][2][2] != recs[i + 1]["grp"][0][2][2]:
                        units.append([recs[i], recs[i + 1]])
                        i += 2
                    else:
                        units.append([recs[i]])
                        i += 1
                LA = 1
                for i in range(len(units) + LA):
                    if i < len(units):
                        emitA(units[i])
                    if i - LA >= 0:
                        for rc in units[i - LA]:
                            emitB(rc)
                sch.fence_all("sp")
                sch.fence_all("pool")

        SEL = _os.environ.get("P2SEL", "na0,na1,dl0,dl1,mm").split(",")
        if 2 in phases:
            for ps_ in range(2):
                if f"na{ps_}" not in SEL:
                    continue
                slots = [(h // 2, h // 2, h % 2, h, h, h) for h in range(4)]
                attn_pass(f"na{ps_}", [qk_scr[T_NAQ + 2 * ps_ + i] for i in range(2)],
                          [qk_scr[T_NAK + 2 * ps_ + i] for i in range(2)], S, v_scr, [(ps_ * 256, 4)], slots,
                          lambda n: [(m, tid, [0, 1, 2, 3]) for (m, tid) in na_steps[n]], 4,
                          (na_bias, ps_ * 4, 4), ps_ * 256)
            for ps_ in range(2):
                if f"dl{ps_}" not in SEL:
                    continue
                slots = []
                for g in range(3):
                    for hh in range(2):
                        slots.append((g, g, hh, g * 2 + hh, hh, hh))

                def dsteps(n):
                    st = []
                    for t, (g, o) in enumerate(dil_tl):
                        m = n + o
                        if 0 <= m < NT:
                            st.append((m, t, [g * 2, g * 2 + 1]))
                    return st
                attn_pass(f"dl{ps_}", [qk_scr[T_DQ + 2 * g + ps_] for g in range(3)],
                          [qk_scr[T_DK + 2 * g + ps_] for g in range(3)], S, v_scr,
                          [(512 + g * 256 + ps_ * 128, 2) for g in range(3)], slots, dsteps, 2,
                          (dil_bias, ps_ * 2, 2), 512 + ps_ * 128)
            slots = [(h // 2, h // 2, h % 2, h, h, 0) for h in range(4)]
            if "mm" in SEL:
              attn_pass("mm", [qk_scr[T_MQ + i] for i in range(2)], [km_scr[i] for i in range(2)], 256, vm_scr,
                      [(0, 4)], slots, lambda n: [(0, None, [0, 1, 2, 3]), (1, None, [0, 1, 2, 3])], 4, None, 768)


        g1 = sb("g1", [128, NT], F32)
        g2 = sb("g2", [128, NT], F32)
        dst1i = sb("dst1i", [128, NT], I32)
        dst2i = sb("dst2i", [128, NT], I32)
        widx = sb("widx", [128, NBLK * 2], I32)
        bRt = sch.buf("routeout")
        if 3 in phases:
            with ExitStack() as e3:
                cur = [e3]
                def sb3(name, shape, dt):
                    return cur[0].enter_context(nc.sbuf_tensor("p3" + name, list(shape), dt))
                L = sb3("L", [128, NT, 36], F32)
                bL = sch.buf("L")
                e3m = ExitStack()
                e3m.__enter__()
                cur[0] = e3m
                Wo = sb3("Wo", [128, 8, D], BF16)
                bWo = sch.buf("Wo")
                wst = Ring(sch, cur[0], nc, "p3wst", [128, D], F32, 2)
                for kc in range(8):
                    t, b = wst.next()
                    sch.dma("sp", t[:], w_out[kc * 128:(kc + 1) * 128, :], writes=[b])
                    sch.op("dve", lambda e, t=t, kc=kc: e.tensor_copy(out=Wo[:, kc, :], in_=t[:]), reads=[b], writes=[bWo])
                wr = sb3("wr", [128, 8, 36], F32)
                br = sb3("br", [128, 36], F32)
                gB = sb3("gB", [128, D], F32)
                bwr = sch.buf("wr")
                sch.dma("sp", wr[:], w_r.rearrange("(kc p) n -> p kc n", p=128), writes=[bwr])
                sch.dma("sp", br[:], b_r[:, :], writes=[bwr])
                sch.dma("sp", gB[:], gffn_b[:, :], writes=[bwr])
                for kc in range(8):
                    sch.op("dve", lambda e, kc=kc: e.tensor_scalar(out=wr[:, kc, :], in0=wr[:, kc, :],
                                                                    scalar1=gv[:, 16 + kc:17 + kc], scalar2=None, op0=ALU.mult),
                           reads=[bwr, b_const], writes=[bwr])
                wrh = sb3("wrh", [128, 8, 36], BF16)
                wrl = sb3("wrl", [128, 8, 36], BF16)
                sch.op("dve", lambda e: e.tensor_copy(out=wrh[:], in_=wr[:]), reads=[bwr], writes=[bwr])
                sch.op("dve", lambda e: e.tensor_tensor(out=wrl[:], in0=wr[:], in1=wrh[:], op=ALU.subtract), reads=[bwr], writes=[bwr])
                mx_r = Ring(sch, cur[0], nc, "p3mx", [128, D], BF16, 2)
                xt_r = Ring(sch, cur[0], nc, "p3xt", [128, D], F32, 2)
                mT_r = Ring(sch, cur[0], nc, "p3mT", [128, 8, 128], BF16, 2)
                x1_r = Ring(sch, cur[0], nc, "p3x1", [128, D], F32, 2)
                hn_r = Ring(sch, cur[0], nc, "p3hn", [128, D], F32, 2)
                hi_r = Ring(sch, cur[0], nc, "p3hi", [128, D], BF16, 3)
                lo_r = Ring(sch, cur[0], nc, "p3lo", [128, D], BF16, 3)
                hg_r = Ring(sch, cur[0], nc, "p3hg", [128, D], BF16, 2)
                hiT_r = Ring(sch, cur[0], nc, "p3hiT", [128, 8, 128], BF16, 2)
                loT_r = Ring(sch, cur[0], nc, "p3loT", [128, 8, 128], BF16, 2)
                st_r = Ring(sch, cur[0], nc, "p3st", [128, 4], F32, 2)
                junk = sb3("junk", [128, D], BF16)
                bjunk = sch.buf("p3junk")
                with ExitStack() as e3p:
                    tpm = e3p.enter_context(nc.psum_tensor("p3tpm", [128, 8, 128], BF16))
                    btpm = sch.buf("p3tpm")
                    yps = Ring(sch, e3p, nc, "p3yps", [128, 512], F32, 4, psum=True)
                    tph = e3p.enter_context(nc.psum_tensor("p3tph", [128, 8, 128], BF16))
                    tpl = e3p.enter_context(nc.psum_tensor("p3tpl", [128, 8, 128], BF16))
                    btph, btpl = sch.buf("p3tph"), sch.buf("p3tpl")
                    lps = e3p.enter_context(nc.psum_tensor("p3lps", [128, 512], F32))
                    blps = sch.buf("p3lps")
                    def p3A(n):
                        r0_ = n * 128
                        mxt, bmx = mx_r.next()
                        xt, bxt = xt_r.next()
                        sch.dma("sp", mxt[:], mix_scr[r0_:r0_ + 128, :], writes=[bmx])
                        sch.dma("sp", xt[:], x[r0_:r0_ + 128, :], writes=[bxt])
                        for kc in range(8):
                            sch.op("pe", lambda e, kc=kc, mxt=mxt: e.transpose(out=tpm[:, kc, :], in_=mxt[:, kc * 128:(kc + 1) * 128],
                                                                      identity=identb[:]), reads=[bmx, b_const], writes=[btpm])
                        mT, bmT = mT_r.next()
                        sch.op("dve", lambda e, mT=mT: e.tensor_copy(out=mT[:], in_=tpm[:]), reads=[btpm], writes=[bmT])
                        x1, bx1 = x1_r.next()
                        for hf in range(2):
                            yp, byp = yps.next()
                            for kc in range(8):
                                sch.op("pe", lambda e, kc=kc, hf=hf, yp=yp, mT=mT: e.matmul(
                                    yp[:], mT[:, kc, :], Wo[:, kc, hf * 512:(hf + 1) * 512], start=(kc == 0), stop=(kc == 7)),
                                    reads=[bmT, bWo], writes=[byp])
                            sch.op("dve", lambda e, hf=hf, yp=yp, x1=x1, xt=xt: e.tensor_tensor(
                                out=x1[:, hf * 512:(hf + 1) * 512], in0=yp[:], in1=xt[:, hf * 512:(hf + 1) * 512], op=ALU.add),
                                reads=[byp, bxt], writes=[bx1])
                        sch.dma("pool", out[r0_:r0_ + 128, :], x1[:], reads=[bx1])
                        stt, bst = st_r.next()
                        sch.op("act", lambda e, x1=x1, stt=stt: e.activation(out=junk[:], in_=x1[:], func=AF.Square,
                                                                           accum_out=stt[:, 0:1]), reads=[bx1], writes=[bjunk, bst])
                        sch.op("act", lambda e, stt=stt: e.activation(out=stt[:, 1:2], in_=stt[:, 0:1], func=AF.Sqrt,
                                                                       bias=EPS, scale=1.0 / D), reads=[bst], writes=[bst])
                        sch.op("dve", lambda e, stt=stt: e.reciprocal(out=stt[:, 1:2], in_=stt[:, 1:2]), reads=[bst], writes=[bst])
                        hn, bhn = hn_r.next()
                        sch.op("act", lambda e, hn=hn, x1=x1, stt=stt: e.activation(out=hn[:], in_=x1[:], func=AF.Copy,
                                                                                   scale=stt[:, 1:2]), reads=[bx1, bst], writes=[bhn])
                        hi, bhi = hi_r.next()
                        lo, blo = lo_r.next()
                        hg, bhg = hg_r.next()
                        sch.op("act", lambda e, hi=hi, hn=hn: e.activation(out=hi[:], in_=hn[:], func=AF.Copy), reads=[bhn], writes=[bhi])
                        sch.op("dve", lambda e, hi=hi, lo=lo, hn=hn: e.tensor_tensor(out=lo[:], in0=hn[:], in1=hi[:], op=ALU.subtract),
                               reads=[bhn, bhi], writes=[blo])
                        sch.op("pool", lambda e, hg=hg, hn=hn: e.tensor_tensor(out=hg[:], in0=hn[:], in1=gB[:], op=ALU.mult),
                               reads=[bhn, bwr], writes=[bhg])
                        sch.dma("pool", h2_scr[r0_:r0_ + 128, :], hg[:], reads=[bhg])
                        return hi, bhi, lo, blo

                    def p3B(n, hi, bhi, lo, blo):
                        for kc in range(8):
                            sch.op("pe", lambda e, kc=kc, hi=hi: e.transpose(out=tph[:, kc, :], in_=hi[:, kc * 128:(kc + 1) * 128],
                                                                            identity=identb[:]), reads=[bhi, b_const], writes=[btph])
                        for kc in range(8):
                            sch.op("pe", lambda e, kc=kc, lo=lo: e.transpose(out=tpl[:, kc, :], in_=lo[:, kc * 128:(kc + 1) * 128],
                                                                            identity=identb[:]), reads=[blo, b_const], writes=[btpl])
                        hiT, bhiT = hiT_r.next()
                        loT, bloT = loT_r.next()
                        sch.op("act", lambda e, hiT=hiT: e.activation(out=hiT[:], in_=tph[:], func=AF.Copy), reads=[btph], writes=[bhiT])
                        sch.op("act", lambda e, loT=loT: e.activation(out=loT[:], in_=tpl[:], func=AF.Copy), reads=[btpl], writes=[bloT])
                        k = 0
                        for (aa, ba, ww) in ((hiT, bhiT, wrh), (hiT, bhiT, wrl), (loT, bloT, wrh)):
                            for kc in range(8):
                                sch.op("pe", lambda e, kc=kc, aa=aa, ww=ww, k=k: e.matmul(lps[:, 0:36], aa[:, kc, :], ww[:, kc, :],
                                                                                     start=(k == 0), stop=(k == 23)),
                                       reads=[ba, bwr], writes=[blps])
                                k += 1
                        sch.op("dve", lambda e, n=n: e.tensor_tensor(out=L[:, n, :], in0=lps[:, 0:36], in1=br[:], op=ALU.add),
                               reads=[blps, bwr], writes=[bL])


                    prev3 = None
                    for n in range(NT + 1):
                        cur3 = p3A(n) if n < NT else None
                        if prev3 is not None:
                            p3B(n - 1, *prev3)
                        prev3 = cur3
                e3m.close()
                cur[0] = e3
                def sbr(name, shape, dt=F32):
                    return e3.enter_context(nc.sbuf_tensor("rt" + name, list(shape), dt))
                bR = sch.buf("route")
                gl = L[:, :, 0:4]
                fl = L[:, :, 4:36]
                gmax = sbr("gmax", [128, NT]); G1 = sbr("G1", [128, NT, 4]); ge = sbr("ge", [128, NT, 4])
                gsum = sbr("gsum", [128, NT]); pen = sbr("pen", [128, NT, 4]); flm = sbr("flm", [128, NT, 32])
                m1 = sbr("m1", [128, NT]); oh1 = sbr("oh1", [128, NT, 32]); m2 = sbr("m2", [128, NT])
                oh2 = sbr("oh2", [128, NT, 32]); dd = sbr("dd", [128, NT])

                def R(eng, fn, extra_w=()):
                    sch.op(eng, fn, reads=[bL, bR, b_const], writes=[bR] + list(extra_w))

                def bc(a, n):
                    return a[:, :].unsqueeze(2).broadcast_to([128, NT, n])
                R("dve", lambda e: e.tensor_reduce(out=gmax[:], in_=gl, axis=AX.X, op=ALU.max))
                R("dve", lambda e: e.tensor_tensor(out=G1[:], in0=gl, in1=bc(gmax, 4), op=ALU.is_ge))
                R("dve", lambda e: e.tensor_tensor(out=ge[:], in0=gl, in1=bc(gmax, 4), op=ALU.subtract))
                R("act", lambda e: e.activation(out=ge[:], in_=ge[:], func=AF.Exp))
                R("dve", lambda e: e.tensor_reduce(out=gsum[:], in_=ge[:], axis=AX.X, op=ALU.add))
                R("dve", lambda e: e.reciprocal(out=gsum[:], in_=gsum[:]))
                R("dve", lambda e: e.tensor_scalar(out=pen[:], in0=G1[:], scalar1=1e9, scalar2=-1e9, op0=ALU.mult, op1=ALU.add))
                R("dve", lambda e: e.tensor_tensor(
                    out=flm[:].rearrange("p t (g e) -> p t g e", e=8), in0=fl.rearrange("p t (g e) -> p t g e", e=8),
                    in1=pen[:].unsqueeze(3).broadcast_to([128, NT, 4, 8]), op=ALU.add))
                R("dve", lambda e: e.tensor_reduce(out=m1[:], in_=flm[:], axis=AX.X, op=ALU.max))
                R("dve", lambda e: e.tensor_tensor(out=oh1[:], in0=flm[:], in1=bc(m1, 32), op=ALU.is_ge))
                R("dve", lambda e: e.scalar_tensor_tensor(out=flm[:], in0=oh1[:], scalar=-1e9, in1=flm[:], op0=ALU.mult, op1=ALU.add))
                R("dve", lambda e: e.tensor_reduce(out=m2[:], in_=flm[:], axis=AX.X, op=ALU.max))
                R("dve", lambda e: e.tensor_tensor(out=oh2[:], in0=flm[:], in1=bc(m2, 32), op=ALU.is_ge))
                R("dve", lambda e: e.tensor_tensor(out=dd[:], in0=m2[:], in1=m1[:], op=ALU.subtract))
                R("act", lambda e: e.activation(out=dd[:], in_=dd[:], func=AF.Exp))
                R("dve", lambda e: e.tensor_scalar(out=g1[:], in0=dd[:], scalar1=1.0, scalar2=None, op0=ALU.add), [bRt])
                R("dve", lambda e: e.reciprocal(out=g1[:], in_=g1[:]), [bRt])
                R("dve", lambda e: e.tensor_tensor(out=g1[:], in0=g1[:], in1=gsum[:], op=ALU.mult), [bRt])
                R("dve", lambda e: e.tensor_tensor(out=g2[:], in0=g1[:], in1=dd[:], op=ALU.mult), [bRt])
                selb = sbr("selb", [128, NT * 32], BF16)
                trif = sbr("trif", [128, 128]); trib = sbr("trib", [128, 128], BF16); oneb = sbr("oneb", [128, 128], BF16)
                iot = sbr("iot", [128, 256]); pid2 = sbr("pid2", [128, 1])
                sch.dma("sp", trif[:], tri_in[:, :], writes=[bR])
                sch.dma("sp", iot[:], iota_in[:, :], writes=[bR])
                sch.dma("sp", pid2[:], pidx_in[:, :], writes=[bR])
                R("dve", lambda e: e.tensor_copy(out=trib[:], in_=trif[:]))
                R("dve", lambda e: e.memset(oneb[:], 1.0))
                R("dve", lambda e: e.tensor_scalar(out=pid2[:], in0=pid2[:], scalar1=2.0, scalar2=None, op0=ALU.mult))
                R("dve", lambda e: e.tensor_tensor(out=selb[:], in0=oh1[:].rearrange("p t e -> p (t e)"),
                                                   in1=oh2[:].rearrange("p t e -> p (t e)"), op=ALU.add))
                Cs = sbr("Cs", [128, NT, 32]); Ta = sbr("Ta", [128, NT, 32]); Tb = sbr("Tb", [128, NT, 32]); T0 = sbr("T0", [128, NT, 32])
                with ExitStack() as e3r:
                    cps = [e3r.enter_context(nc.psum_tensor(f"rtc{j}", [128, 512], F32)) for j in range(4)]
                    tps = [e3r.enter_context(nc.psum_tensor(f"rtt{j}", [128, 512], F32)) for j in range(4)]
                    bcp, btp = sch.buf("rtc"), sch.buf("rtt")
                    Cf = Cs[:].rearrange("p t e -> p (t e)")
                    T0f = T0[:].rearrange("p t e -> p (t e)")
                    for j in range(4):
                        sch.op("pe", lambda e, j=j: e.matmul(cps[j][:], trib[:], selb[:, j * 512:(j + 1) * 512], start=True, stop=True),
                               reads=[bR], writes=[bcp])
                        sch.op("pe", lambda e, j=j: e.matmul(tps[j][:], oneb[:], selb[:, j * 512:(j + 1) * 512], start=True, stop=True),
                               reads=[bR], writes=[btp])
                    for j in range(4):
                        sch.op("act", lambda e, j=j: e.activation(out=Cf[:, j * 512:(j + 1) * 512], in_=cps[j][:], func=AF.Copy),
                               reads=[bcp], writes=[bR])
                        sch.op("dve", lambda e, j=j: e.tensor_copy(out=T0f[:, j * 512:(j + 1) * 512], in_=tps[j][:]),
                               reads=[btp], writes=[bR])
                src, dstb = T0, Ta
                for sft in (1, 2, 4, 8, 16, 32):
                    R("dve", lambda e, src=src, dstb=dstb, sft=sft: e.tensor_copy(out=dstb[:, 0:sft, :], in_=src[:, 0:sft, :]))
                    R("dve", lambda e, src=src, dstb=dstb, sft=sft: e.tensor_tensor(
                        out=dstb[:, sft:NT, :], in0=src[:, sft:NT, :], in1=src[:, 0:NT - sft, :], op=ALU.add))
                    src, dstb = dstb, (Tb if dstb is Ta else Ta)
                Inc = src
                cnt = sbr("cnt", [128, 32]); nbk = sbr("nbk", [128, 32]); cmp1 = sbr("cmp1", [128, 32, 128])
                i128 = sbr("i128", [128, 128]); sa = sbr("sa", [128, 32]); sb_ = sbr("sb_", [128, 32])
                psr = sbr("psr", [128, 32]); pend = sbr("pend", [128, 32])
                R("dve", lambda e: e.tensor_copy(out=cnt[:], in_=Inc[:, NT - 1, :]))
                R("dve", lambda e: e.tensor_scalar(out=i128[:], in0=iot[:, 0:128], scalar1=float(MOE_B), scalar2=None, op0=ALU.mult))
                R("dve", lambda e: e.tensor_tensor(out=cmp1[:], in0=cnt[:, :].unsqueeze(2).broadcast_to([128, 32, 128]),
                                                   in1=i128[:, :].unsqueeze(1).broadcast_to([128, 32, 128]), op=ALU.is_gt))
                R("dve", lambda e: e.tensor_reduce(out=nbk[:], in_=cmp1[:], axis=AX.X, op=ALU.add))
                src, dstb = nbk, sa
                for sft in (1, 2, 4, 8, 16):
                    R("dve", lambda e, src=src, dstb=dstb, sft=sft: e.tensor_copy(out=dstb[:, 0:sft], in_=src[:, 0:sft]))
                    R("dve", lambda e, src=src, dstb=dstb, sft=sft: e.tensor_tensor(
                        out=dstb[:, sft:32], in0=src[:, sft:32], in1=src[:, 0:32 - sft], op=ALU.add))
                    src, dstb = dstb, (sb_ if dstb is sa else sa)
                R("dve", lambda e, src=src: e.tensor_copy(out=pend[:], in_=src[:]))
                R("dve", lambda e: e.tensor_tensor(out=psr[:], in0=pend[:], in1=nbk[:], op=ALU.subtract))
                R("dve", lambda e: e.tensor_scalar(out=psr[:], in0=psr[:], scalar1=float(MOE_B), scalar2=None, op0=ALU.mult))
                R("dve", lambda e, Inc=Inc: e.tensor_tensor(out=Cs[:], in0=Cs[:], in1=Inc[:], op=ALU.add))
                R("dve", lambda e: e.tensor_tensor(out=Cs[:], in0=Cs[:], in1=T0[:], op=ALU.subtract))
                R("dve", lambda e: e.tensor_tensor(out=Cs[:], in0=Cs[:], in1=psr[:, :].unsqueeze(1).broadcast_to([128, NT, 32]), op=ALU.add))
                d1 = sbr("d1", [128, NT]); d2 = sbr("d2", [128, NT])
                R("dve", lambda e: e.tensor_tensor(out=Ta[:], in0=Cs[:], in1=oh1[:], op=ALU.mult))
                R("dve", lambda e: e.tensor_reduce(out=d1[:], in_=Ta[:], axis=AX.X, op=ALU.add))
                R("dve", lambda e: e.tensor_tensor(out=Tb[:], in0=Cs[:], in1=oh2[:], op=ALU.mult))
                R("dve", lambda e: e.tensor_reduce(out=d2[:], in_=Tb[:], axis=AX.X, op=ALU.add))
                R("dve", lambda e: e.tensor_copy(out=dst1i[:], in_=d1[:]), [bRt])
                R("dve", lambda e: e.tensor_copy(out=dst2i[:], in_=d2[:]), [bRt])
                cmp2 = sbr("cmp2", [128, NBLK, 32]); bex = sbr("bex", [128, NBLK]); need = sbr("need", [128, NBLK])
                w0 = sbr("w0", [128, NBLK]); w1f = sbr("w1f", [128, NBLK, 2])
                R("dve", lambda e: e.tensor_tensor(out=cmp2[:], in0=iot[:, 0:NBLK].unsqueeze(2).broadcast_to([128, NBLK, 32]),
                                                   in1=pend[:, :].unsqueeze(1).broadcast_to([128, NBLK, 32]), op=ALU.is_ge))
                R("dve", lambda e: e.tensor_reduce(out=bex[:], in_=cmp2[:], axis=AX.X, op=ALU.add))
                R("dve", lambda e: e.tensor_scalar(out=bex[:], in0=bex[:], scalar1=float(N_EXP - 1), scalar2=None, op0=ALU.min))
                R("dve", lambda e: e.memset(need[:], 1.0))
                if WSKIP:
                    R("dve", lambda e: e.tensor_tensor(out=need[:, NSET:NBLK], in0=bex[:, NSET:NBLK], in1=bex[:, 0:NBLK - NSET], op=ALU.not_equal))
                R("dve", lambda e: e.tensor_scalar(out=w0[:], in0=bex[:], scalar1=256.0, scalar2=pid2[:, 0:1], op0=ALU.mult, op1=ALU.add))
                R("dve", lambda e: e.tensor_tensor(out=w0[:], in0=w0[:], in1=need[:], op=ALU.mult))
                R("dve", lambda e: e.tensor_scalar(out=need[:], in0=need[:], scalar1=-float(1 << 30), scalar2=float(1 << 30),
                                                   op0=ALU.mult, op1=ALU.add))
                R("dve", lambda e: e.tensor_tensor(out=w0[:], in0=w0[:], in1=need[:], op=ALU.add))
                R("dve", lambda e: e.tensor_copy(out=w1f[:, :, 0], in_=w0[:]))
                R("dve", lambda e: e.tensor_scalar(out=w1f[:, :, 1], in0=w0[:], scalar1=1.0, scalar2=None, op0=ALU.add))
                R("dve", lambda e: e.tensor_copy(out=widx[:], in_=w1f[:].rearrange("p b h -> p (b h)")), [bRt])
                if debug:
                    dbg = sbr("dbg", [128, 4 * NT + 2 * NBLK])
                    R("dve", lambda e: e.tensor_copy(out=dbg[:, 0:NT], in_=d1[:]))
                    R("dve", lambda e: e.tensor_copy(out=dbg[:, NT:2 * NT], in_=d2[:]))
                    R("dve", lambda e: e.tensor_copy(out=dbg[:, 2 * NT:3 * NT], in_=g1[:]))
                    R("dve", lambda e: e.tensor_copy(out=dbg[:, 3 * NT:4 * NT], in_=g2[:]))
                    R("dve", lambda e: e.tensor_copy(out=dbg[:, 4 * NT:4 * NT + NBLK], in_=bex[:]))
                    R("dve", lambda e: e.tensor_copy(out=dbg[:, 4 * NT + NBLK:4 * NT + 2 * NBLK], in_=w0[:]))
                    sch.dma("sp", dbg_out[:, :], dbg[:], reads=[bR])
                sch.fence_all("sp")
                sch.fence_all("pool")
                hs_r = Ring(sch, e3, nc, "p3hs", [128, D], BF16, 4)
                for n in range(NT):
                    hs, bhs = hs_r.next()
                    sch.dma("sp", hs[:], h2_scr[n * 128:(n + 1) * 128, :], writes=[bhs])
                    for dsti in (dst1i, dst2i):
                        sch.dma("pool", xs_scr[:, :], hs[:], reads=[bhs, bRt],
                                indirect=dict(out_offset=bass.IndirectOffsetOnAxis(dsti[:, n:n + 1], 0), in_offset=None))
                sch.fence_all("sp")
                sch.fence_all("pool")

        if 4 in phases:
            with ExitStack() as e4:
                def sb4(name, shape, dt):
                    return e4.enter_context(nc.sbuf_tensor("p4" + name, list(shape), dt))
                SUB = MOE_B // 128
                Wb = [[sb4(f"W{i}_{s_}", [128, 4096], BF16) for s_ in range(NSET)] for i in range(3)]
                bWb = [[[sch.buf(f"p4W{i}_{s_}_{h}") for h in range(2)] for s_ in range(NSET)] for i in range(3)]
                wsrc = (w1, w3, w2)
                bnd_reg = nc.gpsimd.alloc_register("wbnd")
                nc.gpsimd.reg_mov(bnd_reg, N_EXP * 256 - 1)
                xs_r = Ring(sch, e4, nc, "p4xs", [128, D], BF16, 3)
                xT_r = Ring(sch, e4, nc, "p4xT", [128, 8, 128], BF16, 3)
                sg_r = Ring(sch, e4, nc, "p4sg", [128, 512], BF16, 2)
                am_r = Ring(sch, e4, nc, "p4am", [128, 512], BF16, 2)
                aT_r = Ring(sch, e4, nc, "p4aT", [128, 4, 128], BF16, 2 * SUB + 1)
                ys_r = Ring(sch, e4, nc, "p4ys", [128, D], F32, 3)
                with ExitStack() as e4p:
                    tpx = e4p.enter_context(nc.psum_tensor("p4tpx", [128, 8, 128], BF16))
                    btpx = sch.buf("p4tpx")
                    a1p = Ring(sch, e4p, nc, "p4a1", [128, 512], F32, 2, psum=True)
                    a3p = Ring(sch, e4p, nc, "p4a3", [128, 512], F32, 2, psum=True)
                    ypp = Ring(sch, e4p, nc, "p4yp", [128, 512], F32, 2, psum=True)
                    tpa = e4p.enter_context(nc.psum_tensor("p4tpa", [128, 4, 128], BF16))
                    btpa = sch.buf("p4tpa")

                    def stageX(b):
                        st_ = b % NSET
                        for i in range(3):
                            for hf in range(2):
                                sch.dma("pool", Wb[i][st_][:, hf * 2048:(hf + 1) * 2048], wsrc[i][:, :], reads=[bRt], writes=[bWb[i][st_][hf]],
                                        indirect=dict(out_offset=None, in_offset=bass.IndirectOffsetOnAxis(widx[:, 2 * b + hf:2 * b + hf + 1], 0),
                                                      bounds_check=bnd_reg, oob_is_err=False))
                        res = []
                        for sb_ in range(SUB):
                            r0_ = b * MOE_B + sb_ * 128
                            xs, bxs = xs_r.next()
                            sch.dma("sp", xs[:], xs_scr[r0_:r0_ + 128, :], writes=[bxs])
                            for kc in range(8):
                                sch.op("pe", lambda e, kc=kc, xs=xs: e.transpose(out=tpx[:, kc, :], in_=xs[:, kc * 128:(kc + 1) * 128],
                                                                                identity=identb[:]), reads=[bxs, b_const], writes=[btpx])
                            xT, bxT = xT_r.next()
                            sch.op("dve", lambda e, xT=xT: e.tensor_copy(out=xT[:], in_=tpx[:]), reads=[btpx], writes=[bxT])
                            a1, ba1 = a1p.next()
                            a3, ba3 = a3p.next()
                            for (ap_, bap, Wx, bWx) in ((a1, ba1, Wb[0][st_], bWb[0][st_]), (a3, ba3, Wb[1][st_], bWb[1][st_])):
                                for kc in range(8):
                                    sch.op("pe", lambda e, kc=kc, ap_=ap_, Wx=Wx, xT=xT: e.matmul(
                                        ap_[:], xT[:, kc, :], Wx[:, kc * 512:(kc + 1) * 512],
                                        start=(kc == 0), stop=(kc == 7)), reads=[bWx[kc // 4], bxT], writes=[bap])
                            sg, bsg = sg_r.next()
                            sch.op("act", lambda e, a1=a1, sg=sg: e.activation(out=sg[:], in_=a1[:], func=AF.Silu), reads=[ba1], writes=[bsg])
                            am, bam = am_r.next()
                            sch.op("dve", lambda e, am=am, a3=a3, sg=sg: e.tensor_tensor(out=am[:], in0=a3[:], in1=sg[:], op=ALU.mult),
                                   reads=[ba3, bsg], writes=[bam])
                            for nch in range(4):
                                sch.op("pe", lambda e, nch=nch, am=am: e.transpose(out=tpa[:, nch, :], in_=am[:, nch * 128:(nch + 1) * 128],
                                                                                  identity=identb[:]), reads=[bam, b_const], writes=[btpa])
                            aT, baT = aT_r.next()
                            sch.op("act", lambda e, aT=aT: e.activation(out=aT[:], in_=tpa[:], func=AF.Copy), reads=[btpa], writes=[baT])
                            res.append((aT, baT))
                        return res

                    def stageY(b, res):
                        st_ = b % NSET
                        W2b = Wb[2][st_]
                        for sb_, (aT, baT) in enumerate(res):
                            r0_ = b * MOE_B + sb_ * 128
                            ys, bys = ys_r.next()
                            for hf in range(2):
                                yp, byp = ypp.next()
                                for nch in range(4):
                                    sch.op("pe", lambda e, nch=nch, hf=hf, yp=yp, aT=aT, W2b=W2b: e.matmul(
                                        yp[:], aT[:, nch, :], W2b[:, nch * 1024 + hf * 512:nch * 1024 + (hf + 1) * 512],
                                        start=(nch == 0), stop=(nch == 3)), reads=[baT, bWb[2][st_][nch // 2]], writes=[byp])
                                sch.op("act", lambda e, hf=hf, yp=yp, ys=ys: e.activation(out=ys[:, hf * 512:(hf + 1) * 512], in_=yp[:], func=AF.Copy),
                                       reads=[byp], writes=[bys])
                            sch.dma("act", yb_scr[r0_:r0_ + 128, :], ys[:], reads=[bys])

                    prevx = None
                    for b in range(NBLK + 1):
                        curx = stageX(b) if b < NBLK else None
                        if prevx is not None:
                            stageY(b - 1, prevx)
                        prevx = curx
                sch.fence_all("sp")
                sch.fence_all("pool")
                sch.fence_all("act")
                y1_r = Ring(sch, e4, nc, "p4y1", [128, D], F32, 3)
                y2_r = Ring(sch, e4, nc, "p4y2", [128, D], F32, 3)
                xo_r = Ring(sch, e4, nc, "p4xo", [128, D], F32, 3)
                for n in range(NT):
                    y1, by1 = y1_r.next()
                    y2, by2 = y2_r.next()
                    xo, bxo = xo_r.next()
                    sch.dma("pool", y1[:], yb_scr[:, :], reads=[bRt], writes=[by1],
                            indirect=dict(out_offset=None, in_offset=bass.IndirectOffsetOnAxis(dst1i[:, n:n + 1], 0)))
                    sch.dma("pool", y2[:], yb_scr[:, :], reads=[bRt], writes=[by2],
                            indirect=dict(out_offset=None, in_offset=bass.IndirectOffsetOnAxis(dst2i[:, n:n + 1], 0)))
                    sch.dma("sp", xo[:], out[n * 128:(n + 1) * 128, :], writes=[bxo])
                    sch.op("dve", lambda e, n=n, y1=y1, xo=xo: e.scalar_tensor_tensor(
                        out=xo[:], in0=y1[:], scalar=g1[:, n:n + 1], in1=xo[:], op0=ALU.mult, op1=ALU.add),
                        reads=[by1, bxo, bRt], writes=[bxo])
                    sch.op("dve", lambda e, n=n, y2=y2, xo=xo: e.scalar_tensor_tensor(
                        out=xo[:], in0=y2[:], scalar=g2[:, n:n + 1], in1=xo[:], op0=ALU.mult, op1=ALU.add),
                        reads=[by2, bxo, bRt], writes=[bxo])
                    sch.dma("act", out[n * 128:(n + 1) * 128, :], xo[:], reads=[bxo])
                sch.fence_all("sp")
                sch.fence_all("pool")
                sch.fence_all("act")

        for en in ("sp", "pool", "act", "dve", "pe"):
            sch.fence_all(en)
        if _os.environ.get("DRYPRINT"):
            print("counts", {k: v.count for k, v in sch.engs.items()}, "nsem", sch.nsem)
    return nc


_CACHE = {}


def kernel(x, mem, g_mix, w_in, qk_gain, na_rpb, t5_table, g_mem, w_mem_kv, w_out, g_ffn, w_r1, b_r1, w_r2, b_r2,
           w1, w3, w2, _debug=False, _phases=(1, 2, 3, 4), _cores=8):
    f32 = np.float32
    x = np.asarray(x, f32); mem = np.asarray(mem, f32)
    na_steps, na_keys = na_plan()
    dil_tl = dil_plan()
    nab = na_bias_tiles(np.asarray(na_rpb, f32)[0], na_keys)
    dlb = dil_bias_tiles(np.asarray(t5_table, f32), dil_tl)
    key = (len(na_keys), len(dil_tl), _debug, tuple(_phases))
    if key not in _CACHE:
        _CACHE[key] = build_program(len(na_keys), len(dil_tl), na_steps, dil_tl, debug=_debug, phases=_phases)
    nc = _CACHE[key]

    def pk(v):
        return np.asarray(v, f32).reshape(8, 128).T
    gvec = np.ascontiguousarray(np.concatenate([pk(g_mix[0]), pk(g_mem[0]), pk(g_ffn[0])], axis=1))
    qg = np.asarray(qk_gain, f32)[0]
    gains = np.ascontiguousarray(np.tile(qg.reshape(6, 64), (1, 2)).T)
    shared = {
        "w_in": np.ascontiguousarray(np.asarray(w_in, f32)[0]),
        "w_mem": np.ascontiguousarray(np.asarray(w_mem_kv, f32)[0]),
        "w_out": np.ascontiguousarray(np.asarray(w_out, f32)[0]),
        "gvec": gvec, "gains": gains,
        "gffn_b": np.ascontiguousarray(np.broadcast_to(np.asarray(g_ffn, f32)[0][None, :], (128, D))),
        "ident": np.eye(128, dtype=f32),
        "na_bias": nab, "dil_bias": dlb,
        "w_r": np.ascontiguousarray(np.concatenate([np.asarray(w_r1, f32)[0], np.asarray(w_r2, f32)[0]], axis=1)),
        "b_r": np.ascontiguousarray(np.broadcast_to(
            np.concatenate([np.asarray(b_r1, f32)[0], np.asarray(b_r2, f32)[0]])[None, :], (128, 36))),
        "w1": np.ascontiguousarray(np.asarray(w1, f32)[0].reshape(N_EXP, 8, 128, 512).transpose(0, 2, 1, 3)).reshape(N_EXP * 256, 2048),
        "w3": np.ascontiguousarray(np.asarray(w3, f32)[0].reshape(N_EXP, 8, 128, 512).transpose(0, 2, 1, 3)).reshape(N_EXP * 256, 2048),
        "w2": np.ascontiguousarray(np.asarray(w2, f32)[0].reshape(N_EXP, 4, 128, 1024).transpose(0, 2, 1, 3)).reshape(N_EXP * 256, 2048),
        "iota": np.ascontiguousarray(np.broadcast_to(np.arange(256, dtype=f32)[None, :], (128, 256))),
        "pidx": np.arange(128, dtype=f32).reshape(128, 1),
        "tri": np.triu(np.ones((128, 128), f32), 1),
    }
    in_maps = []
    for c in range(_cores):
        m = dict(shared)
        m["x"] = np.ascontiguousarray(x[c])
        m["mem"] = np.ascontiguousarray(mem[c])
        in_maps.append(m)
    res = run_bass_kernel_spmd(nc, in_maps, core_ids=list(range(_cores)))
    if _debug:
        return res.results
    return np.stack([r["out"] for r in res.results], axis=0)
```

```python
import math
import os as _os
from contextlib import ExitStack
import numpy as np
import concourse.bass as bass
import concourse.mybir as mybir
from concourse.bass_utils import run_bass_kernel_spmd

F32 = mybir.dt.float32
BF16 = mybir.dt.bfloat16
I32 = mybir.dt.int32
AF = mybir.ActivationFunctionType
ALU = mybir.AluOpType
AX = mybir.AxisListType

S = 8192
D = 1024
NT = S // 128
EPS = 1e-6
NEG = -30000.0
N_EXP = 32
MOE_B = 256
NSET = 3
CAP = 2 * S + N_EXP * MOE_B
NBLK = CAP // MOE_B
SAME_ENGINE_SYNC = True
WSKIP = True
LNEXP = bool(int(_os.environ.get("LNEXP", "1")))


class Eng:
    def __init__(self, name, eng, sem):
        self.name, self.eng, self.sem = name, eng, sem
        self.count = 0
        self.waited = {}


class Buf:
    def __init__(self, name):
        self.name = name
        self.w = {}
        self.r = {}
        self.dw = None
        self.dr = None


class Sched:
    def __init__(self, nc, es):
        self.nc, self.es = nc, es
        self.engs = {}
        for nm, e in (("pe", nc.tensor), ("act", nc.scalar), ("dve", nc.vector), ("pool", nc.gpsimd), ("sp", nc.sync)):
            self.engs[nm] = Eng(nm, e, es.enter_context(nc.semaphore("sem_" + nm)))
        self.nsem = 5
        self.all_bufs = []

    def buf(self, name):
        b = Buf(name)
        self.all_bufs.append(b)
        return b

    def newsem(self, name):
        self.nsem += 1
        return self.es.enter_context(self.nc.semaphore(name))

    def _wait(self, E, key, sem, val):
        if val <= 0 or E.waited.get(key, 0) >= val:
            return
        E.eng.wait_ge(sem, val)
        E.waited[key] = val

    def _deps(self, E, reads, writes, skip_dw=False):
        for b in reads:
            for f, n in b.w.items():
                self._dep_eng(E, f, n)
            if b.dw is not None:
                self._wait(E, id(b.dw[0]), b.dw[0], b.dw[1])
        for b in writes:
            for f, n in b.w.items():
                self._dep_eng(E, f, n)
            for f, n in b.r.items():
                self._dep_eng(E, f, n)
            if b.dw is not None and not skip_dw:
                self._wait(E, id(b.dw[0]), b.dw[0], b.dw[1])
            if b.dr is not None:
                self._wait(E, id(b.dr[0]), b.dr[0], b.dr[1])

    def _dep_eng(self, E, f, n):
        if f == E.name and (f == "pe" or not SAME_ENGINE_SYNC):
            return
        F = self.engs[f]
        self._wait(E, f, F.sem, n)

    def op(self, ename, ins_fn, reads=(), writes=()):
        E = self.engs[ename]
        self._deps(E, reads, writes)
        ins = ins_fn(E.eng)
        E.count += 1
        ins.then_inc(E.sem, 1)
        for b in reads:
            b.r[ename] = E.count
        for b in writes:
            b.w[ename] = E.count
        return ins

    def dma(self, ename, out, in_, reads=(), writes=(), extra_wait=(), indirect=None, disjoint=False, **kw):
        E = self.engs[ename]
        self._deps(E, reads, writes, skip_dw=disjoint)
        for b in extra_wait:
            self._deps(E, [b], [])
        if indirect is None:
            ins = E.eng.dma_start(out=out, in_=in_, **kw)
        else:
            ins = E.eng.indirect_dma_start(out=out, in_=in_, **indirect, **kw)
        if writes:
            b = writes[0]
            if b.dw is None:
                b.dw = [self.newsem("ld_" + b.name), 0]
            b.dw[1] += 16
            ins.then_inc(b.dw[0], 16)
            for b2 in writes[1:]:
                b2.dw = b.dw
        elif reads:
            b = reads[0]
            if b.dr is None:
                b.dr = [self.newsem("st_" + b.name), 0]
            b.dr[1] += 16
            ins.then_inc(b.dr[0], 16)
        return ins

    def fence_stores(self, ename):
        E = self.engs[ename]
        for b in self.all_bufs:
            if b.dr is not None:
                self._wait(E, id(b.dr[0]), b.dr[0], b.dr[1])

    def fence_all(self, ename):
        E = self.engs[ename]
        for f, F in self.engs.items():
            if f != ename and F.count > 0:
                self._wait(E, f, F.sem, F.count)
        self.fence_stores(ename)


class Ring:
    def __init__(self, sch, es, nc, name, shape, dtype, n, psum=False):
        self.tiles, self.bufs, self.i = [], [], 0
        for k in range(n):
            nm = f"{name}{k}"
            t = es.enter_context(nc.psum_tensor(nm, shape, dtype) if psum else nc.sbuf_tensor(nm, shape, dtype))
            self.tiles.append(t)
            self.bufs.append(sch.buf(nm))

    def next(self):
        k = self.i % len(self.tiles)
        self.i += 1
        return self.tiles[k], self.bufs[k]


def t5_bucket_np(rel):
    nb = 16
    max_exact = 8
    n = np.abs(rel)
    upper = (rel > 0).astype(np.int32) * nb
    nf = np.maximum(n, 1).astype(np.float32)
    large = max_exact + (np.log(nf / max_exact) / math.log(1024 / max_exact) * (nb - max_exact)).astype(np.int32)
    large = np.minimum(large, nb - 1)
    return upper + np.where(n < max_exact, n, large)


def na_plan():
    def r0(r):
        return min(max(r - 4, 0), 120)
    tiles = {}
    steps = []
    for n in range(64):
        rows = (2 * n, 2 * n + 1)
        lo = min(r0(r) for r in rows)
        hi = max(r0(r) + 7 for r in rows)
        st = []
        for m in range(lo // 2, hi // 2 + 1):
            key = []
            for kl in range(2):
                kr = 2 * m + kl
                for ql in range(2):
                    r = rows[ql]
                    ok = r0(r) <= kr <= r0(r) + 7
                    key.append(kr - r + 7 if ok else -1)
            key = tuple(key)
            if all(k < 0 for k in key):
                continue
            if key not in tiles:
                tiles[key] = len(tiles)
            st.append((m, tiles[key]))
        steps.append(st)
    return steps, list(tiles.keys())


def na_bias_tiles(rpb, tile_keys):
    cols = np.arange(64)
    c0 = np.clip(cols - 8, 0, 48)
    kc = cols[:, None]
    qc = cols[None, :]
    colok = (kc >= c0[None, :]) & (kc < c0[None, :] + 16)
    dc = np.clip(kc - qc + 15, 0, 30)
    out = np.full((len(tile_keys), 128, 8, 128), NEG, np.float32)
    for t, key in enumerate(tile_keys):
        i = 0
        for kl in range(2):
            for ql in range(2):
                dr = key[i]
                i += 1
                if dr < 0:
                    continue
                blk = np.where(colok[None], rpb[:, dr][:, dc], NEG)
                out[t, kl * 64:(kl + 1) * 64, :, ql * 64:(ql + 1) * 64] = blk.transpose(1, 0, 2)
    return out


DIL = ((128, 1), (512, 4), (2048, 16))


def dil_plan():
    tl = []
    for g, (win, d) in enumerate(DIL):
        half = win // 2
        lo = -((half + 127) // 128)
        hi = (half + 127) // 128
        for o in range(lo, hi + 1):
            tl.append((g, o))
    return tl


def dil_bias_tiles(t5_table, tl):
    t5 = t5_table.reshape(32, 3, 4)
    out = np.full((len(tl), 128, 4, 128), NEG, np.float32)
    kk = np.arange(128)[:, None]
    qq = np.arange(128)[None, :]
    for t, (g, o) in enumerate(tl):
        win, d = DIL[g]
        rel = o * 128 + kk - qq
        ok = (np.abs(rel) <= win // 2) & (rel % d == 0)
        bk = t5_bucket_np(rel)
        for h in range(4):
            out[t, :, h, :] = np.where(ok, t5[bk, g, h], NEG)
    return out


QK_TILES = []
for i in range(4):
    QK_TILES.append((0 + 128 * i, 0))
for i in range(4):
    QK_TILES.append((512 + 128 * i, 1))
for i in range(6):
    QK_TILES.append((1536 + 128 * i, 2))
for i in range(6):
    QK_TILES.append((2304 + 128 * i, 3))
for i in range(2):
    QK_TILES.append((3840 + 128 * i, 4))
T_NAQ, T_NAK, T_DQ, T_DK, T_MQ = 0, 4, 8, 14, 20
NQK = len(QK_TILES)
V_SEGS = ((1024, 512, 0), (3072, 512, 512), (3584, 256, 1024))


def build_program(n_na_tiles, n_dil_tiles, na_steps, dil_tl, debug=False, phases=(1, 2, 3, 4)):
    nc = bass.Bass("TRN2", target_bir_lowering=False)
    dk = "ExternalOutput" if debug else "Internal"

    def din(name, shape, dt=F32):
        return nc.dram_tensor(name, list(shape), dt, kind="ExternalInput").ap()

    x = din("x", [S, D])
    mem = din("mem", [256, D])
    w_in = din("w_in", [D, 4096])
    w_mem = din("w_mem", [D, 512])
    w_out = din("w_out", [D, D])
    gvec = din("gvec", [128, 24])
    gains = din("gains", [128, 6])
    gffn_b = din("gffn_b", [128, D])
    ident_in = din("ident", [128, 128])
    na_bias = din("na_bias", [n_na_tiles, 128, 8, 128])
    dil_bias = din("dil_bias", [n_dil_tiles, 128, 4, 128])
    w_r = din("w_r", [D, 36])
    b_r = din("b_r", [128, 36])
    w1 = din("w1", [N_EXP * 256, 2048])
    w3 = din("w3", [N_EXP * 256, 2048])
    w2 = din("w2", [N_EXP * 256, 2048])
    iota_in = din("iota", [128, 256])
    pidx_in = din("pidx", [128, 1])
    tri_in = din("tri", [128, 128])
    out = nc.dram_tensor("out", [S, D], F32, kind="ExternalOutput").ap()

    qk_scr = nc.dram_tensor("qk_scr", [NQK, 128, S], BF16, kind=dk).ap()
    v_scr = nc.dram_tensor("v_scr", [S, 1280], BF16, kind=dk).ap()
    mix_scr = nc.dram_tensor("mix_scr", [S, D], BF16, kind=dk).ap()
    km_scr = nc.dram_tensor("km_scr", [2, 128, 256], BF16, kind=dk).ap()
    vm_scr = nc.dram_tensor("vm_scr", [256, 256], BF16, kind=dk).ap()
    h2_scr = nc.dram_tensor("h2_scr", [S, D], BF16, kind=dk).ap()
    xs_scr = nc.dram_tensor("xs_scr", [CAP, D], BF16, kind=dk).ap()
    yb_scr = nc.dram_tensor("yb_scr", [CAP, D], F32, kind=dk).ap()
    dbg_out = nc.dram_tensor("dbg_out", [128, 4 * NT + 2 * NBLK], F32, kind=dk).ap()

    with ExitStack() as es:
        sch = Sched(nc, es)

        def sb(name, shape, dt):
            return es.enter_context(nc.sbuf_tensor(name, list(shape), dt))

        def ps(name, shape, dt=F32):
            return es.enter_context(nc.psum_tensor(name, list(shape), dt))

        identf = sb("identf", [128, 128], F32)
        identb = sb("identb", [128, 128], BF16)
        blk1 = sb("blk1", [128, 128], BF16)
        gv = sb("gv", [128, 24], F32)
        gn = sb("gn", [128, 6], F32)
        epsb = sb("epsb", [128, 1], F32)
        b_const = sch.buf("const")
        sch.dma("sp", identf[:], ident_in[:, :], writes=[b_const])
        sch.dma("sp", gv[:], gvec[:, :], writes=[b_const])
        sch.dma("sp", gn[:], gains[:, :], writes=[b_const])
        sch.op("dve", lambda e: e.tensor_copy(out=identb[:], in_=identf[:]), reads=[b_const], writes=[b_const])
        sch.op("dve", lambda e: e.memset(blk1[:], 0.0), writes=[b_const])
        sch.op("dve", lambda e: e.memset(epsb[:], EPS), writes=[b_const])
        sch.op("dve", lambda e: e.memset(blk1[0:64, 0:64], 1.0), writes=[b_const])
        sch.op("dve", lambda e: e.memset(blk1[64:128, 64:128], 1.0), writes=[b_const])
        gq = gn[:].rearrange("p (a b) -> p a b", b=2)[:, :, 0:1]
        sch.op("dve", lambda e: e.tensor_scalar(out=gq, in0=gq, scalar1=0.125, scalar2=None, op0=ALU.mult),
               reads=[b_const], writes=[b_const])

        def projection(es1, src, n_tok, wsrc, ncols, gcol0, qk_tiles, qk_dst, v_segs, v_dst, tag):
            def sb1(name, shape, dt):
                return es1.enter_context(nc.sbuf_tensor(tag + name, list(shape), dt))
            W = sb1("W", [128, 8, ncols], BF16)
            bW = sch.buf(tag + "W")
            wst = Ring(sch, es1, nc, tag + "wst", [128, 2048], F32, 2)
            nhalf = (ncols + 2047) // 2048
            for kc in range(8):
                for hf in range(nhalf):
                    c0 = hf * 2048
                    cw = min(2048, ncols - c0)
                    t, b = wst.next()
                    sch.dma("sp", t[:, 0:cw], wsrc[kc * 128:(kc + 1) * 128, c0:c0 + cw], writes=[b])
                    if (kc * nhalf + hf) % 2 == 0:
                        sch.op("dve", lambda e, t=t, kc=kc, c0=c0, cw=cw: e.tensor_scalar(
                            out=W[:, kc, c0:c0 + cw], in0=t[:, 0:cw], scalar1=gv[:, gcol0 + kc:gcol0 + kc + 1],
                            scalar2=None, op0=ALU.mult), reads=[b, b_const], writes=[bW])
                    else:
                        sch.op("act", lambda e, t=t, kc=kc, c0=c0, cw=cw: e.activation(
                            out=W[:, kc, c0:c0 + cw], in_=t[:, 0:cw], func=AF.Copy, scale=gv[:, gcol0 + kc:gcol0 + kc + 1]),
                            reads=[b, b_const], writes=[bW])
            CH = min(512, n_tok)
            TT = CH // 128
            xt_r = Ring(sch, es1, nc, tag + "xt", [128, TT, D], F32, 2)
            hn_r = Ring(sch, es1, nc, tag + "hn", [128, TT, D], BF16, 2)
            hT_r = Ring(sch, es1, nc, tag + "hT", [128, 8, CH], BF16, 2)
            st_r = Ring(sch, es1, nc, tag + "st", [128, 8], F32, 2)
            junk = sb1("junk", [128, D], BF16)
            bjunk = sch.buf(tag + "junk")
            sq_r = Ring(sch, es1, nc, tag + "sq", [128, CH], BF16, 2)
            sd_r = Ring(sch, es1, nc, tag + "sd", [128, CH], F32, 2)
            rs_r = Ring(sch, es1, nc, tag + "rs", [128, CH], F32, 2)
            qn_r = Ring(sch, es1, nc, tag + "qn", [128, CH], BF16, 3)
            vo_r = Ring(sch, es1, nc, tag + "vo", [128, 1280], BF16, 2)
            tpb = [es1.enter_context(nc.psum_tensor(f"{tag}tp{i}", [128, 2, 512], BF16)) for i in range(1)]
            tpbuf = [sch.buf(tag + "tp0")]
            pbank = [None] + [es1.enter_context(nc.psum_tensor(f"{tag}pb{i}", [128, 512], F32)) for i in range(1, 8)]
            pbuf = [None] + [sch.buf(f"{tag}pb{i}") for i in range(1, 8)]
            def front1(ck):
                t0 = ck * CH
                xt, bxt = xt_r.next()
                sch.dma("sp", xt[:], src[t0:t0 + CH, :].rearrange("(t p) d -> p t d", p=128), writes=[bxt])
                stt, bst = st_r.next()
                for t in range(TT):
                    sch.op("act", lambda e, t=t: e.activation(out=junk[:], in_=xt[:, t, :], func=AF.Square,
                                                               accum_out=stt[:, t:t + 1]),
                           reads=[bxt], writes=[bjunk, bst])
                sch.op("act", lambda e: e.activation(out=stt[:, 4:4 + TT], in_=stt[:, 0:TT], func=AF.Sqrt,
                                                      bias=EPS, scale=1.0 / D), reads=[bst], writes=[bst])
                sch.op("dve", lambda e: e.reciprocal(out=stt[:, 4:4 + TT], in_=stt[:, 4:4 + TT]),
                       reads=[bst], writes=[bst])
                hn, bhn = hn_r.next()
                for t in range(TT):
                    sch.op("act", lambda e, t=t: e.activation(out=hn[:, t, :], in_=xt[:, t, :], func=AF.Copy,
                                                               scale=stt[:, 4 + t:5 + t]),
                           reads=[bxt, bst], writes=[bhn])
                return hn, bhn

            def front2(hn, bhn):
                hT_, bhT_ = hT_r.next()
                for kc in range(8):
                    bank = 0
                    sl = kc % 2
                    for t in range(TT):
                        sch.op("pe", lambda e, t=t, kc=kc, bank=bank, sl=sl: e.transpose(
                            out=tpb[bank][:, sl, t * 128:(t + 1) * 128], in_=hn[:, t, kc * 128:(kc + 1) * 128],
                            identity=identb[:]), reads=[bhn, b_const], writes=[tpbuf[bank]])
                    if sl == 1:
                        sch.op("dve", lambda e, kc=kc, bank=bank: e.tensor_copy(
                            out=hT_[:, kc - 1:kc + 1, :], in_=tpb[bank][:, :, 0:CH]),
                            reads=[tpbuf[bank]], writes=[bhT_])
                return hT_, bhT_

            NCK = n_tok // CH
            cur_hT = front2(*front1(0))
            for ck in range(NCK):
                t0 = ck * CH
                hT, bhT = cur_hT
                nxt1 = front1(ck + 1) if ck + 1 < NCK else None
                nxt_hT = None
                def qk_mm(j):
                    c0, gc = qk_tiles[j]
                    qb = 1 + (j % 3)
                    for kc in range(8):
                        sch.op("pe", lambda e, kc=kc, c0=c0, qb=qb: e.matmul(
                            pbank[qb][:, 0:CH], W[:, kc, c0:c0 + 128], hT[:, kc, :], start=(kc == 0), stop=(kc == 7)),
                            reads=[bW, bhT], writes=[pbuf[qb]])
                    sq, bsq = sq_r.next()
                    sch.op("act", lambda e, qb=qb, sq=sq: e.activation(out=sq[:], in_=pbank[qb][:, 0:CH], func=AF.Square),
                           reads=[pbuf[qb]], writes=[bsq])
                    return sq, bsq

                def qk_epi(j, sq, bsq):
                    c0, gc = qk_tiles[j]
                    qb = 1 + (j % 3)
                    sbk = 4 + (j % 2)
                    sch.op("pe", lambda e, sbk=sbk, sq=sq: e.matmul(pbank[sbk][:, 0:CH], blk1[:], sq[:], start=True, stop=True),
                           reads=[bsq, b_const], writes=[pbuf[sbk]])
                    sd, bsd = sd_r.next()
                    rs, brs = rs_r.next()
                    if LNEXP:
                        sch.op("act", lambda e, sbk=sbk, sd=sd: e.activation(out=sd[:], in_=pbank[sbk][:, 0:CH], func=AF.Ln,
                                                                             bias=epsb[:, 0:1], scale=1.0 / 64),
                               reads=[pbuf[sbk], b_const], writes=[bsd])
                        sch.op("act", lambda e, sd=sd, rs=rs: e.activation(out=rs[:], in_=sd[:], func=AF.Exp, scale=-0.5),
                               reads=[bsd], writes=[brs])
                    else:
                        sch.op("act", lambda e, sbk=sbk, sd=sd: e.activation(out=sd[:], in_=pbank[sbk][:, 0:CH], func=AF.Sqrt,
                                                                             bias=EPS, scale=1.0 / 64),
                               reads=[pbuf[sbk]], writes=[bsd])
                        sch.op("dve", lambda e, sd=sd, rs=rs: e.reciprocal(out=rs[:], in_=sd[:]), reads=[bsd], writes=[brs])
                    qn, bqn = qn_r.next()
                    sch.op("dve", lambda e, qb=qb, rs=rs, qn=qn, gc=gc: e.scalar_tensor_tensor(
                        out=qn[:], in0=pbank[qb][:, 0:CH], scalar=gn[:, gc:gc + 1], in1=rs[:], op0=ALU.mult, op1=ALU.mult),
                        reads=[pbuf[qb], brs, b_const], writes=[bqn])
                    sch.dma("pool", qk_dst(j, t0, CH), qn[:], reads=[bqn])

                prev = None
                for j in range(len(qk_tiles) + 1):
                    cur_ = qk_mm(j) if j < len(qk_tiles) else None
                    if prev is not None:
                        qk_epi(j - 1, *prev)
                    prev = cur_
                    if j == len(qk_tiles) // 2 and nxt1 is not None and nxt_hT is None:
                        nxt_hT = front2(*nxt1)
                if nxt1 is not None and nxt_hT is None:
                    nxt_hT = front2(*nxt1)
                for t in range(TT):
                    vo, bvo = vo_r.next()
                    for si, (c0, cw, d0) in enumerate(v_segs):
                        vb = 6 + ((t * len(v_segs) + si) % 2)
                        for kc in range(8):
                            sch.op("pe", lambda e, kc=kc, c0=c0, cw=cw, vb=vb, t=t: e.matmul(
                                pbank[vb][:, 0:cw], hT[:, kc, t * 128:(t + 1) * 128], W[:, kc, c0:c0 + cw],
                                start=(kc == 0), stop=(kc == 7)), reads=[bW, bhT], writes=[pbuf[vb]])
                        sch.op("act", lambda e, vb=vb, cw=cw, d0=d0, vo=vo: e.activation(
                            out=vo[:, d0:d0 + cw], in_=pbank[vb][:, 0:cw], func=AF.Copy),
                            reads=[pbuf[vb]], writes=[bvo])
                    vw = sum(s_[1] for s_ in v_segs)
                    sch.dma("pool", v_dst(t0 + t * 128, vw), vo[:, 0:vw], reads=[bvo])
                cur_hT = nxt_hT

        if 1 in phases:
            with ExitStack() as es1:
                projection(es1, x, S, w_in, 4096, 0, QK_TILES,
                           lambda j, t0, n: qk_scr[j, :, t0:t0 + n], V_SEGS,
                           lambda t0, vw: v_scr[t0:t0 + 128, 0:vw], "p1")
                sch.fence_all("sp")
                sch.fence_all("pool")
            with ExitStack() as es1:
                projection(es1, mem, 256, w_mem, 512, 8, [(0, 5), (128, 5)],
                           lambda j, t0, n: km_scr[j, :, t0:t0 + n], ((256, 256, 0),),
                           lambda t0, vw: vm_scr[t0:t0 + 128, 0:vw], "pm")
                sch.fence_all("sp")
                sch.fence_all("pool")


        def attn_pass(tag, qsrc, ksrc, sk, vsrc, vruns, slots, steps_fn, OH, bias, mix_col0):
            with ExitStack() as e2:
                def sb2(name, shape, dt):
                    return e2.enter_context(nc.sbuf_tensor(tag + name, list(shape), dt))
                nkb = sk // 128
                QT = [sb2(f"QT{i}", [128, S], BF16) for i in range(len(qsrc))]
                KT = [sb2(f"KT{i}", [128, sk], BF16) for i in range(len(ksrc))]
                nv = sum(c for _, c in vruns)
                V1 = sb2("V1", [128, nkb, nv, 65], BF16)
                bin_ = sch.buf(tag + "in")
                SKIP = _os.environ.get("P2SKIP", "")
                for i, a in enumerate(qsrc if "q" not in SKIP else []):
                    for hf in range(2):
                        sch.dma("sp", QT[i][:, hf * S // 2:(hf + 1) * S // 2], a[:, hf * S // 2:(hf + 1) * S // 2], writes=[bin_], disjoint=True)
                for i, a in enumerate(ksrc if "k" not in SKIP else []):
                    sch.dma("sp", KT[i][:], a, writes=[bin_], disjoint=True)
                s0 = 0
                for (vc0, cnt) in (vruns if "v" not in SKIP else []):
                    for c in range(cnt):
                        vv = vsrc[:, vc0 + c * 64:vc0 + (c + 1) * 64].rearrange("(b p) d -> p b d", p=128)
                        for b0 in range(0, nkb, 16):
                            b1 = min(nkb, b0 + 16)
                            sch.dma("sp", V1[:, b0:b1, s0 + c, 0:64], vv[:, b0:b1, :], writes=[bin_], disjoint=True)
                    s0 += cnt
                if "m" not in SKIP:
                    sch.op("pool", lambda e: e.memset(V1[:, :, :, 64:65], 1.0), writes=[bin_])
                EB = None
                if bias is not None:
                    bd, h0, Hs = bias
                    ntile = bd.shape[0]
                    Hh = Hs // 2
                    EB = sb2("EB", [128, 2, ntile, Hh, 128], BF16)
                    ebs = Ring(sch, e2, nc, tag + "ebs", [128, Hs, 128], F32, 1)
                    for t in range(ntile):
                        st, bst = ebs.next()
                        sch.dma("sp", st[:], bd[t, :, h0:h0 + Hs, :], writes=[bst])
                        sch.op("act", lambda e, st=st, t=t: e.activation(
                            out=EB[:, :, t, :, :], in_=st[:].rearrange("p (i f) q -> p f i q", f=2), func=AF.Exp),
                               reads=[bst], writes=[bin_])
                sps = Ring(sch, e2, nc, tag + "sps", [128, 512], F32, 4, psum=True)
                ops_ = Ring(sch, e2, nc, tag + "ops", [128, 512], F32, 2, psum=True)
                pr = Ring(sch, e2, nc, tag + "P", [128, 512], BF16, 5)
                rd_r = Ring(sch, e2, nc, tag + "rd", [128, 8], F32, 2)
                mx_r = Ring(sch, e2, nc, tag + "mx", [128, 4, OH * 64], BF16, 2)
                mx_state = [None, None]
                recs = []
                for n in range(NT):
                    pend = ([], [])
                    groups = []
                    for (m, tid, sl) in steps_fn(n):
                        for si in sl:
                            hf = slots[si][2]
                            pend[hf].append((m, tid, slots[si]))
                            if len(pend[hf]) == 4:
                                groups.append(list(pend[hf]))
                                pend[hf].clear()
                    for hf in range(2):
                        if pend[hf]:
                            groups.append(list(pend[hf]))
                    for gi, grp in enumerate(groups):
                        recs.append(dict(n=n, grp=grp, first=(gi == 0), last=(gi == len(groups) - 1)))

                def emitA(rcs):
                    for rc in rcs:
                        rc["sp"], rc["bsp"] = sps.next()
                    for i in range(4):
                        for rc in rcs:
                            grp = rc["grp"]
                            if i >= len(grp):
                                continue
                            n = rc["n"]
                            (m, tid, (qt, kt, half, vs, oh, bh)) = grp[i]
                            pl = slice(half * 64, half * 64 + 64)
                            sp_ = rc["sp"]
                            sch.op("pe", lambda e, i=i, m=m, qt=qt, kt=kt, pl=pl, sp_=sp_, n=n: e.matmul(
                                sp_[:, i * 128:(i + 1) * 128], KT[kt][pl, m * 128:(m + 1) * 128],
                                QT[qt][pl, n * 128:(n + 1) * 128], start=True, stop=True),
                                reads=[bin_], writes=[rc["bsp"]])
                    for rc in rcs:
                        grp, sp_, bsp = rc["grp"], rc["sp"], rc["bsp"]
                        w = len(grp) * 128
                        P, bP = pr.next()
                        rc["P"], rc["bP"] = P, bP
                        sch.op("act", lambda e, sp_=sp_, P=P, w=w: e.activation(out=P[:, 0:w], in_=sp_[:, 0:w], func=AF.Exp),
                               reads=[bsp], writes=[bP])
                        if EB is not None:
                            def eoff(job):
                                return (job[2][2] * ntile + job[1]) * Hh + job[2][5] // 2
                            i = 0
                            while i < len(grp):
                                off = eoff(grp[i])
                                j = i + 1
                                while j < len(grp) and eoff(grp[j]) == off + (j - i):
                                    j += 1
                                ebv = EB[:].rearrange("p f t h q -> p (f t h) q")[:, off:off + (j - i), :]
                                pv = P[:, i * 128:j * 128].rearrange("p (a q) -> p a q", q=128)
                                sch.op("dve", lambda e, pv=pv, ebv=ebv: e.tensor_tensor(out=pv, in0=pv, in1=ebv, op=ALU.mult),
                                       reads=[bP, bin_], writes=[bP])
                                i = j

                cur_o = [None, None]

                def emitB(rc):
                    n, grp, P, bP = rc["n"], rc["grp"], rc["P"], rc["bP"]
                    if rc["first"]:
                        cur_o[0], cur_o[1] = ops_.next()
                    oacc, boacc = cur_o
                    for i, (m, tid, (qt, kt, half, vs, oh, bh)) in enumerate(grp):
                        fst = rc["first"] and i == 0
                        lst = rc["last"] and i == len(grp) - 1
                        sch.op("pe", lambda e, i=i, m=m, vs=vs, oh=oh, P=P, oacc=oacc, fst=fst, lst=lst: e.matmul(
                            oacc[:, oh * 65:(oh + 1) * 65], P[:, i * 128:(i + 1) * 128], V1[:, m, vs, :],
                            start=fst, stop=lst, skip_group_check=True),
                            reads=[bP, bin_], writes=[boacc])
                    if not rc["last"]:
                        return
                    rd, brd = rd_r.next()
                    ov = oacc[:, 0:OH * 65].rearrange("p (h c) -> p h c", c=65)
                    sch.op("dve", lambda e, rd=rd, ov=ov: e.reciprocal(out=rd[:, 0:OH], in_=ov[:, :, 64]),
                           reads=[boacc], writes=[brd])
                    if n % 4 == 0:
                        mx_state[0], mx_state[1] = mx_r.next()
                    mx, bmx = mx_state
                    for h in range(OH):
                        sch.op("dve", lambda e, h=h, rd=rd, oacc=oacc, mx=mx: e.tensor_scalar(
                            out=mx[:, n % 4, h * 64:(h + 1) * 64], in0=oacc[:, h * 65:h * 65 + 64],
                            scalar1=rd[:, h:h + 1], scalar2=None, op0=ALU.mult),
                            reads=[boacc, brd], writes=[bmx])
                    if n % 4 == 3:
                        r0_ = (n - 3) * 128
                        sch.dma("pool", mix_scr[r0_:r0_ + 512, mix_col0:mix_col0 + OH * 64].rearrange("(t p) c -> p t c", p=128),
                                mx[:], reads=[bmx])

                units = []
                i = 0
                while i < len(recs):
                    if i + 1 < len(recs) and recs[i]["grp"][0][2][2] != recs[i + 1]["grp"][0][2][2]:
                        units.append([recs[i], recs[i + 1]])
                        i += 2
                    else:
                        units.append([recs[i]])
                        i += 1
                LA = 1
                for i in range(len(units) + LA):
                    if i < len(units):
                        emitA(units[i])
                    if i - LA >= 0:
                        for rc in units[i - LA]:
                            emitB(rc)
                sch.fence_all("sp")
                sch.fence_all("pool")

        SEL = _os.environ.get("P2SEL", "na0,na1,dl0,dl1,mm").split(",")
        if 2 in phases:
            for ps_ in range(2):
                if f"na{ps_}" not in SEL:
                    continue
                slots = [(h // 2, h // 2, h % 2, h, h, h) for h in range(4)]
                attn_pass(f"na{ps_}", [qk_scr[T_NAQ + 2 * ps_ + i] for i in range(2)],
                          [qk_scr[T_NAK + 2 * ps_ + i] for i in range(2)], S, v_scr, [(ps_ * 256, 4)], slots,
                          lambda n: [(m, tid, [0, 1, 2, 3]) for (m, tid) in na_steps[n]], 4,
                          (na_bias, ps_ * 4, 4), ps_ * 256)
            for ps_ in range(2):
                if f"dl{ps_}" not in SEL:
                    continue
                slots = []
                for g in range(3):
                    for hh in range(2):
                        slots.append((g, g, hh, g * 2 + hh, hh, hh))

                def dsteps(n):
                    st = []
                    for t, (g, o) in enumerate(dil_tl):
                        m = n + o
                        if 0 <= m < NT:
                            st.append((m, t, [g * 2, g * 2 + 1]))
                    return st
                attn_pass(f"dl{ps_}", [qk_scr[T_DQ + 2 * g + ps_] for g in range(3)],
                          [qk_scr[T_DK + 2 * g + ps_] for g in range(3)], S, v_scr,
                          [(512 + g * 256 + ps_ * 128, 2) for g in range(3)], slots, dsteps, 2,
                          (dil_bias, ps_ * 2, 2), 512 + ps_ * 128)
            slots = [(h // 2, h // 2, h % 2, h, h, 0) for h in range(4)]
            if "mm" in SEL:
              attn_pass("mm", [qk_scr[T_MQ + i] for i in range(2)], [km_scr[i] for i in range(2)], 256, vm_scr,
                      [(0, 4)], slots, lambda n: [(0, None, [0, 1, 2, 3]), (1, None, [0, 1, 2, 3])], 4, None, 768)


        g1 = sb("g1", [128, NT], F32)
        g2 = sb("g2", [128, NT], F32)
        dst1i = sb("dst1i", [128, NT], I32)
        dst2i = sb("dst2i", [128, NT], I32)
        widx = sb("widx", [128, NBLK * 2], I32)
        bRt = sch.buf("routeout")
        if 3 in phases:
            with ExitStack() as e3:
                cur = [e3]
                def sb3(name, shape, dt):
                    return cur[0].enter_context(nc.sbuf_tensor("p3" + name, list(shape), dt))
                L = sb3("L", [128, NT, 36], F32)
                bL = sch.buf("L")
                e3m = ExitStack()
                e3m.__enter__()
                cur[0] = e3m
                Wo = sb3("Wo", [128, 8, D], BF16)
                bWo = sch.buf("Wo")
                wst = Ring(sch, cur[0], nc, "p3wst", [128, D], F32, 2)
                for kc in range(8):
                    t, b = wst.next()
                    sch.dma("sp", t[:], w_out[kc * 128:(kc + 1) * 128, :], writes=[b])
                    sch.op("dve", lambda e, t=t, kc=kc: e.tensor_copy(out=Wo[:, kc, :], in_=t[:]), reads=[b], writes=[bWo])
                wr = sb3("wr", [128, 8, 36], F32)
                br = sb3("br", [128, 36], F32)
                gB = sb3("gB", [128, D], F32)
                bwr = sch.buf("wr")
                sch.dma("sp", wr[:], w_r.rearrange("(kc p) n -> p kc n", p=128), writes=[bwr])
                sch.dma("sp", br[:], b_r[:, :], writes=[bwr])
                sch.dma("sp", gB[:], gffn_b[:, :], writes=[bwr])
                for kc in range(8):
                    sch.op("dve", lambda e, kc=kc: e.tensor_scalar(out=wr[:, kc, :], in0=wr[:, kc, :],
                                                                    scalar1=gv[:, 16 + kc:17 + kc], scalar2=None, op0=ALU.mult),
                           reads=[bwr, b_const], writes=[bwr])
                wrh = sb3("wrh", [128, 8, 36], BF16)
                wrl = sb3("wrl", [128, 8, 36], BF16)
                sch.op("dve", lambda e: e.tensor_copy(out=wrh[:], in_=wr[:]), reads=[bwr], writes=[bwr])
                sch.op("dve", lambda e: e.tensor_tensor(out=wrl[:], in0=wr[:], in1=wrh[:], op=ALU.subtract), reads=[bwr], writes=[bwr])
                mx_r = Ring(sch, cur[0], nc, "p3mx", [128, D], BF16, 2)
                xt_r = Ring(sch, cur[0], nc, "p3xt", [128, D], F32, 2)
                mT_r = Ring(sch, cur[0], nc, "p3mT", [128, 8, 128], BF16, 2)
                x1_r = Ring(sch, cur[0], nc, "p3x1", [128, D], F32, 2)
                hn_r = Ring(sch, cur[0], nc, "p3hn", [128, D], F32, 2)
                hi_r = Ring(sch, cur[0], nc, "p3hi", [128, D], BF16, 3)
                lo_r = Ring(sch, cur[0], nc, "p3lo", [128, D], BF16, 3)
                hg_r = Ring(sch, cur[0], nc, "p3hg", [128, D], BF16, 2)
                hiT_r = Ring(sch, cur[0], nc, "p3hiT", [128, 8, 128], BF16, 2)
                loT_r = Ring(sch, cur[0], nc, "p3loT", [128, 8, 128], BF16, 2)
                st_r = Ring(sch, cur[0], nc, "p3st", [128, 4], F32, 2)
                junk = sb3("junk", [128, D], BF16)
                bjunk = sch.buf("p3junk")
                with ExitStack() as e3p:
                    tpm = e3p.enter_context(nc.psum_tensor("p3tpm", [128, 8, 128], BF16))
                    btpm = sch.buf("p3tpm")
                    yps = Ring(sch, e3p, nc, "p3yps", [128, 512], F32, 4, psum=True)
                    tph = e3p.enter_context(nc.psum_tensor("p3tph", [128, 8, 128], BF16))
                    tpl = e3p.enter_context(nc.psum_tensor("p3tpl", [128, 8, 128], BF16))
                    btph, btpl = sch.buf("p3tph"), sch.buf("p3tpl")
                    lps = e3p.enter_context(nc.psum_tensor("p3lps", [128, 512], F32))
                    blps = sch.buf("p3lps")
                    def p3A(n):
                        r0_ = n * 128
                        mxt, bmx = mx_r.next()
                        xt, bxt = xt_r.next()
                        sch.dma("sp", mxt[:], mix_scr[r0_:r0_ + 128, :], writes=[bmx])
                        sch.dma("sp", xt[:], x[r0_:r0_ + 128, :], writes=[bxt])
                        for kc in range(8):
                            sch.op("pe", lambda e, kc=kc, mxt=mxt: e.transpose(out=tpm[:, kc, :], in_=mxt[:, kc * 128:(kc + 1) * 128],
                                                                      identity=identb[:]), reads=[bmx, b_const], writes=[btpm])
                        mT, bmT = mT_r.next()
                        sch.op("dve", lambda e, mT=mT: e.tensor_copy(out=mT[:], in_=tpm[:]), reads=[btpm], writes=[bmT])
                        x1, bx1 = x1_r.next()
                        for hf in range(2):
                            yp, byp = yps.next()
                            for kc in range(8):
                                sch.op("pe", lambda e, kc=kc, hf=hf, yp=yp, mT=mT: e.matmul(
                                    yp[:], mT[:, kc, :], Wo[:, kc, hf * 512:(hf + 1) * 512], start=(kc == 0), stop=(kc == 7)),
                                    reads=[bmT, bWo], writes=[byp])
                            sch.op("dve", lambda e, hf=hf, yp=yp, x1=x1, xt=xt: e.tensor_tensor(
                                out=x1[:, hf * 512:(hf + 1) * 512], in0=yp[:], in1=xt[:, hf * 512:(hf + 1) * 512], op=ALU.add),
                                reads=[byp, bxt], writes=[bx1])
                        sch.dma("pool", out[r0_:r0_ + 128, :], x1[:], reads=[bx1])
                        stt, bst = st_r.next()
                        sch.op("act", lambda e, x1=x1, stt=stt: e.activation(out=junk[:], in_=x1[:], func=AF.Square,
                                                                           accum_out=stt[:, 0:1]), reads=[bx1], writes=[bjunk, bst])
                        sch.op("act", lambda e, stt=stt: e.activation(out=stt[:, 1:2], in_=stt[:, 0:1], func=AF.Sqrt,
                                                                       bias=EPS, scale=1.0 / D), reads=[bst], writes=[bst])
                        sch.op("dve", lambda e, stt=stt: e.reciprocal(out=stt[:, 1:2], in_=stt[:, 1:2]), reads=[bst], writes=[bst])
                        hn, bhn = hn_r.next()
                        sch.op("act", lambda e, hn=hn, x1=x1, stt=stt: e.activation(out=hn[:], in_=x1[:], func=AF.Copy,
                                                                                   scale=stt[:, 1:2]), reads=[bx1, bst], writes=[bhn])
                        hi, bhi = hi_r.next()
                        lo, blo = lo_r.next()
                        hg, bhg = hg_r.next()
                        sch.op("act", lambda e, hi=hi, hn=hn: e.activation(out=hi[:], in_=hn[:], func=AF.Copy), reads=[bhn], writes=[bhi])
                        sch.op("dve", lambda e, hi=hi, lo=lo, hn=hn: e.tensor_tensor(out=lo[:], in0=hn[:], in1=hi[:], op=ALU.subtract),
                               reads=[bhn, bhi], writes=[blo])
                        sch.op("pool", lambda e, hg=hg, hn=hn: e.tensor_tensor(out=hg[:], in0=hn[:], in1=gB[:], op=ALU.mult),
                               reads=[bhn, bwr], writes=[bhg])
                        sch.dma("pool", h2_scr[r0_:r0_ + 128, :], hg[:], reads=[bhg])
                        return hi, bhi, lo, blo

                    def p3B(n, hi, bhi, lo, blo):
                        for kc in range(8):
                            sch.op("pe", lambda e, kc=kc, hi=hi: e.transpose(out=tph[:, kc, :], in_=hi[:, kc * 128:(kc + 1) * 128],
                                                                            identity=identb[:]), reads=[bhi, b_const], writes=[btph])
                        for kc in range(8):
                            sch.op("pe", lambda e, kc=kc, lo=lo: e.transpose(out=tpl[:, kc, :], in_=lo[:, kc * 128:(kc + 1) * 128],
                                                                            identity=identb[:]), reads=[blo, b_const], writes=[btpl])
                        hiT, bhiT = hiT_r.next()
                        loT, bloT = loT_r.next()
                        sch.op("act", lambda e, hiT=hiT: e.activation(out=hiT[:], in_=tph[:], func=AF.Copy), reads=[btph], writes=[bhiT])
                        sch.op("act", lambda e, loT=loT: e.activation(out=loT[:], in_=tpl[:], func=AF.Copy), reads=[btpl], writes=[bloT])
                        k = 0
                        for (aa, ba, ww) in ((hiT, bhiT, wrh), (hiT, bhiT, wrl), (loT, bloT, wrh)):
                            for kc in range(8):
                                sch.op("pe", lambda e, kc=kc, aa=aa, ww=ww, k=k: e.matmul(lps[:, 0:36], aa[:, kc, :], ww[:, kc, :],
                                                                                     start=(k == 0), stop=(k == 23)),
                                       reads=[ba, bwr], writes=[blps])
                                k += 1
                        sch.op("dve", lambda e, n=n: e.tensor_tensor(out=L[:, n, :], in0=lps[:, 0:36], in1=br[:], op=ALU.add),
                               reads=[blps, bwr], writes=[bL])


                    prev3 = None
                    for n in range(NT + 1):
                        cur3 = p3A(n) if n < NT else None
                        if prev3 is not None:
                            p3B(n - 1, *prev3)
                        prev3 = cur3
                e3m.close()
                cur[0] = e3
                def sbr(name, shape, dt=F32):
                    return e3.enter_context(nc.sbuf_tensor("rt" + name, list(shape), dt))
                bR = sch.buf("route")
                gl = L[:, :, 0:4]
                fl = L[:, :, 4:36]
                gmax = sbr("gmax", [128, NT]); G1 = sbr("G1", [128, NT, 4]); ge = sbr("ge", [128, NT, 4])
                gsum = sbr("gsum", [128, NT]); pen = sbr("pen", [128, NT, 4]); flm = sbr("flm", [128, NT, 32])
                m1 = sbr("m1", [128, NT]); oh1 = sbr("oh1", [128, NT, 32]); m2 = sbr("m2", [128, NT])
                oh2 = sbr("oh2", [128, NT, 32]); dd = sbr("dd", [128, NT])

                def R(eng, fn, extra_w=()):
                    sch.op(eng, fn, reads=[bL, bR, b_const], writes=[bR] + list(extra_w))

                def bc(a, n):
                    return a[:, :].unsqueeze(2).broadcast_to([128, NT, n])
                R("dve", lambda e: e.tensor_reduce(out=gmax[:], in_=gl, axis=AX.X, op=ALU.max))
                R("dve", lambda e: e.tensor_tensor(out=G1[:], in0=gl, in1=bc(gmax, 4), op=ALU.is_ge))
                R("dve", lambda e: e.tensor_tensor(out=ge[:], in0=gl, in1=bc(gmax, 4), op=ALU.subtract))
                R("act", lambda e: e.activation(out=ge[:], in_=ge[:], func=AF.Exp))
                R("dve", lambda e: e.tensor_reduce(out=gsum[:], in_=ge[:], axis=AX.X, op=ALU.add))
                R("dve", lambda e: e.reciprocal(out=gsum[:], in_=gsum[:]))
                R("dve", lambda e: e.tensor_scalar(out=pen[:], in0=G1[:], scalar1=1e9, scalar2=-1e9, op0=ALU.mult, op1=ALU.add))
                R("dve", lambda e: e.tensor_tensor(
                    out=flm[:].rearrange("p t (g e) -> p t g e", e=8), in0=fl.rearrange("p t (g e) -> p t g e", e=8),
                    in1=pen[:].unsqueeze(3).broadcast_to([128, NT, 4, 8]), op=ALU.add))
                R("dve", lambda e: e.tensor_reduce(out=m1[:], in_=flm[:], axis=AX.X, op=ALU.max))
                R("dve", lambda e: e.tensor_tensor(out=oh1[:], in0=flm[:], in1=bc(m1, 32), op=ALU.is_ge))
                R("dve", lambda e: e.scalar_tensor_tensor(out=flm[:], in0=oh1[:], scalar=-1e9, in1=flm[:], op0=ALU.mult, op1=ALU.add))
                R("dve", lambda e: e.tensor_reduce(out=m2[:], in_=flm[:], axis=AX.X, op=ALU.max))
                R("dve", lambda e: e.tensor_tensor(out=oh2[:], in0=flm[:], in1=bc(m2, 32), op=ALU.is_ge))
                R("dve", lambda e: e.tensor_tensor(out=dd[:], in0=m2[:], in1=m1[:], op=ALU.subtract))
                R("act", lambda e: e.activation(out=dd[:], in_=dd[:], func=AF.Exp))
                R("dve", lambda e: e.tensor_scalar(out=g1[:], in0=dd[:], scalar1=1.0, scalar2=None, op0=ALU.add), [bRt])
                R("dve", lambda e: e.reciprocal(out=g1[:], in_=g1[:]), [bRt])
                R("dve", lambda e: e.tensor_tensor(out=g1[:], in0=g1[:], in1=gsum[:], op=ALU.mult), [bRt])
                R("dve", lambda e: e.tensor_tensor(out=g2[:], in0=g1[:], in1=dd[:], op=ALU.mult), [bRt])
                selb = sbr("selb", [128, NT * 32], BF16)
                trif = sbr("trif", [128, 128]); trib = sbr("trib", [128, 128], BF16); oneb = sbr("oneb", [128, 128], BF16)
                iot = sbr("iot", [128, 256]); pid2 = sbr("pid2", [128, 1])
                sch.dma("sp", trif[:], tri_in[:, :], writes=[bR])
                sch.dma("sp", iot[:], iota_in[:, :], writes=[bR])
                sch.dma("sp", pid2[:], pidx_in[:, :], writes=[bR])
                R("dve", lambda e: e.tensor_copy(out=trib[:], in_=trif[:]))
                R("dve", lambda e: e.memset(oneb[:], 1.0))
                R("dve", lambda e: e.tensor_scalar(out=pid2[:], in0=pid2[:], scalar1=2.0, scalar2=None, op0=ALU.mult))
                R("dve", lambda e: e.tensor_tensor(out=selb[:], in0=oh1[:].rearrange("p t e -> p (t e)"),
                                                   in1=oh2[:].rearrange("p t e -> p (t e)"), op=ALU.add))
                Cs = sbr("Cs", [128, NT, 32]); Ta = sbr("Ta", [128, NT, 32]); Tb = sbr("Tb", [128, NT, 32]); T0 = sbr("T0", [128, NT, 32])
                with ExitStack() as e3r:
                    cps = [e3r.enter_context(nc.psum_tensor(f"rtc{j}", [128, 512], F32)) for j in range(4)]
                    tps = [e3r.enter_context(nc.psum_tensor(f"rtt{j}", [128, 512], F32)) for j in range(4)]
                    bcp, btp = sch.buf("rtc"), sch.buf("rtt")
                    Cf = Cs[:].rearrange("p t e -> p (t e)")
                    T0f = T0[:].rearrange("p t e -> p (t e)")
                    for j in range(4):
                        sch.op("pe", lambda e, j=j: e.matmul(cps[j][:], trib[:], selb[:, j * 512:(j + 1) * 512], start=True, stop=True),
                               reads=[bR], writes=[bcp])
                        sch.op("pe", lambda e, j=j: e.matmul(tps[j][:], oneb[:], selb[:, j * 512:(j + 1) * 512], start=True, stop=True),
                               reads=[bR], writes=[btp])
                    for j in range(4):
                        sch.op("act", lambda e, j=j: e.activation(out=Cf[:, j * 512:(j + 1) * 512], in_=cps[j][:], func=AF.Copy),
                               reads=[bcp], writes=[bR])
                        sch.op("dve", lambda e, j=j: e.tensor_copy(out=T0f[:, j * 512:(j + 1) * 512], in_=tps[j][:]),
                               reads=[btp], writes=[bR])
                src, dstb = T0, Ta
                for sft in (1, 2, 4, 8, 16, 32):
                    R("dve", lambda e, src=src, dstb=dstb, sft=sft: e.tensor_copy(out=dstb[:, 0:sft, :], in_=src[:, 0:sft, :]))
                    R("dve", lambda e, src=src, dstb=dstb, sft=sft: e.tensor_tensor(
                        out=dstb[:, sft:NT, :], in0=src[:, sft:NT, :], in1=src[:, 0:NT - sft, :], op=ALU.add))
                    src, dstb = dstb, (Tb if dstb is Ta else Ta)
                Inc = src
                cnt = sbr("cnt", [128, 32]); nbk = sbr("nbk", [128, 32]); cmp1 = sbr("cmp1", [128, 32, 128])
                i128 = sbr("i128", [128, 128]); sa = sbr("sa", [128, 32]); sb_ = sbr("sb_", [128, 32])
                psr = sbr("psr", [128, 32]); pend = sbr("pend", [128, 32])
                R("dve", lambda e: e.tensor_copy(out=cnt[:], in_=Inc[:, NT - 1, :]))
                R("dve", lambda e: e.tensor_scalar(out=i128[:], in0=iot[:, 0:128], scalar1=float(MOE_B), scalar2=None, op0=ALU.mult))
                R("dve", lambda e: e.tensor_tensor(out=cmp1[:], in0=cnt[:, :].unsqueeze(2).broadcast_to([128, 32, 128]),
                                                   in1=i128[:, :].unsqueeze(1).broadcast_to([128, 32, 128]), op=ALU.is_gt))
                R("dve", lambda e: e.tensor_reduce(out=nbk[:], in_=cmp1[:], axis=AX.X, op=ALU.add))
                src, dstb = nbk, sa
                for sft in (1, 2, 4, 8, 16):
                    R("dve", lambda e, src=src, dstb=dstb, sft=sft: e.tensor_copy(out=dstb[:, 0:sft], in_=src[:, 0:sft]))
                    R("dve", lambda e, src=src, dstb=dstb, sft=sft: e.tensor_tensor(
                        out=dstb[:, sft:32], in0=src[:, sft:32], in1=src[:, 0:32 - sft], op=ALU.add))
                    src, dstb = dstb, (sb_ if dstb is sa else sa)
                R("dve", lambda e, src=src: e.tensor_copy(out=pend[:], in_=src[:]))
                R("dve", lambda e: e.tensor_tensor(out=psr[:], in0=pend[:], in1=nbk[:], op=ALU.subtract))
                R("dve", lambda e: e.tensor_scalar(out=psr[:], in0=psr[:], scalar1=float(MOE_B), scalar2=None, op0=ALU.mult))
                R("dve", lambda e, Inc=Inc: e.tensor_tensor(out=Cs[:], in0=Cs[:], in1=Inc[:], op=ALU.add))
                R("dve", lambda e: e.tensor_tensor(out=Cs[:], in0=Cs[:], in1=T0[:], op=ALU.subtract))
                R("dve", lambda e: e.tensor_tensor(out=Cs[:], in0=Cs[:], in1=psr[:, :].unsqueeze(1).broadcast_to([128, NT, 32]), op=ALU.add))
                d1 = sbr("d1", [128, NT]); d2 = sbr("d2", [128, NT])
                R("dve", lambda e: e.tensor_tensor(out=Ta[:], in0=Cs[:], in1=oh1[:], op=ALU.mult))
                R("dve", lambda e: e.tensor_reduce(out=d1[:], in_=Ta[:], axis=AX.X, op=ALU.add))
                R("dve", lambda e: e.tensor_tensor(out=Tb[:], in0=Cs[:], in1=oh2[:], op=ALU.mult))
                R("dve", lambda e: e.tensor_reduce(out=d2[:], in_=Tb[:], axis=AX.X, op=ALU.add))
                R("dve", lambda e: e.tensor_copy(out=dst1i[:], in_=d1[:]), [bRt])
                R("dve", lambda e: e.tensor_copy(out=dst2i[:], in_=d2[:]), [bRt])
                cmp2 = sbr("cmp2", [128, NBLK, 32]); bex = sbr("bex", [128, NBLK]); need = sbr("need", [128, NBLK])
                w0 = sbr("w0", [128, NBLK]); w1f = sbr("w1f", [128, NBLK, 2])
                R("dve", lambda e: e.tensor_tensor(out=cmp2[:], in0=iot[:, 0:NBLK].unsqueeze(2).broadcast_to([128, NBLK, 32]),
                                                   in1=pend[:, :].unsqueeze(1).broadcast_to([128, NBLK, 32]), op=ALU.is_ge))
                R("dve", lambda e: e.tensor_reduce(out=bex[:], in_=cmp2[:], axis=AX.X, op=ALU.add))
                R("dve", lambda e: e.tensor_scalar(out=bex[:], in0=bex[:], scalar1=float(N_EXP - 1), scalar2=None, op0=ALU.min))
                R("dve", lambda e: e.memset(need[:], 1.0))
                if WSKIP:
                    R("dve", lambda e: e.tensor_tensor(out=need[:, NSET:NBLK], in0=bex[:, NSET:NBLK], in1=bex[:, 0:NBLK - NSET], op=ALU.not_equal))
                R("dve", lambda e: e.tensor_scalar(out=w0[:], in0=bex[:], scalar1=256.0, scalar2=pid2[:, 0:1], op0=ALU.mult, op1=ALU.add))
                R("dve", lambda e: e.tensor_tensor(out=w0[:], in0=w0[:], in1=need[:], op=ALU.mult))
                R("dve", lambda e: e.tensor_scalar(out=need[:], in0=need[:], scalar1=-float(1 << 30), scalar2=float(1 << 30),
                                                   op0=ALU.mult, op1=ALU.add))
                R("dve", lambda e: e.tensor_tensor(out=w0[:], in0=w0[:], in1=need[:], op=ALU.add))
                R("dve", lambda e: e.tensor_copy(out=w1f[:, :, 0], in_=w0[:]))
                R("dve", lambda e: e.tensor_scalar(out=w1f[:, :, 1], in0=w0[:], scalar1=1.0, scalar2=None, op0=ALU.add))
                R("dve", lambda e: e.tensor_copy(out=widx[:], in_=w1f[:].rearrange("p b h -> p (b h)")), [bRt])
                if debug:
                    dbg = sbr("dbg", [128, 4 * NT + 2 * NBLK])
                    R("dve", lambda e: e.tensor_copy(out=dbg[:, 0:NT], in_=d1[:]))
                    R("dve", lambda e: e.tensor_copy(out=dbg[:, NT:2 * NT], in_=d2[:]))
                    R("dve", lambda e: e.tensor_copy(out=dbg[:, 2 * NT:3 * NT], in_=g1[:]))
                    R("dve", lambda e: e.tensor_copy(out=dbg[:, 3 * NT:4 * NT], in_=g2[:]))
                    R("dve", lambda e: e.tensor_copy(out=dbg[:, 4 * NT:4 * NT + NBLK], in_=bex[:]))
                    R("dve", lambda e: e.tensor_copy(out=dbg[:, 4 * NT + NBLK:4 * NT + 2 * NBLK], in_=w0[:]))
                    sch.dma("sp", dbg_out[:, :], dbg[:], reads=[bR])
                sch.fence_all("sp")
                sch.fence_all("pool")
                hs_r = Ring(sch, e3, nc, "p3hs", [128, D], BF16, 4)
                for n in range(NT):
                    hs, bhs = hs_r.next()
                    sch.dma("sp", hs[:], h2_scr[n * 128:(n + 1) * 128, :], writes=[bhs])
                    for dsti in (dst1i, dst2i):
                        sch.dma("pool", xs_scr[:, :], hs[:], reads=[bhs, bRt],
                                indirect=dict(out_offset=bass.IndirectOffsetOnAxis(dsti[:, n:n + 1], 0), in_offset=None))
                sch.fence_all("sp")
                sch.fence_all("pool")

        if 4 in phases:
            with ExitStack() as e4:
                def sb4(name, shape, dt):
                    return e4.enter_context(nc.sbuf_tensor("p4" + name, list(shape), dt))
                SUB = MOE_B // 128
                Wb = [[sb4(f"W{i}_{s_}", [128, 4096], BF16) for s_ in range(NSET)] for i in range(3)]
                bWb = [[[sch.buf(f"p4W{i}_{s_}_{h}") for h in range(2)] for s_ in range(NSET)] for i in range(3)]
                wsrc = (w1, w3, w2)
                bnd_reg = nc.gpsimd.alloc_register("wbnd")
                nc.gpsimd.reg_mov(bnd_reg, N_EXP * 256 - 1)
                xs_r = Ring(sch, e4, nc, "p4xs", [128, D], BF16, 3)
                xT_r = Ring(sch, e4, nc, "p4xT", [128, 8, 128], BF16, 3)
                sg_r = Ring(sch, e4, nc, "p4sg", [128, 512], BF16, 2)
                am_r = Ring(sch, e4, nc, "p4am", [128, 512], BF16, 2)
                aT_r = Ring(sch, e4, nc, "p4aT", [128, 4, 128], BF16, 2 * SUB + 1)
                ys_r = Ring(sch, e4, nc, "p4ys", [128, D], F32, 3)
                with ExitStack() as e4p:
                    tpx = e4p.enter_context(nc.psum_tensor("p4tpx", [128, 8, 128], BF16))
                    btpx = sch.buf("p4tpx")
                    a1p = Ring(sch, e4p, nc, "p4a1", [128, 512], F32, 2, psum=True)
                    a3p = Ring(sch, e4p, nc, "p4a3", [128, 512], F32, 2, psum=True)
                    ypp = Ring(sch, e4p, nc, "p4yp", [128, 512], F32, 2, psum=True)
                    tpa = e4p.enter_context(nc.psum_tensor("p4tpa", [128, 4, 128], BF16))
                    btpa = sch.buf("p4tpa")

                    def stageX(b):
                        st_ = b % NSET
                        for i in range(3):
                            for hf in range(2):
                                sch.dma("pool", Wb[i][st_][:, hf * 2048:(hf + 1) * 2048], wsrc[i][:, :], reads=[bRt], writes=[bWb[i][st_][hf]],
                                        indirect=dict(out_offset=None, in_offset=bass.IndirectOffsetOnAxis(widx[:, 2 * b + hf:2 * b + hf + 1], 0),
                                                      bounds_check=bnd_reg, oob_is_err=False))
                        res = []
                        for sb_ in range(SUB):
                            r0_ = b * MOE_B + sb_ * 128
                            xs, bxs = xs_r.next()
                            sch.dma("sp", xs[:], xs_scr[r0_:r0_ + 128, :], writes=[bxs])
                            for kc in range(8):
                                sch.op("pe", lambda e, kc=kc, xs=xs: e.transpose(out=tpx[:, kc, :], in_=xs[:, kc * 128:(kc + 1) * 128],
                                                                                identity=identb[:]), reads=[bxs, b_const], writes=[btpx])
                            xT, bxT = xT_r.next()
                            sch.op("dve", lambda e, xT=xT: e.tensor_copy(out=xT[:], in_=tpx[:]), reads=[btpx], writes=[bxT])
                            a1, ba1 = a1p.next()
                            a3, ba3 = a3p.next()
                            for (ap_, bap, Wx, bWx) in ((a1, ba1, Wb[0][st_], bWb[0][st_]), (a3, ba3, Wb[1][st_], bWb[1][st_])):
                                for kc in range(8):
                                    sch.op("pe", lambda e, kc=kc, ap_=ap_, Wx=Wx, xT=xT: e.matmul(
                                        ap_[:], xT[:, kc, :], Wx[:, kc * 512:(kc + 1) * 512],
                                        start=(kc == 0), stop=(kc == 7)), reads=[bWx[kc // 4], bxT], writes=[bap])
                            sg, bsg = sg_r.next()
                            sch.op("act", lambda e, a1=a1, sg=sg: e.activation(out=sg[:], in_=a1[:], func=AF.Silu), reads=[ba1], writes=[bsg])
                            am, bam = am_r.next()
                            sch.op("dve", lambda e, am=am, a3=a3, sg=sg: e.tensor_tensor(out=am[:], in0=a3[:], in1=sg[:], op=ALU.mult),
                                   reads=[ba3, bsg], writes=[bam])
                            for nch in range(4):
                                sch.op("pe", lambda e, nch=nch, am=am: e.transpose(out=tpa[:, nch, :], in_=am[:, nch * 128:(nch + 1) * 128],
                                                                                  identity=identb[:]), reads=[bam, b_const], writes=[btpa])
                            aT, baT = aT_r.next()
                            sch.op("act", lambda e, aT=aT: e.activation(out=aT[:], in_=tpa[:], func=AF.Copy), reads=[btpa], writes=[baT])
                            res.append((aT, baT))
                        return res

                    def stageY(b, res):
                        st_ = b % NSET
                        W2b = Wb[2][st_]
                        for sb_, (aT, baT) in enumerate(res):
                            r0_ = b * MOE_B + sb_ * 128
                            ys, bys = ys_r.next()
                            for hf in range(2):
                                yp, byp = ypp.next()
                                for nch in range(4):
                                    sch.op("pe", lambda e, nch=nch, hf=hf, yp=yp, aT=aT, W2b=W2b: e.matmul(
                                        yp[:], aT[:, nch, :], W2b[:, nch * 1024 + hf * 512:nch * 1024 + (hf + 1) * 512],
                                        start=(nch == 0), stop=(nch == 3)), reads=[baT, bWb[2][st_][nch // 2]], writes=[byp])
                                sch.op("act", lambda e, hf=hf, yp=yp, ys=ys: e.activation(out=ys[:, hf * 512:(hf + 1) * 512], in_=yp[:], func=AF.Copy),
                                       reads=[byp], writes=[bys])
                            sch.dma("act", yb_scr[r0_:r0_ + 128, :], ys[:], reads=[bys])

                    prevx = None
                    for b in range(NBLK + 1):
                        curx = stageX(b) if b < NBLK else None
                        if prevx is not None:
                            stageY(b - 1, prevx)
                        prevx = curx
                sch.fence_all("sp")
                sch.fence_all("pool")
                sch.fence_all("act")
                y1_r = Ring(sch, e4, nc, "p4y1", [128, D], F32, 3)
                y2_r = Ring(sch, e4, nc, "p4y2", [128, D], F32, 3)
                xo_r = Ring(sch, e4, nc, "p4xo", [128, D], F32, 3)
                for n in range(NT):
                    y1, by1 = y1_r.next()
                    y2, by2 = y2_r.next()
                    xo, bxo = xo_r.next()
                    sch.dma("pool", y1[:], yb_scr[:, :], reads=[bRt], writes=[by1],
                            indirect=dict(out_offset=None, in_offset=bass.IndirectOffsetOnAxis(dst1i[:, n:n + 1], 0)))
                    sch.dma("pool", y2[:], yb_scr[:, :], reads=[bRt], writes=[by2],
                            indirect=dict(out_offset=None, in_offset=bass.IndirectOffsetOnAxis(dst2i[:, n:n + 1], 0)))
                    sch.dma("sp", xo[:], out[n * 128:(n + 1) * 128, :], writes=[bxo])
                    sch.op("dve", lambda e, n=n, y1=y1, xo=xo: e.scalar_tensor_tensor(
                        out=xo[:], in0=y1[:], scalar=g1[:, n:n + 1], in1=xo[:], op0=ALU.mult, op1=ALU.add),
                        reads=[by1, bxo, bRt], writes=[bxo])
                    sch.op("dve", lambda e, n=n, y2=y2, xo=xo: e.scalar_tensor_tensor(
                        out=xo[:], in0=y2[:], scalar=g2[:, n:n + 1], in1=xo[:], op0=ALU.mult, op1=ALU.add),
                        reads=[by2, bxo, bRt], writes=[bxo])
                    sch.dma("act", out[n * 128:(n + 1) * 128, :], xo[:], reads=[bxo])
                sch.fence_all("sp")
                sch.fence_all("pool")
                sch.fence_all("act")

        for en in ("sp", "pool", "act", "dve", "pe"):
            sch.fence_all(en)
        if _os.environ.get("DRYPRINT"):
            print("counts", {k: v.count for k, v in sch.engs.items()}, "nsem", sch.nsem)
    return nc


_CACHE = {}


def kernel(x, mem, g_mix, w_in, qk_gain, na_rpb, t5_table, g_mem, w_mem_kv, w_out, g_ffn, w_r1, b_r1, w_r2, b_r2,
           w1, w3, w2, _debug=False, _phases=(1, 2, 3, 4), _cores=8):
    f32 = np.float32
    x = np.asarray(x, f32); mem = np.asarray(mem, f32)
    na_steps, na_keys = na_plan()
    dil_tl = dil_plan()
    nab = na_bias_tiles(np.asarray(na_rpb, f32)[0], na_keys)
    dlb = dil_bias_tiles(np.asarray(t5_table, f32), dil_tl)
    key = (len(na_keys), len(dil_tl), _debug, tuple(_phases))
    if key not in _CACHE:
        _CACHE[key] = build_program(len(na_keys), len(dil_tl), na_steps, dil_tl, debug=_debug, phases=_phases)
    nc = _CACHE[key]

    def pk(v):
        return np.asarray(v, f32).reshape(8, 128).T
    gvec = np.ascontiguousarray(np.concatenate([pk(g_mix[0]), pk(g_mem[0]), pk(g_ffn[0])], axis=1))
    qg = np.asarray(qk_gain, f32)[0]
    gains = np.ascontiguousarray(np.tile(qg.reshape(6, 64), (1, 2)).T)
    shared = {
        "w_in": np.ascontiguousarray(np.asarray(w_in, f32)[0]),
        "w_mem": np.ascontiguousarray(np.asarray(w_mem_kv, f32)[0]),
        "w_out": np.ascontiguousarray(np.asarray(w_out, f32)[0]),
        "gvec": gvec, "gains": gains,
        "gffn_b": np.ascontiguousarray(np.broadcast_to(np.asarray(g_ffn, f32)[0][None, :], (128, D))),
        "ident": np.eye(128, dtype=f32),
        "na_bias": nab, "dil_bias": dlb,
        "w_r": np.ascontiguousarray(np.concatenate([np.asarray(w_r1, f32)[0], np.asarray(w_r2, f32)[0]], axis=1)),
        "b_r": np.ascontiguousarray(np.broadcast_to(
            np.concatenate([np.asarray(b_r1, f32)[0], np.asarray(b_r2, f32)[0]])[None, :], (128, 36))),
        "w1": np.ascontiguousarray(np.asarray(w1, f32)[0].reshape(N_EXP, 8, 128, 512).transpose(0, 2, 1, 3)).reshape(N_EXP * 256, 2048),
        "w3": np.ascontiguousarray(np.asarray(w3, f32)[0].reshape(N_EXP, 8, 128, 512).transpose(0, 2, 1, 3)).reshape(N_EXP * 256, 2048),
        "w2": np.ascontiguousarray(np.asarray(w2, f32)[0].reshape(N_EXP, 4, 128, 1024).transpose(0, 2, 1, 3)).reshape(N_EXP * 256, 2048),
        "iota": np.ascontiguousarray(np.broadcast_to(np.arange(256, dtype=f32)[None, :], (128, 256))),
        "pidx": np.arange(128, dtype=f32).reshape(128, 1),
        "tri": np.triu(np.ones((128, 128), f32), 1),
    }
    in_maps = []
    for c in range(_cores):
        m = dict(shared)
        m["x"] = np.ascontiguousarray(x[c])
        m["mem"] = np.ascontiguousarray(mem[c])
        in_maps.append(m)
    res = run_bass_kernel_spmd(nc, in_maps, core_ids=list(range(_cores)))
    if _debug:
        return res.results
    return np.stack([r["out"] for r in res.results], axis=0)
```

```python
import math
import os as _os
from contextlib import ExitStack
import numpy as np
import concourse.bass as bass
import concourse.mybir as mybir
from concourse.bass_utils import run_bass_kernel_spmd

F32 = mybir.dt.float32
BF16 = mybir.dt.bfloat16
I32 = mybir.dt.int32
AF = mybir.ActivationFunctionType
ALU = mybir.AluOpType
AX = mybir.AxisListType

S = 8192
D = 1024
NT = S // 128
EPS = 1e-6
NEG = -30000.0
N_EXP = 32
MOE_B = 256
NSET = 3
CAP = 2 * S + N_EXP * MOE_B
NBLK = CAP // MOE_B
SAME_ENGINE_SYNC = True
WSKIP = True
LNEXP = bool(int(_os.environ.get("LNEXP", "1")))


class Eng:
    def __init__(self, name, eng, sem):
        self.name, self.eng, self.sem = name, eng, sem
        self.count = 0
        self.waited = {}


class Buf:
    def __init__(self, name):
        self.name = name
        self.w = {}
        self.r = {}
        self.dw = None
        self.dr = None


class Sched:
    def __init__(self, nc, es):
        self.nc, self.es = nc, es
        self.engs = {}
        for nm, e in (("pe", nc.tensor), ("act", nc.scalar), ("dve", nc.vector), ("pool", nc.gpsimd), ("sp", nc.sync)):
            self.engs[nm] = Eng(nm, e, es.enter_context(nc.semaphore("sem_" + nm)))
        self.nsem = 5
        self.all_bufs = []
        self.free_sems = []

    def buf(self, name):
        b = Buf(name)
        self.all_bufs.append(b)
        return b

    def newsem(self, name):
        self.nsem += 1
        return self.es.enter_context(self.nc.semaphore(name))

    def getsem(self, name):
        if self.free_sems:
            return self.free_sems.pop()
        return [self.newsem(name), 0]

    def release_since(self, mark):
        for b in self.all_bufs[mark:]:
            seen = set()
            for attr in ("dw", "dr"):
                v = getattr(b, attr)
                if v is not None and id(v) not in seen:
                    seen.add(id(v))
                    self.free_sems.append(v)
                setattr(b, attr, None)
        del self.all_bufs[mark:]

    def _wait(self, E, key, sem, val):
        if val <= 0 or E.waited.get(key, 0) >= val:
            return
        E.eng.wait_ge(sem, val)
        E.waited[key] = val

    def _deps(self, E, reads, writes, skip_dw=False):
        for b in reads:
            for f, n in b.w.items():
                self._dep_eng(E, f, n)
            if b.dw is not None:
                self._wait(E, id(b.dw[0]), b.dw[0], b.dw[1])
        for b in writes:
            for f, n in b.w.items():
                self._dep_eng(E, f, n)
            for f, n in b.r.items():
                self._dep_eng(E, f, n)
            if b.dw is not None and not skip_dw:
                self._wait(E, id(b.dw[0]), b.dw[0], b.dw[1])
            if b.dr is not None:
                self._wait(E, id(b.dr[0]), b.dr[0], b.dr[1])

    def _dep_eng(self, E, f, n):
        if f == E.name and (f == "pe" or not SAME_ENGINE_SYNC):
            return
        F = self.engs[f]
        self._wait(E, f, F.sem, n)

    def op(self, ename, ins_fn, reads=(), writes=()):
        E = self.engs[ename]
        self._deps(E, reads, writes)
        ins = ins_fn(E.eng)
        E.count += 1
        ins.then_inc(E.sem, 1)
        for b in reads:
            b.r[ename] = E.count
        for b in writes:
            b.w[ename] = E.count
        return ins

    def dma(self, ename, out, in_, reads=(), writes=(), extra_wait=(), indirect=None, disjoint=False, **kw):
        E = self.engs[ename]
        self._deps(E, reads, writes, skip_dw=disjoint)
        for b in extra_wait:
            self._deps(E, [b], [])
        if indirect is None:
            ins = E.eng.dma_start(out=out, in_=in_, **kw)
        else:
            ins = E.eng.indirect_dma_start(out=out, in_=in_, **indirect, **kw)
        if writes:
            b = writes[0]
            if b.dw is None:
                b.dw = self.getsem("ld_" + b.name)
            b.dw[1] += 16
            ins.then_inc(b.dw[0], 16)
            for b2 in writes[1:]:
                b2.dw = b.dw
        elif reads:
            b = reads[0]
            if b.dr is None:
                b.dr = self.getsem("st_" + b.name)
            b.dr[1] += 16
            ins.then_inc(b.dr[0], 16)
        return ins

    def fence_stores(self, ename):
        E = self.engs[ename]
        for b in self.all_bufs:
            if b.dr is not None:
                self._wait(E, id(b.dr[0]), b.dr[0], b.dr[1])

    def fence_all(self, ename):
        E = self.engs[ename]
        for f, F in self.engs.items():
            if f != ename and F.count > 0:
                self._wait(E, f, F.sem, F.count)
        self.fence_stores(ename)


class Ring:
    def __init__(self, sch, es, nc, name, shape, dtype, n, psum=False):
        self.tiles, self.bufs, self.i = [], [], 0
        for k in range(n):
            nm = f"{name}{k}"
            t = es.enter_context(nc.psum_tensor(nm, shape, dtype) if psum else nc.sbuf_tensor(nm, shape, dtype))
            self.tiles.append(t)
            self.bufs.append(sch.buf(nm))

    def next(self):
        k = self.i % len(self.tiles)
        self.i += 1
        return self.tiles[k], self.bufs[k]


def t5_bucket_np(rel):
    nb = 16
    max_exact = 8
    n = np.abs(rel)
    upper = (rel > 0).astype(np.int32) * nb
    nf = np.maximum(n, 1).astype(np.float32)
    large = max_exact + (np.log(nf / max_exact) / math.log(1024 / max_exact) * (nb - max_exact)).astype(np.int32)
    large = np.minimum(large, nb - 1)
    return upper + np.where(n < max_exact, n, large)


def na_plan():
    def r0(r):
        return min(max(r - 4, 0), 120)
    tiles = {}
    steps = []
    for n in range(64):
        rows = (2 * n, 2 * n + 1)
        lo = min(r0(r) for r in rows)
        hi = max(r0(r) + 7 for r in rows)
        st = []
        for m in range(lo // 2, hi // 2 + 1):
            key = []
            for kl in range(2):
                kr = 2 * m + kl
                for ql in range(2):
                    r = rows[ql]
                    ok = r0(r) <= kr <= r0(r) + 7
                    key.append(kr - r + 7 if ok else -1)
            key = tuple(key)
            if all(k < 0 for k in key):
                continue
            if key not in tiles:
                tiles[key] = len(tiles)
            st.append((m, tiles[key]))
        steps.append(st)
    return steps, list(tiles.keys())


def na_bias_tiles(rpb, tile_keys):
    cols = np.arange(64)
    c0 = np.clip(cols - 8, 0, 48)
    kc = cols[:, None]
    qc = cols[None, :]
    colok = (kc >= c0[None, :]) & (kc < c0[None, :] + 16)
    dc = np.clip(kc - qc + 15, 0, 30)
    out = np.full((len(tile_keys), 128, 8, 128), NEG, np.float32)
    for t, key in enumerate(tile_keys):
        i = 0
        for kl in range(2):
            for ql in range(2):
                dr = key[i]
                i += 1
                if dr < 0:
                    continue
                blk = np.where(colok[None], rpb[:, dr][:, dc], NEG)
                out[t, kl * 64:(kl + 1) * 64, :, ql * 64:(ql + 1) * 64] = blk.transpose(1, 0, 2)
    return out


DIL = ((128, 1), (512, 4), (2048, 16))


def dil_plan():
    tl = []
    for g, (win, d) in enumerate(DIL):
        half = win // 2
        lo = -((half + 127) // 128)
        hi = (half + 127) // 128
        for o in range(lo, hi + 1):
            tl.append((g, o))
    return tl


def dil_bias_tiles(t5_table, tl):
    t5 = t5_table.reshape(32, 3, 4)
    out = np.full((len(tl), 128, 4, 128), NEG, np.float32)
    kk = np.arange(128)[:, None]
    qq = np.arange(128)[None, :]
    for t, (g, o) in enumerate(tl):
        win, d = DIL[g]
        rel = o * 128 + kk - qq
        ok = (np.abs(rel) <= win // 2) & (rel % d == 0)
        bk = t5_bucket_np(rel)
        for h in range(4):
            out[t, :, h, :] = np.where(ok, t5[bk, g, h], NEG)
    return out


def dil_res_bias_tiles(t5_table):
    t5 = t5_table.reshape(32, 3, 4)
    out = np.full((9, 128, 4, 128), NEG, np.float32)
    kk = np.arange(128)[:, None]
    qq = np.arange(128)[None, :]
    for g, (win, d) in enumerate(DIL):
        ns = win // 2 // d
        for o in (-1, 0, 1):
            du = o * 128 + kk - qq
            ok = np.abs(du) <= ns
            bk = t5_bucket_np(du * d)
            for h in range(4):
                out[g * 3 + o + 1, :, h, :] = np.where(ok, t5[bk, g, h], NEG)
    return out


QK_TILES = []
for i in range(4):
    QK_TILES.append((0 + 128 * i, 0))
for i in range(4):
    QK_TILES.append((512 + 128 * i, 1))
for i in range(6):
    QK_TILES.append((1536 + 128 * i, 2))
for i in range(6):
    QK_TILES.append((2304 + 128 * i, 3))
for i in range(2):
    QK_TILES.append((3840 + 128 * i, 4))
T_NAQ, T_NAK, T_DQ, T_DK, T_MQ = 0, 4, 8, 14, 20
NQK = len(QK_TILES)
V_SEGS = ((1024, 512, 0), (3072, 512, 512), (3584, 256, 1024))


def build_program(n_na_tiles, n_dil_tiles, na_steps, dil_tl, debug=False, phases=(1, 2, 3, 4)):
    nc = bass.Bass("TRN2", target_bir_lowering=False)
    dk = "ExternalOutput" if debug else "Internal"

    def din(name, shape, dt=F32):
        return nc.dram_tensor(name, list(shape), dt, kind="ExternalInput").ap()

    x = din("x", [S, D])
    mem = din("mem", [256, D])
    w_in = din("w_in", [D, 4096])
    w_mem = din("w_mem", [D, 512])
    w_out = din("w_out", [D, D])
    gvec = din("gvec", [128, 24])
    gains = din("gains", [128, 6])
    gffn_b = din("gffn_b", [128, D])
    ident_in = din("ident", [128, 128])
    na_bias = din("na_bias", [n_na_tiles, 128, 8, 128])
    dil_bias = din("dil_bias", [9, 128, 4, 128])
    w_r = din("w_r", [D, 36])
    b_r = din("b_r", [128, 36])
    w1 = din("w1", [N_EXP * 256, 2048])
    w3 = din("w3", [N_EXP * 256, 2048])
    w2 = din("w2", [N_EXP * 256, 2048])
    iota_in = din("iota", [128, 256])
    pidx_in = din("pidx", [128, 1])
    tri_in = din("tri", [128, 128])
    out = nc.dram_tensor("out", [S, D], F32, kind="ExternalOutput").ap()

    qk_scr = nc.dram_tensor("qk_scr", [NQK, 128, S], BF16, kind=dk).ap()
    v_scr = nc.dram_tensor("v_scr", [S, 1280], BF16, kind=dk).ap()
    mix_scr = nc.dram_tensor("mix_scr", [S, D], BF16, kind=dk).ap()
    km_scr = nc.dram_tensor("km_scr", [2, 128, 256], BF16, kind=dk).ap()
    vm_scr = nc.dram_tensor("vm_scr", [256, 256], BF16, kind=dk).ap()
    h2_scr = nc.dram_tensor("h2_scr", [S, D], BF16, kind=dk).ap()
    dn_scr = nc.dram_tensor("dn_scr", [3, S, 260], F32, kind=dk).ap()
    xs_scr = nc.dram_tensor("xs_scr", [CAP, D], BF16, kind=dk).ap()
    yb_scr = nc.dram_tensor("yb_scr", [CAP, D], F32, kind=dk).ap()
    dbg_out = nc.dram_tensor("dbg_out", [128, 4 * NT + 2 * NBLK], F32, kind=dk).ap()

    with ExitStack() as es:
        sch = Sched(nc, es)

        def sb(name, shape, dt):
            return es.enter_context(nc.sbuf_tensor(name, list(shape), dt))

        def ps(name, shape, dt=F32):
            return es.enter_context(nc.psum_tensor(name, list(shape), dt))

        identf = sb("identf", [128, 128], F32)
        identb = sb("identb", [128, 128], BF16)
        blk1 = sb("blk1", [128, 128], BF16)
        gv = sb("gv", [128, 24], F32)
        gn = sb("gn", [128, 6], F32)
        epsb = sb("epsb", [128, 1], F32)
        b_const = sch.buf("const")
        sch.dma("sp", identf[:], ident_in[:, :], writes=[b_const])
        sch.dma("sp", gv[:], gvec[:, :], writes=[b_const])
        sch.dma("sp", gn[:], gains[:, :], writes=[b_const])
        sch.op("dve", lambda e: e.tensor_copy(out=identb[:], in_=identf[:]), reads=[b_const], writes=[b_const])
        sch.op("dve", lambda e: e.memset(blk1[:], 0.0), writes=[b_const])
        sch.op("dve", lambda e: e.memset(epsb[:], EPS), writes=[b_const])
        sch.op("dve", lambda e: e.memset(blk1[0:64, 0:64], 1.0), writes=[b_const])
        sch.op("dve", lambda e: e.memset(blk1[64:128, 64:128], 1.0), writes=[b_const])
        gq = gn[:].rearrange("p (a b) -> p a b", b=2)[:, :, 0:1]
        sch.op("dve", lambda e: e.tensor_scalar(out=gq, in0=gq, scalar1=0.125, scalar2=None, op0=ALU.mult),
               reads=[b_const], writes=[b_const])

        def projection(es1, src, n_tok, wsrc, ncols, gcol0, qk_tiles, qk_dst, v_segs, v_dst, tag):
            def sb1(name, shape, dt):
                return es1.enter_context(nc.sbuf_tensor(tag + name, list(shape), dt))
            W = sb1("W", [128, 8, ncols], BF16)
            bW = sch.buf(tag + "W")
            wst = Ring(sch, es1, nc, tag + "wst", [128, 2048], F32, 2)
            nhalf = (ncols + 2047) // 2048
            for kc in range(8):
                for hf in range(nhalf):
                    c0 = hf * 2048
                    cw = min(2048, ncols - c0)
                    t, b = wst.next()
                    sch.dma("sp", t[:, 0:cw], wsrc[kc * 128:(kc + 1) * 128, c0:c0 + cw], writes=[b])
                    if (kc * nhalf + hf) % 2 == 0:
                        sch.op("dve", lambda e, t=t, kc=kc, c0=c0, cw=cw: e.tensor_scalar(
                            out=W[:, kc, c0:c0 + cw], in0=t[:, 0:cw], scalar1=gv[:, gcol0 + kc:gcol0 + kc + 1],
                            scalar2=None, op0=ALU.mult), reads=[b, b_const], writes=[bW])
                    else:
                        sch.op("act", lambda e, t=t, kc=kc, c0=c0, cw=cw: e.activation(
                            out=W[:, kc, c0:c0 + cw], in_=t[:, 0:cw], func=AF.Copy, scale=gv[:, gcol0 + kc:gcol0 + kc + 1]),
                            reads=[b, b_const], writes=[bW])
            CH = min(512, n_tok)
            TT = CH // 128
            xt_r = Ring(sch, es1, nc, tag + "xt", [128, TT, D], F32, 2)
            hn_r = Ring(sch, es1, nc, tag + "hn", [128, TT, D], BF16, 2)
            hT_r = Ring(sch, es1, nc, tag + "hT", [128, 8, CH], BF16, 2)
            st_r = Ring(sch, es1, nc, tag + "st", [128, 8], F32, 2)
            junk = sb1("junk", [128, D], BF16)
            bjunk = sch.buf(tag + "junk")
            sq_r = Ring(sch, es1, nc, tag + "sq", [128, CH], BF16, 2)
            sd_r = Ring(sch, es1, nc, tag + "sd", [128, CH], F32, 2)
            rs_r = Ring(sch, es1, nc, tag + "rs", [128, CH], F32, 2)
            qn_r = Ring(sch, es1, nc, tag + "qn", [128, CH], BF16, 3)
            vo_r = Ring(sch, es1, nc, tag + "vo", [128, 1280], BF16, 2)
            tpb = [es1.enter_context(nc.psum_tensor(f"{tag}tp{i}", [128, 2, 512], BF16)) for i in range(1)]
            tpbuf = [sch.buf(tag + "tp0")]
            pbank = [None] + [es1.enter_context(nc.psum_tensor(f"{tag}pb{i}", [128, 512], F32)) for i in range(1, 8)]
            pbuf = [None] + [sch.buf(f"{tag}pb{i}") for i in range(1, 8)]
            def front1(ck):
                t0 = ck * CH
                xt, bxt = xt_r.next()
                sch.dma("sp", xt[:], src[t0:t0 + CH, :].rearrange("(t p) d -> p t d", p=128), writes=[bxt])
                stt, bst = st_r.next()
                for t in range(TT):
                    sch.op("act", lambda e, t=t: e.activation(out=junk[:], in_=xt[:, t, :], func=AF.Square,
                                                               accum_out=stt[:, t:t + 1]),
                           reads=[bxt], writes=[bjunk, bst])
                sch.op("act", lambda e: e.activation(out=stt[:, 4:4 + TT], in_=stt[:, 0:TT], func=AF.Sqrt,
                                                      bias=EPS, scale=1.0 / D), reads=[bst], writes=[bst])
                sch.op("dve", lambda e: e.reciprocal(out=stt[:, 4:4 + TT], in_=stt[:, 4:4 + TT]),
                       reads=[bst], writes=[bst])
                hn, bhn = hn_r.next()
                for t in range(TT):
                    sch.op("act", lambda e, t=t: e.activation(out=hn[:, t, :], in_=xt[:, t, :], func=AF.Copy,
                                                               scale=stt[:, 4 + t:5 + t]),
                           reads=[bxt, bst], writes=[bhn])
                return hn, bhn

            def front2(hn, bhn):
                hT_, bhT_ = hT_r.next()
                for kc in range(8):
                    bank = 0
                    sl = kc % 2
                    for t in range(TT):
                        sch.op("pe", lambda e, t=t, kc=kc, bank=bank, sl=sl: e.transpose(
                            out=tpb[bank][:, sl, t * 128:(t + 1) * 128], in_=hn[:, t, kc * 128:(kc + 1) * 128],
                            identity=identb[:]), reads=[bhn, b_const], writes=[tpbuf[bank]])
                    if sl == 1:
                        sch.op("dve", lambda e, kc=kc, bank=bank: e.tensor_copy(
                            out=hT_[:, kc - 1:kc + 1, :], in_=tpb[bank][:, :, 0:CH]),
                            reads=[tpbuf[bank]], writes=[bhT_])
                return hT_, bhT_

            NCK = n_tok // CH
            cur_hT = front2(*front1(0))
            for ck in range(NCK):
                t0 = ck * CH
                hT, bhT = cur_hT
                nxt1 = front1(ck + 1) if ck + 1 < NCK else None
                nxt_hT = None
                def qk_mm(j):
                    c0, gc = qk_tiles[j]
                    qb = 1 + (j % 3)
                    for kc in range(8):
                        sch.op("pe", lambda e, kc=kc, c0=c0, qb=qb: e.matmul(
                            pbank[qb][:, 0:CH], W[:, kc, c0:c0 + 128], hT[:, kc, :], start=(kc == 0), stop=(kc == 7)),
                            reads=[bW, bhT], writes=[pbuf[qb]])
                    sq, bsq = sq_r.next()
                    sch.op("act", lambda e, qb=qb, sq=sq: e.activation(out=sq[:], in_=pbank[qb][:, 0:CH], func=AF.Square),
                           reads=[pbuf[qb]], writes=[bsq])
                    return sq, bsq

                def qk_epi(j, sq, bsq):
                    c0, gc = qk_tiles[j]
                    qb = 1 + (j % 3)
                    sbk = 4 + (j % 2)
                    sch.op("pe", lambda e, sbk=sbk, sq=sq: e.matmul(pbank[sbk][:, 0:CH], blk1[:], sq[:], start=True, stop=True),
                           reads=[bsq, b_const], writes=[pbuf[sbk]])
                    sd, bsd = sd_r.next()
                    rs, brs = rs_r.next()
                    if LNEXP:
                        sch.op("act", lambda e, sbk=sbk, sd=sd: e.activation(out=sd[:], in_=pbank[sbk][:, 0:CH], func=AF.Ln,
                                                                             bias=epsb[:, 0:1], scale=1.0 / 64),
                               reads=[pbuf[sbk], b_const], writes=[bsd])
                        sch.op("act", lambda e, sd=sd, rs=rs: e.activation(out=rs[:], in_=sd[:], func=AF.Exp, scale=-0.5),
                               reads=[bsd], writes=[brs])
                    else:
                        sch.op("act", lambda e, sbk=sbk, sd=sd: e.activation(out=sd[:], in_=pbank[sbk][:, 0:CH], func=AF.Sqrt,
                                                                             bias=EPS, scale=1.0 / 64),
                               reads=[pbuf[sbk]], writes=[bsd])
                        sch.op("dve", lambda e, sd=sd, rs=rs: e.reciprocal(out=rs[:], in_=sd[:]), reads=[bsd], writes=[brs])
                    qn, bqn = qn_r.next()
                    sch.op("dve", lambda e, qb=qb, rs=rs, qn=qn, gc=gc: e.scalar_tensor_tensor(
                        out=qn[:], in0=pbank[qb][:, 0:CH], scalar=gn[:, gc:gc + 1], in1=rs[:], op0=ALU.mult, op1=ALU.mult),
                        reads=[pbuf[qb], brs, b_const], writes=[bqn])
                    sch.dma("pool", qk_dst(j, t0, CH), qn[:], reads=[bqn])

                prev = None
                for j in range(len(qk_tiles) + 1):
                    cur_ = qk_mm(j) if j < len(qk_tiles) else None
                    if prev is not None:
                        qk_epi(j - 1, *prev)
                    prev = cur_
                    if j == len(qk_tiles) // 2 and nxt1 is not None and nxt_hT is None:
                        nxt_hT = front2(*nxt1)
                if nxt1 is not None and nxt_hT is None:
                    nxt_hT = front2(*nxt1)
                for t in range(TT):
                    vo, bvo = vo_r.next()
                    for si, (c0, cw, d0) in enumerate(v_segs):
                        vb = 6 + ((t * len(v_segs) + si) % 2)
                        for kc in range(8):
                            sch.op("pe", lambda e, kc=kc, c0=c0, cw=cw, vb=vb, t=t: e.matmul(
                                pbank[vb][:, 0:cw], hT[:, kc, t * 128:(t + 1) * 128], W[:, kc, c0:c0 + cw],
                                start=(kc == 0), stop=(kc == 7)), reads=[bW, bhT], writes=[pbuf[vb]])
                        sch.op("act", lambda e, vb=vb, cw=cw, d0=d0, vo=vo: e.activation(
                            out=vo[:, d0:d0 + cw], in_=pbank[vb][:, 0:cw], func=AF.Copy),
                            reads=[pbuf[vb]], writes=[bvo])
                    vw = sum(s_[1] for s_ in v_segs)
                    sch.dma("pool", v_dst(t0 + t * 128, vw), vo[:, 0:vw], reads=[bvo])
                cur_hT = nxt_hT

        if 1 in phases:
            mark1 = len(sch.all_bufs)
            with ExitStack() as es1:
                projection(es1, x, S, w_in, 4096, 0, QK_TILES,
                           lambda j, t0, n: qk_scr[j, :, t0:t0 + n], V_SEGS,
                           lambda t0, vw: v_scr[t0:t0 + 128, 0:vw], "p1")
                sch.fence_all("sp")
                sch.fence_all("pool")
            with ExitStack() as es1:
                projection(es1, mem, 256, w_mem, 512, 8, [(0, 5), (128, 5)],
                           lambda j, t0, n: km_scr[j, :, t0:t0 + n], ((256, 256, 0),),
                           lambda t0, vw: vm_scr[t0:t0 + 128, 0:vw], "pm")
                sch.fence_all("sp")
                sch.fence_all("pool")


        def attn_pass(tag, qsrc, ksrc, sk, vsrc, vruns, slots, steps_fn, OH, bias, mix_col0, perm=None, raw_dst=None):
            mark_ = len(sch.all_bufs)
            with ExitStack() as e2:
                def sb2(name, shape, dt):
                    return e2.enter_context(nc.sbuf_tensor(tag + name, list(shape), dt))
                nkb = sk // 128
                QT = [sb2(f"QT{i}", [128, S], BF16) for i in range(len(qsrc))]
                KT = [sb2(f"KT{i}", [128, sk], BF16) for i in range(len(ksrc))]
                nv = sum(c for _, c in vruns)
                V1 = sb2("V1", [128, nkb, nv, 65], BF16)
                bin_ = sch.buf(tag + "in")
                SKIP = _os.environ.get("P2SKIP", "")
                tmp_r = None
                if perm is not None and any(d_ > 1 for d_ in perm):
                    tmp_r = Ring(sch, e2, nc, tag + "tmpN", [128, S], BF16, 1)
                pk = 0
                for (dstT, srcs) in ((QT, qsrc), (KT, ksrc)):
                    for i, a in enumerate(srcs):
                        d_ = 1 if perm is None else perm[i]
                        if d_ == 1:
                            for hf in range(2):
                                w_ = a.shape[1] // 2
                                sch.dma("sp", dstT[i][:, hf * w_:(hf + 1) * w_], a[:, hf * w_:(hf + 1) * w_], writes=[bin_], disjoint=True)
                        else:
                            tn, btn = tmp_r.next()
                            sch.dma("sp", tn[:], a, writes=[btn])
                            eng = ("act", "dve", "pool")[pk % 3]
                            pk += 1
                            o_ap = dstT[i][:, :].rearrange("p (r u) -> p r u", r=d_)
                            i_ap = tn[:, :].rearrange("p (u r) -> p r u", r=d_)
                            if eng == "act":
                                sch.op("act", lambda e, o_ap=o_ap, i_ap=i_ap: e.activation(out=o_ap, in_=i_ap, func=AF.Copy),
                                       reads=[btn], writes=[bin_])
                            else:
                                sch.op(eng, lambda e, o_ap=o_ap, i_ap=i_ap: e.tensor_copy(out=o_ap, in_=i_ap),
                                       reads=[btn], writes=[bin_])
                s0 = 0
                for ri, (vc0, cnt) in enumerate(vruns):
                    d_ = 1 if perm is None else perm[ri]
                    for c in range(cnt):
                        vcol = vsrc[:, vc0 + c * 64:vc0 + (c + 1) * 64]
                        if d_ == 1:
                            vv = vcol.rearrange("(b p) d -> p b d", p=128)
                            for b0 in range(0, nkb, 16):
                                b1 = min(nkb, b0 + 16)
                                sch.dma("sp", V1[:, b0:b1, s0 + c, 0:64], vv[:, b0:b1, :], writes=[bin_], disjoint=True)
                        else:
                            SB_ = nkb // d_
                            vv = vcol.rearrange("(ub p r) c -> r p ub c", p=128, r=d_)
                            for rho in range(d_):
                                sch.dma("sp", V1[:, rho * SB_:(rho + 1) * SB_, s0 + c, 0:64], vv[rho], writes=[bin_], disjoint=True)
                    s0 += cnt
                if "m" not in SKIP:
                    sch.op("pool", lambda e: e.memset(V1[:, :, :, 64:65], 1.0), writes=[bin_])
                EB = None
                if bias is not None:
                    bd, h0, Hs = bias
                    ntile = bd.shape[0]
                    Hh = Hs // 2
                    EB = sb2("EB", [128, 2, ntile, Hh, 128], BF16)
                    ebs = Ring(sch, e2, nc, tag + "ebs", [128, Hs, 128], F32, 1)
                    for t in range(ntile):
                        st, bst = ebs.next()
                        sch.dma("sp", st[:], bd[t, :, h0:h0 + Hs, :], writes=[bst])
                        sch.op("act", lambda e, st=st, t=t: e.activation(
                            out=EB[:, :, t, :, :], in_=st[:].rearrange("p (i f) q -> p f i q", f=2), func=AF.Exp),
                               reads=[bst], writes=[bin_])
                sps = Ring(sch, e2, nc, tag + "sps", [128, 512], F32, 4, psum=True)
                ops_ = Ring(sch, e2, nc, tag + "ops", [128, 512], F32, 2, psum=True)
                pr = Ring(sch, e2, nc, tag + "P", [128, 512], BF16, 5)
                rd_r = Ring(sch, e2, nc, tag + "rd", [128, 8], F32, 2)
                mx_r = Ring(sch, e2, nc, tag + "mx", [128, 4, OH * 64], BF16, 2) if raw_dst is None else None
                ro_r = Ring(sch, e2, nc, tag + "ro", [128, 512], F32, 2) if raw_dst is not None else None
                mx_state = [None, None]
                recs = []
                for n in range(NT):
                    pend = ([], [])
                    groups = []
                    for (m, tid, sl) in steps_fn(n):
                        for si in sl:
                            hf = slots[si][2]
                            pend[hf].append((m, tid, slots[si]))
                            if len(pend[hf]) == 4:
                                groups.append(list(pend[hf]))
                                pend[hf].clear()
                    for hf in range(2):
                        if pend[hf]:
                            groups.append(list(pend[hf]))
                    for gi, grp in enumerate(groups):
                        recs.append(dict(n=n, grp=grp, first=(gi == 0), last=(gi == len(groups) - 1)))

                def emitA(rcs):
                    for rc in rcs:
                        rc["sp"], rc["bsp"] = sps.next()
                    for i in range(4):
                        for rc in rcs:
                            grp = rc["grp"]
                            if i >= len(grp):
                                continue
                            n = rc["n"]
                            (m, tid, (qt, kt, half, vs, oh, bh)) = grp[i]
                            pl = slice(half * 64, half * 64 + 64)
                            sp_ = rc["sp"]
                            sch.op("pe", lambda e, i=i, m=m, qt=qt, kt=kt, pl=pl, sp_=sp_, n=n: e.matmul(
                                sp_[:, i * 128:(i + 1) * 128], KT[kt][pl, m * 128:(m + 1) * 128],
                                QT[qt][pl, n * 128:(n + 1) * 128], start=True, stop=True),
                                reads=[bin_], writes=[rc["bsp"]])
                    for rc in rcs:
                        grp, sp_, bsp = rc["grp"], rc["sp"], rc["bsp"]
                        w = len(grp) * 128
                        P, bP = pr.next()
                        rc["P"], rc["bP"] = P, bP
                        sch.op("act", lambda e, sp_=sp_, P=P, w=w: e.activation(out=P[:, 0:w], in_=sp_[:, 0:w], func=AF.Exp),
                               reads=[bsp], writes=[bP])
                        if EB is not None:
                            def eoff(job):
                                return (job[2][2] * ntile + job[1]) * Hh + job[2][5] // 2
                            i = 0
                            while i < len(grp):
                                off = eoff(grp[i])
                                j = i + 1
                                while j < len(grp) and eoff(grp[j]) == off + (j - i):
                                    j += 1
                                ebv = EB[:].rearrange("p f t h q -> p (f t h) q")[:, off:off + (j - i), :]
                                pv = P[:, i * 128:j * 128].rearrange("p (a q) -> p a q", q=128)
                                sch.op("dve", lambda e, pv=pv, ebv=ebv: e.tensor_tensor(out=pv, in0=pv, in1=ebv, op=ALU.mult),
                                       reads=[bP, bin_], writes=[bP])
                                i = j

                cur_o = [None, None]

                def emitB(rc):
                    n, grp, P, bP = rc["n"], rc["grp"], rc["P"], rc["bP"]
                    if rc["first"]:
                        cur_o[0], cur_o[1] = ops_.next()
                    oacc, boacc = cur_o
                    for i, (m, tid, (qt, kt, half, vs, oh, bh)) in enumerate(grp):
                        fst = rc["first"] and i == 0
                        lst = rc["last"] and i == len(grp) - 1
                        sch.op("pe", lambda e, i=i, m=m, vs=vs, oh=oh, P=P, oacc=oacc, fst=fst, lst=lst: e.matmul(
                            oacc[:, oh * 65:(oh + 1) * 65], P[:, i * 128:(i + 1) * 128], V1[:, m, vs, :],
                            start=fst, stop=lst, skip_group_check=True),
                            reads=[bP, bin_], writes=[boacc])
                    if not rc["last"]:
                        return
                    if raw_dst is not None:
                        ro, bro = ro_r.next()
                        sch.op("dve", lambda e, ro=ro, oacc=oacc: e.tensor_copy(out=ro[:, 0:OH * 65], in_=oacc[:, 0:OH * 65]),
                               reads=[boacc], writes=[bro])
                        for g_ in range(3):
                            sch.dma("pool", raw_dst(g_, n), ro[:, g_ * 130:(g_ + 1) * 130], reads=[bro])
                        return
                    rd, brd = rd_r.next()
                    ov = oacc[:, 0:OH * 65].rearrange("p (h c) -> p h c", c=65)
                    sch.op("dve", lambda e, rd=rd, ov=ov: e.reciprocal(out=rd[:, 0:OH], in_=ov[:, :, 64]),
                           reads=[boacc], writes=[brd])
                    if n % 4 == 0:
                        mx_state[0], mx_state[1] = mx_r.next()
                    mx, bmx = mx_state
                    for h in range(OH):
                        sch.op("dve", lambda e, h=h, rd=rd, oacc=oacc, mx=mx: e.tensor_scalar(
                            out=mx[:, n % 4, h * 64:(h + 1) * 64], in0=oacc[:, h * 65:h * 65 + 64],
                            scalar1=rd[:, h:h + 1], scalar2=None, op0=ALU.mult),
                            reads=[boacc, brd], writes=[bmx])
                    if n % 4 == 3:
                        r0_ = (n - 3) * 128
                        sch.dma("pool", mix_scr[r0_:r0_ + 512, mix_col0:mix_col0 + OH * 64].rearrange("(t p) c -> p t c", p=128),
                                mx[:], reads=[bmx])

                units = []
                i = 0
                while i < len(recs):
                    if i + 1 < len(recs) and recs[i]["grp"][0][2][2] != recs[i + 1]["grp"][0][2][2]:
                        units.append([recs[i], recs[i + 1]])
                        i += 2
                    else:
                        units.append([recs[i]])
                        i += 1
                LA = 1
                for i in range(len(units) + LA):
                    if i < len(units):
                        emitA(units[i])
                    if i - LA >= 0:
                        for rc in units[i - LA]:
                            emitB(rc)
                sch.fence_all("sp")
                sch.fence_all("pool")
                for en_ in ("act", "dve", "pe"):
                    sch.fence_all(en_)
                sch.release_since(mark_)

        SEL = _os.environ.get("P2SEL", "na0,na1,dl0,dl1,mm").split(",")
        if 2 in phases:
            for ps_ in range(2):
                if f"na{ps_}" not in SEL:
                    continue
                slots = [(h // 2, h // 2, h % 2, h, h, h) for h in range(4)]
                attn_pass(f"na{ps_}", [qk_scr[T_NAQ + 2 * ps_ + i] for i in range(2)],
                          [qk_scr[T_NAK + 2 * ps_ + i] for i in range(2)], S, v_scr, [(ps_ * 256, 4)], slots,
                          lambda n: [(m, tid, [0, 1, 2, 3]) for (m, tid) in na_steps[n]], 4,
                          (na_bias, ps_ * 4, 4), ps_ * 256)
            DD = [d_ for (_, d_) in DIL]
            for ps_ in range(2):
                if f"dl{ps_}" not in SEL:
                    continue
                slots = []
                for g in range(3):
                    for hh in range(2):
                        slots.append((g, g, hh, g * 2 + hh, g * 2 + hh, hh))

                def dsteps(n):
                    st = []
                    for g in range(3):
                        SB_ = NT // DD[g]
                        for o in (-1, 0, 1):
                            m = n + o
                            if 0 <= m < NT and m // SB_ == n // SB_:
                                st.append((m, g * 3 + o + 1, [g * 2, g * 2 + 1]))
                    return st

                def raw_dst(g, n, ps_=ps_):
                    d_ = DD[g]
                    SB_ = NT // d_
                    v = dn_scr[g][:, ps_ * 130:(ps_ + 1) * 130].rearrange("(ub p r) c -> r ub p c", p=128, r=d_)
                    return v[n // SB_, n % SB_]
                attn_pass(f"dl{ps_}", [qk_scr[T_DQ + 2 * g + ps_] for g in range(3)],
                          [qk_scr[T_DK + 2 * g + ps_] for g in range(3)], S, v_scr,
                          [(512 + g * 256 + ps_ * 128, 2) for g in range(3)], slots, dsteps, 6,
                          (dil_bias, ps_ * 2, 2), 0, perm=DD, raw_dst=raw_dst)
            if "dl0" in SEL and "dl1" in SEL:
                mark_c = len(sch.all_bufs)
                with ExitStack() as ec:
                    dn_r = Ring(sch, ec, nc, "dcdn", [128, 3, 260], F32, 3)
                    sm_r = Ring(sch, ec, nc, "dcsm", [128, 260], F32, 2)
                    rc_r = Ring(sch, ec, nc, "dcrc", [128, 4], F32, 2)
                    mo_r = Ring(sch, ec, nc, "dcmo", [128, 256], BF16, 3)
                    for t in range(NT):
                        dn, bdn = dn_r.next()
                        sch.dma("sp", dn[:], dn_scr[:, t * 128:(t + 1) * 128, :].rearrange("g p c -> p g c"), writes=[bdn])
                        sm, bsm = sm_r.next()
                        sch.op("dve", lambda e, dn=dn, sm=sm: e.tensor_tensor(out=sm[:], in0=dn[:, 0, :], in1=dn[:, 1, :], op=ALU.add),
                               reads=[bdn], writes=[bsm])
                        sch.op("dve", lambda e, dn=dn, sm=sm: e.tensor_tensor(out=sm[:], in0=sm[:], in1=dn[:, 2, :], op=ALU.add),
                               reads=[bdn, bsm], writes=[bsm])
                        rcp, brc = rc_r.next()
                        smv = sm[:].rearrange("p (h c) -> p h c", c=65)
                        sch.op("dve", lambda e, rcp=rcp, smv=smv: e.reciprocal(out=rcp[:], in_=smv[:, :, 64]), reads=[bsm], writes=[brc])
                        mo, bmo = mo_r.next()
                        for h in range(4):
                            sch.op("act", lambda e, h=h, mo=mo, sm=sm, rcp=rcp: e.activation(
                                out=mo[:, h * 64:(h + 1) * 64], in_=sm[:, h * 65:h * 65 + 64], func=AF.Copy, scale=rcp[:, h:h + 1]),
                                reads=[bsm, brc], writes=[bmo])
                        sch.dma("pool", mix_scr[t * 128:(t + 1) * 128, 512:768], mo[:], reads=[bmo])
                    for en_ in ("sp", "pool", "act", "dve", "pe"):
                        sch.fence_all(en_)
                    sch.release_since(mark_c)
            slots = [(h // 2, h // 2, h % 2, h, h, 0) for h in range(4)]
            if "mm" in SEL:
              attn_pass("mm", [qk_scr[T_MQ + i] for i in range(2)], [km_scr[i] for i in range(2)], 256, vm_scr,
                      [(0, 4)], slots, lambda n: [(0, None, [0, 1, 2, 3]), (1, None, [0, 1, 2, 3])], 4, None, 768)


        g1 = sb("g1", [128, NT], F32)
        g2 = sb("g2", [128, NT], F32)
        dst1i = sb("dst1i", [128, NT], I32)
        dst2i = sb("dst2i", [128, NT], I32)
        widx = sb("widx", [128, NBLK * 2], I32)
        bRt = sch.buf("routeout")
        if 3 in phases:
            with ExitStack() as e3:
                cur = [e3]
                def sb3(name, shape, dt):
                    return cur[0].enter_context(nc.sbuf_tensor("p3" + name, list(shape), dt))
                L = sb3("L", [128, NT, 36], F32)
                bL = sch.buf("L")
                e3m = ExitStack()
                e3m.__enter__()
                cur[0] = e3m
                Wo = sb3("Wo", [128, 8, D], BF16)
                bWo = sch.buf("Wo")
                wst = Ring(sch, cur[0], nc, "p3wst", [128, D], F32, 2)
                for kc in range(8):
                    t, b = wst.next()
                    sch.dma("sp", t[:], w_out[kc * 128:(kc + 1) * 128, :], writes=[b])
                    sch.op("dve", lambda e, t=t, kc=kc: e.tensor_copy(out=Wo[:, kc, :], in_=t[:]), reads=[b], writes=[bWo])
                wr = sb3("wr", [128, 8, 36], F32)
                br = sb3("br", [128, 36], F32)
                gB = sb3("gB", [128, D], F32)
                bwr = sch.buf("wr")
                sch.dma("sp", wr[:], w_r.rearrange("(kc p) n -> p kc n", p=128), writes=[bwr])
                sch.dma("sp", br[:], b_r[:, :], writes=[bwr])
                sch.dma("sp", gB[:], gffn_b[:, :], writes=[bwr])
                for kc in range(8):
                    sch.op("dve", lambda e, kc=kc: e.tensor_scalar(out=wr[:, kc, :], in0=wr[:, kc, :],
                                                                    scalar1=gv[:, 16 + kc:17 + kc], scalar2=None, op0=ALU.mult),
                           reads=[bwr, b_const], writes=[bwr])
                wrh = sb3("wrh", [128, 8, 36], BF16)
                wrl = sb3("wrl", [128, 8, 36], BF16)
                sch.op("dve", lambda e: e.tensor_copy(out=wrh[:], in_=wr[:]), reads=[bwr], writes=[bwr])
                sch.op("dve", lambda e: e.tensor_tensor(out=wrl[:], in0=wr[:], in1=wrh[:], op=ALU.subtract), reads=[bwr], writes=[bwr])
                mx_r = Ring(sch, cur[0], nc, "p3mx", [128, D], BF16, 2)
                xt_r = Ring(sch, cur[0], nc, "p3xt", [128, D], F32, 2)
                mT_r = Ring(sch, cur[0], nc, "p3mT", [128, 8, 128], BF16, 2)
                x1_r = Ring(sch, cur[0], nc, "p3x1", [128, D], F32, 2)
                hn_r = Ring(sch, cur[0], nc, "p3hn", [128, D], F32, 2)
                hi_r = Ring(sch, cur[0], nc, "p3hi", [128, D], BF16, 3)
                lo_r = Ring(sch, cur[0], nc, "p3lo", [128, D], BF16, 3)
                hg_r = Ring(sch, cur[0], nc, "p3hg", [128, D], BF16, 2)
                hiT_r = Ring(sch, cur[0], nc, "p3hiT", [128, 8, 128], BF16, 2)
                loT_r = Ring(sch, cur[0], nc, "p3loT", [128, 8, 128], BF16, 2)
                st_r = Ring(sch, cur[0], nc, "p3st", [128, 4], F32, 2)
                junk = sb3("junk", [128, D], BF16)
                bjunk = sch.buf("p3junk")
                with ExitStack() as e3p:
                    tpm = e3p.enter_context(nc.psum_tensor("p3tpm", [128, 8, 128], BF16))
                    btpm = sch.buf("p3tpm")
                    yps = Ring(sch, e3p, nc, "p3yps", [128, 512], F32, 4, psum=True)
                    tph = e3p.enter_context(nc.psum_tensor("p3tph", [128, 8, 128], BF16))
                    tpl = e3p.enter_context(nc.psum_tensor("p3tpl", [128, 8, 128], BF16))
                    btph, btpl = sch.buf("p3tph"), sch.buf("p3tpl")
                    lps = e3p.enter_context(nc.psum_tensor("p3lps", [128, 512], F32))
                    blps = sch.buf("p3lps")
                    def p3A(n):
                        r0_ = n * 128
                        mxt, bmx = mx_r.next()
                        xt, bxt = xt_r.next()
                        sch.dma("sp", mxt[:], mix_scr[r0_:r0_ + 128, :], writes=[bmx])
                        sch.dma("sp", xt[:], x[r0_:r0_ + 128, :], writes=[bxt])
                        for kc in range(8):
                            sch.op("pe", lambda e, kc=kc, mxt=mxt: e.transpose(out=tpm[:, kc, :], in_=mxt[:, kc * 128:(kc + 1) * 128],
                                                                      identity=identb[:]), reads=[bmx, b_const], writes=[btpm])
                        mT, bmT = mT_r.next()
                        sch.op("dve", lambda e, mT=mT: e.tensor_copy(out=mT[:], in_=tpm[:]), reads=[btpm], writes=[bmT])
                        x1, bx1 = x1_r.next()
                        for hf in range(2):
                            yp, byp = yps.next()
                            for kc in range(8):
                                sch.op("pe", lambda e, kc=kc, hf=hf, yp=yp, mT=mT: e.matmul(
                                    yp[:], mT[:, kc, :], Wo[:, kc, hf * 512:(hf + 1) * 512], start=(kc == 0), stop=(kc == 7)),
                                    reads=[bmT, bWo], writes=[byp])
                            sch.op("dve", lambda e, hf=hf, yp=yp, x1=x1, xt=xt: e.tensor_tensor(
                                out=x1[:, hf * 512:(hf + 1) * 512], in0=yp[:], in1=xt[:, hf * 512:(hf + 1) * 512], op=ALU.add),
                                reads=[byp, bxt], writes=[bx1])
                        sch.dma("pool", out[r0_:r0_ + 128, :], x1[:], reads=[bx1])
                        stt, bst = st_r.next()
                        sch.op("act", lambda e, x1=x1, stt=stt: e.activation(out=junk[:], in_=x1[:], func=AF.Square,
                                                                           accum_out=stt[:, 0:1]), reads=[bx1], writes=[bjunk, bst])
                        sch.op("act", lambda e, stt=stt: e.activation(out=stt[:, 1:2], in_=stt[:, 0:1], func=AF.Sqrt,
                                                                       bias=EPS, scale=1.0 / D), reads=[bst], writes=[bst])
                        sch.op("dve", lambda e, stt=stt: e.reciprocal(out=stt[:, 1:2], in_=stt[:, 1:2]), reads=[bst], writes=[bst])
                        hn, bhn = hn_r.next()
                        sch.op("act", lambda e, hn=hn, x1=x1, stt=stt: e.activation(out=hn[:], in_=x1[:], func=AF.Copy,
                                                                                   scale=stt[:, 1:2]), reads=[bx1, bst], writes=[bhn])
                        hi, bhi = hi_r.next()
                        lo, blo = lo_r.next()
                        hg, bhg = hg_r.next()
                        sch.op("act", lambda e, hi=hi, hn=hn: e.activation(out=hi[:], in_=hn[:], func=AF.Copy), reads=[bhn], writes=[bhi])
                        sch.op("dve", lambda e, hi=hi, lo=lo, hn=hn: e.tensor_tensor(out=lo[:], in0=hn[:], in1=hi[:], op=ALU.subtract),
                               reads=[bhn, bhi], writes=[blo])
                        sch.op("pool", lambda e, hg=hg, hn=hn: e.tensor_tensor(out=hg[:], in0=hn[:], in1=gB[:], op=ALU.mult),
                               reads=[bhn, bwr], writes=[bhg])
                        sch.dma("pool", h2_scr[r0_:r0_ + 128, :], hg[:], reads=[bhg])
                        return hi, bhi, lo, blo

                    def p3B(n, hi, bhi, lo, blo):
                        for kc in range(8):
                            sch.op("pe", lambda e, kc=kc, hi=hi: e.transpose(out=tph[:, kc, :], in_=hi[:, kc * 128:(kc + 1) * 128],
                                                                            identity=identb[:]), reads=[bhi, b_const], writes=[btph])
                        for kc in range(8):
                            sch.op("pe", lambda e, kc=kc, lo=lo: e.transpose(out=tpl[:, kc, :], in_=lo[:, kc * 128:(kc + 1) * 128],
                                                                            identity=identb[:]), reads=[blo, b_const], writes=[btpl])
                        hiT, bhiT = hiT_r.next()
                        loT, bloT = loT_r.next()
                        sch.op("act", lambda e, hiT=hiT: e.activation(out=hiT[:], in_=tph[:], func=AF.Copy), reads=[btph], writes=[bhiT])
                        sch.op("act", lambda e, loT=loT: e.activation(out=loT[:], in_=tpl[:], func=AF.Copy), reads=[btpl], writes=[bloT])
                        k = 0
                        for (aa, ba, ww) in ((hiT, bhiT, wrh), (hiT, bhiT, wrl), (loT, bloT, wrh)):
                            for kc in range(8):
                                sch.op("pe", lambda e, kc=kc, aa=aa, ww=ww, k=k: e.matmul(lps[:, 0:36], aa[:, kc, :], ww[:, kc, :],
                                                                                     start=(k == 0), stop=(k == 23)),
                                       reads=[ba, bwr], writes=[blps])
                                k += 1
                        sch.op("dve", lambda e, n=n: e.tensor_tensor(out=L[:, n, :], in0=lps[:, 0:36], in1=br[:], op=ALU.add),
                               reads=[blps, bwr], writes=[bL])


                    prev3 = None
                    for n in range(NT + 1):
                        cur3 = p3A(n) if n < NT else None
                        if prev3 is not None:
                            p3B(n - 1, *prev3)
                        prev3 = cur3
                e3m.close()
                cur[0] = e3
                def sbr(name, shape, dt=F32):
                    return e3.enter_context(nc.sbuf_tensor("rt" + name, list(shape), dt))
                bR = sch.buf("route")
                gl = L[:, :, 0:4]
                fl = L[:, :, 4:36]
                gmax = sbr("gmax", [128, NT]); G1 = sbr("G1", [128, NT, 4]); ge = sbr("ge", [128, NT, 4])
                gsum = sbr("gsum", [128, NT]); pen = sbr("pen", [128, NT, 4]); flm = sbr("flm", [128, NT, 32])
                m1 = sbr("m1", [128, NT]); oh1 = sbr("oh1", [128, NT, 32]); m2 = sbr("m2", [128, NT])
                oh2 = sbr("oh2", [128, NT, 32]); dd = sbr("dd", [128, NT])

                def R(eng, fn, extra_w=()):
                    sch.op(eng, fn, reads=[bL, bR, b_const], writes=[bR] + list(extra_w))

                def bc(a, n):
                    return a[:, :].unsqueeze(2).broadcast_to([128, NT, n])
                R("dve", lambda e: e.tensor_reduce(out=gmax[:], in_=gl, axis=AX.X, op=ALU.max))
                R("dve", lambda e: e.tensor_tensor(out=G1[:], in0=gl, in1=bc(gmax, 4), op=ALU.is_ge))
                R("dve", lambda e: e.tensor_tensor(out=ge[:], in0=gl, in1=bc(gmax, 4), op=ALU.subtract))
                R("act", lambda e: e.activation(out=ge[:], in_=ge[:], func=AF.Exp))
                R("dve", lambda e: e.tensor_reduce(out=gsum[:], in_=ge[:], axis=AX.X, op=ALU.add))
                R("dve", lambda e: e.reciprocal(out=gsum[:], in_=gsum[:]))
                R("dve", lambda e: e.tensor_scalar(out=pen[:], in0=G1[:], scalar1=1e9, scalar2=-1e9, op0=ALU.mult, op1=ALU.add))
                R("dve", lambda e: e.tensor_tensor(
                    out=flm[:].rearrange("p t (g e) -> p t g e", e=8), in0=fl.rearrange("p t (g e) -> p t g e", e=8),
                    in1=pen[:].unsqueeze(3).broadcast_to([128, NT, 4, 8]), op=ALU.add))
                R("dve", lambda e: e.tensor_reduce(out=m1[:], in_=flm[:], axis=AX.X, op=ALU.max))
                R("dve", lambda e: e.tensor_tensor(out=oh1[:], in0=flm[:], in1=bc(m1, 32), op=ALU.is_ge))
                R("dve", lambda e: e.scalar_tensor_tensor(out=flm[:], in0=oh1[:], scalar=-1e9, in1=flm[:], op0=ALU.mult, op1=ALU.add))
                R("dve", lambda e: e.tensor_reduce(out=m2[:], in_=flm[:], axis=AX.X, op=ALU.max))
                R("dve", lambda e: e.tensor_tensor(out=oh2[:], in0=flm[:], in1=bc(m2, 32), op=ALU.is_ge))
                R("dve", lambda e: e.tensor_tensor(out=dd[:], in0=m2[:], in1=m1[:], op=ALU.subtract))
                R("act", lambda e: e.activation(out=dd[:], in_=dd[:], func=AF.Exp))
                R("dve", lambda e: e.tensor_scalar(out=g1[:], in0=dd[:], scalar1=1.0, scalar2=None, op0=ALU.add), [bRt])
                R("dve", lambda e: e.reciprocal(out=g1[:], in_=g1[:]), [bRt])
                R("dve", lambda e: e.tensor_tensor(out=g1[:], in0=g1[:], in1=gsum[:], op=ALU.mult), [bRt])
                R("dve", lambda e: e.tensor_tensor(out=g2[:], in0=g1[:], in1=dd[:], op=ALU.mult), [bRt])
                selb = sbr("selb", [128, NT * 32], BF16)
                trif = sbr("trif", [128, 128]); trib = sbr("trib", [128, 128], BF16); oneb = sbr("oneb", [128, 128], BF16)
                iot = sbr("iot", [128, 256]); pid2 = sbr("pid2", [128, 1])
                sch.dma("sp", trif[:], tri_in[:, :], writes=[bR])
                sch.dma("sp", iot[:], iota_in[:, :], writes=[bR])
                sch.dma("sp", pid2[:], pidx_in[:, :], writes=[bR])
                R("dve", lambda e: e.tensor_copy(out=trib[:], in_=trif[:]))
                R("dve", lambda e: e.memset(oneb[:], 1.0))
                R("dve", lambda e: e.tensor_scalar(out=pid2[:], in0=pid2[:], scalar1=2.0, scalar2=None, op0=ALU.mult))
                R("dve", lambda e: e.tensor_tensor(out=selb[:], in0=oh1[:].rearrange("p t e -> p (t e)"),
                                                   in1=oh2[:].rearrange("p t e -> p (t e)"), op=ALU.add))
                Cs = sbr("Cs", [128, NT, 32]); Ta = sbr("Ta", [128, NT, 32]); Tb = sbr("Tb", [128, NT, 32]); T0 = sbr("T0", [128, NT, 32])
                with ExitStack() as e3r:
                    cps = [e3r.enter_context(nc.psum_tensor(f"rtc{j}", [128, 512], F32)) for j in range(4)]
                    tps = [e3r.enter_context(nc.psum_tensor(f"rtt{j}", [128, 512], F32)) for j in range(4)]
                    bcp, btp = sch.buf("rtc"), sch.buf("rtt")
                    Cf = Cs[:].rearrange("p t e -> p (t e)")
                    T0f = T0[:].rearrange("p t e -> p (t e)")
                    for j in range(4):
                        sch.op("pe", lambda e, j=j: e.matmul(cps[j][:], trib[:], selb[:, j * 512:(j + 1) * 512], start=True, stop=True),
                               reads=[bR], writes=[bcp])
                        sch.op("pe", lambda e, j=j: e.matmul(tps[j][:], oneb[:], selb[:, j * 512:(j + 1) * 512], start=True, stop=True),
                               reads=[bR], writes=[btp])
                    for j in range(4):
                        sch.op("act", lambda e, j=j: e.activation(out=Cf[:, j * 512:(j + 1) * 512], in_=cps[j][:], func=AF.Copy),
                               reads=[bcp], writes=[bR])
                        sch.op("dve", lambda e, j=j: e.tensor_copy(out=T0f[:, j * 512:(j + 1) * 512], in_=tps[j][:]),
                               reads=[btp], writes=[bR])
                src, dstb = T0, Ta
                for sft in (1, 2, 4, 8, 16, 32):
                    R("dve", lambda e, src=src, dstb=dstb, sft=sft: e.tensor_copy(out=dstb[:, 0:sft, :], in_=src[:, 0:sft, :]))
                    R("dve", lambda e, src=src, dstb=dstb, sft=sft: e.tensor_tensor(
                        out=dstb[:, sft:NT, :], in0=src[:, sft:NT, :], in1=src[:, 0:NT - sft, :], op=ALU.add))
                    src, dstb = dstb, (Tb if dstb is Ta else Ta)
                Inc = src
                cnt = sbr("cnt", [128, 32]); nbk = sbr("nbk", [128, 32]); cmp1 = sbr("cmp1", [128, 32, 128])
                i128 = sbr("i128", [128, 128]); sa = sbr("sa", [128, 32]); sb_ = sbr("sb_", [128, 32])
                psr = sbr("psr", [128, 32]); pend = sbr("pend", [128, 32])
                R("dve", lambda e: e.tensor_copy(out=cnt[:], in_=Inc[:, NT - 1, :]))
                R("dve", lambda e: e.tensor_scalar(out=i128[:], in0=iot[:, 0:128], scalar1=float(MOE_B), scalar2=None, op0=ALU.mult))
                R("dve", lambda e: e.tensor_tensor(out=cmp1[:], in0=cnt[:, :].unsqueeze(2).broadcast_to([128, 32, 128]),
                                                   in1=i128[:, :].unsqueeze(1).broadcast_to([128, 32, 128]), op=ALU.is_gt))
                R("dve", lambda e: e.tensor_reduce(out=nbk[:], in_=cmp1[:], axis=AX.X, op=ALU.add))
                src, dstb = nbk, sa
                for sft in (1, 2, 4, 8, 16):
                    R("dve", lambda e, src=src, dstb=dstb, sft=sft: e.tensor_copy(out=dstb[:, 0:sft], in_=src[:, 0:sft]))
                    R("dve", lambda e, src=src, dstb=dstb, sft=sft: e.tensor_tensor(
                        out=dstb[:, sft:32], in0=src[:, sft:32], in1=src[:, 0:32 - sft], op=ALU.add))
                    src, dstb = dstb, (sb_ if dstb is sa else sa)
                R("dve", lambda e, src=src: e.tensor_copy(out=pend[:], in_=src[:]))
                R("dve", lambda e: e.tensor_tensor(out=psr[:], in0=pend[:], in1=nbk[:], op=ALU.subtract))
                R("dve", lambda e: e.tensor_scalar(out=psr[:], in0=psr[:], scalar1=float(MOE_B), scalar2=None, op0=ALU.mult))
                R("dve", lambda e, Inc=Inc: e.tensor_tensor(out=Cs[:], in0=Cs[:], in1=Inc[:], op=ALU.add))
                R("dve", lambda e: e.tensor_tensor(out=Cs[:], in0=Cs[:], in1=T0[:], op=ALU.subtract))
                R("dve", lambda e: e.tensor_tensor(out=Cs[:], in0=Cs[:], in1=psr[:, :].unsqueeze(1).broadcast_to([128, NT, 32]), op=ALU.add))
                d1 = sbr("d1", [128, NT]); d2 = sbr("d2", [128, NT])
                R("dve", lambda e: e.tensor_tensor(out=Ta[:], in0=Cs[:], in1=oh1[:], op=ALU.mult))
                R("dve", lambda e: e.tensor_reduce(out=d1[:], in_=Ta[:], axis=AX.X, op=ALU.add))
                R("dve", lambda e: e.tensor_tensor(out=Tb[:], in0=Cs[:], in1=oh2[:], op=ALU.mult))
                R("dve", lambda e: e.tensor_reduce(out=d2[:], in_=Tb[:], axis=AX.X, op=ALU.add))
                R("dve", lambda e: e.tensor_copy(out=dst1i[:], in_=d1[:]), [bRt])
                R("dve", lambda e: e.tensor_copy(out=dst2i[:], in_=d2[:]), [bRt])
                cmp2 = sbr("cmp2", [128, NBLK, 32]); bex = sbr("bex", [128, NBLK]); need = sbr("need", [128, NBLK])
                w0 = sbr("w0", [128, NBLK]); w1f = sbr("w1f", [128, NBLK, 2])
                R("dve", lambda e: e.tensor_tensor(out=cmp2[:], in0=iot[:, 0:NBLK].unsqueeze(2).broadcast_to([128, NBLK, 32]),
                                                   in1=pend[:, :].unsqueeze(1).broadcast_to([128, NBLK, 32]), op=ALU.is_ge))
                R("dve", lambda e: e.tensor_reduce(out=bex[:], in_=cmp2[:], axis=AX.X, op=ALU.add))
                R("dve", lambda e: e.tensor_scalar(out=bex[:], in0=bex[:], scalar1=float(N_EXP - 1), scalar2=None, op0=ALU.min))
                R("dve", lambda e: e.memset(need[:], 1.0))
                if WSKIP:
                    R("dve", lambda e: e.tensor_tensor(out=need[:, NSET:NBLK], in0=bex[:, NSET:NBLK], in1=bex[:, 0:NBLK - NSET], op=ALU.not_equal))
                R("dve", lambda e: e.tensor_scalar(out=w0[:], in0=bex[:], scalar1=256.0, scalar2=pid2[:, 0:1], op0=ALU.mult, op1=ALU.add))
                R("dve", lambda e: e.tensor_tensor(out=w0[:], in0=w0[:], in1=need[:], op=ALU.mult))
                R("dve", lambda e: e.tensor_scalar(out=need[:], in0=need[:], scalar1=-float(1 << 30), scalar2=float(1 << 30),
                                                   op0=ALU.mult, op1=ALU.add))
                R("dve", lambda e: e.tensor_tensor(out=w0[:], in0=w0[:], in1=need[:], op=ALU.add))
                R("dve", lambda e: e.tensor_copy(out=w1f[:, :, 0], in_=w0[:]))
                R("dve", lambda e: e.tensor_scalar(out=w1f[:, :, 1], in0=w0[:], scalar1=1.0, scalar2=None, op0=ALU.add))
                R("dve", lambda e: e.tensor_copy(out=widx[:], in_=w1f[:].rearrange("p b h -> p (b h)")), [bRt])
                if debug:
                    dbg = sbr("dbg", [128, 4 * NT + 2 * NBLK])
                    R("dve", lambda e: e.tensor_copy(out=dbg[:, 0:NT], in_=d1[:]))
                    R("dve", lambda e: e.tensor_copy(out=dbg[:, NT:2 * NT], in_=d2[:]))
                    R("dve", lambda e: e.tensor_copy(out=dbg[:, 2 * NT:3 * NT], in_=g1[:]))
                    R("dve", lambda e: e.tensor_copy(out=dbg[:, 3 * NT:4 * NT], in_=g2[:]))
                    R("dve", lambda e: e.tensor_copy(out=dbg[:, 4 * NT:4 * NT + NBLK], in_=bex[:]))
                    R("dve", lambda e: e.tensor_copy(out=dbg[:, 4 * NT + NBLK:4 * NT + 2 * NBLK], in_=w0[:]))
                    sch.dma("sp", dbg_out[:, :], dbg[:], reads=[bR])
                sch.fence_all("sp")
                sch.fence_all("pool")
                hs_r = Ring(sch, e3, nc, "p3hs", [128, D], BF16, 4)
                for n in range(NT):
                    hs, bhs = hs_r.next()
                    sch.dma("sp", hs[:], h2_scr[n * 128:(n + 1) * 128, :], writes=[bhs])
                    for dsti in (dst1i, dst2i):
                        sch.dma("pool", xs_scr[:, :], hs[:], reads=[bhs, bRt],
                                indirect=dict(out_offset=bass.IndirectOffsetOnAxis(dsti[:, n:n + 1], 0), in_offset=None))
                sch.fence_all("sp")
                sch.fence_all("pool")

        if 4 in phases:
            with ExitStack() as e4:
                def sb4(name, shape, dt):
                    return e4.enter_context(nc.sbuf_tensor("p4" + name, list(shape), dt))
                SUB = MOE_B // 128
                Wb = [[sb4(f"W{i}_{s_}", [128, 4096], BF16) for s_ in range(NSET)] for i in range(3)]
                bWb = [[[sch.buf(f"p4W{i}_{s_}_{h}") for h in range(2)] for s_ in range(NSET)] for i in range(3)]
                wsrc = (w1, w3, w2)
                bnd_reg = nc.gpsimd.alloc_register("wbnd")
                nc.gpsimd.reg_mov(bnd_reg, N_EXP * 256 - 1)
                xs_r = Ring(sch, e4, nc, "p4xs", [128, D], BF16, 3)
                xT_r = Ring(sch, e4, nc, "p4xT", [128, 8, 128], BF16, 3)
                sg_r = Ring(sch, e4, nc, "p4sg", [128, 512], BF16, 2)
                am_r = Ring(sch, e4, nc, "p4am", [128, 512], BF16, 2)
                aT_r = Ring(sch, e4, nc, "p4aT", [128, 4, 128], BF16, 2 * SUB + 1)
                ys_r = Ring(sch, e4, nc, "p4ys", [128, D], F32, 3)
                with ExitStack() as e4p:
                    tpx = e4p.enter_context(nc.psum_tensor("p4tpx", [128, 8, 128], BF16))
                    btpx = sch.buf("p4tpx")
                    a1p = Ring(sch, e4p, nc, "p4a1", [128, 512], F32, 2, psum=True)
                    a3p = Ring(sch, e4p, nc, "p4a3", [128, 512], F32, 2, psum=True)
                    ypp = Ring(sch, e4p, nc, "p4yp", [128, 512], F32, 2, psum=True)
                    tpa = e4p.enter_context(nc.psum_tensor("p4tpa", [128, 4, 128], BF16))
                    btpa = sch.buf("p4tpa")

                    def stageX(b):
                        st_ = b % NSET
                        for i in range(3):
                            for hf in range(2):
                                sch.dma("pool", Wb[i][st_][:, hf * 2048:(hf + 1) * 2048], wsrc[i][:, :], reads=[bRt], writes=[bWb[i][st_][hf]],
                                        indirect=dict(out_offset=None, in_offset=bass.IndirectOffsetOnAxis(widx[:, 2 * b + hf:2 * b + hf + 1], 0),
                                                      bounds_check=bnd_reg, oob_is_err=False))
                        res = []
                        for sb_ in range(SUB):
                            r0_ = b * MOE_B + sb_ * 128
                            xs, bxs = xs_r.next()
                            sch.dma("sp", xs[:], xs_scr[r0_:r0_ + 128, :], writes=[bxs])
                            for kc in range(8):
                                sch.op("pe", lambda e, kc=kc, xs=xs: e.transpose(out=tpx[:, kc, :], in_=xs[:, kc * 128:(kc + 1) * 128],
                                                                                identity=identb[:]), reads=[bxs, b_const], writes=[btpx])
                            xT, bxT = xT_r.next()
                            sch.op("dve", lambda e, xT=xT: e.tensor_copy(out=xT[:], in_=tpx[:]), reads=[btpx], writes=[bxT])
                            a1, ba1 = a1p.next()
                            a3, ba3 = a3p.next()
                            for (ap_, bap, Wx, bWx) in ((a1, ba1, Wb[0][st_], bWb[0][st_]), (a3, ba3, Wb[1][st_], bWb[1][st_])):
                                for kc in range(8):
                                    sch.op("pe", lambda e, kc=kc, ap_=ap_, Wx=Wx, xT=xT: e.matmul(
                                        ap_[:], xT[:, kc, :], Wx[:, kc * 512:(kc + 1) * 512],
                                        start=(kc == 0), stop=(kc == 7)), reads=[bWx[kc // 4], bxT], writes=[bap])
                            sg, bsg = sg_r.next()
                            sch.op("act", lambda e, a1=a1, sg=sg: e.activation(out=sg[:], in_=a1[:], func=AF.Silu), reads=[ba1], writes=[bsg])
                            am, bam = am_r.next()
                            sch.op("dve", lambda e, am=am, a3=a3, sg=sg: e.tensor_tensor(out=am[:], in0=a3[:], in1=sg[:], op=ALU.mult),
                                   reads=[ba3, bsg], writes=[bam])
                            for nch in range(4):
                                sch.op("pe", lambda e, nch=nch, am=am: e.transpose(out=tpa[:, nch, :], in_=am[:, nch * 128:(nch + 1) * 128],
                                                                                  identity=identb[:]), reads=[bam, b_const], writes=[btpa])
                            aT, baT = aT_r.next()
                            sch.op("act", lambda e, aT=aT: e.activation(out=aT[:], in_=tpa[:], func=AF.Copy), reads=[btpa], writes=[baT])
                            res.append((aT, baT))
                        return res

                    def stageY(b, res):
                        st_ = b % NSET
                        W2b = Wb[2][st_]
                        for sb_, (aT, baT) in enumerate(res):
                            r0_ = b * MOE_B + sb_ * 128
                            ys, bys = ys_r.next()
                            for hf in range(2):
                                yp, byp = ypp.next()
                                for nch in range(4):
                                    sch.op("pe", lambda e, nch=nch, hf=hf, yp=yp, aT=aT, W2b=W2b: e.matmul(
                                        yp[:], aT[:, nch, :], W2b[:, nch * 1024 + hf * 512:nch * 1024 + (hf + 1) * 512],
                                        start=(nch == 0), stop=(nch == 3)), reads=[baT, bWb[2][st_][nch // 2]], writes=[byp])
                                sch.op("act", lambda e, hf=hf, yp=yp, ys=ys: e.activation(out=ys[:, hf * 512:(hf + 1) * 512], in_=yp[:], func=AF.Copy),
                                       reads=[byp], writes=[bys])
                            sch.dma("act", yb_scr[r0_:r0_ + 128, :], ys[:], reads=[bys])

                    prevx = None
                    for b in range(NBLK + 1):
                        curx = stageX(b) if b < NBLK else None
                        if prevx is not None:
                            stageY(b - 1, prevx)
                        prevx = curx
                sch.fence_all("sp")
                sch.fence_all("pool")
                sch.fence_all("act")
                y1_r = Ring(sch, e4, nc, "p4y1", [128, D], F32, 3)
                y2_r = Ring(sch, e4, nc, "p4y2", [128, D], F32, 3)
                xo_r = Ring(sch, e4, nc, "p4xo", [128, D], F32, 3)
                for n in range(NT):
                    y1, by1 = y1_r.next()
                    y2, by2 = y2_r.next()
                    xo, bxo = xo_r.next()
                    sch.dma("pool", y1[:], yb_scr[:, :], reads=[bRt], writes=[by1],
                            indirect=dict(out_offset=None, in_offset=bass.IndirectOffsetOnAxis(dst1i[:, n:n + 1], 0)))
                    sch.dma("pool", y2[:], yb_scr[:, :], reads=[bRt], writes=[by2],
                            indirect=dict(out_offset=None, in_offset=bass.IndirectOffsetOnAxis(dst2i[:, n:n + 1], 0)))
                    sch.dma("sp", xo[:], out[n * 128:(n + 1) * 128, :], writes=[bxo])
                    sch.op("dve", lambda e, n=n, y1=y1, xo=xo: e.scalar_tensor_tensor(
                        out=xo[:], in0=y1[:], scalar=g1[:, n:n + 1], in1=xo[:], op0=ALU.mult, op1=ALU.add),
                        reads=[by1, bxo, bRt], writes=[bxo])
                    sch.op("dve", lambda e, n=n, y2=y2, xo=xo: e.scalar_tensor_tensor(
                        out=xo[:], in0=y2[:], scalar=g2[:, n:n + 1], in1=xo[:], op0=ALU.mult, op1=ALU.add),
                        reads=[by2, bxo, bRt], writes=[bxo])
                    sch.dma("act", out[n * 128:(n + 1) * 128, :], xo[:], reads=[bxo])
                sch.fence_all("sp")
                sch.fence_all("pool")
                sch.fence_all("act")

        for en in ("sp", "pool", "act", "dve", "pe"):
            sch.fence_all(en)
        if _os.environ.get("DRYPRINT"):
            print("counts", {k: v.count for k, v in sch.engs.items()}, "nsem", sch.nsem)
    return nc


_CACHE = {}


def kernel(x, mem, g_mix, w_in, qk_gain, na_rpb, t5_table, g_mem, w_mem_kv, w_out, g_ffn, w_r1, b_r1, w_r2, b_r2,
           w1, w3, w2, _debug=False, _phases=(1, 2, 3, 4), _cores=8):
    f32 = np.float32
    x = np.asarray(x, f32); mem = np.asarray(mem, f32)
    na_steps, na_keys = na_plan()
    dil_tl = dil_plan()
    nab = na_bias_tiles(np.asarray(na_rpb, f32)[0], na_keys)
    dlb = dil_res_bias_tiles(np.asarray(t5_table, f32))
    key = (len(na_keys), len(dil_tl), _debug, tuple(_phases))
    if key not in _CACHE:
        _CACHE[key] = build_program(len(na_keys), len(dil_tl), na_steps, dil_tl, debug=_debug, phases=_phases)
    nc = _CACHE[key]

    def pk(v):
        return np.asarray(v, f32).reshape(8, 128).T
    gvec = np.ascontiguousarray(np.concatenate([pk(g_mix[0]), pk(g_mem[0]), pk(g_ffn[0])], axis=1))
    qg = np.asarray(qk_gain, f32)[0]
    gains = np.ascontiguousarray(np.tile(qg.reshape(6, 64), (1, 2)).T)
    shared = {
        "w_in": np.ascontiguousarray(np.asarray(w_in, f32)[0]),
        "w_mem": np.ascontiguousarray(np.asarray(w_mem_kv, f32)[0]),
        "w_out": np.ascontiguousarray(np.asarray(w_out, f32)[0]),
        "gvec": gvec, "gains": gains,
        "gffn_b": np.ascontiguousarray(np.broadcast_to(np.asarray(g_ffn, f32)[0][None, :], (128, D))),
        "ident": np.eye(128, dtype=f32),
        "na_bias": nab, "dil_bias": dlb,
        "w_r": np.ascontiguousarray(np.concatenate([np.asarray(w_r1, f32)[0], np.asarray(w_r2, f32)[0]], axis=1)),
        "b_r": np.ascontiguousarray(np.broadcast_to(
            np.concatenate([np.asarray(b_r1, f32)[0], np.asarray(b_r2, f32)[0]])[None, :], (128, 36))),
        "w1": np.ascontiguousarray(np.asarray(w1, f32)[0].reshape(N_EXP, 8, 128, 512).transpose(0, 2, 1, 3)).reshape(N_EXP * 256, 2048),
        "w3": np.ascontiguousarray(np.asarray(w3, f32)[0].reshape(N_EXP, 8, 128, 512).transpose(0, 2, 1, 3)).reshape(N_EXP * 256, 2048),
        "w2": np.ascontiguousarray(np.asarray(w2, f32)[0].reshape(N_EXP, 4, 128, 1024).transpose(0, 2, 1, 3)).reshape(N_EXP * 256, 2048),
        "iota": np.ascontiguousarray(np.broadcast_to(np.arange(256, dtype=f32)[None, :], (128, 256))),
        "pidx": np.arange(128, dtype=f32).reshape(128, 1),
        "tri": np.triu(np.ones((128, 128), f32), 1),
    }
    in_maps = []
    for c in range(_cores):
        m = dict(shared)
        m["x"] = np.ascontiguousarray(x[c])
        m["mem"] = np.ascontiguousarray(mem[c])
        in_maps.append(m)
    res = run_bass_kernel_spmd(nc, in_maps, core_ids=list(range(_cores)))
    if _debug:
        return res.results
    return np.stack([r["out"] for r in res.results], axis=0)
```

```python
import math
import os as _os
from contextlib import ExitStack
import numpy as np
import concourse.bass as bass
import concourse.mybir as mybir
from concourse.bass_utils import run_bass_kernel_spmd

F32 = mybir.dt.float32
BF16 = mybir.dt.bfloat16
I32 = mybir.dt.int32
AF = mybir.ActivationFunctionType
ALU = mybir.AluOpType
AX = mybir.AxisListType

S = 8192
D = 1024
NT = S // 128
EPS = 1e-6
NEG = -30000.0
N_EXP = 32
MOE_B = 256
NSET = 3
CAP = 2 * S + N_EXP * MOE_B
NBLK = CAP // MOE_B
SAME_ENGINE_SYNC = True
WSKIP = True
LNEXP = bool(int(_os.environ.get("LNEXP", "1")))


class Eng:
    def __init__(self, name, eng, sem):
        self.name, self.eng, self.sem = name, eng, sem
        self.count = 0
        self.waited = {}


class Buf:
    def __init__(self, name):
        self.name = name
        self.w = {}
        self.r = {}
        self.dw = None
        self.dr = None


class Sched:
    def __init__(self, nc, es):
        self.nc, self.es = nc, es
        self.engs = {}
        for nm, e in (("pe", nc.tensor), ("act", nc.scalar), ("dve", nc.vector), ("pool", nc.gpsimd), ("sp", nc.sync)):
            self.engs[nm] = Eng(nm, e, es.enter_context(nc.semaphore("sem_" + nm)))
        self.nsem = 5
        self.all_bufs = []
        self.free_sems = []

    def buf(self, name):
        b = Buf(name)
        self.all_bufs.append(b)
        return b

    def newsem(self, name):
        self.nsem += 1
        return self.es.enter_context(self.nc.semaphore(name))

    def getsem(self, name):
        if self.free_sems:
            return self.free_sems.pop()
        return [self.newsem(name), 0]

    def release_since(self, mark):
        for b in self.all_bufs[mark:]:
            seen = set()
            for attr in ("dw", "dr"):
                v = getattr(b, attr)
                if v is not None and id(v) not in seen:
                    seen.add(id(v))
                    self.free_sems.append(v)
                setattr(b, attr, None)
        del self.all_bufs[mark:]

    def _wait(self, E, key, sem, val):
        if val <= 0 or E.waited.get(key, 0) >= val:
            return
        E.eng.wait_ge(sem, val)
        E.waited[key] = val

    def _deps(self, E, reads, writes, skip_dw=False):
        for b in reads:
            for f, n in b.w.items():
                self._dep_eng(E, f, n)
            if b.dw is not None:
                self._wait(E, id(b.dw[0]), b.dw[0], b.dw[1])
        for b in writes:
            for f, n in b.w.items():
                self._dep_eng(E, f, n)
            for f, n in b.r.items():
                self._dep_eng(E, f, n)
            if b.dw is not None and not skip_dw:
                self._wait(E, id(b.dw[0]), b.dw[0], b.dw[1])
            if b.dr is not None:
                self._wait(E, id(b.dr[0]), b.dr[0], b.dr[1])

    def _dep_eng(self, E, f, n):
        if f == E.name and (f == "pe" or not SAME_ENGINE_SYNC):
            return
        F = self.engs[f]
        self._wait(E, f, F.sem, n)

    def op(self, ename, ins_fn, reads=(), writes=()):
        E = self.engs[ename]
        self._deps(E, reads, writes)
        ins = ins_fn(E.eng)
        E.count += 1
        ins.then_inc(E.sem, 1)
        for b in reads:
            b.r[ename] = E.count
        for b in writes:
            b.w[ename] = E.count
        return ins

    def dma(self, ename, out, in_, reads=(), writes=(), extra_wait=(), indirect=None, disjoint=False, **kw):
        E = self.engs[ename]
        self._deps(E, reads, writes, skip_dw=disjoint)
        for b in extra_wait:
            self._deps(E, [b], [])
        if indirect is None:
            ins = E.eng.dma_start(out=out, in_=in_, **kw)
        else:
            ins = E.eng.indirect_dma_start(out=out, in_=in_, **indirect, **kw)
        if writes:
            b = writes[0]
            if b.dw is None:
                b.dw = self.getsem("ld_" + b.name)
            b.dw[1] += 16
            ins.then_inc(b.dw[0], 16)
            for b2 in writes[1:]:
                b2.dw = b.dw
        elif reads:
            b = reads[0]
            if b.dr is None:
                b.dr = self.getsem("st_" + b.name)
            b.dr[1] += 16
            ins.then_inc(b.dr[0], 16)
        return ins

    def fence_stores(self, ename):
        E = self.engs[ename]
        for b in self.all_bufs:
            if b.dr is not None:
                self._wait(E, id(b.dr[0]), b.dr[0], b.dr[1])

    def fence_all(self, ename):
        E = self.engs[ename]
        for f, F in self.engs.items():
            if f != ename and F.count > 0:
                self._wait(E, f, F.sem, F.count)
        self.fence_stores(ename)


class Ring:
    def __init__(self, sch, es, nc, name, shape, dtype, n, psum=False):
        self.tiles, self.bufs, self.i = [], [], 0
        for k in range(n):
            nm = f"{name}{k}"
            t = es.enter_context(nc.psum_tensor(nm, shape, dtype) if psum else nc.sbuf_tensor(nm, shape, dtype))
            self.tiles.append(t)
            self.bufs.append(sch.buf(nm))

    def next(self):
        k = self.i % len(self.tiles)
        self.i += 1
        return self.tiles[k], self.bufs[k]


def t5_bucket_np(rel):
    nb = 16
    max_exact = 8
    n = np.abs(rel)
    upper = (rel > 0).astype(np.int32) * nb
    nf = np.maximum(n, 1).astype(np.float32)
    large = max_exact + (np.log(nf / max_exact) / math.log(1024 / max_exact) * (nb - max_exact)).astype(np.int32)
    large = np.minimum(large, nb - 1)
    return upper + np.where(n < max_exact, n, large)


def na_plan():
    def r0(r):
        return min(max(r - 4, 0), 120)
    tiles = {}
    steps = []
    for n in range(64):
        rows = (2 * n, 2 * n + 1)
        lo = min(r0(r) for r in rows)
        hi = max(r0(r) + 7 for r in rows)
        st = []
        for m in range(lo // 2, hi // 2 + 1):
            key = []
            for kl in range(2):
                kr = 2 * m + kl
                for ql in range(2):
                    r = rows[ql]
                    ok = r0(r) <= kr <= r0(r) + 7
                    key.append(kr - r + 7 if ok else -1)
            key = tuple(key)
            if all(k < 0 for k in key):
                continue
            if key not in tiles:
                tiles[key] = len(tiles)
            st.append((m, tiles[key]))
        steps.append(st)
    return steps, list(tiles.keys())


def na_bias_tiles(rpb, tile_keys):
    cols = np.arange(64)
    c0 = np.clip(cols - 8, 0, 48)
    kc = cols[:, None]
    qc = cols[None, :]
    colok = (kc >= c0[None, :]) & (kc < c0[None, :] + 16)
    dc = np.clip(kc - qc + 15, 0, 30)
    out = np.full((len(tile_keys), 128, 8, 128), NEG, np.float32)
    for t, key in enumerate(tile_keys):
        i = 0
        for kl in range(2):
            for ql in range(2):
                dr = key[i]
                i += 1
                if dr < 0:
                    continue
                blk = np.where(colok[None], rpb[:, dr][:, dc], NEG)
                out[t, kl * 64:(kl + 1) * 64, :, ql * 64:(ql + 1) * 64] = blk.transpose(1, 0, 2)
    return out


DIL = ((128, 1), (512, 4), (2048, 16))


def dil_plan():
    tl = []
    for g, (win, d) in enumerate(DIL):
        half = win // 2
        lo = -((half + 127) // 128)
        hi = (half + 127) // 128
        for o in range(lo, hi + 1):
            tl.append((g, o))
    return tl


def dil_bias_tiles(t5_table, tl):
    t5 = t5_table.reshape(32, 3, 4)
    out = np.full((len(tl), 128, 4, 128), NEG, np.float32)
    kk = np.arange(128)[:, None]
    qq = np.arange(128)[None, :]
    for t, (g, o) in enumerate(tl):
        win, d = DIL[g]
        rel = o * 128 + kk - qq
        ok = (np.abs(rel) <= win // 2) & (rel % d == 0)
        bk = t5_bucket_np(rel)
        for h in range(4):
            out[t, :, h, :] = np.where(ok, t5[bk, g, h], NEG)
    return out


def dil_res_bias_tiles(t5_table):
    t5 = t5_table.reshape(32, 3, 4)
    out = np.full((9, 128, 4, 128), NEG, np.float32)
    kk = np.arange(128)[:, None]
    qq = np.arange(128)[None, :]
    for g, (win, d) in enumerate(DIL):
        ns = win // 2 // d
        for o in (-1, 0, 1):
            du = o * 128 + kk - qq
            ok = np.abs(du) <= ns
            bk = t5_bucket_np(du * d)
            for h in range(4):
                out[g * 3 + o + 1, :, h, :] = np.where(ok, t5[bk, g, h], NEG)
    return out


QK_TILES = []
for i in range(4):
    QK_TILES.append((0 + 128 * i, 0))
for i in range(4):
    QK_TILES.append((512 + 128 * i, 1))
for i in range(6):
    QK_TILES.append((1536 + 128 * i, 2))
for i in range(6):
    QK_TILES.append((2304 + 128 * i, 3))
for i in range(2):
    QK_TILES.append((3840 + 128 * i, 4))
T_NAQ, T_NAK, T_DQ, T_DK, T_MQ = 0, 4, 8, 14, 20
NQK = len(QK_TILES)
V_SEGS = ((1024, 512, 0), (3072, 512, 512), (3584, 256, 1024))


def build_program(n_na_tiles, n_dil_tiles, na_steps, dil_tl, debug=False, phases=(1, 2, 3, 4)):
    nc = bass.Bass("TRN2", target_bir_lowering=False)
    dk = "ExternalOutput" if debug else "Internal"

    def din(name, shape, dt=F32):
        return nc.dram_tensor(name, list(shape), dt, kind="ExternalInput").ap()

    x = din("x", [S, D])
    mem = din("mem", [256, D])
    w_in = din("w_in", [D, 4096])
    w_mem = din("w_mem", [D, 512])
    w_out = din("w_out", [D, D])
    gvec = din("gvec", [128, 24])
    gains = din("gains", [128, 6])
    gffn_b = din("gffn_b", [128, D])
    ident_in = din("ident", [128, 128])
    na_bias = din("na_bias", [n_na_tiles, 128, 8, 128])
    dil_bias = din("dil_bias", [9, 128, 4, 128])
    w_r = din("w_r", [D, 36])
    b_r = din("b_r", [128, 36])
    w1 = din("w1", [N_EXP * 256, 2048])
    w3 = din("w3", [N_EXP * 256, 2048])
    w2 = din("w2", [N_EXP * 256, 2048])
    iota_in = din("iota", [128, 256])
    pidx_in = din("pidx", [128, 1])
    tri_in = din("tri", [128, 128])
    out = nc.dram_tensor("out", [S, D], F32, kind="ExternalOutput").ap()

    qk_scr = nc.dram_tensor("qk_scr", [NQK, 128, S], BF16, kind=dk).ap()
    v_scr = nc.dram_tensor("v_scr", [S, 1280], BF16, kind=dk).ap()
    mix_scr = nc.dram_tensor("mix_scr", [S, D], BF16, kind=dk).ap()
    km_scr = nc.dram_tensor("km_scr", [2, 128, 256], BF16, kind=dk).ap()
    vm_scr = nc.dram_tensor("vm_scr", [256, 256], BF16, kind=dk).ap()
    h2_scr = nc.dram_tensor("h2_scr", [S, D], BF16, kind=dk).ap()
    dn_scr = nc.dram_tensor("dn_scr", [3, S, 260], F32, kind=dk).ap()
    xs_scr = nc.dram_tensor("xs_scr", [CAP, D], BF16, kind=dk).ap()
    yb_scr = nc.dram_tensor("yb_scr", [CAP, D], F32, kind=dk).ap()
    dbg_out = nc.dram_tensor("dbg_out", [128, 4 * NT + 2 * NBLK], F32, kind=dk).ap()

    with ExitStack() as es:
        sch = Sched(nc, es)

        def sb(name, shape, dt):
            return es.enter_context(nc.sbuf_tensor(name, list(shape), dt))

        def ps(name, shape, dt=F32):
            return es.enter_context(nc.psum_tensor(name, list(shape), dt))

        identf = sb("identf", [128, 128], F32)
        identb = sb("identb", [128, 128], BF16)
        blk1 = sb("blk1", [128, 128], BF16)
        gv = sb("gv", [128, 24], F32)
        gn = sb("gn", [128, 6], F32)
        epsb = sb("epsb", [128, 1], F32)
        b_const = sch.buf("const")
        sch.dma("sp", identf[:], ident_in[:, :], writes=[b_const])
        sch.dma("sp", gv[:], gvec[:, :], writes=[b_const])
        sch.dma("sp", gn[:], gains[:, :], writes=[b_const])
        sch.op("dve", lambda e: e.tensor_copy(out=identb[:], in_=identf[:]), reads=[b_const], writes=[b_const])
        sch.op("dve", lambda e: e.memset(blk1[:], 0.0), writes=[b_const])
        sch.op("dve", lambda e: e.memset(epsb[:], EPS), writes=[b_const])
        sch.op("dve", lambda e: e.memset(blk1[0:64, 0:64], 1.0), writes=[b_const])
        sch.op("dve", lambda e: e.memset(blk1[64:128, 64:128], 1.0), writes=[b_const])
        gq = gn[:].rearrange("p (a b) -> p a b", b=2)[:, :, 0:1]
        sch.op("dve", lambda e: e.tensor_scalar(out=gq, in0=gq, scalar1=0.125, scalar2=None, op0=ALU.mult),
               reads=[b_const], writes=[b_const])

        def projection(es1, src, n_tok, wsrc, ncols, gcol0, qk_tiles, qk_dst, v_segs, v_dst, tag):
            def sb1(name, shape, dt):
                return es1.enter_context(nc.sbuf_tensor(tag + name, list(shape), dt))
            W = sb1("W", [128, 8, ncols], BF16)
            bW = sch.buf(tag + "W")
            wst = Ring(sch, es1, nc, tag + "wst", [128, 2048], F32, 2)
            nhalf = (ncols + 2047) // 2048
            for kc in range(8):
                for hf in range(nhalf):
                    c0 = hf * 2048
                    cw = min(2048, ncols - c0)
                    t, b = wst.next()
                    sch.dma("sp", t[:, 0:cw], wsrc[kc * 128:(kc + 1) * 128, c0:c0 + cw], writes=[b])
                    if (kc * nhalf + hf) % 2 == 0:
                        sch.op("dve", lambda e, t=t, kc=kc, c0=c0, cw=cw: e.tensor_scalar(
                            out=W[:, kc, c0:c0 + cw], in0=t[:, 0:cw], scalar1=gv[:, gcol0 + kc:gcol0 + kc + 1],
                            scalar2=None, op0=ALU.mult), reads=[b, b_const], writes=[bW])
                    else:
                        sch.op("act", lambda e, t=t, kc=kc, c0=c0, cw=cw: e.activation(
                            out=W[:, kc, c0:c0 + cw], in_=t[:, 0:cw], func=AF.Copy, scale=gv[:, gcol0 + kc:gcol0 + kc + 1]),
                            reads=[b, b_const], writes=[bW])
            CH = min(512, n_tok)
            TT = CH // 128
            xt_r = Ring(sch, es1, nc, tag + "xt", [128, TT, D], F32, 2)
            hn_r = Ring(sch, es1, nc, tag + "hn", [128, TT, D], BF16, 2)
            hT_r = Ring(sch, es1, nc, tag + "hT", [128, 8, CH], BF16, 2)
            st_r = Ring(sch, es1, nc, tag + "st", [128, 8], F32, 2)
            junk = sb1("junk", [128, D], BF16)
            bjunk = sch.buf(tag + "junk")
            sq_r = Ring(sch, es1, nc, tag + "sq", [128, CH], BF16, 2)
            sd_r = Ring(sch, es1, nc, tag + "sd", [128, CH], F32, 2)
            rs_r = Ring(sch, es1, nc, tag + "rs", [128, CH], F32, 2)
            qn_r = Ring(sch, es1, nc, tag + "qn", [128, CH], BF16, 3)
            vo_r = Ring(sch, es1, nc, tag + "vo", [128, 1280], BF16, 2)
            tpb = [es1.enter_context(nc.psum_tensor(f"{tag}tp{i}", [128, 2, 512], BF16)) for i in range(1)]
            tpbuf = [sch.buf(tag + "tp0")]
            pbank = [None] + [es1.enter_context(nc.psum_tensor(f"{tag}pb{i}", [128, 512], F32)) for i in range(1, 8)]
            pbuf = [None] + [sch.buf(f"{tag}pb{i}") for i in range(1, 8)]
            def front1(ck):
                t0 = ck * CH
                xt, bxt = xt_r.next()
                sch.dma("sp", xt[:], src[t0:t0 + CH, :].rearrange("(t p) d -> p t d", p=128), writes=[bxt])
                stt, bst = st_r.next()
                for t in range(TT):
                    sch.op("act", lambda e, t=t: e.activation(out=junk[:], in_=xt[:, t, :], func=AF.Square,
                                                               accum_out=stt[:, t:t + 1]),
                           reads=[bxt], writes=[bjunk, bst])
                sch.op("act", lambda e: e.activation(out=stt[:, 4:4 + TT], in_=stt[:, 0:TT], func=AF.Sqrt,
                                                      bias=EPS, scale=1.0 / D), reads=[bst], writes=[bst])
                sch.op("dve", lambda e: e.reciprocal(out=stt[:, 4:4 + TT], in_=stt[:, 4:4 + TT]),
                       reads=[bst], writes=[bst])
                hn, bhn = hn_r.next()
                for t in range(TT):
                    sch.op("act", lambda e, t=t: e.activation(out=hn[:, t, :], in_=xt[:, t, :], func=AF.Copy,
                                                               scale=stt[:, 4 + t:5 + t]),
                           reads=[bxt, bst], writes=[bhn])
                return hn, bhn

            def front2(hn, bhn):
                hT_, bhT_ = hT_r.next()
                for kc in range(8):
                    bank = 0
                    sl = kc % 2
                    for t in range(TT):
                        sch.op("pe", lambda e, t=t, kc=kc, bank=bank, sl=sl: e.transpose(
                            out=tpb[bank][:, sl, t * 128:(t + 1) * 128], in_=hn[:, t, kc * 128:(kc + 1) * 128],
                            identity=identb[:]), reads=[bhn, b_const], writes=[tpbuf[bank]])
                    if sl == 1:
                        sch.op("dve", lambda e, kc=kc, bank=bank: e.tensor_copy(
                            out=hT_[:, kc - 1:kc + 1, :], in_=tpb[bank][:, :, 0:CH]),
                            reads=[tpbuf[bank]], writes=[bhT_])
                return hT_, bhT_

            NCK = n_tok // CH
            cur_hT = front2(*front1(0))
            for ck in range(NCK):
                t0 = ck * CH
                hT, bhT = cur_hT
                nxt1 = front1(ck + 1) if ck + 1 < NCK else None
                nxt_hT = None
                def qk_mm(j):
                    c0, gc = qk_tiles[j]
                    qb = 1 + (j % 3)
                    for kc in range(8):
                        sch.op("pe", lambda e, kc=kc, c0=c0, qb=qb: e.matmul(
                            pbank[qb][:, 0:CH], W[:, kc, c0:c0 + 128], hT[:, kc, :], start=(kc == 0), stop=(kc == 7)),
                            reads=[bW, bhT], writes=[pbuf[qb]])
                    sq, bsq = sq_r.next()
                    sch.op("act", lambda e, qb=qb, sq=sq: e.activation(out=sq[:], in_=pbank[qb][:, 0:CH], func=AF.Square),
                           reads=[pbuf[qb]], writes=[bsq])
                    return sq, bsq

                def qk_epi(j, sq, bsq):
                    c0, gc = qk_tiles[j]
                    qb = 1 + (j % 3)
                    sbk = 4 + (j % 2)
                    sch.op("pe", lambda e, sbk=sbk, sq=sq: e.matmul(pbank[sbk][:, 0:CH], blk1[:], sq[:], start=True, stop=True),
                           reads=[bsq, b_const], writes=[pbuf[sbk]])
                    sd, bsd = sd_r.next()
                    rs, brs = rs_r.next()
                    if LNEXP:
                        sch.op("act", lambda e, sbk=sbk, sd=sd: e.activation(out=sd[:], in_=pbank[sbk][:, 0:CH], func=AF.Ln,
                                                                             bias=epsb[:, 0:1], scale=1.0 / 64),
                               reads=[pbuf[sbk], b_const], writes=[bsd])
                        sch.op("act", lambda e, sd=sd, rs=rs: e.activation(out=rs[:], in_=sd[:], func=AF.Exp, scale=-0.5),
                               reads=[bsd], writes=[brs])
                    else:
                        sch.op("act", lambda e, sbk=sbk, sd=sd: e.activation(out=sd[:], in_=pbank[sbk][:, 0:CH], func=AF.Sqrt,
                                                                             bias=EPS, scale=1.0 / 64),
                               reads=[pbuf[sbk]], writes=[bsd])
                        sch.op("dve", lambda e, sd=sd, rs=rs: e.reciprocal(out=rs[:], in_=sd[:]), reads=[bsd], writes=[brs])
                    qn, bqn = qn_r.next()
                    sch.op("dve", lambda e, qb=qb, rs=rs, qn=qn, gc=gc: e.scalar_tensor_tensor(
                        out=qn[:], in0=pbank[qb][:, 0:CH], scalar=gn[:, gc:gc + 1], in1=rs[:], op0=ALU.mult, op1=ALU.mult),
                        reads=[pbuf[qb], brs, b_const], writes=[bqn])
                    sch.dma("pool", qk_dst(j, t0, CH), qn[:], reads=[bqn])

                prev = None
                for j in range(len(qk_tiles) + 1):
                    cur_ = qk_mm(j) if j < len(qk_tiles) else None
                    if prev is not None:
                        qk_epi(j - 1, *prev)
                    prev = cur_
                    if j == len(qk_tiles) // 2 and nxt1 is not None and nxt_hT is None:
                        nxt_hT = front2(*nxt1)
                if nxt1 is not None and nxt_hT is None:
                    nxt_hT = front2(*nxt1)
                for t in range(TT):
                    vo, bvo = vo_r.next()
                    for si, (c0, cw, d0) in enumerate(v_segs):
                        vb = 6 + ((t * len(v_segs) + si) % 2)
                        for kc in range(8):
                            sch.op("pe", lambda e, kc=kc, c0=c0, cw=cw, vb=vb, t=t: e.matmul(
                                pbank[vb][:, 0:cw], hT[:, kc, t * 128:(t + 1) * 128], W[:, kc, c0:c0 + cw],
                                start=(kc == 0), stop=(kc == 7)), reads=[bW, bhT], writes=[pbuf[vb]])
                        sch.op("act", lambda e, vb=vb, cw=cw, d0=d0, vo=vo: e.activation(
                            out=vo[:, d0:d0 + cw], in_=pbank[vb][:, 0:cw], func=AF.Copy),
                            reads=[pbuf[vb]], writes=[bvo])
                    vw = sum(s_[1] for s_ in v_segs)
                    sch.dma("pool", v_dst(t0 + t * 128, vw), vo[:, 0:vw], reads=[bvo])
                cur_hT = nxt_hT

        if 1 in phases:
            mark1 = len(sch.all_bufs)
            with ExitStack() as es1:
                projection(es1, x, S, w_in, 4096, 0, QK_TILES,
                           lambda j, t0, n: qk_scr[j, :, t0:t0 + n], V_SEGS,
                           lambda t0, vw: v_scr[t0:t0 + 128, 0:vw], "p1")
                sch.fence_all("sp")
                sch.fence_all("pool")
            with ExitStack() as es1:
                projection(es1, mem, 256, w_mem, 512, 8, [(0, 5), (128, 5)],
                           lambda j, t0, n: km_scr[j, :, t0:t0 + n], ((256, 256, 0),),
                           lambda t0, vw: vm_scr[t0:t0 + 128, 0:vw], "pm")
                sch.fence_all("sp")
                sch.fence_all("pool")


        def attn_pass(tag, qsrc, ksrc, sk, vsrc, vruns, slots, steps_fn, OH, bias, mix_col0, perm=None, raw_dst=None):
            mark_ = len(sch.all_bufs)
            with ExitStack() as e2:
                def sb2(name, shape, dt):
                    return e2.enter_context(nc.sbuf_tensor(tag + name, list(shape), dt))
                nkb = sk // 128
                QT = [sb2(f"QT{i}", [128, S], BF16) for i in range(len(qsrc))]
                KT = [sb2(f"KT{i}", [128, sk], BF16) for i in range(len(ksrc))]
                nv = sum(c for _, c in vruns)
                V1 = sb2("V1", [128, nkb, nv, 65], BF16)
                bin_ = sch.buf(tag + "in")
                SKIP = _os.environ.get("P2SKIP", "")
                tmp_r = None
                if perm is not None and any(d_ > 1 for d_ in perm):
                    tmp_r = Ring(sch, e2, nc, tag + "tmpN", [128, S], BF16, 1)
                pk = 0
                for (dstT, srcs) in ((QT, qsrc), (KT, ksrc)):
                    for i, a in enumerate(srcs):
                        d_ = 1 if perm is None else perm[i]
                        if d_ == 1:
                            for hf in range(2):
                                w_ = a.shape[1] // 2
                                sch.dma("sp", dstT[i][:, hf * w_:(hf + 1) * w_], a[:, hf * w_:(hf + 1) * w_], writes=[bin_], disjoint=True)
                        else:
                            tn, btn = tmp_r.next()
                            sch.dma("sp", tn[:], a, writes=[btn])
                            eng = ("act", "dve")[pk % 2]
                            pk += 1
                            o_ap = dstT[i][:, :].rearrange("p (r u) -> p r u", r=d_)
                            i_ap = tn[:, :].rearrange("p (u r) -> p r u", r=d_)
                            if eng == "act":
                                sch.op("act", lambda e, o_ap=o_ap, i_ap=i_ap: e.activation(out=o_ap, in_=i_ap, func=AF.Copy),
                                       reads=[btn], writes=[bin_])
                            else:
                                sch.op(eng, lambda e, o_ap=o_ap, i_ap=i_ap: e.tensor_copy(out=o_ap, in_=i_ap),
                                       reads=[btn], writes=[bin_])
                s0 = 0
                for ri, (vc0, cnt) in enumerate(vruns):
                    d_ = 1 if perm is None else perm[ri]
                    for c in range(cnt):
                        vcol = vsrc[:, vc0 + c * 64:vc0 + (c + 1) * 64]
                        if d_ == 1:
                            vv = vcol.rearrange("(b p) d -> p b d", p=128)
                            for b0 in range(0, nkb, 16):
                                b1 = min(nkb, b0 + 16)
                                sch.dma("sp", V1[:, b0:b1, s0 + c, 0:64], vv[:, b0:b1, :], writes=[bin_], disjoint=True)
                        else:
                            SB_ = nkb // d_
                            vv = vcol.rearrange("(ub p r) c -> r p ub c", p=128, r=d_)
                            for rho in range(d_):
                                sch.dma("sp", V1[:, rho * SB_:(rho + 1) * SB_, s0 + c, 0:64], vv[rho], writes=[bin_], disjoint=True)
                    s0 += cnt
                if "m" not in SKIP:
                    sch.op("pool", lambda e: e.memset(V1[:, :, :, 64:65], 1.0), writes=[bin_])
                EB = None
                if bias is not None:
                    bd, h0, Hs = bias
                    ntile = bd.shape[0]
                    Hh = Hs // 2
                    EB = sb2("EB", [128, 2, ntile, Hh, 128], BF16)
                    ebs = Ring(sch, e2, nc, tag + "ebs", [128, Hs, 128], F32, 1)
                    for t in range(ntile):
                        st, bst = ebs.next()
                        sch.dma("sp", st[:], bd[t, :, h0:h0 + Hs, :], writes=[bst])
                        sch.op("act", lambda e, st=st, t=t: e.activation(
                            out=EB[:, :, t, :, :], in_=st[:].rearrange("p (i f) q -> p f i q", f=2), func=AF.Exp),
                               reads=[bst], writes=[bin_])
                sps = Ring(sch, e2, nc, tag + "sps", [128, 512], F32, 4, psum=True)
                ops_ = Ring(sch, e2, nc, tag + "ops", [128, 512], F32, 2, psum=True)
                pr = Ring(sch, e2, nc, tag + "P", [128, 512], BF16, 5)
                rd_r = Ring(sch, e2, nc, tag + "rd", [128, 8], F32, 2)
                mx_r = Ring(sch, e2, nc, tag + "mx", [128, 4, OH * 64], BF16, 2) if raw_dst is None else None
                ro_r = Ring(sch, e2, nc, tag + "ro", [128, 512], F32, 2) if raw_dst is not None else None
                mx_state = [None, None]
                recs = []
                for n in range(NT):
                    pend = ([], [])
                    groups = []
                    for (m, tid, sl) in steps_fn(n):
                        for si in sl:
                            hf = slots[si][2]
                            pend[hf].append((m, tid, slots[si]))
                            if len(pend[hf]) == 4:
                                groups.append(list(pend[hf]))
                                pend[hf].clear()
                    for hf in range(2):
                        if pend[hf]:
                            groups.append(list(pend[hf]))
                    for gi, grp in enumerate(groups):
                        recs.append(dict(n=n, grp=grp, first=(gi == 0), last=(gi == len(groups) - 1)))

                def emitA(rcs):
                    for rc in rcs:
                        rc["sp"], rc["bsp"] = sps.next()
                    for i in range(4):
                        for rc in rcs:
                            grp = rc["grp"]
                            if i >= len(grp):
                                continue
                            n = rc["n"]
                            (m, tid, (qt, kt, half, vs, oh, bh)) = grp[i]
                            pl = slice(half * 64, half * 64 + 64)
                            sp_ = rc["sp"]
                            sch.op("pe", lambda e, i=i, m=m, qt=qt, kt=kt, pl=pl, sp_=sp_, n=n: e.matmul(
                                sp_[:, i * 128:(i + 1) * 128], KT[kt][pl, m * 128:(m + 1) * 128],
                                QT[qt][pl, n * 128:(n + 1) * 128], start=True, stop=True),
                                reads=[bin_], writes=[rc["bsp"]])
                    for rc in rcs:
                        grp, sp_, bsp = rc["grp"], rc["sp"], rc["bsp"]
                        w = len(grp) * 128
                        P, bP = pr.next()
                        rc["P"], rc["bP"] = P, bP
                        sch.op("act", lambda e, sp_=sp_, P=P, w=w: e.activation(out=P[:, 0:w], in_=sp_[:, 0:w], func=AF.Exp),
                               reads=[bsp], writes=[bP])
                        if EB is not None:
                            def eoff(job):
                                return (job[2][2] * ntile + job[1]) * Hh + job[2][5] // 2
                            i = 0
                            while i < len(grp):
                                off = eoff(grp[i])
                                j = i + 1
                                while j < len(grp) and eoff(grp[j]) == off + (j - i):
                                    j += 1
                                ebv = EB[:].rearrange("p f t h q -> p (f t h) q")[:, off:off + (j - i), :]
                                pv = P[:, i * 128:j * 128].rearrange("p (a q) -> p a q", q=128)
                                sch.op("dve", lambda e, pv=pv, ebv=ebv: e.tensor_tensor(out=pv, in0=pv, in1=ebv, op=ALU.mult),
                                       reads=[bP, bin_], writes=[bP])
                                i = j

                cur_o = [None, None]

                def emitB(rc):
                    n, grp, P, bP = rc["n"], rc["grp"], rc["P"], rc["bP"]
                    if rc["first"]:
                        cur_o[0], cur_o[1] = ops_.next()
                    oacc, boacc = cur_o
                    for i, (m, tid, (qt, kt, half, vs, oh, bh)) in enumerate(grp):
                        fst = rc["first"] and i == 0
                        lst = rc["last"] and i == len(grp) - 1
                        sch.op("pe", lambda e, i=i, m=m, vs=vs, oh=oh, P=P, oacc=oacc, fst=fst, lst=lst: e.matmul(
                            oacc[:, oh * 65:(oh + 1) * 65], P[:, i * 128:(i + 1) * 128], V1[:, m, vs, :],
                            start=fst, stop=lst, skip_group_check=True),
                            reads=[bP, bin_], writes=[boacc])
                    if not rc["last"]:
                        return
                    if raw_dst is not None:
                        ro, bro = ro_r.next()
                        sch.op("dve", lambda e, ro=ro, oacc=oacc: e.tensor_copy(out=ro[:, 0:OH * 65], in_=oacc[:, 0:OH * 65]),
                               reads=[boacc], writes=[bro])
                        for g_ in range(3):
                            sch.dma("pool", raw_dst(g_, n), ro[:, g_ * 130:(g_ + 1) * 130], reads=[bro])
                        return
                    rd, brd = rd_r.next()
                    ov = oacc[:, 0:OH * 65].rearrange("p (h c) -> p h c", c=65)
                    sch.op("dve", lambda e, rd=rd, ov=ov: e.reciprocal(out=rd[:, 0:OH], in_=ov[:, :, 64]),
                           reads=[boacc], writes=[brd])
                    if n % 4 == 0:
                        mx_state[0], mx_state[1] = mx_r.next()
                    mx, bmx = mx_state
                    for h in range(OH):
                        sch.op("dve", lambda e, h=h, rd=rd, oacc=oacc, mx=mx: e.tensor_scalar(
                            out=mx[:, n % 4, h * 64:(h + 1) * 64], in0=oacc[:, h * 65:h * 65 + 64],
                            scalar1=rd[:, h:h + 1], scalar2=None, op0=ALU.mult),
                            reads=[boacc, brd], writes=[bmx])
                    if n % 4 == 3:
                        r0_ = (n - 3) * 128
                        sch.dma("pool", mix_scr[r0_:r0_ + 512, mix_col0:mix_col0 + OH * 64].rearrange("(t p) c -> p t c", p=128),
                                mx[:], reads=[bmx])

                units = []
                i = 0
                while i < len(recs):
                    if i + 1 < len(recs) and recs[i]["grp"][0][2][2] != recs[i + 1]["grp"][0][2][2]:
                        units.append([recs[i], recs[i + 1]])
                        i += 2
                    else:
                        units.append([recs[i]])
                        i += 1
                LA = 1
                for i in range(len(units) + LA):
                    if i < len(units):
                        emitA(units[i])
                    if i - LA >= 0:
                        for rc in units[i - LA]:
                            emitB(rc)
                sch.fence_all("sp")
                sch.fence_all("pool")
                for en_ in ("act", "dve", "pe"):
                    sch.fence_all(en_)
                sch.release_since(mark_)

        SEL = _os.environ.get("P2SEL", "na0,na1,dl0,dl1,mm").split(",")
        if 2 in phases:
            for ps_ in range(2):
                if f"na{ps_}" not in SEL:
                    continue
                slots = [(h // 2, h // 2, h % 2, h, h, h) for h in range(4)]
                attn_pass(f"na{ps_}", [qk_scr[T_NAQ + 2 * ps_ + i] for i in range(2)],
                          [qk_scr[T_NAK + 2 * ps_ + i] for i in range(2)], S, v_scr, [(ps_ * 256, 4)], slots,
                          lambda n: [(m, tid, [0, 1, 2, 3]) for (m, tid) in na_steps[n]], 4,
                          (na_bias, ps_ * 4, 4), ps_ * 256)
            DD = [d_ for (_, d_) in DIL]
            for ps_ in range(2):
                if f"dl{ps_}" not in SEL:
                    continue
                slots = []
                for g in range(3):
                    for hh in range(2):
                        slots.append((g, g, hh, g * 2 + hh, g * 2 + hh, hh))

                def dsteps(n):
                    st = []
                    for g in range(3):
                        SB_ = NT // DD[g]
                        for o in (-1, 0, 1):
                            m = n + o
                            if 0 <= m < NT and m // SB_ == n // SB_:
                                st.append((m, g * 3 + o + 1, [g * 2, g * 2 + 1]))
                    return st

                def raw_dst(g, n, ps_=ps_):
                    d_ = DD[g]
                    SB_ = NT // d_
                    v = dn_scr[g][:, ps_ * 130:(ps_ + 1) * 130].rearrange("(ub p r) c -> r ub p c", p=128, r=d_)
                    return v[n // SB_, n % SB_]
                attn_pass(f"dl{ps_}", [qk_scr[T_DQ + 2 * g + ps_] for g in range(3)],
                          [qk_scr[T_DK + 2 * g + ps_] for g in range(3)], S, v_scr,
                          [(512 + g * 256 + ps_ * 128, 2) for g in range(3)], slots, dsteps, 6,
                          (dil_bias, ps_ * 2, 2), 0, perm=DD, raw_dst=raw_dst)
            if "dl0" in SEL and "dl1" in SEL:
                mark_c = len(sch.all_bufs)
                with ExitStack() as ec:
                    dn_r = Ring(sch, ec, nc, "dcdn", [128, 3, 260], F32, 3)
                    sm_r = Ring(sch, ec, nc, "dcsm", [128, 260], F32, 2)
                    rc_r = Ring(sch, ec, nc, "dcrc", [128, 4], F32, 2)
                    mo_r = Ring(sch, ec, nc, "dcmo", [128, 256], BF16, 3)
                    for t in range(NT):
                        dn, bdn = dn_r.next()
                        sch.dma("sp", dn[:], dn_scr[:, t * 128:(t + 1) * 128, :].rearrange("g p c -> p g c"), writes=[bdn])
                        sm, bsm = sm_r.next()
                        sch.op("dve", lambda e, dn=dn, sm=sm: e.tensor_tensor(out=sm[:], in0=dn[:, 0, :], in1=dn[:, 1, :], op=ALU.add),
                               reads=[bdn], writes=[bsm])
                        sch.op("dve", lambda e, dn=dn, sm=sm: e.tensor_tensor(out=sm[:], in0=sm[:], in1=dn[:, 2, :], op=ALU.add),
                               reads=[bdn, bsm], writes=[bsm])
                        rcp, brc = rc_r.next()
                        smv = sm[:].rearrange("p (h c) -> p h c", c=65)
                        sch.op("dve", lambda e, rcp=rcp, smv=smv: e.reciprocal(out=rcp[:], in_=smv[:, :, 64]), reads=[bsm], writes=[brc])
                        mo, bmo = mo_r.next()
                        for h in range(4):
                            sch.op("act", lambda e, h=h, mo=mo, sm=sm, rcp=rcp: e.activation(
                                out=mo[:, h * 64:(h + 1) * 64], in_=sm[:, h * 65:h * 65 + 64], func=AF.Copy, scale=rcp[:, h:h + 1]),
                                reads=[bsm, brc], writes=[bmo])
                        sch.dma("pool", mix_scr[t * 128:(t + 1) * 128, 512:768], mo[:], reads=[bmo])
                    for en_ in ("sp", "pool", "act", "dve", "pe"):
                        sch.fence_all(en_)
                    sch.release_since(mark_c)
            slots = [(h // 2, h // 2, h % 2, h, h, 0) for h in range(4)]
            if "mm" in SEL:
              attn_pass("mm", [qk_scr[T_MQ + i] for i in range(2)], [km_scr[i] for i in range(2)], 256, vm_scr,
                      [(0, 4)], slots, lambda n: [(0, None, [0, 1, 2, 3]), (1, None, [0, 1, 2, 3])], 4, None, 768)


        g1 = sb("g1", [128, NT], F32)
        g2 = sb("g2", [128, NT], F32)
        dst1i = sb("dst1i", [128, NT], I32)
        dst2i = sb("dst2i", [128, NT], I32)
        widx = sb("widx", [128, NBLK * 2], I32)
        bRt = sch.buf("routeout")
        if 3 in phases:
            with ExitStack() as e3:
                cur = [e3]
                def sb3(name, shape, dt):
                    return cur[0].enter_context(nc.sbuf_tensor("p3" + name, list(shape), dt))
                L = sb3("L", [128, NT, 36], F32)
                bL = sch.buf("L")
                e3m = ExitStack()
                e3m.__enter__()
                cur[0] = e3m
                Wo = sb3("Wo", [128, 8, D], BF16)
                bWo = sch.buf("Wo")
                wst = Ring(sch, cur[0], nc, "p3wst", [128, D], F32, 2)
                for kc in range(8):
                    t, b = wst.next()
                    sch.dma("sp", t[:], w_out[kc * 128:(kc + 1) * 128, :], writes=[b])
                    sch.op("dve", lambda e, t=t, kc=kc: e.tensor_copy(out=Wo[:, kc, :], in_=t[:]), reads=[b], writes=[bWo])
                wr = sb3("wr", [128, 8, 36], F32)
                br = sb3("br", [128, 36], F32)
                gB = sb3("gB", [128, D], F32)
                bwr = sch.buf("wr")
                sch.dma("sp", wr[:], w_r.rearrange("(kc p) n -> p kc n", p=128), writes=[bwr])
                sch.dma("sp", br[:], b_r[:, :], writes=[bwr])
                sch.dma("sp", gB[:], gffn_b[:, :], writes=[bwr])
                for kc in range(8):
                    sch.op("dve", lambda e, kc=kc: e.tensor_scalar(out=wr[:, kc, :], in0=wr[:, kc, :],
                                                                    scalar1=gv[:, 16 + kc:17 + kc], scalar2=None, op0=ALU.mult),
                           reads=[bwr, b_const], writes=[bwr])
                wrh = sb3("wrh", [128, 8, 36], BF16)
                wrl = sb3("wrl", [128, 8, 36], BF16)
                sch.op("dve", lambda e: e.tensor_copy(out=wrh[:], in_=wr[:]), reads=[bwr], writes=[bwr])
                sch.op("dve", lambda e: e.tensor_tensor(out=wrl[:], in0=wr[:], in1=wrh[:], op=ALU.subtract), reads=[bwr], writes=[bwr])
                mx_r = Ring(sch, cur[0], nc, "p3mx", [128, D], BF16, 2)
                xt_r = Ring(sch, cur[0], nc, "p3xt", [128, D], F32, 2)
                mT_r = Ring(sch, cur[0], nc, "p3mT", [128, 8, 128], BF16, 2)
                x1_r = Ring(sch, cur[0], nc, "p3x1", [128, D], F32, 2)
                hn_r = Ring(sch, cur[0], nc, "p3hn", [128, D], F32, 2)
                hi_r = Ring(sch, cur[0], nc, "p3hi", [128, D], BF16, 3)
                lo_r = Ring(sch, cur[0], nc, "p3lo", [128, D], BF16, 3)
                hg_r = Ring(sch, cur[0], nc, "p3hg", [128, D], BF16, 2)
                hiT_r = Ring(sch, cur[0], nc, "p3hiT", [128, 8, 128], BF16, 2)
                loT_r = Ring(sch, cur[0], nc, "p3loT", [128, 8, 128], BF16, 2)
                st_r = Ring(sch, cur[0], nc, "p3st", [128, 4], F32, 2)
                junk = sb3("junk", [128, D], BF16)
                bjunk = sch.buf("p3junk")
                with ExitStack() as e3p:
                    tpm = e3p.enter_context(nc.psum_tensor("p3tpm", [128, 8, 128], BF16))
                    btpm = sch.buf("p3tpm")
                    yps = Ring(sch, e3p, nc, "p3yps", [128, 512], F32, 4, psum=True)
                    tph = e3p.enter_context(nc.psum_tensor("p3tph", [128, 8, 128], BF16))
                    tpl = e3p.enter_context(nc.psum_tensor("p3tpl", [128, 8, 128], BF16))
                    btph, btpl = sch.buf("p3tph"), sch.buf("p3tpl")
                    lps = e3p.enter_context(nc.psum_tensor("p3lps", [128, 512], F32))
                    blps = sch.buf("p3lps")
                    def p3A(n):
                        r0_ = n * 128
                        mxt, bmx = mx_r.next()
                        xt, bxt = xt_r.next()
                        sch.dma("sp", mxt[:], mix_scr[r0_:r0_ + 128, :], writes=[bmx])
                        sch.dma("sp", xt[:], x[r0_:r0_ + 128, :], writes=[bxt])
                        for kc in range(8):
                            sch.op("pe", lambda e, kc=kc, mxt=mxt: e.transpose(out=tpm[:, kc, :], in_=mxt[:, kc * 128:(kc + 1) * 128],
                                                                      identity=identb[:]), reads=[bmx, b_const], writes=[btpm])
                        mT, bmT = mT_r.next()
                        sch.op("dve", lambda e, mT=mT: e.tensor_copy(out=mT[:], in_=tpm[:]), reads=[btpm], writes=[bmT])
                        x1, bx1 = x1_r.next()
                        for hf in range(2):
                            yp, byp = yps.next()
                            for kc in range(8):
                                sch.op("pe", lambda e, kc=kc, hf=hf, yp=yp, mT=mT: e.matmul(
                                    yp[:], mT[:, kc, :], Wo[:, kc, hf * 512:(hf + 1) * 512], start=(kc == 0), stop=(kc == 7)),
                                    reads=[bmT, bWo], writes=[byp])
                            sch.op("dve", lambda e, hf=hf, yp=yp, x1=x1, xt=xt: e.tensor_tensor(
                                out=x1[:, hf * 512:(hf + 1) * 512], in0=yp[:], in1=xt[:, hf * 512:(hf + 1) * 512], op=ALU.add),
                                reads=[byp, bxt], writes=[bx1])
                        sch.dma("pool", out[r0_:r0_ + 128, :], x1[:], reads=[bx1])
                        stt, bst = st_r.next()
                        sch.op("act", lambda e, x1=x1, stt=stt: e.activation(out=junk[:], in_=x1[:], func=AF.Square,
                                                                           accum_out=stt[:, 0:1]), reads=[bx1], writes=[bjunk, bst])
                        sch.op("act", lambda e, stt=stt: e.activation(out=stt[:, 1:2], in_=stt[:, 0:1], func=AF.Sqrt,
                                                                       bias=EPS, scale=1.0 / D), reads=[bst], writes=[bst])
                        sch.op("dve", lambda e, stt=stt: e.reciprocal(out=stt[:, 1:2], in_=stt[:, 1:2]), reads=[bst], writes=[bst])
                        hn, bhn = hn_r.next()
                        sch.op("act", lambda e, hn=hn, x1=x1, stt=stt: e.activation(out=hn[:], in_=x1[:], func=AF.Copy,
                                                                                   scale=stt[:, 1:2]), reads=[bx1, bst], writes=[bhn])
                        hi, bhi = hi_r.next()
                        lo, blo = lo_r.next()
                        hg, bhg = hg_r.next()
                        sch.op("act", lambda e, hi=hi, hn=hn: e.activation(out=hi[:], in_=hn[:], func=AF.Copy), reads=[bhn], writes=[bhi])
                        sch.op("dve", lambda e, hi=hi, lo=lo, hn=hn: e.tensor_tensor(out=lo[:], in0=hn[:], in1=hi[:], op=ALU.subtract),
                               reads=[bhn, bhi], writes=[blo])
                        sch.op("pool", lambda e, hg=hg, hn=hn: e.tensor_tensor(out=hg[:], in0=hn[:], in1=gB[:], op=ALU.mult),
                               reads=[bhn, bwr], writes=[bhg])
                        sch.dma("pool", h2_scr[r0_:r0_ + 128, :], hg[:], reads=[bhg])
                        return hi, bhi, lo, blo

                    def p3B(n, hi, bhi, lo, blo):
                        for kc in range(8):
                            sch.op("pe", lambda e, kc=kc, hi=hi: e.transpose(out=tph[:, kc, :], in_=hi[:, kc * 128:(kc + 1) * 128],
                                                                            identity=identb[:]), reads=[bhi, b_const], writes=[btph])
                        for kc in range(8):
                            sch.op("pe", lambda e, kc=kc, lo=lo: e.transpose(out=tpl[:, kc, :], in_=lo[:, kc * 128:(kc + 1) * 128],
                                                                            identity=identb[:]), reads=[blo, b_const], writes=[btpl])
                        hiT, bhiT = hiT_r.next()
                        loT, bloT = loT_r.next()
                        sch.op("act", lambda e, hiT=hiT: e.activation(out=hiT[:], in_=tph[:], func=AF.Copy), reads=[btph], writes=[bhiT])
                        sch.op("act", lambda e, loT=loT: e.activation(out=loT[:], in_=tpl[:], func=AF.Copy), reads=[btpl], writes=[bloT])
                        k = 0
                        for (aa, ba, ww) in ((hiT, bhiT, wrh), (hiT, bhiT, wrl), (loT, bloT, wrh)):
                            for kc in range(8):
                                sch.op("pe", lambda e, kc=kc, aa=aa, ww=ww, k=k: e.matmul(lps[:, 0:36], aa[:, kc, :], ww[:, kc, :],
                                                                                     start=(k == 0), stop=(k == 23)),
                                       reads=[ba, bwr], writes=[blps])
                                k += 1
                        sch.op("dve", lambda e, n=n: e.tensor_tensor(out=L[:, n, :], in0=lps[:, 0:36], in1=br[:], op=ALU.add),
                               reads=[blps, bwr], writes=[bL])


                    prev3 = None
                    for n in range(NT + 1):
                        cur3 = p3A(n) if n < NT else None
                        if prev3 is not None:
                            p3B(n - 1, *prev3)
                        prev3 = cur3
                e3m.close()
                cur[0] = e3
                def sbr(name, shape, dt=F32):
                    return e3.enter_context(nc.sbuf_tensor("rt" + name, list(shape), dt))
                bR = sch.buf("route")
                gl = L[:, :, 0:4]
                fl = L[:, :, 4:36]
                gmax = sbr("gmax", [128, NT]); G1 = sbr("G1", [128, NT, 4]); ge = sbr("ge", [128, NT, 4])
                gsum = sbr("gsum", [128, NT]); pen = sbr("pen", [128, NT, 4]); flm = sbr("flm", [128, NT, 32])
                m1 = sbr("m1", [128, NT]); oh1 = sbr("oh1", [128, NT, 32]); m2 = sbr("m2", [128, NT])
                oh2 = sbr("oh2", [128, NT, 32]); dd = sbr("dd", [128, NT])

                def R(eng, fn, extra_w=()):
                    sch.op(eng, fn, reads=[bL, bR, b_const], writes=[bR] + list(extra_w))

                def bc(a, n):
                    return a[:, :].unsqueeze(2).broadcast_to([128, NT, n])
                R("dve", lambda e: e.tensor_reduce(out=gmax[:], in_=gl, axis=AX.X, op=ALU.max))
                R("dve", lambda e: e.tensor_tensor(out=G1[:], in0=gl, in1=bc(gmax, 4), op=ALU.is_ge))
                R("dve", lambda e: e.tensor_tensor(out=ge[:], in0=gl, in1=bc(gmax, 4), op=ALU.subtract))
                R("act", lambda e: e.activation(out=ge[:], in_=ge[:], func=AF.Exp))
                R("dve", lambda e: e.tensor_reduce(out=gsum[:], in_=ge[:], axis=AX.X, op=ALU.add))
                R("dve", lambda e: e.reciprocal(out=gsum[:], in_=gsum[:]))
                R("dve", lambda e: e.tensor_scalar(out=pen[:], in0=G1[:], scalar1=1e9, scalar2=-1e9, op0=ALU.mult, op1=ALU.add))
                R("dve", lambda e: e.tensor_tensor(
                    out=flm[:].rearrange("p t (g e) -> p t g e", e=8), in0=fl.rearrange("p t (g e) -> p t g e", e=8),
                    in1=pen[:].unsqueeze(3).broadcast_to([128, NT, 4, 8]), op=ALU.add))
                R("dve", lambda e: e.tensor_reduce(out=m1[:], in_=flm[:], axis=AX.X, op=ALU.max))
                R("dve", lambda e: e.tensor_tensor(out=oh1[:], in0=flm[:], in1=bc(m1, 32), op=ALU.is_ge))
                R("dve", lambda e: e.scalar_tensor_tensor(out=flm[:], in0=oh1[:], scalar=-1e9, in1=flm[:], op0=ALU.mult, op1=ALU.add))
                R("dve", lambda e: e.tensor_reduce(out=m2[:], in_=flm[:], axis=AX.X, op=ALU.max))
                R("dve", lambda e: e.tensor_tensor(out=oh2[:], in0=flm[:], in1=bc(m2, 32), op=ALU.is_ge))
                R("dve", lambda e: e.tensor_tensor(out=dd[:], in0=m2[:], in1=m1[:], op=ALU.subtract))
                R("act", lambda e: e.activation(out=dd[:], in_=dd[:], func=AF.Exp))
                R("dve", lambda e: e.tensor_scalar(out=g1[:], in0=dd[:], scalar1=1.0, scalar2=None, op0=ALU.add), [bRt])
                R("dve", lambda e: e.reciprocal(out=g1[:], in_=g1[:]), [bRt])
                R("dve", lambda e: e.tensor_tensor(out=g1[:], in0=g1[:], in1=gsum[:], op=ALU.mult), [bRt])
                R("dve", lambda e: e.tensor_tensor(out=g2[:], in0=g1[:], in1=dd[:], op=ALU.mult), [bRt])
                selb = sbr("selb", [128, NT * 32], BF16)
                trif = sbr("trif", [128, 128]); trib = sbr("trib", [128, 128], BF16); oneb = sbr("oneb", [128, 128], BF16)
                iot = sbr("iot", [128, 256]); pid2 = sbr("pid2", [128, 1])
                sch.dma("sp", trif[:], tri_in[:, :], writes=[bR])
                sch.dma("sp", iot[:], iota_in[:, :], writes=[bR])
                sch.dma("sp", pid2[:], pidx_in[:, :], writes=[bR])
                R("dve", lambda e: e.tensor_copy(out=trib[:], in_=trif[:]))
                R("dve", lambda e: e.memset(oneb[:], 1.0))
                R("dve", lambda e: e.tensor_scalar(out=pid2[:], in0=pid2[:], scalar1=2.0, scalar2=None, op0=ALU.mult))
                R("dve", lambda e: e.tensor_tensor(out=selb[:], in0=oh1[:].rearrange("p t e -> p (t e)"),
                                                   in1=oh2[:].rearrange("p t e -> p (t e)"), op=ALU.add))
                Cs = sbr("Cs", [128, NT, 32]); Ta = sbr("Ta", [128, NT, 32]); Tb = sbr("Tb", [128, NT, 32]); T0 = sbr("T0", [128, NT, 32])
                with ExitStack() as e3r:
                    cps = [e3r.enter_context(nc.psum_tensor(f"rtc{j}", [128, 512], F32)) for j in range(4)]
                    tps = [e3r.enter_context(nc.psum_tensor(f"rtt{j}", [128, 512], F32)) for j in range(4)]
                    bcp, btp = sch.buf("rtc"), sch.buf("rtt")
                    Cf = Cs[:].rearrange("p t e -> p (t e)")
                    T0f = T0[:].rearrange("p t e -> p (t e)")
                    for j in range(4):
                        sch.op("pe", lambda e, j=j: e.matmul(cps[j][:], trib[:], selb[:, j * 512:(j + 1) * 512], start=True, stop=True),
                               reads=[bR], writes=[bcp])
                        sch.op("pe", lambda e, j=j: e.matmul(tps[j][:], oneb[:], selb[:, j * 512:(j + 1) * 512], start=True, stop=True),
                               reads=[bR], writes=[btp])
                    for j in range(4):
                        sch.op("act", lambda e, j=j: e.activation(out=Cf[:, j * 512:(j + 1) * 512], in_=cps[j][:], func=AF.Copy),
                               reads=[bcp], writes=[bR])
                        sch.op("dve", lambda e, j=j: e.tensor_copy(out=T0f[:, j * 512:(j + 1) * 512], in_=tps[j][:]),
                               reads=[btp], writes=[bR])
                src, dstb = T0, Ta
                for sft in (1, 2, 4, 8, 16, 32):
                    R("dve", lambda e, src=src, dstb=dstb, sft=sft: e.tensor_copy(out=dstb[:, 0:sft, :], in_=src[:, 0:sft, :]))
                    R("dve", lambda e, src=src, dstb=dstb, sft=sft: e.tensor_tensor(
                        out=dstb[:, sft:NT, :], in0=src[:, sft:NT, :], in1=src[:, 0:NT - sft, :], op=ALU.add))
                    src, dstb = dstb, (Tb if dstb is Ta else Ta)
                Inc = src
                cnt = sbr("cnt", [128, 32]); nbk = sbr("nbk", [128, 32]); cmp1 = sbr("cmp1", [128, 32, 128])
                i128 = sbr("i128", [128, 128]); sa = sbr("sa", [128, 32]); sb_ = sbr("sb_", [128, 32])
                psr = sbr("psr", [128, 32]); pend = sbr("pend", [128, 32])
                R("dve", lambda e: e.tensor_copy(out=cnt[:], in_=Inc[:, NT - 1, :]))
                R("dve", lambda e: e.tensor_scalar(out=i128[:], in0=iot[:, 0:128], scalar1=float(MOE_B), scalar2=None, op0=ALU.mult))
                R("dve", lambda e: e.tensor_tensor(out=cmp1[:], in0=cnt[:, :].unsqueeze(2).broadcast_to([128, 32, 128]),
                                                   in1=i128[:, :].unsqueeze(1).broadcast_to([128, 32, 128]), op=ALU.is_gt))
                R("dve", lambda e: e.tensor_reduce(out=nbk[:], in_=cmp1[:], axis=AX.X, op=ALU.add))
                src, dstb = nbk, sa
                for sft in (1, 2, 4, 8, 16):
                    R("dve", lambda e, src=src, dstb=dstb, sft=sft: e.tensor_copy(out=dstb[:, 0:sft], in_=src[:, 0:sft]))
                    R("dve", lambda e, src=src, dstb=dstb, sft=sft: e.tensor_tensor(
                        out=dstb[:, sft:32], in0=src[:, sft:32], in1=src[:, 0:32 - sft], op=ALU.add))
                    src, dstb = dstb, (sb_ if dstb is sa else sa)
                R("dve", lambda e, src=src: e.tensor_copy(out=pend[:], in_=src[:]))
                R("dve", lambda e: e.tensor_tensor(out=psr[:], in0=pend[:], in1=nbk[:], op=ALU.subtract))
                R("dve", lambda e: e.tensor_scalar(out=psr[:], in0=psr[:], scalar1=float(MOE_B), scalar2=None, op0=ALU.mult))
                R("dve", lambda e, Inc=Inc: e.tensor_tensor(out=Cs[:], in0=Cs[:], in1=Inc[:], op=ALU.add))
                R("dve", lambda e: e.tensor_tensor(out=Cs[:], in0=Cs[:], in1=T0[:], op=ALU.subtract))
                R("dve", lambda e: e.tensor_tensor(out=Cs[:], in0=Cs[:], in1=psr[:, :].unsqueeze(1).broadcast_to([128, NT, 32]), op=ALU.add))
                d1 = sbr("d1", [128, NT]); d2 = sbr("d2", [128, NT])
                R("dve", lambda e: e.tensor_tensor(out=Ta[:], in0=Cs[:], in1=oh1[:], op=ALU.mult))
                R("dve", lambda e: e.tensor_reduce(out=d1[:], in_=Ta[:], axis=AX.X, op=ALU.add))
                R("dve", lambda e: e.tensor_tensor(out=Tb[:], in0=Cs[:], in1=oh2[:], op=ALU.mult))
                R("dve", lambda e: e.tensor_reduce(out=d2[:], in_=Tb[:], axis=AX.X, op=ALU.add))
                R("dve", lambda e: e.tensor_copy(out=dst1i[:], in_=d1[:]), [bRt])
                R("dve", lambda e: e.tensor_copy(out=dst2i[:], in_=d2[:]), [bRt])
                cmp2 = sbr("cmp2", [128, NBLK, 32]); bex = sbr("bex", [128, NBLK]); need = sbr("need", [128, NBLK])
                w0 = sbr("w0", [128, NBLK]); w1f = sbr("w1f", [128, NBLK, 2])
                R("dve", lambda e: e.tensor_tensor(out=cmp2[:], in0=iot[:, 0:NBLK].unsqueeze(2).broadcast_to([128, NBLK, 32]),
                                                   in1=pend[:, :].unsqueeze(1).broadcast_to([128, NBLK, 32]), op=ALU.is_ge))
                R("dve", lambda e: e.tensor_reduce(out=bex[:], in_=cmp2[:], axis=AX.X, op=ALU.add))
                R("dve", lambda e: e.tensor_scalar(out=bex[:], in0=bex[:], scalar1=float(N_EXP - 1), scalar2=None, op0=ALU.min))
                R("dve", lambda e: e.memset(need[:], 1.0))
                if WSKIP:
                    R("dve", lambda e: e.tensor_tensor(out=need[:, NSET:NBLK], in0=bex[:, NSET:NBLK], in1=bex[:, 0:NBLK - NSET], op=ALU.not_equal))
                R("dve", lambda e: e.tensor_scalar(out=w0[:], in0=bex[:], scalar1=256.0, scalar2=pid2[:, 0:1], op0=ALU.mult, op1=ALU.add))
                R("dve", lambda e: e.tensor_tensor(out=w0[:], in0=w0[:], in1=need[:], op=ALU.mult))
                R("dve", lambda e: e.tensor_scalar(out=need[:], in0=need[:], scalar1=-float(1 << 30), scalar2=float(1 << 30),
                                                   op0=ALU.mult, op1=ALU.add))
                R("dve", lambda e: e.tensor_tensor(out=w0[:], in0=w0[:], in1=need[:], op=ALU.add))
                R("dve", lambda e: e.tensor_copy(out=w1f[:, :, 0], in_=w0[:]))
                R("dve", lambda e: e.tensor_scalar(out=w1f[:, :, 1], in0=w0[:], scalar1=1.0, scalar2=None, op0=ALU.add))
                R("dve", lambda e: e.tensor_copy(out=widx[:], in_=w1f[:].rearrange("p b h -> p (b h)")), [bRt])
                if debug:
                    dbg = sbr("dbg", [128, 4 * NT + 2 * NBLK])
                    R("dve", lambda e: e.tensor_copy(out=dbg[:, 0:NT], in_=d1[:]))
                    R("dve", lambda e: e.tensor_copy(out=dbg[:, NT:2 * NT], in_=d2[:]))
                    R("dve", lambda e: e.tensor_copy(out=dbg[:, 2 * NT:3 * NT], in_=g1[:]))
                    R("dve", lambda e: e.tensor_copy(out=dbg[:, 3 * NT:4 * NT], in_=g2[:]))
                    R("dve", lambda e: e.tensor_copy(out=dbg[:, 4 * NT:4 * NT + NBLK], in_=bex[:]))
                    R("dve", lambda e: e.tensor_copy(out=dbg[:, 4 * NT + NBLK:4 * NT + 2 * NBLK], in_=w0[:]))
                    sch.dma("sp", dbg_out[:, :], dbg[:], reads=[bR])
                sch.fence_all("sp")
                sch.fence_all("pool")
                hs_r = Ring(sch, e3, nc, "p3hs", [128, D], BF16, 4)
                for n in range(NT):
                    hs, bhs = hs_r.next()
                    sch.dma("sp", hs[:], h2_scr[n * 128:(n + 1) * 128, :], writes=[bhs])
                    for dsti in (dst1i, dst2i):
                        sch.dma("pool", xs_scr[:, :], hs[:], reads=[bhs, bRt],
                                indirect=dict(out_offset=bass.IndirectOffsetOnAxis(dsti[:, n:n + 1], 0), in_offset=None))
                sch.fence_all("sp")
                sch.fence_all("pool")

        if 4 in phases:
            with ExitStack() as e4:
                def sb4(name, shape, dt):
                    return e4.enter_context(nc.sbuf_tensor("p4" + name, list(shape), dt))
                SUB = MOE_B // 128
                Wb = [[sb4(f"W{i}_{s_}", [128, 4096], BF16) for s_ in range(NSET)] for i in range(3)]
                bWb = [[[sch.buf(f"p4W{i}_{s_}_{h}") for h in range(2)] for s_ in range(NSET)] for i in range(3)]
                wsrc = (w1, w3, w2)
                bnd_reg = nc.gpsimd.alloc_register("wbnd")
                nc.gpsimd.reg_mov(bnd_reg, N_EXP * 256 - 1)
                xs_r = Ring(sch, e4, nc, "p4xs", [128, D], BF16, 3)
                xT_r = Ring(sch, e4, nc, "p4xT", [128, 8, 128], BF16, 3)
                sg_r = Ring(sch, e4, nc, "p4sg", [128, 512], BF16, 2)
                am_r = Ring(sch, e4, nc, "p4am", [128, 512], BF16, 2)
                aT_r = Ring(sch, e4, nc, "p4aT", [128, 4, 128], BF16, 2 * SUB + 1)
                ys_r = Ring(sch, e4, nc, "p4ys", [128, D], F32, 3)
                with ExitStack() as e4p:
                    tpx = e4p.enter_context(nc.psum_tensor("p4tpx", [128, 8, 128], BF16))
                    btpx = sch.buf("p4tpx")
                    a1p = Ring(sch, e4p, nc, "p4a1", [128, 512], F32, 2, psum=True)
                    a3p = Ring(sch, e4p, nc, "p4a3", [128, 512], F32, 2, psum=True)
                    ypp = Ring(sch, e4p, nc, "p4yp", [128, 512], F32, 2, psum=True)
                    tpa = e4p.enter_context(nc.psum_tensor("p4tpa", [128, 4, 128], BF16))
                    btpa = sch.buf("p4tpa")

                    def stageX(b):
                        st_ = b % NSET
                        for i in range(3):
                            for hf in range(2):
                                sch.dma("pool", Wb[i][st_][:, hf * 2048:(hf + 1) * 2048], wsrc[i][:, :], reads=[bRt], writes=[bWb[i][st_][hf]],
                                        indirect=dict(out_offset=None, in_offset=bass.IndirectOffsetOnAxis(widx[:, 2 * b + hf:2 * b + hf + 1], 0),
                                                      bounds_check=bnd_reg, oob_is_err=False))
                        res = []
                        mids = []
                        for sb_ in range(SUB):
                            r0_ = b * MOE_B + sb_ * 128
                            xs, bxs = xs_r.next()
                            sch.dma("sp", xs[:], xs_scr[r0_:r0_ + 128, :], writes=[bxs])
                            for kc in range(8):
                                sch.op("pe", lambda e, kc=kc, xs=xs: e.transpose(out=tpx[:, kc, :], in_=xs[:, kc * 128:(kc + 1) * 128],
                                                                                identity=identb[:]), reads=[bxs, b_const], writes=[btpx])
                            xT, bxT = xT_r.next()
                            sch.op("dve", lambda e, xT=xT: e.tensor_copy(out=xT[:], in_=tpx[:]), reads=[btpx], writes=[bxT])
                            a1, ba1 = a1p.next()
                            a3, ba3 = a3p.next()
                            for (ap_, bap, Wx, bWx) in ((a1, ba1, Wb[0][st_], bWb[0][st_]), (a3, ba3, Wb[1][st_], bWb[1][st_])):
                                for kc in range(8):
                                    sch.op("pe", lambda e, kc=kc, ap_=ap_, Wx=Wx, xT=xT: e.matmul(
                                        ap_[:], xT[:, kc, :], Wx[:, kc * 512:(kc + 1) * 512],
                                        start=(kc == 0), stop=(kc == 7)), reads=[bWx[kc // 4], bxT], writes=[bap])
                            sg, bsg = sg_r.next()
                            sch.op("act", lambda e, a1=a1, sg=sg: e.activation(out=sg[:], in_=a1[:], func=AF.Silu), reads=[ba1], writes=[bsg])
                            am, bam = am_r.next()
                            sch.op("dve", lambda e, am=am, a3=a3, sg=sg: e.tensor_tensor(out=am[:], in0=a3[:], in1=sg[:], op=ALU.mult),
                                   reads=[ba3, bsg], writes=[bam])
                            mids.append((am, bam))
                        for (am, bam) in mids:
                            for nch in range(4):
                                sch.op("pe", lambda e, nch=nch, am=am: e.transpose(out=tpa[:, nch, :], in_=am[:, nch * 128:(nch + 1) * 128],
                                                                                  identity=identb[:]), reads=[bam, b_const], writes=[btpa])
                            aT, baT = aT_r.next()
                            sch.op("act", lambda e, aT=aT: e.activation(out=aT[:], in_=tpa[:], func=AF.Copy), reads=[btpa], writes=[baT])
                            res.append((aT, baT))
                        return res

                    def stageY(b, res):
                        st_ = b % NSET
                        W2b = Wb[2][st_]
                        for sb_, (aT, baT) in enumerate(res):
                            r0_ = b * MOE_B + sb_ * 128
                            ys, bys = ys_r.next()
                            for hf in range(2):
                                yp, byp = ypp.next()
                                for nch in range(4):
                                    sch.op("pe", lambda e, nch=nch, hf=hf, yp=yp, aT=aT, W2b=W2b: e.matmul(
                                        yp[:], aT[:, nch, :], W2b[:, nch * 1024 + hf * 512:nch * 1024 + (hf + 1) * 512],
                                        start=(nch == 0), stop=(nch == 3)), reads=[baT, bWb[2][st_][nch // 2]], writes=[byp])
                                sch.op("act", lambda e, hf=hf, yp=yp, ys=ys: e.activation(out=ys[:, hf * 512:(hf + 1) * 512], in_=yp[:], func=AF.Copy),
                                       reads=[byp], writes=[bys])
                            sch.dma("act", yb_scr[r0_:r0_ + 128, :], ys[:], reads=[bys])

                    prevx = None
                    for b in range(NBLK + 1):
                        curx = stageX(b) if b < NBLK else None
                        if prevx is not None:
                            stageY(b - 1, prevx)
                        prevx = curx
                sch.fence_all("sp")
                sch.fence_all("pool")
                sch.fence_all("act")
                y1_r = Ring(sch, e4, nc, "p4y1", [128, D], F32, 3)
                y2_r = Ring(sch, e4, nc, "p4y2", [128, D], F32, 3)
                xo_r = Ring(sch, e4, nc, "p4xo", [128, D], F32, 3)
                for n in range(NT):
                    y1, by1 = y1_r.next()
                    y2, by2 = y2_r.next()
                    xo, bxo = xo_r.next()
                    sch.dma("pool", y1[:], yb_scr[:, :], reads=[bRt], writes=[by1],
                            indirect=dict(out_offset=None, in_offset=bass.IndirectOffsetOnAxis(dst1i[:, n:n + 1], 0)))
                    sch.dma("pool", y2[:], yb_scr[:, :], reads=[bRt], writes=[by2],
                            indirect=dict(out_offset=None, in_offset=bass.IndirectOffsetOnAxis(dst2i[:, n:n + 1], 0)))
                    sch.dma("sp", xo[:], out[n * 128:(n + 1) * 128, :], writes=[bxo])
                    sch.op("dve", lambda e, n=n, y1=y1, xo=xo: e.scalar_tensor_tensor(
                        out=xo[:], in0=y1[:], scalar=g1[:, n:n + 1], in1=xo[:], op0=ALU.mult, op1=ALU.add),
                        reads=[by1, bxo, bRt], writes=[bxo])
                    sch.op("dve", lambda e, n=n, y2=y2, xo=xo: e.scalar_tensor_tensor(
                        out=xo[:], in0=y2[:], scalar=g2[:, n:n + 1], in1=xo[:], op0=ALU.mult, op1=ALU.add),
                        reads=[by2, bxo, bRt], writes=[bxo])
                    sch.dma("act", out[n * 128:(n + 1) * 128, :], xo[:], reads=[bxo])
                sch.fence_all("sp")
                sch.fence_all("pool")
                sch.fence_all("act")

        for en in ("sp", "pool", "act", "dve", "pe"):
            sch.fence_all(en)
        if _os.environ.get("DRYPRINT"):
            print("counts", {k: v.count for k, v in sch.engs.items()}, "nsem", sch.nsem)
    return nc


_CACHE = {}


def kernel(x, mem, g_mix, w_in, qk_gain, na_rpb, t5_table, g_mem, w_mem_kv, w_out, g_ffn, w_r1, b_r1, w_r2, b_r2,
           w1, w3, w2, _debug=False, _phases=(1, 2, 3, 4), _cores=8):
    f32 = np.float32
    x = np.asarray(x, f32); mem = np.asarray(mem, f32)
    na_steps, na_keys = na_plan()
    dil_tl = dil_plan()
    nab = na_bias_tiles(np.asarray(na_rpb, f32)[0], na_keys)
    dlb = dil_res_bias_tiles(np.asarray(t5_table, f32))
    key = (len(na_keys), len(dil_tl), _debug, tuple(_phases))
    if key not in _CACHE:
        _CACHE[key] = build_program(len(na_keys), len(dil_tl), na_steps, dil_tl, debug=_debug, phases=_phases)
    nc = _CACHE[key]

    def pk(v):
        return np.asarray(v, f32).reshape(8, 128).T
    gvec = np.ascontiguousarray(np.concatenate([pk(g_mix[0]), pk(g_mem[0]), pk(g_ffn[0])], axis=1))
    qg = np.asarray(qk_gain, f32)[0]
    gains = np.ascontiguousarray(np.tile(qg.reshape(6, 64), (1, 2)).T)
    shared = {
        "w_in": np.ascontiguousarray(np.asarray(w_in, f32)[0]),
        "w_mem": np.ascontiguousarray(np.asarray(w_mem_kv, f32)[0]),
        "w_out": np.ascontiguousarray(np.asarray(w_out, f32)[0]),
        "gvec": gvec, "gains": gains,
        "gffn_b": np.ascontiguousarray(np.broadcast_to(np.asarray(g_ffn, f32)[0][None, :], (128, D))),
        "ident": np.eye(128, dtype=f32),
        "na_bias": nab, "dil_bias": dlb,
        "w_r": np.ascontiguousarray(np.concatenate([np.asarray(w_r1, f32)[0], np.asarray(w_r2, f32)[0]], axis=1)),
        "b_r": np.ascontiguousarray(np.broadcast_to(
            np.concatenate([np.asarray(b_r1, f32)[0], np.asarray(b_r2, f32)[0]])[None, :], (128, 36))),
        "w1": np.ascontiguousarray(np.asarray(w1, f32)[0].reshape(N_EXP, 8, 128, 512).transpose(0, 2, 1, 3)).reshape(N_EXP * 256, 2048),
        "w3": np.ascontiguousarray(np.asarray(w3, f32)[0].reshape(N_EXP, 8, 128, 512).transpose(0, 2, 1, 3)).reshape(N_EXP * 256, 2048),
        "w2": np.ascontiguousarray(np.asarray(w2, f32)[0].reshape(N_EXP, 4, 128, 1024).transpose(0, 2, 1, 3)).reshape(N_EXP * 256, 2048),
        "iota": np.ascontiguousarray(np.broadcast_to(np.arange(256, dtype=f32)[None, :], (128, 256))),
        "pidx": np.arange(128, dtype=f32).reshape(128, 1),
        "tri": np.triu(np.ones((128, 128), f32), 1),
    }
    in_maps = []
    for c in range(_cores):
        m = dict(shared)
        m["x"] = np.ascontiguousarray(x[c])
        m["mem"] = np.ascontiguousarray(mem[c])
        in_maps.append(m)
    res = run_bass_kernel_spmd(nc, in_maps, core_ids=list(range(_cores)))
    if _debug:
        return res.results
    return np.stack([r["out"] for r in res.results], axis=0)
```

```python
import math
import os as _os
from contextlib import ExitStack
import numpy as np
import concourse.bass as bass
import concourse.mybir as mybir
from concourse.bass_utils import run_bass_kernel_spmd

F32 = mybir.dt.float32
BF16 = mybir.dt.bfloat16
I32 = mybir.dt.int32
AF = mybir.ActivationFunctionType
ALU = mybir.AluOpType
AX = mybir.AxisListType

S = 8192
D = 1024
NT = S // 128
EPS = 1e-6
NEG = -30000.0
N_EXP = 32
MOE_B = 256
NSET = 3
CAP = 2 * S + N_EXP * MOE_B
NBLK = CAP // MOE_B
SAME_ENGINE_SYNC = True
WSKIP = True
LNEXP = bool(int(_os.environ.get("LNEXP", "1")))


class Eng:
    def __init__(self, name, eng, sem):
        self.name, self.eng, self.sem = name, eng, sem
        self.count = 0
        self.waited = {}


class Buf:
    def __init__(self, name):
        self.name = name
        self.w = {}
        self.r = {}
        self.dw = None
        self.dr = None


class Sched:
    def __init__(self, nc, es):
        self.nc, self.es = nc, es
        self.engs = {}
        for nm, e in (("pe", nc.tensor), ("act", nc.scalar), ("dve", nc.vector), ("pool", nc.gpsimd), ("sp", nc.sync)):
            self.engs[nm] = Eng(nm, e, es.enter_context(nc.semaphore("sem_" + nm)))
        self.nsem = 5
        self.all_bufs = []
        self.free_sems = []

    def buf(self, name):
        b = Buf(name)
        self.all_bufs.append(b)
        return b

    def newsem(self, name):
        self.nsem += 1
        return self.es.enter_context(self.nc.semaphore(name))

    def getsem(self, name):
        if self.free_sems:
            return self.free_sems.pop()
        return [self.newsem(name), 0]

    def release_since(self, mark):
        for b in self.all_bufs[mark:]:
            seen = set()
            for attr in ("dw", "dr"):
                v = getattr(b, attr)
                if v is not None and id(v) not in seen:
                    seen.add(id(v))
                    self.free_sems.append(v)
                setattr(b, attr, None)
        del self.all_bufs[mark:]

    def _wait(self, E, key, sem, val):
        if val <= 0 or E.waited.get(key, 0) >= val:
            return
        E.eng.wait_ge(sem, val)
        E.waited[key] = val

    def _deps(self, E, reads, writes, skip_dw=False):
        for b in reads:
            for f, n in b.w.items():
                self._dep_eng(E, f, n)
            if b.dw is not None:
                self._wait(E, id(b.dw[0]), b.dw[0], b.dw[1])
        for b in writes:
            for f, n in b.w.items():
                self._dep_eng(E, f, n)
            for f, n in b.r.items():
                self._dep_eng(E, f, n)
            if b.dw is not None and not skip_dw:
                self._wait(E, id(b.dw[0]), b.dw[0], b.dw[1])
            if b.dr is not None:
                self._wait(E, id(b.dr[0]), b.dr[0], b.dr[1])

    def _dep_eng(self, E, f, n):
        if f == E.name and (f == "pe" or not SAME_ENGINE_SYNC):
            return
        F = self.engs[f]
        self._wait(E, f, F.sem, n)

    def op(self, ename, ins_fn, reads=(), writes=()):
        E = self.engs[ename]
        self._deps(E, reads, writes)
        ins = ins_fn(E.eng)
        E.count += 1
        ins.then_inc(E.sem, 1)
        for b in reads:
            b.r[ename] = E.count
        for b in writes:
            b.w[ename] = E.count
        return ins

    def dma(self, ename, out, in_, reads=(), writes=(), extra_wait=(), indirect=None, disjoint=False, **kw):
        E = self.engs[ename]
        self._deps(E, reads, writes, skip_dw=disjoint)
        for b in extra_wait:
            self._deps(E, [b], [])
        if indirect is None:
            ins = E.eng.dma_start(out=out, in_=in_, **kw)
        else:
            ins = E.eng.indirect_dma_start(out=out, in_=in_, **indirect, **kw)
        if writes:
            b = writes[0]
            if b.dw is None:
                b.dw = self.getsem("ld_" + b.name)
            b.dw[1] += 16
            ins.then_inc(b.dw[0], 16)
            for b2 in writes[1:]:
                b2.dw = b.dw
        elif reads:
            b = reads[0]
            if b.dr is None:
                b.dr = self.getsem("st_" + b.name)
            b.dr[1] += 16
            ins.then_inc(b.dr[0], 16)
        return ins

    def fence_stores(self, ename):
        E = self.engs[ename]
        for b in self.all_bufs:
            if b.dr is not None:
                self._wait(E, id(b.dr[0]), b.dr[0], b.dr[1])

    def fence_all(self, ename):
        E = self.engs[ename]
        for f, F in self.engs.items():
            if f != ename and F.count > 0:
                self._wait(E, f, F.sem, F.count)
        self.fence_stores(ename)


class Ring:
    def __init__(self, sch, es, nc, name, shape, dtype, n, psum=False):
        self.tiles, self.bufs, self.i = [], [], 0
        for k in range(n):
            nm = f"{name}{k}"
            t = es.enter_context(nc.psum_tensor(nm, shape, dtype) if psum else nc.sbuf_tensor(nm, shape, dtype))
            self.tiles.append(t)
            self.bufs.append(sch.buf(nm))

    def next(self):
        k = self.i % len(self.tiles)
        self.i += 1
        return self.tiles[k], self.bufs[k]


def t5_bucket_np(rel):
    nb = 16
    max_exact = 8
    n = np.abs(rel)
    upper = (rel > 0).astype(np.int32) * nb
    nf = np.maximum(n, 1).astype(np.float32)
    large = max_exact + (np.log(nf / max_exact) / math.log(1024 / max_exact) * (nb - max_exact)).astype(np.int32)
    large = np.minimum(large, nb - 1)
    return upper + np.where(n < max_exact, n, large)


def na_plan():
    def r0(r):
        return min(max(r - 4, 0), 120)
    tiles = {}
    steps = []
    for n in range(64):
        rows = (2 * n, 2 * n + 1)
        lo = min(r0(r) for r in rows)
        hi = max(r0(r) + 7 for r in rows)
        st = []
        for m in range(lo // 2, hi // 2 + 1):
            key = []
            for kl in range(2):
                kr = 2 * m + kl
                for ql in range(2):
                    r = rows[ql]
                    ok = r0(r) <= kr <= r0(r) + 7
                    key.append(kr - r + 7 if ok else -1)
            key = tuple(key)
            if all(k < 0 for k in key):
                continue
            if key not in tiles:
                tiles[key] = len(tiles)
            st.append((m, tiles[key]))
        steps.append(st)
    return steps, list(tiles.keys())


def na_bias_tiles(rpb, tile_keys):
    cols = np.arange(64)
    c0 = np.clip(cols - 8, 0, 48)
    kc = cols[:, None]
    qc = cols[None, :]
    colok = (kc >= c0[None, :]) & (kc < c0[None, :] + 16)
    dc = np.clip(kc - qc + 15, 0, 30)
    out = np.full((len(tile_keys), 128, 8, 128), NEG, np.float32)
    for t, key in enumerate(tile_keys):
        i = 0
        for kl in range(2):
            for ql in range(2):
                dr = key[i]
                i += 1
                if dr < 0:
                    continue
                blk = np.where(colok[None], rpb[:, dr][:, dc], NEG)
                out[t, kl * 64:(kl + 1) * 64, :, ql * 64:(ql + 1) * 64] = blk.transpose(1, 0, 2)
    return out


DIL = ((128, 1), (512, 4), (2048, 16))


def dil_plan():
    tl = []
    for g, (win, d) in enumerate(DIL):
        half = win // 2
        lo = -((half + 127) // 128)
        hi = (half + 127) // 128
        for o in range(lo, hi + 1):
            tl.append((g, o))
    return tl


def dil_bias_tiles(t5_table, tl):
    t5 = t5_table.reshape(32, 3, 4)
    out = np.full((len(tl), 128, 4, 128), NEG, np.float32)
    kk = np.arange(128)[:, None]
    qq = np.arange(128)[None, :]
    for t, (g, o) in enumerate(tl):
        win, d = DIL[g]
        rel = o * 128 + kk - qq
        ok = (np.abs(rel) <= win // 2) & (rel % d == 0)
        bk = t5_bucket_np(rel)
        for h in range(4):
            out[t, :, h, :] = np.where(ok, t5[bk, g, h], NEG)
    return out


def dil_res_bias_tiles(t5_table):
    t5 = t5_table.reshape(32, 3, 4)
    out = np.full((9, 128, 4, 128), NEG, np.float32)
    kk = np.arange(128)[:, None]
    qq = np.arange(128)[None, :]
    for g, (win, d) in enumerate(DIL):
        ns = win // 2 // d
        for o in (-1, 0, 1):
            du = o * 128 + kk - qq
            ok = np.abs(du) <= ns
            bk = t5_bucket_np(du * d)
            for h in range(4):
                out[g * 3 + o + 1, :, h, :] = np.where(ok, t5[bk, g, h], NEG)
    return out


QK_TILES = []
for i in range(4):
    QK_TILES.append((0 + 128 * i, 0))
for i in range(4):
    QK_TILES.append((512 + 128 * i, 1))
for i in range(6):
    QK_TILES.append((1536 + 128 * i, 2))
for i in range(6):
    QK_TILES.append((2304 + 128 * i, 3))
for i in range(2):
    QK_TILES.append((3840 + 128 * i, 4))
T_NAQ, T_NAK, T_DQ, T_DK, T_MQ = 0, 4, 8, 14, 20
NQK = len(QK_TILES)
V_SEGS = ((1024, 512, 0), (3072, 512, 512), (3584, 256, 1024))


def build_program(n_na_tiles, n_dil_tiles, na_steps, dil_tl, debug=False, phases=(1, 2, 3, 4)):
    nc = bass.Bass("TRN2", target_bir_lowering=False)
    dk = "ExternalOutput" if debug else "Internal"

    def din(name, shape, dt=F32):
        return nc.dram_tensor(name, list(shape), dt, kind="ExternalInput").ap()

    x = din("x", [S, D])
    mem = din("mem", [256, D])
    w_in = din("w_in", [D, 4096])
    w_mem = din("w_mem", [D, 512])
    w_out = din("w_out", [D, D])
    gvec = din("gvec", [128, 24])
    gains = din("gains", [128, 6])
    gffn_b = din("gffn_b", [128, D])
    ident_in = din("ident", [128, 128])
    na_bias = din("na_bias", [n_na_tiles, 128, 8, 128])
    dil_bias = din("dil_bias", [9, 128, 4, 128])
    w_r = din("w_r", [D, 36])
    b_r = din("b_r", [128, 36])
    w1 = din("w1", [N_EXP * 256, 2048])
    w3 = din("w3", [N_EXP * 256, 2048])
    w2 = din("w2", [N_EXP * 256, 2048])
    iota_in = din("iota", [128, 256])
    pidx_in = din("pidx", [128, 1])
    tri_in = din("tri", [128, 128])
    out = nc.dram_tensor("out", [S, D], F32, kind="ExternalOutput").ap()

    qk_scr = nc.dram_tensor("qk_scr", [NQK, 128, S], BF16, kind=dk).ap()
    v_scr = nc.dram_tensor("v_scr", [S, 1280], BF16, kind=dk).ap()
    mix_scr = nc.dram_tensor("mix_scr", [S, D], BF16, kind=dk).ap()
    km_scr = nc.dram_tensor("km_scr", [2, 128, 256], BF16, kind=dk).ap()
    vm_scr = nc.dram_tensor("vm_scr", [256, 256], BF16, kind=dk).ap()
    h2_scr = nc.dram_tensor("h2_scr", [S, D], BF16, kind=dk).ap()
    dn_scr = nc.dram_tensor("dn_scr", [3, S, 260], F32, kind=dk).ap()
    xs_scr = nc.dram_tensor("xs_scr", [CAP, D], BF16, kind=dk).ap()
    yb_scr = nc.dram_tensor("yb_scr", [CAP, D], F32, kind=dk).ap()
    dbg_out = nc.dram_tensor("dbg_out", [128, 4 * NT + 2 * NBLK], F32, kind=dk).ap()

    with ExitStack() as es:
        sch = Sched(nc, es)

        def sb(name, shape, dt):
            return es.enter_context(nc.sbuf_tensor(name, list(shape), dt))

        def ps(name, shape, dt=F32):
            return es.enter_context(nc.psum_tensor(name, list(shape), dt))

        identf = sb("identf", [128, 128], F32)
        identb = sb("identb", [128, 128], BF16)
        blk1 = sb("blk1", [128, 128], BF16)
        gv = sb("gv", [128, 24], F32)
        gn = sb("gn", [128, 6], F32)
        epsb = sb("epsb", [128, 1], F32)
        b_const = sch.buf("const")
        sch.dma("sp", identf[:], ident_in[:, :], writes=[b_const])
        sch.dma("sp", gv[:], gvec[:, :], writes=[b_const])
        sch.dma("sp", gn[:], gains[:, :], writes=[b_const])
        sch.op("dve", lambda e: e.tensor_copy(out=identb[:], in_=identf[:]), reads=[b_const], writes=[b_const])
        sch.op("dve", lambda e: e.memset(blk1[:], 0.0), writes=[b_const])
        sch.op("dve", lambda e: e.memset(epsb[:], EPS), writes=[b_const])
        sch.op("dve", lambda e: e.memset(blk1[0:64, 0:64], 1.0), writes=[b_const])
        sch.op("dve", lambda e: e.memset(blk1[64:128, 64:128], 1.0), writes=[b_const])
        gq = gn[:].rearrange("p (a b) -> p a b", b=2)[:, :, 0:1]
        sch.op("dve", lambda e: e.tensor_scalar(out=gq, in0=gq, scalar1=0.125, scalar2=None, op0=ALU.mult),
               reads=[b_const], writes=[b_const])

        def projection(es1, src, n_tok, wsrc, ncols, gcol0, qk_tiles, qk_dst, v_segs, v_dst, tag):
            def sb1(name, shape, dt):
                return es1.enter_context(nc.sbuf_tensor(tag + name, list(shape), dt))
            W = sb1("W", [128, 8, ncols], BF16)
            bW = sch.buf(tag + "W")
            wst = Ring(sch, es1, nc, tag + "wst", [128, 2048], F32, 2)
            nhalf = (ncols + 2047) // 2048
            for kc in range(8):
                for hf in range(nhalf):
                    c0 = hf * 2048
                    cw = min(2048, ncols - c0)
                    t, b = wst.next()
                    sch.dma("sp", t[:, 0:cw], wsrc[kc * 128:(kc + 1) * 128, c0:c0 + cw], writes=[b])
                    if (kc * nhalf + hf) % 2 == 0:
                        sch.op("dve", lambda e, t=t, kc=kc, c0=c0, cw=cw: e.tensor_scalar(
                            out=W[:, kc, c0:c0 + cw], in0=t[:, 0:cw], scalar1=gv[:, gcol0 + kc:gcol0 + kc + 1],
                            scalar2=None, op0=ALU.mult), reads=[b, b_const], writes=[bW])
                    else:
                        sch.op("act", lambda e, t=t, kc=kc, c0=c0, cw=cw: e.activation(
                            out=W[:, kc, c0:c0 + cw], in_=t[:, 0:cw], func=AF.Copy, scale=gv[:, gcol0 + kc:gcol0 + kc + 1]),
                            reads=[b, b_const], writes=[bW])
            CH = min(512, n_tok)
            TT = CH // 128
            xt_r = Ring(sch, es1, nc, tag + "xt", [128, TT, D], F32, 2)
            hn_r = Ring(sch, es1, nc, tag + "hn", [128, TT, D], BF16, 2)
            hT_r = Ring(sch, es1, nc, tag + "hT", [128, 8, CH], BF16, 2)
            st_r = Ring(sch, es1, nc, tag + "st", [128, 8], F32, 2)
            junk = sb1("junk", [128, D], BF16)
            bjunk = sch.buf(tag + "junk")
            sq_r = Ring(sch, es1, nc, tag + "sq", [128, CH], BF16, 2)
            sd_r = Ring(sch, es1, nc, tag + "sd", [128, CH], F32, 2)
            rs_r = Ring(sch, es1, nc, tag + "rs", [128, CH], F32, 2)
            qn_r = Ring(sch, es1, nc, tag + "qn", [128, CH], BF16, 3)
            vo_r = Ring(sch, es1, nc, tag + "vo", [128, 1280], BF16, 2)
            tpb = [es1.enter_context(nc.psum_tensor(f"{tag}tp{i}", [128, 2, 512], BF16)) for i in range(1)]
            tpbuf = [sch.buf(tag + "tp0")]
            pbank = [None] + [es1.enter_context(nc.psum_tensor(f"{tag}pb{i}", [128, 512], F32)) for i in range(1, 8)]
            pbuf = [None] + [sch.buf(f"{tag}pb{i}") for i in range(1, 8)]
            def front1(ck):
                t0 = ck * CH
                xt, bxt = xt_r.next()
                sch.dma("sp", xt[:], src[t0:t0 + CH, :].rearrange("(t p) d -> p t d", p=128), writes=[bxt])
                stt, bst = st_r.next()
                for t in range(TT):
                    sch.op("act", lambda e, t=t: e.activation(out=junk[:], in_=xt[:, t, :], func=AF.Square,
                                                               accum_out=stt[:, t:t + 1]),
                           reads=[bxt], writes=[bjunk, bst])
                sch.op("act", lambda e: e.activation(out=stt[:, 4:4 + TT], in_=stt[:, 0:TT], func=AF.Sqrt,
                                                      bias=EPS, scale=1.0 / D), reads=[bst], writes=[bst])
                sch.op("dve", lambda e: e.reciprocal(out=stt[:, 4:4 + TT], in_=stt[:, 4:4 + TT]),
                       reads=[bst], writes=[bst])
                hn, bhn = hn_r.next()
                for t in range(TT):
                    sch.op("act", lambda e, t=t: e.activation(out=hn[:, t, :], in_=xt[:, t, :], func=AF.Copy,
                                                               scale=stt[:, 4 + t:5 + t]),
                           reads=[bxt, bst], writes=[bhn])
                return hn, bhn

            def front2(hn, bhn):
                hT_, bhT_ = hT_r.next()
                for kc in range(8):
                    bank = 0
                    sl = kc % 2
                    for t in range(TT):
                        sch.op("pe", lambda e, t=t, kc=kc, bank=bank, sl=sl: e.transpose(
                            out=tpb[bank][:, sl, t * 128:(t + 1) * 128], in_=hn[:, t, kc * 128:(kc + 1) * 128],
                            identity=identb[:]), reads=[bhn, b_const], writes=[tpbuf[bank]])
                    if sl == 1:
                        sch.op("dve", lambda e, kc=kc, bank=bank: e.tensor_copy(
                            out=hT_[:, kc - 1:kc + 1, :], in_=tpb[bank][:, :, 0:CH]),
                            reads=[tpbuf[bank]], writes=[bhT_])
                return hT_, bhT_

            NCK = n_tok // CH
            cur_hT = front2(*front1(0))
            for ck in range(NCK):
                t0 = ck * CH
                hT, bhT = cur_hT
                nxt1 = front1(ck + 1) if ck + 1 < NCK else None
                nxt_hT = None
                def qk_mm(j):
                    c0, gc = qk_tiles[j]
                    qb = 1 + (j % 3)
                    for kc in range(8):
                        sch.op("pe", lambda e, kc=kc, c0=c0, qb=qb: e.matmul(
                            pbank[qb][:, 0:CH], W[:, kc, c0:c0 + 128], hT[:, kc, :], start=(kc == 0), stop=(kc == 7)),
                            reads=[bW, bhT], writes=[pbuf[qb]])
                    sq, bsq = sq_r.next()
                    sch.op("act", lambda e, qb=qb, sq=sq: e.activation(out=sq[:], in_=pbank[qb][:, 0:CH], func=AF.Square),
                           reads=[pbuf[qb]], writes=[bsq])
                    return sq, bsq

                def qk_epi(j, sq, bsq):
                    c0, gc = qk_tiles[j]
                    qb = 1 + (j % 3)
                    sbk = 4 + (j % 2)
                    sch.op("pe", lambda e, sbk=sbk, sq=sq: e.matmul(pbank[sbk][:, 0:CH], blk1[:], sq[:], start=True, stop=True),
                           reads=[bsq, b_const], writes=[pbuf[sbk]])
                    sd, bsd = sd_r.next()
                    rs, brs = rs_r.next()
                    if LNEXP:
                        sch.op("act", lambda e, sbk=sbk, sd=sd: e.activation(out=sd[:], in_=pbank[sbk][:, 0:CH], func=AF.Ln,
                                                                             bias=epsb[:, 0:1], scale=1.0 / 64),
                               reads=[pbuf[sbk], b_const], writes=[bsd])
                        sch.op("act", lambda e, sd=sd, rs=rs: e.activation(out=rs[:], in_=sd[:], func=AF.Exp, scale=-0.5),
                               reads=[bsd], writes=[brs])
                    else:
                        sch.op("act", lambda e, sbk=sbk, sd=sd: e.activation(out=sd[:], in_=pbank[sbk][:, 0:CH], func=AF.Sqrt,
                                                                             bias=EPS, scale=1.0 / 64),
                               reads=[pbuf[sbk]], writes=[bsd])
                        sch.op("dve", lambda e, sd=sd, rs=rs: e.reciprocal(out=rs[:], in_=sd[:]), reads=[bsd], writes=[brs])
                    qn, bqn = qn_r.next()
                    sch.op("dve", lambda e, qb=qb, rs=rs, qn=qn, gc=gc: e.scalar_tensor_tensor(
                        out=qn[:], in0=pbank[qb][:, 0:CH], scalar=gn[:, gc:gc + 1], in1=rs[:], op0=ALU.mult, op1=ALU.mult),
                        reads=[pbuf[qb], brs, b_const], writes=[bqn])
                    sch.dma("pool", qk_dst(j, t0, CH), qn[:], reads=[bqn])

                prev = None
                for j in range(len(qk_tiles) + 1):
                    cur_ = qk_mm(j) if j < len(qk_tiles) else None
                    if prev is not None:
                        qk_epi(j - 1, *prev)
                    prev = cur_
                    if j == len(qk_tiles) // 2 and nxt1 is not None and nxt_hT is None:
                        nxt_hT = front2(*nxt1)
                if nxt1 is not None and nxt_hT is None:
                    nxt_hT = front2(*nxt1)
                for t in range(TT):
                    vo, bvo = vo_r.next()
                    for si, (c0, cw, d0) in enumerate(v_segs):
                        vb = 6 + ((t * len(v_segs) + si) % 2)
                        for kc in range(8):
                            sch.op("pe", lambda e, kc=kc, c0=c0, cw=cw, vb=vb, t=t: e.matmul(
                                pbank[vb][:, 0:cw], hT[:, kc, t * 128:(t + 1) * 128], W[:, kc, c0:c0 + cw],
                                start=(kc == 0), stop=(kc == 7)), reads=[bW, bhT], writes=[pbuf[vb]])
                        sch.op("act", lambda e, vb=vb, cw=cw, d0=d0, vo=vo: e.activation(
                            out=vo[:, d0:d0 + cw], in_=pbank[vb][:, 0:cw], func=AF.Copy),
                            reads=[pbuf[vb]], writes=[bvo])
                    vw = sum(s_[1] for s_ in v_segs)
                    sch.dma("pool", v_dst(t0 + t * 128, vw), vo[:, 0:vw], reads=[bvo])
                cur_hT = nxt_hT

        if 1 in phases:
            mark1 = len(sch.all_bufs)
            with ExitStack() as es1:
                projection(es1, x, S, w_in, 4096, 0, QK_TILES,
                           lambda j, t0, n: qk_scr[j, :, t0:t0 + n], V_SEGS,
                           lambda t0, vw: v_scr[t0:t0 + 128, 0:vw], "p1")
                sch.fence_all("sp")
                sch.fence_all("pool")
            with ExitStack() as es1:
                projection(es1, mem, 256, w_mem, 512, 8, [(0, 5), (128, 5)],
                           lambda j, t0, n: km_scr[j, :, t0:t0 + n], ((256, 256, 0),),
                           lambda t0, vw: vm_scr[t0:t0 + 128, 0:vw], "pm")
                sch.fence_all("sp")
                sch.fence_all("pool")


        def attn_pass(tag, qsrc, ksrc, sk, vsrc, vruns, slots, steps_fn, OH, bias, mix_col0, perm=None, raw_dst=None):
            mark_ = len(sch.all_bufs)
            with ExitStack() as e2:
                def sb2(name, shape, dt):
                    return e2.enter_context(nc.sbuf_tensor(tag + name, list(shape), dt))
                nkb = sk // 128
                QT = [sb2(f"QT{i}", [128, S], BF16) for i in range(len(qsrc))]
                KT = [sb2(f"KT{i}", [128, sk], BF16) for i in range(len(ksrc))]
                nv = sum(c for _, c in vruns)
                V1 = sb2("V1", [128, nkb, nv, 65], BF16)
                bin_ = sch.buf(tag + "in")
                bv_ = sch.buf(tag + "inv")
                beb_ = sch.buf(tag + "ineb")
                SKIP = _os.environ.get("P2SKIP", "")
                s0 = 0
                for ri, (vc0, cnt) in enumerate(vruns):
                    d_ = 1 if perm is None else perm[ri]
                    for c in range(cnt):
                        vcol = vsrc[:, vc0 + c * 64:vc0 + (c + 1) * 64]
                        if d_ == 1:
                            vv = vcol.rearrange("(b p) d -> p b d", p=128)
                            for b0 in range(0, nkb, 16):
                                b1 = min(nkb, b0 + 16)
                                sch.dma("sp", V1[:, b0:b1, s0 + c, 0:64], vv[:, b0:b1, :], writes=[bv_], disjoint=True)
                        else:
                            SB_ = nkb // d_
                            vv = vcol.rearrange("(ub p r) c -> r p ub c", p=128, r=d_)
                            for rho in range(d_):
                                sch.dma("sp", V1[:, rho * SB_:(rho + 1) * SB_, s0 + c, 0:64], vv[rho], writes=[bv_], disjoint=True)
                    s0 += cnt
                tmp_r = None
                if perm is not None and any(d_ > 1 for d_ in perm):
                    tmp_r = Ring(sch, e2, nc, tag + "tmpN", [128, S], BF16, 1)
                pk = 0
                for (dstT, srcs) in ((QT, qsrc), (KT, ksrc)):
                    for i, a in enumerate(srcs):
                        d_ = 1 if perm is None else perm[i]
                        if d_ == 1:
                            for hf in range(2):
                                w_ = a.shape[1] // 2
                                sch.dma("sp", dstT[i][:, hf * w_:(hf + 1) * w_], a[:, hf * w_:(hf + 1) * w_], writes=[bin_], disjoint=True)
                        else:
                            tn, btn = tmp_r.next()
                            sch.dma("sp", tn[:], a, writes=[btn])
                            eng = ("act", "dve")[pk % 2]
                            pk += 1
                            o_ap = dstT[i][:, :].rearrange("p (r u) -> p r u", r=d_)
                            i_ap = tn[:, :].rearrange("p (u r) -> p r u", r=d_)
                            if eng == "act":
                                sch.op("act", lambda e, o_ap=o_ap, i_ap=i_ap: e.activation(out=o_ap, in_=i_ap, func=AF.Copy),
                                       reads=[btn], writes=[bin_])
                            else:
                                sch.op(eng, lambda e, o_ap=o_ap, i_ap=i_ap: e.tensor_copy(out=o_ap, in_=i_ap),
                                       reads=[btn], writes=[bin_])
                if "m" not in SKIP:
                    sch.op("pool", lambda e: e.memset(V1[:, :, :, 64:65], 1.0), writes=[bv_])
                EB = None
                if bias is not None:
                    bd, h0, Hs = bias
                    ntile = bd.shape[0]
                    Hh = Hs // 2
                    EB = sb2("EB", [128, 2, ntile, Hh, 128], BF16)
                    ebs = Ring(sch, e2, nc, tag + "ebs", [128, Hs, 128], F32, 1)
                    for t in range(ntile):
                        st, bst = ebs.next()
                        sch.dma("sp", st[:], bd[t, :, h0:h0 + Hs, :], writes=[bst])
                        sch.op("act", lambda e, st=st, t=t: e.activation(
                            out=EB[:, :, t, :, :], in_=st[:].rearrange("p (i f) q -> p f i q", f=2), func=AF.Exp),
                               reads=[bst], writes=[beb_])
                sps = Ring(sch, e2, nc, tag + "sps", [128, 512], F32, 4, psum=True)
                ops_ = Ring(sch, e2, nc, tag + "ops", [128, 512], F32, 2, psum=True)
                pr = Ring(sch, e2, nc, tag + "P", [128, 512], BF16, 5)
                rd_r = Ring(sch, e2, nc, tag + "rd", [128, 8], F32, 2)
                mx_r = Ring(sch, e2, nc, tag + "mx", [128, 4, OH * 64], BF16, 2) if raw_dst is None else None
                ro_r = Ring(sch, e2, nc, tag + "ro", [128, 512], F32, 2) if raw_dst is not None else None
                mx_state = [None, None]
                recs = []
                for n in range(NT):
                    pend = ([], [])
                    groups = []
                    for (m, tid, sl) in steps_fn(n):
                        for si in sl:
                            hf = slots[si][2]
                            pend[hf].append((m, tid, slots[si]))
                            if len(pend[hf]) == 4:
                                groups.append(list(pend[hf]))
                                pend[hf].clear()
                    for hf in range(2):
                        if pend[hf]:
                            groups.append(list(pend[hf]))
                    for gi, grp in enumerate(groups):
                        recs.append(dict(n=n, grp=grp, first=(gi == 0), last=(gi == len(groups) - 1)))

                def emitA(rcs):
                    for rc in rcs:
                        rc["sp"], rc["bsp"] = sps.next()
                    for i in range(4):
                        for rc in rcs:
                            grp = rc["grp"]
                            if i >= len(grp):
                                continue
                            n = rc["n"]
                            (m, tid, (qt, kt, half, vs, oh, bh)) = grp[i]
                            pl = slice(half * 64, half * 64 + 64)
                            sp_ = rc["sp"]
                            sch.op("pe", lambda e, i=i, m=m, qt=qt, kt=kt, pl=pl, sp_=sp_, n=n: e.matmul(
                                sp_[:, i * 128:(i + 1) * 128], KT[kt][pl, m * 128:(m + 1) * 128],
                                QT[qt][pl, n * 128:(n + 1) * 128], start=True, stop=True),
                                reads=[bin_], writes=[rc["bsp"]])
                    for rc in rcs:
                        grp, sp_, bsp = rc["grp"], rc["sp"], rc["bsp"]
                        w = len(grp) * 128
                        P, bP = pr.next()
                        rc["P"], rc["bP"] = P, bP
                        sch.op("act", lambda e, sp_=sp_, P=P, w=w: e.activation(out=P[:, 0:w], in_=sp_[:, 0:w], func=AF.Exp),
                               reads=[bsp], writes=[bP])
                        if EB is not None:
                            def eoff(job):
                                return (job[2][2] * ntile + job[1]) * Hh + job[2][5] // 2
                            i = 0
                            while i < len(grp):
                                off = eoff(grp[i])
                                j = i + 1
                                while j < len(grp) and eoff(grp[j]) == off + (j - i):
                                    j += 1
                                ebv = EB[:].rearrange("p f t h q -> p (f t h) q")[:, off:off + (j - i), :]
                                pv = P[:, i * 128:j * 128].rearrange("p (a q) -> p a q", q=128)
                                sch.op("dve", lambda e, pv=pv, ebv=ebv: e.tensor_tensor(out=pv, in0=pv, in1=ebv, op=ALU.mult),
                                       reads=[bP, beb_], writes=[bP])
                                i = j

                cur_o = [None, None]

                def emitB(rc):
                    n, grp, P, bP = rc["n"], rc["grp"], rc["P"], rc["bP"]
                    if rc["first"]:
                        cur_o[0], cur_o[1] = ops_.next()
                    oacc, boacc = cur_o
                    for i, (m, tid, (qt, kt, half, vs, oh, bh)) in enumerate(grp):
                        fst = rc["first"] and i == 0
                        lst = rc["last"] and i == len(grp) - 1
                        sch.op("pe", lambda e, i=i, m=m, vs=vs, oh=oh, P=P, oacc=oacc, fst=fst, lst=lst: e.matmul(
                            oacc[:, oh * 65:(oh + 1) * 65], P[:, i * 128:(i + 1) * 128], V1[:, m, vs, :],
                            start=fst, stop=lst, skip_group_check=True),
                            reads=[bP, bv_], writes=[boacc])
                    if not rc["last"]:
                        return
                    if raw_dst is not None:
                        ro, bro = ro_r.next()
                        sch.op("dve", lambda e, ro=ro, oacc=oacc: e.tensor_copy(out=ro[:, 0:OH * 65], in_=oacc[:, 0:OH * 65]),
                               reads=[boacc], writes=[bro])
                        for g_ in range(3):
                            sch.dma("pool", raw_dst(g_, n), ro[:, g_ * 130:(g_ + 1) * 130], reads=[bro])
                        return
                    rd, brd = rd_r.next()
                    ov = oacc[:, 0:OH * 65].rearrange("p (h c) -> p h c", c=65)
                    sch.op("dve", lambda e, rd=rd, ov=ov: e.reciprocal(out=rd[:, 0:OH], in_=ov[:, :, 64]),
                           reads=[boacc], writes=[brd])
                    if n % 4 == 0:
                        mx_state[0], mx_state[1] = mx_r.next()
                    mx, bmx = mx_state
                    for h in range(OH):
                        sch.op("dve", lambda e, h=h, rd=rd, oacc=oacc, mx=mx: e.tensor_scalar(
                            out=mx[:, n % 4, h * 64:(h + 1) * 64], in0=oacc[:, h * 65:h * 65 + 64],
                            scalar1=rd[:, h:h + 1], scalar2=None, op0=ALU.mult),
                            reads=[boacc, brd], writes=[bmx])
                    if n % 4 == 3:
                        r0_ = (n - 3) * 128
                        sch.dma("pool", mix_scr[r0_:r0_ + 512, mix_col0:mix_col0 + OH * 64].rearrange("(t p) c -> p t c", p=128),
                                mx[:], reads=[bmx])

                units = []
                i = 0
                while i < len(recs):
                    if i + 1 < len(recs) and recs[i]["grp"][0][2][2] != recs[i + 1]["grp"][0][2][2]:
                        units.append([recs[i], recs[i + 1]])
                        i += 2
                    else:
                        units.append([recs[i]])
                        i += 1
                LA = 1
                for i in range(len(units) + LA):
                    if i < len(units):
                        emitA(units[i])
                    if i - LA >= 0:
                        for rc in units[i - LA]:
                            emitB(rc)
                sch.fence_all("sp")
                sch.fence_all("pool")
                for en_ in ("act", "dve", "pe"):
                    sch.fence_all(en_)
                sch.release_since(mark_)

        SEL = _os.environ.get("P2SEL", "na0,na1,dl0,dl1,mm").split(",")
        if 2 in phases:
            for ps_ in range(2):
                if f"na{ps_}" not in SEL:
                    continue
                slots = [(h // 2, h // 2, h % 2, h, h, h) for h in range(4)]
                attn_pass(f"na{ps_}", [qk_scr[T_NAQ + 2 * ps_ + i] for i in range(2)],
                          [qk_scr[T_NAK + 2 * ps_ + i] for i in range(2)], S, v_scr, [(ps_ * 256, 4)], slots,
                          lambda n: [(m, tid, [0, 1, 2, 3]) for (m, tid) in na_steps[n]], 4,
                          (na_bias, ps_ * 4, 4), ps_ * 256)
            DD = [d_ for (_, d_) in DIL]
            for ps_ in range(2):
                if f"dl{ps_}" not in SEL:
                    continue
                slots = []
                for g in range(3):
                    for hh in range(2):
                        slots.append((g, g, hh, g * 2 + hh, g * 2 + hh, hh))

                def dsteps(n):
                    st = []
                    for g in range(3):
                        SB_ = NT // DD[g]
                        for o in (-1, 0, 1):
                            m = n + o
                            if 0 <= m < NT and m // SB_ == n // SB_:
                                st.append((m, g * 3 + o + 1, [g * 2, g * 2 + 1]))
                    return st

                def raw_dst(g, n, ps_=ps_):
                    d_ = DD[g]
                    SB_ = NT // d_
                    v = dn_scr[g][:, ps_ * 130:(ps_ + 1) * 130].rearrange("(ub p r) c -> r ub p c", p=128, r=d_)
                    return v[n // SB_, n % SB_]
                attn_pass(f"dl{ps_}", [qk_scr[T_DQ + 2 * g + ps_] for g in range(3)],
                          [qk_scr[T_DK + 2 * g + ps_] for g in range(3)], S, v_scr,
                          [(512 + g * 256 + ps_ * 128, 2) for g in range(3)], slots, dsteps, 6,
                          (dil_bias, ps_ * 2, 2), 0, perm=DD, raw_dst=raw_dst)
            if "dl0" in SEL and "dl1" in SEL:
                mark_c = len(sch.all_bufs)
                with ExitStack() as ec:
                    dn_r = Ring(sch, ec, nc, "dcdn", [128, 3, 260], F32, 3)
                    sm_r = Ring(sch, ec, nc, "dcsm", [128, 260], F32, 2)
                    rc_r = Ring(sch, ec, nc, "dcrc", [128, 4], F32, 2)
                    mo_r = Ring(sch, ec, nc, "dcmo", [128, 256], BF16, 3)
                    for t in range(NT):
                        dn, bdn = dn_r.next()
                        sch.dma("sp", dn[:], dn_scr[:, t * 128:(t + 1) * 128, :].rearrange("g p c -> p g c"), writes=[bdn])
                        sm, bsm = sm_r.next()
                        sch.op("dve", lambda e, dn=dn, sm=sm: e.tensor_tensor(out=sm[:], in0=dn[:, 0, :], in1=dn[:, 1, :], op=ALU.add),
                               reads=[bdn], writes=[bsm])
                        sch.op("dve", lambda e, dn=dn, sm=sm: e.tensor_tensor(out=sm[:], in0=sm[:], in1=dn[:, 2, :], op=ALU.add),
                               reads=[bdn, bsm], writes=[bsm])
                        rcp, brc = rc_r.next()
                        smv = sm[:].rearrange("p (h c) -> p h c", c=65)
                        sch.op("dve", lambda e, rcp=rcp, smv=smv: e.reciprocal(out=rcp[:], in_=smv[:, :, 64]), reads=[bsm], writes=[brc])
                        mo, bmo = mo_r.next()
                        for h in range(4):
                            sch.op("act", lambda e, h=h, mo=mo, sm=sm, rcp=rcp: e.activation(
                                out=mo[:, h * 64:(h + 1) * 64], in_=sm[:, h * 65:h * 65 + 64], func=AF.Copy, scale=rcp[:, h:h + 1]),
                                reads=[bsm, brc], writes=[bmo])
                        sch.dma("pool", mix_scr[t * 128:(t + 1) * 128, 512:768], mo[:], reads=[bmo])
                    for en_ in ("sp", "pool", "act", "dve", "pe"):
                        sch.fence_all(en_)
                    sch.release_since(mark_c)
            slots = [(h // 2, h // 2, h % 2, h, h, 0) for h in range(4)]
            if "mm" in SEL:
              attn_pass("mm", [qk_scr[T_MQ + i] for i in range(2)], [km_scr[i] for i in range(2)], 256, vm_scr,
                      [(0, 4)], slots, lambda n: [(0, None, [0, 1, 2, 3]), (1, None, [0, 1, 2, 3])], 4, None, 768)


        g1 = sb("g1", [128, NT], F32)
        g2 = sb("g2", [128, NT], F32)
        dst1i = sb("dst1i", [128, NT], I32)
        dst2i = sb("dst2i", [128, NT], I32)
        widx = sb("widx", [128, NBLK * 2], I32)
        bRt = sch.buf("routeout")
        if 3 in phases:
            with ExitStack() as e3:
                cur = [e3]
                def sb3(name, shape, dt):
                    return cur[0].enter_context(nc.sbuf_tensor("p3" + name, list(shape), dt))
                L = sb3("L", [128, NT, 36], F32)
                bL = sch.buf("L")
                e3m = ExitStack()
                e3m.__enter__()
                cur[0] = e3m
                Wo = sb3("Wo", [128, 8, D], BF16)
                bWo = sch.buf("Wo")
                wst = Ring(sch, cur[0], nc, "p3wst", [128, D], F32, 2)
                for kc in range(8):
                    t, b = wst.next()
                    sch.dma("sp", t[:], w_out[kc * 128:(kc + 1) * 128, :], writes=[b])
                    sch.op("dve", lambda e, t=t, kc=kc: e.tensor_copy(out=Wo[:, kc, :], in_=t[:]), reads=[b], writes=[bWo])
                wr = sb3("wr", [128, 8, 36], F32)
                br = sb3("br", [128, 36], F32)
                gB = sb3("gB", [128, D], F32)
                bwr = sch.buf("wr")
                sch.dma("sp", wr[:], w_r.rearrange("(kc p) n -> p kc n", p=128), writes=[bwr])
                sch.dma("sp", br[:], b_r[:, :], writes=[bwr])
                sch.dma("sp", gB[:], gffn_b[:, :], writes=[bwr])
                for kc in range(8):
                    sch.op("dve", lambda e, kc=kc: e.tensor_scalar(out=wr[:, kc, :], in0=wr[:, kc, :],
                                                                    scalar1=gv[:, 16 + kc:17 + kc], scalar2=None, op0=ALU.mult),
                           reads=[bwr, b_const], writes=[bwr])
                wrh = sb3("wrh", [128, 8, 36], BF16)
                wrl = sb3("wrl", [128, 8, 36], BF16)
                sch.op("dve", lambda e: e.tensor_copy(out=wrh[:], in_=wr[:]), reads=[bwr], writes=[bwr])
                sch.op("dve", lambda e: e.tensor_tensor(out=wrl[:], in0=wr[:], in1=wrh[:], op=ALU.subtract), reads=[bwr], writes=[bwr])
                mx_r = Ring(sch, cur[0], nc, "p3mx", [128, D], BF16, 2)
                xt_r = Ring(sch, cur[0], nc, "p3xt", [128, D], F32, 2)
                mT_r = Ring(sch, cur[0], nc, "p3mT", [128, 8, 128], BF16, 2)
                x1_r = Ring(sch, cur[0], nc, "p3x1", [128, D], F32, 2)
                hn_r = Ring(sch, cur[0], nc, "p3hn", [128, D], F32, 2)
                hi_r = Ring(sch, cur[0], nc, "p3hi", [128, D], BF16, 3)
                lo_r = Ring(sch, cur[0], nc, "p3lo", [128, D], BF16, 3)
                hg_r = Ring(sch, cur[0], nc, "p3hg", [128, D], BF16, 2)
                hiT_r = Ring(sch, cur[0], nc, "p3hiT", [128, 8, 128], BF16, 2)
                loT_r = Ring(sch, cur[0], nc, "p3loT", [128, 8, 128], BF16, 2)
                st_r = Ring(sch, cur[0], nc, "p3st", [128, 4], F32, 2)
                junk = sb3("junk", [128, D], BF16)
                bjunk = sch.buf("p3junk")
                with ExitStack() as e3p:
                    tpm = e3p.enter_context(nc.psum_tensor("p3tpm", [128, 8, 128], BF16))
                    btpm = sch.buf("p3tpm")
                    yps = Ring(sch, e3p, nc, "p3yps", [128, 512], F32, 4, psum=True)
                    tph = e3p.enter_context(nc.psum_tensor("p3tph", [128, 8, 128], BF16))
                    tpl = e3p.enter_context(nc.psum_tensor("p3tpl", [128, 8, 128], BF16))
                    btph, btpl = sch.buf("p3tph"), sch.buf("p3tpl")
                    lps = e3p.enter_context(nc.psum_tensor("p3lps", [128, 512], F32))
                    blps = sch.buf("p3lps")
                    def p3A(n):
                        r0_ = n * 128
                        mxt, bmx = mx_r.next()
                        xt, bxt = xt_r.next()
                        sch.dma("sp", mxt[:], mix_scr[r0_:r0_ + 128, :], writes=[bmx])
                        sch.dma("sp", xt[:], x[r0_:r0_ + 128, :], writes=[bxt])
                        for kc in range(8):
                            sch.op("pe", lambda e, kc=kc, mxt=mxt: e.transpose(out=tpm[:, kc, :], in_=mxt[:, kc * 128:(kc + 1) * 128],
                                                                      identity=identb[:]), reads=[bmx, b_const], writes=[btpm])
                        mT, bmT = mT_r.next()
                        sch.op("dve", lambda e, mT=mT: e.tensor_copy(out=mT[:], in_=tpm[:]), reads=[btpm], writes=[bmT])
                        x1, bx1 = x1_r.next()
                        for hf in range(2):
                            yp, byp = yps.next()
                            for kc in range(8):
                                sch.op("pe", lambda e, kc=kc, hf=hf, yp=yp, mT=mT: e.matmul(
                                    yp[:], mT[:, kc, :], Wo[:, kc, hf * 512:(hf + 1) * 512], start=(kc == 0), stop=(kc == 7)),
                                    reads=[bmT, bWo], writes=[byp])
                            sch.op("dve", lambda e, hf=hf, yp=yp, x1=x1, xt=xt: e.tensor_tensor(
                                out=x1[:, hf * 512:(hf + 1) * 512], in0=yp[:], in1=xt[:, hf * 512:(hf + 1) * 512], op=ALU.add),
                                reads=[byp, bxt], writes=[bx1])
                        sch.dma("pool", out[r0_:r0_ + 128, :], x1[:], reads=[bx1])
                        stt, bst = st_r.next()
                        sch.op("act", lambda e, x1=x1, stt=stt: e.activation(out=junk[:], in_=x1[:], func=AF.Square,
                                                                           accum_out=stt[:, 0:1]), reads=[bx1], writes=[bjunk, bst])
                        sch.op("act", lambda e, stt=stt: e.activation(out=stt[:, 1:2], in_=stt[:, 0:1], func=AF.Sqrt,
                                                                       bias=EPS, scale=1.0 / D), reads=[bst], writes=[bst])
                        sch.op("dve", lambda e, stt=stt: e.reciprocal(out=stt[:, 1:2], in_=stt[:, 1:2]), reads=[bst], writes=[bst])
                        hn, bhn = hn_r.next()
                        sch.op("act", lambda e, hn=hn, x1=x1, stt=stt: e.activation(out=hn[:], in_=x1[:], func=AF.Copy,
                                                                                   scale=stt[:, 1:2]), reads=[bx1, bst], writes=[bhn])
                        hi, bhi = hi_r.next()
                        lo, blo = lo_r.next()
                        hg, bhg = hg_r.next()
                        sch.op("act", lambda e, hi=hi, hn=hn: e.activation(out=hi[:], in_=hn[:], func=AF.Copy), reads=[bhn], writes=[bhi])
                        sch.op("dve", lambda e, hi=hi, lo=lo, hn=hn: e.tensor_tensor(out=lo[:], in0=hn[:], in1=hi[:], op=ALU.subtract),
                               reads=[bhn, bhi], writes=[blo])
                        sch.op("pool", lambda e, hg=hg, hn=hn: e.tensor_tensor(out=hg[:], in0=hn[:], in1=gB[:], op=ALU.mult),
                               reads=[bhn, bwr], writes=[bhg])
                        sch.dma("pool", h2_scr[r0_:r0_ + 128, :], hg[:], reads=[bhg])
                        return hi, bhi, lo, blo

                    def p3B(n, hi, bhi, lo, blo):
                        for kc in range(8):
                            sch.op("pe", lambda e, kc=kc, hi=hi: e.transpose(out=tph[:, kc, :], in_=hi[:, kc * 128:(kc + 1) * 128],
                                                                            identity=identb[:]), reads=[bhi, b_const], writes=[btph])
                        for kc in range(8):
                            sch.op("pe", lambda e, kc=kc, lo=lo: e.transpose(out=tpl[:, kc, :], in_=lo[:, kc * 128:(kc + 1) * 128],
                                                                            identity=identb[:]), reads=[blo, b_const], writes=[btpl])
                        hiT, bhiT = hiT_r.next()
                        loT, bloT = loT_r.next()
                        sch.op("act", lambda e, hiT=hiT: e.activation(out=hiT[:], in_=tph[:], func=AF.Copy), reads=[btph], writes=[bhiT])
                        sch.op("act", lambda e, loT=loT: e.activation(out=loT[:], in_=tpl[:], func=AF.Copy), reads=[btpl], writes=[bloT])
                        k = 0
                        for (aa, ba, ww) in ((hiT, bhiT, wrh), (hiT, bhiT, wrl), (loT, bloT, wrh)):
                            for kc in range(8):
                                sch.op("pe", lambda e, kc=kc, aa=aa, ww=ww, k=k: e.matmul(lps[:, 0:36], aa[:, kc, :], ww[:, kc, :],
                                                                                     start=(k == 0), stop=(k == 23)),
                                       reads=[ba, bwr], writes=[blps])
                                k += 1
                        sch.op("dve", lambda e, n=n: e.tensor_tensor(out=L[:, n, :], in0=lps[:, 0:36], in1=br[:], op=ALU.add),
                               reads=[blps, bwr], writes=[bL])


                    prev3 = None
                    for n in range(NT + 1):
                        cur3 = p3A(n) if n < NT else None
                        if prev3 is not None:
                            p3B(n - 1, *prev3)
                        prev3 = cur3
                e3m.close()
                cur[0] = e3
                def sbr(name, shape, dt=F32):
                    return e3.enter_context(nc.sbuf_tensor("rt" + name, list(shape), dt))
                bR = sch.buf("route")
                gl = L[:, :, 0:4]
                fl = L[:, :, 4:36]
                gmax = sbr("gmax", [128, NT]); G1 = sbr("G1", [128, NT, 4]); ge = sbr("ge", [128, NT, 4])
                gsum = sbr("gsum", [128, NT]); pen = sbr("pen", [128, NT, 4]); flm = sbr("flm", [128, NT, 32])
                m1 = sbr("m1", [128, NT]); oh1 = sbr("oh1", [128, NT, 32]); m2 = sbr("m2", [128, NT])
                oh2 = sbr("oh2", [128, NT, 32]); dd = sbr("dd", [128, NT])

                def R(eng, fn, extra_w=()):
                    sch.op(eng, fn, reads=[bL, bR, b_const], writes=[bR] + list(extra_w))

                def bc(a, n):
                    return a[:, :].unsqueeze(2).broadcast_to([128, NT, n])
                R("dve", lambda e: e.tensor_reduce(out=gmax[:], in_=gl, axis=AX.X, op=ALU.max))
                R("dve", lambda e: e.tensor_tensor(out=G1[:], in0=gl, in1=bc(gmax, 4), op=ALU.is_ge))
                R("dve", lambda e: e.tensor_tensor(out=ge[:], in0=gl, in1=bc(gmax, 4), op=ALU.subtract))
                R("act", lambda e: e.activation(out=ge[:], in_=ge[:], func=AF.Exp))
                R("dve", lambda e: e.tensor_reduce(out=gsum[:], in_=ge[:], axis=AX.X, op=ALU.add))
                R("dve", lambda e: e.reciprocal(out=gsum[:], in_=gsum[:]))
                R("dve", lambda e: e.tensor_scalar(out=pen[:], in0=G1[:], scalar1=1e9, scalar2=-1e9, op0=ALU.mult, op1=ALU.add))
                R("dve", lambda e: e.tensor_tensor(
                    out=flm[:].rearrange("p t (g e) -> p t g e", e=8), in0=fl.rearrange("p t (g e) -> p t g e", e=8),
                    in1=pen[:].unsqueeze(3).broadcast_to([128, NT, 4, 8]), op=ALU.add))
                R("dve", lambda e: e.tensor_reduce(out=m1[:], in_=flm[:], axis=AX.X, op=ALU.max))
                R("dve", lambda e: e.tensor_tensor(out=oh1[:], in0=flm[:], in1=bc(m1, 32), op=ALU.is_ge))
                R("dve", lambda e: e.scalar_tensor_tensor(out=flm[:], in0=oh1[:], scalar=-1e9, in1=flm[:], op0=ALU.mult, op1=ALU.add))
                R("dve", lambda e: e.tensor_reduce(out=m2[:], in_=flm[:], axis=AX.X, op=ALU.max))
                R("dve", lambda e: e.tensor_tensor(out=oh2[:], in0=flm[:], in1=bc(m2, 32), op=ALU.is_ge))
                R("dve", lambda e: e.tensor_tensor(out=dd[:], in0=m2[:], in1=m1[:], op=ALU.subtract))
                R("act", lambda e: e.activation(out=dd[:], in_=dd[:], func=AF.Exp))
                R("dve", lambda e: e.tensor_scalar(out=g1[:], in0=dd[:], scalar1=1.0, scalar2=None, op0=ALU.add), [bRt])
                R("dve", lambda e: e.reciprocal(out=g1[:], in_=g1[:]), [bRt])
                R("dve", lambda e: e.tensor_tensor(out=g1[:], in0=g1[:], in1=gsum[:], op=ALU.mult), [bRt])
                R("dve", lambda e: e.tensor_tensor(out=g2[:], in0=g1[:], in1=dd[:], op=ALU.mult), [bRt])
                selb = sbr("selb", [128, NT * 32], BF16)
                trif = sbr("trif", [128, 128]); trib = sbr("trib", [128, 128], BF16); oneb = sbr("oneb", [128, 128], BF16)
                iot = sbr("iot", [128, 256]); pid2 = sbr("pid2", [128, 1])
                sch.dma("sp", trif[:], tri_in[:, :], writes=[bR])
                sch.dma("sp", iot[:], iota_in[:, :], writes=[bR])
                sch.dma("sp", pid2[:], pidx_in[:, :], writes=[bR])
                R("dve", lambda e: e.tensor_copy(out=trib[:], in_=trif[:]))
                R("dve", lambda e: e.memset(oneb[:], 1.0))
                R("dve", lambda e: e.tensor_scalar(out=pid2[:], in0=pid2[:], scalar1=2.0, scalar2=None, op0=ALU.mult))
                R("dve", lambda e: e.tensor_tensor(out=selb[:], in0=oh1[:].rearrange("p t e -> p (t e)"),
                                                   in1=oh2[:].rearrange("p t e -> p (t e)"), op=ALU.add))
                Cs = sbr("Cs", [128, NT, 32]); Ta = sbr("Ta", [128, NT, 32]); Tb = sbr("Tb", [128, NT, 32]); T0 = sbr("T0", [128, NT, 32])
                with ExitStack() as e3r:
                    cps = [e3r.enter_context(nc.psum_tensor(f"rtc{j}", [128, 512], F32)) for j in range(4)]
                    tps = [e3r.enter_context(nc.psum_tensor(f"rtt{j}", [128, 512], F32)) for j in range(4)]
                    bcp, btp = sch.buf("rtc"), sch.buf("rtt")
                    Cf = Cs[:].rearrange("p t e -> p (t e)")
                    T0f = T0[:].rearrange("p t e -> p (t e)")
                    for j in range(4):
                        sch.op("pe", lambda e, j=j: e.matmul(cps[j][:], trib[:], selb[:, j * 512:(j + 1) * 512], start=True, stop=True),
                               reads=[bR], writes=[bcp])
                        sch.op("pe", lambda e, j=j: e.matmul(tps[j][:], oneb[:], selb[:, j * 512:(j + 1) * 512], start=True, stop=True),
                               reads=[bR], writes=[btp])
                    for j in range(4):
                        sch.op("act", lambda e, j=j: e.activation(out=Cf[:, j * 512:(j + 1) * 512], in_=cps[j][:], func=AF.Copy),
                               reads=[bcp], writes=[bR])
                        sch.op("dve", lambda e, j=j: e.tensor_copy(out=T0f[:, j * 512:(j + 1) * 512], in_=tps[j][:]),
                               reads=[btp], writes=[bR])
                src, dstb = T0, Ta
                for sft in (1, 2, 4, 8, 16, 32):
                    R("dve", lambda e, src=src, dstb=dstb, sft=sft: e.tensor_copy(out=dstb[:, 0:sft, :], in_=src[:, 0:sft, :]))
                    R("dve", lambda e, src=src, dstb=dstb, sft=sft: e.tensor_tensor(
                        out=dstb[:, sft:NT, :], in0=src[:, sft:NT, :], in1=src[:, 0:NT - sft, :], op=ALU.add))
                    src, dstb = dstb, (Tb if dstb is Ta else Ta)
                Inc = src
                cnt = sbr("cnt", [128, 32]); nbk = sbr("nbk", [128, 32]); cmp1 = sbr("cmp1", [128, 32, 128])
                i128 = sbr("i128", [128, 128]); sa = sbr("sa", [128, 32]); sb_ = sbr("sb_", [128, 32])
                psr = sbr("psr", [128, 32]); pend = sbr("pend", [128, 32])
                R("dve", lambda e: e.tensor_copy(out=cnt[:], in_=Inc[:, NT - 1, :]))
                R("dve", lambda e: e.tensor_scalar(out=i128[:], in0=iot[:, 0:128], scalar1=float(MOE_B), scalar2=None, op0=ALU.mult))
                R("dve", lambda e: e.tensor_tensor(out=cmp1[:], in0=cnt[:, :].unsqueeze(2).broadcast_to([128, 32, 128]),
                                                   in1=i128[:, :].unsqueeze(1).broadcast_to([128, 32, 128]), op=ALU.is_gt))
                R("dve", lambda e: e.tensor_reduce(out=nbk[:], in_=cmp1[:], axis=AX.X, op=ALU.add))
                src, dstb = nbk, sa
                for sft in (1, 2, 4, 8, 16):
                    R("dve", lambda e, src=src, dstb=dstb, sft=sft: e.tensor_copy(out=dstb[:, 0:sft], in_=src[:, 0:sft]))
                    R("dve", lambda e, src=src, dstb=dstb, sft=sft: e.tensor_tensor(
                        out=dstb[:, sft:32], in0=src[:, sft:32], in1=src[:, 0:32 - sft], op=ALU.add))
                    src, dstb = dstb, (sb_ if dstb is sa else sa)
                R("dve", lambda e, src=src: e.tensor_copy(out=pend[:], in_=src[:]))
                R("dve", lambda e: e.tensor_tensor(out=psr[:], in0=pend[:], in1=nbk[:], op=ALU.subtract))
                R("dve", lambda e: e.tensor_scalar(out=psr[:], in0=psr[:], scalar1=float(MOE_B), scalar2=None, op0=ALU.mult))
                R("dve", lambda e, Inc=Inc: e.tensor_tensor(out=Cs[:], in0=Cs[:], in1=Inc[:], op=ALU.add))
                R("dve", lambda e: e.tensor_tensor(out=Cs[:], in0=Cs[:], in1=T0[:], op=ALU.subtract))
                R("dve", lambda e: e.tensor_tensor(out=Cs[:], in0=Cs[:], in1=psr[:, :].unsqueeze(1).broadcast_to([128, NT, 32]), op=ALU.add))
                d1 = sbr("d1", [128, NT]); d2 = sbr("d2", [128, NT])
                R("dve", lambda e: e.tensor_tensor(out=Ta[:], in0=Cs[:], in1=oh1[:], op=ALU.mult))
                R("dve", lambda e: e.tensor_reduce(out=d1[:], in_=Ta[:], axis=AX.X, op=ALU.add))
                R("dve", lambda e: e.tensor_tensor(out=Tb[:], in0=Cs[:], in1=oh2[:], op=ALU.mult))
                R("dve", lambda e: e.tensor_reduce(out=d2[:], in_=Tb[:], axis=AX.X, op=ALU.add))
                R("dve", lambda e: e.tensor_copy(out=dst1i[:], in_=d1[:]), [bRt])
                R("dve", lambda e: e.tensor_copy(out=dst2i[:], in_=d2[:]), [bRt])
                cmp2 = sbr("cmp2", [128, NBLK, 32]); bex = sbr("bex", [128, NBLK]); need = sbr("need", [128, NBLK])
                w0 = sbr("w0", [128, NBLK]); w1f = sbr("w1f", [128, NBLK, 2])
                R("dve", lambda e: e.tensor_tensor(out=cmp2[:], in0=iot[:, 0:NBLK].unsqueeze(2).broadcast_to([128, NBLK, 32]),
                                                   in1=pend[:, :].unsqueeze(1).broadcast_to([128, NBLK, 32]), op=ALU.is_ge))
                R("dve", lambda e: e.tensor_reduce(out=bex[:], in_=cmp2[:], axis=AX.X, op=ALU.add))
                R("dve", lambda e: e.tensor_scalar(out=bex[:], in0=bex[:], scalar1=float(N_EXP - 1), scalar2=None, op0=ALU.min))
                R("dve", lambda e: e.memset(need[:], 1.0))
                if WSKIP:
                    R("dve", lambda e: e.tensor_tensor(out=need[:, NSET:NBLK], in0=bex[:, NSET:NBLK], in1=bex[:, 0:NBLK - NSET], op=ALU.not_equal))
                R("dve", lambda e: e.tensor_scalar(out=w0[:], in0=bex[:], scalar1=256.0, scalar2=pid2[:, 0:1], op0=ALU.mult, op1=ALU.add))
                R("dve", lambda e: e.tensor_tensor(out=w0[:], in0=w0[:], in1=need[:], op=ALU.mult))
                R("dve", lambda e: e.tensor_scalar(out=need[:], in0=need[:], scalar1=-float(1 << 30), scalar2=float(1 << 30),
                                                   op0=ALU.mult, op1=ALU.add))
                R("dve", lambda e: e.tensor_tensor(out=w0[:], in0=w0[:], in1=need[:], op=ALU.add))
                R("dve", lambda e: e.tensor_copy(out=w1f[:, :, 0], in_=w0[:]))
                R("dve", lambda e: e.tensor_scalar(out=w1f[:, :, 1], in0=w0[:], scalar1=1.0, scalar2=None, op0=ALU.add))
                R("dve", lambda e: e.tensor_copy(out=widx[:], in_=w1f[:].rearrange("p b h -> p (b h)")), [bRt])
                if debug:
                    dbg = sbr("dbg", [128, 4 * NT + 2 * NBLK])
                    R("dve", lambda e: e.tensor_copy(out=dbg[:, 0:NT], in_=d1[:]))
                    R("dve", lambda e: e.tensor_copy(out=dbg[:, NT:2 * NT], in_=d2[:]))
                    R("dve", lambda e: e.tensor_copy(out=dbg[:, 2 * NT:3 * NT], in_=g1[:]))
                    R("dve", lambda e: e.tensor_copy(out=dbg[:, 3 * NT:4 * NT], in_=g2[:]))
                    R("dve", lambda e: e.tensor_copy(out=dbg[:, 4 * NT:4 * NT + NBLK], in_=bex[:]))
                    R("dve", lambda e: e.tensor_copy(out=dbg[:, 4 * NT + NBLK:4 * NT + 2 * NBLK], in_=w0[:]))
                    sch.dma("sp", dbg_out[:, :], dbg[:], reads=[bR])
                sch.fence_all("sp")
                sch.fence_all("pool")
                hs_r = Ring(sch, e3, nc, "p3hs", [128, D], BF16, 4)
                for n in range(NT):
                    hs, bhs = hs_r.next()
                    sch.dma("sp", hs[:], h2_scr[n * 128:(n + 1) * 128, :], writes=[bhs])
                    for dsti in (dst1i, dst2i):
                        sch.dma("pool", xs_scr[:, :], hs[:], reads=[bhs, bRt],
                                indirect=dict(out_offset=bass.IndirectOffsetOnAxis(dsti[:, n:n + 1], 0), in_offset=None))
                sch.fence_all("sp")
                sch.fence_all("pool")

        if 4 in phases:
            with ExitStack() as e4:
                def sb4(name, shape, dt):
                    return e4.enter_context(nc.sbuf_tensor("p4" + name, list(shape), dt))
                SUB = MOE_B // 128
                Wb = [[sb4(f"W{i}_{s_}", [128, 4096], BF16) for s_ in range(NSET)] for i in range(3)]
                bWb = [[[sch.buf(f"p4W{i}_{s_}_{h}") for h in range(2)] for s_ in range(NSET)] for i in range(3)]
                wsrc = (w1, w3, w2)
                bnd_reg = nc.gpsimd.alloc_register("wbnd")
                nc.gpsimd.reg_mov(bnd_reg, N_EXP * 256 - 1)
                xs_r = Ring(sch, e4, nc, "p4xs", [128, D], BF16, 3)
                xT_r = Ring(sch, e4, nc, "p4xT", [128, 8, 128], BF16, 3)
                sg_r = Ring(sch, e4, nc, "p4sg", [128, 512], BF16, 2)
                am_r = Ring(sch, e4, nc, "p4am", [128, 512], BF16, 2)
                aT_r = Ring(sch, e4, nc, "p4aT", [128, 4, 128], BF16, 2 * SUB + 1)
                ys_r = Ring(sch, e4, nc, "p4ys", [128, D], F32, 3)
                with ExitStack() as e4p:
                    tpx = e4p.enter_context(nc.psum_tensor("p4tpx", [128, 8, 128], BF16))
                    btpx = sch.buf("p4tpx")
                    a1p = Ring(sch, e4p, nc, "p4a1", [128, 512], F32, 2, psum=True)
                    a3p = Ring(sch, e4p, nc, "p4a3", [128, 512], F32, 2, psum=True)
                    ypp = Ring(sch, e4p, nc, "p4yp", [128, 512], F32, 2, psum=True)
                    tpa = e4p.enter_context(nc.psum_tensor("p4tpa", [128, 4, 128], BF16))
                    btpa = sch.buf("p4tpa")

                    def stageX(b):
                        st_ = b % NSET
                        for i in range(3):
                            for hf in range(2):
                                sch.dma("pool", Wb[i][st_][:, hf * 2048:(hf + 1) * 2048], wsrc[i][:, :], reads=[bRt], writes=[bWb[i][st_][hf]],
                                        indirect=dict(out_offset=None, in_offset=bass.IndirectOffsetOnAxis(widx[:, 2 * b + hf:2 * b + hf + 1], 0),
                                                      bounds_check=bnd_reg, oob_is_err=False))
                        res = []
                        mids = []
                        for sb_ in range(SUB):
                            r0_ = b * MOE_B + sb_ * 128
                            xs, bxs = xs_r.next()
                            sch.dma("sp", xs[:], xs_scr[r0_:r0_ + 128, :], writes=[bxs])
                            for kc in range(8):
                                sch.op("pe", lambda e, kc=kc, xs=xs: e.transpose(out=tpx[:, kc, :], in_=xs[:, kc * 128:(kc + 1) * 128],
                                                                                identity=identb[:]), reads=[bxs, b_const], writes=[btpx])
                            xT, bxT = xT_r.next()
                            sch.op("dve", lambda e, xT=xT: e.tensor_copy(out=xT[:], in_=tpx[:]), reads=[btpx], writes=[bxT])
                            a1, ba1 = a1p.next()
                            a3, ba3 = a3p.next()
                            for (ap_, bap, Wx, bWx) in ((a1, ba1, Wb[0][st_], bWb[0][st_]), (a3, ba3, Wb[1][st_], bWb[1][st_])):
                                for kc in range(8):
                                    sch.op("pe", lambda e, kc=kc, ap_=ap_, Wx=Wx, xT=xT: e.matmul(
                                        ap_[:], xT[:, kc, :], Wx[:, kc * 512:(kc + 1) * 512],
                                        start=(kc == 0), stop=(kc == 7)), reads=[bWx[kc // 4], bxT], writes=[bap])
                            sg, bsg = sg_r.next()
                            sch.op("act", lambda e, a1=a1, sg=sg: e.activation(out=sg[:], in_=a1[:], func=AF.Silu), reads=[ba1], writes=[bsg])
                            am, bam = am_r.next()
                            sch.op("dve", lambda e, am=am, a3=a3, sg=sg: e.tensor_tensor(out=am[:], in0=a3[:], in1=sg[:], op=ALU.mult),
                                   reads=[ba3, bsg], writes=[bam])
                            mids.append((am, bam))
                        for (am, bam) in mids:
                            for nch in range(4):
                                sch.op("pe", lambda e, nch=nch, am=am: e.transpose(out=tpa[:, nch, :], in_=am[:, nch * 128:(nch + 1) * 128],
                                                                                  identity=identb[:]), reads=[bam, b_const], writes=[btpa])
                            aT, baT = aT_r.next()
                            sch.op("act", lambda e, aT=aT: e.activation(out=aT[:], in_=tpa[:], func=AF.Copy), reads=[btpa], writes=[baT])
                            res.append((aT, baT))
                        return res

                    def stageY(b, res):
                        st_ = b % NSET
                        W2b = Wb[2][st_]
                        for sb_, (aT, baT) in enumerate(res):
                            r0_ = b * MOE_B + sb_ * 128
                            ys, bys = ys_r.next()
                            for hf in range(2):
                                yp, byp = ypp.next()
                                for nch in range(4):
                                    sch.op("pe", lambda e, nch=nch, hf=hf, yp=yp, aT=aT, W2b=W2b: e.matmul(
                                        yp[:], aT[:, nch, :], W2b[:, nch * 1024 + hf * 512:nch * 1024 + (hf + 1) * 512],
                                        start=(nch == 0), stop=(nch == 3)), reads=[baT, bWb[2][st_][nch // 2]], writes=[byp])
                                sch.op("act", lambda e, hf=hf, yp=yp, ys=ys: e.activation(out=ys[:, hf * 512:(hf + 1) * 512], in_=yp[:], func=AF.Copy),
                                       reads=[byp], writes=[bys])
                            sch.dma("act", yb_scr[r0_:r0_ + 128, :], ys[:], reads=[bys])

                    prevx = None
                    for b in range(NBLK + 1):
                        curx = stageX(b) if b < NBLK else None
                        if prevx is not None:
                            stageY(b - 1, prevx)
                        prevx = curx
                sch.fence_all("sp")
                sch.fence_all("pool")
                sch.fence_all("act")
                y1_r = Ring(sch, e4, nc, "p4y1", [128, D], F32, 3)
                y2_r = Ring(sch, e4, nc, "p4y2", [128, D], F32, 3)
                xo_r = Ring(sch, e4, nc, "p4xo", [128, D], F32, 3)
                for n in range(NT):
                    y1, by1 = y1_r.next()
                    y2, by2 = y2_r.next()
                    xo, bxo = xo_r.next()
                    sch.dma("pool", y1[:], yb_scr[:, :], reads=[bRt], writes=[by1],
                            indirect=dict(out_offset=None, in_offset=bass.IndirectOffsetOnAxis(dst1i[:, n:n + 1], 0)))
                    sch.dma("pool", y2[:], yb_scr[:, :], reads=[bRt], writes=[by2],
                            indirect=dict(out_offset=None, in_offset=bass.IndirectOffsetOnAxis(dst2i[:, n:n + 1], 0)))
                    sch.dma("sp", xo[:], out[n * 128:(n + 1) * 128, :], writes=[bxo])
                    sch.op("dve", lambda e, n=n, y1=y1, xo=xo: e.scalar_tensor_tensor(
                        out=xo[:], in0=y1[:], scalar=g1[:, n:n + 1], in1=xo[:], op0=ALU.mult, op1=ALU.add),
                        reads=[by1, bxo, bRt], writes=[bxo])
                    sch.op("dve", lambda e, n=n, y2=y2, xo=xo: e.scalar_tensor_tensor(
                        out=xo[:], in0=y2[:], scalar=g2[:, n:n + 1], in1=xo[:], op0=ALU.mult, op1=ALU.add),
                        reads=[by2, bxo, bRt], writes=[bxo])
                    sch.dma("act", out[n * 128:(n + 1) * 128, :], xo[:], reads=[bxo])
                sch.fence_all("sp")
                sch.fence_all("pool")
                sch.fence_all("act")

        for en in ("sp", "pool", "act", "dve", "pe"):
            sch.fence_all(en)
        if _os.environ.get("DRYPRINT"):
            print("counts", {k: v.count for k, v in sch.engs.items()}, "nsem", sch.nsem)
    return nc


_CACHE = {}


def kernel(x, mem, g_mix, w_in, qk_gain, na_rpb, t5_table, g_mem, w_mem_kv, w_out, g_ffn, w_r1, b_r1, w_r2, b_r2,
           w1, w3, w2, _debug=False, _phases=(1, 2, 3, 4), _cores=8):
    f32 = np.float32
    x = np.asarray(x, f32); mem = np.asarray(mem, f32)
    na_steps, na_keys = na_plan()
    dil_tl = dil_plan()
    nab = na_bias_tiles(np.asarray(na_rpb, f32)[0], na_keys)
    dlb = dil_res_bias_tiles(np.asarray(t5_table, f32))
    key = (len(na_keys), len(dil_tl), _debug, tuple(_phases))
    if key not in _CACHE:
        _CACHE[key] = build_program(len(na_keys), len(dil_tl), na_steps, dil_tl, debug=_debug, phases=_phases)
    nc = _CACHE[key]

    def pk(v):
        return np.asarray(v, f32).reshape(8, 128).T
    gvec = np.ascontiguousarray(np.concatenate([pk(g_mix[0]), pk(g_mem[0]), pk(g_ffn[0])], axis=1))
    qg = np.asarray(qk_gain, f32)[0]
    gains = np.ascontiguousarray(np.tile(qg.reshape(6, 64), (1, 2)).T)
    shared = {
        "w_in": np.ascontiguousarray(np.asarray(w_in, f32)[0]),
        "w_mem": np.ascontiguousarray(np.asarray(w_mem_kv, f32)[0]),
        "w_out": np.ascontiguousarray(np.asarray(w_out, f32)[0]),
        "gvec": gvec, "gains": gains,
        "gffn_b": np.ascontiguousarray(np.broadcast_to(np.asarray(g_ffn, f32)[0][None, :], (128, D))),
        "ident": np.eye(128, dtype=f32),
        "na_bias": nab, "dil_bias": dlb,
        "w_r": np.ascontiguousarray(np.concatenate([np.asarray(w_r1, f32)[0], np.asarray(w_r2, f32)[0]], axis=1)),
        "b_r": np.ascontiguousarray(np.broadcast_to(
            np.concatenate([np.asarray(b_r1, f32)[0], np.asarray(b_r2, f32)[0]])[None, :], (128, 36))),
        "w1": np.ascontiguousarray(np.asarray(w1, f32)[0].reshape(N_EXP, 8, 128, 512).transpose(0, 2, 1, 3)).reshape(N_EXP * 256, 2048),
        "w3": np.ascontiguousarray(np.asarray(w3, f32)[0].reshape(N_EXP, 8, 128, 512).transpose(0, 2, 1, 3)).reshape(N_EXP * 256, 2048),
        "w2": np.ascontiguousarray(np.asarray(w2, f32)[0].reshape(N_EXP, 4, 128, 1024).transpose(0, 2, 1, 3)).reshape(N_EXP * 256, 2048),
        "iota": np.ascontiguousarray(np.broadcast_to(np.arange(256, dtype=f32)[None, :], (128, 256))),
        "pidx": np.arange(128, dtype=f32).reshape(128, 1),
        "tri": np.triu(np.ones((128, 128), f32), 1),
    }
    in_maps = []
    for c in range(_cores):
        m = dict(shared)
        m["x"] = np.ascontiguousarray(x[c])
        m["mem"] = np.ascontiguousarray(mem[c])
        in_maps.append(m)
    res = run_bass_kernel_spmd(nc, in_maps, core_ids=list(range(_cores)))
    if _debug:
        return res.results
    return np.stack([r["out"] for r in res.results], axis=0)
```
